# Optimizing a Trainium2 kernel written in Bass

```python
import jax
import jax.numpy as jnp
from jax import lax
import numpy as np


D_MODEL = 1024
BATCH = 8
SEQ = 2048
DEPTH = 2

HEAD_DIM = 64
N_MIXERS = 4
GROUP_WIDTH = D_MODEL // N_MIXERS
RMS_EPS = 1e-6
ROPE_THETA = 500000.0
ROPE_DIM = HEAD_DIM // 4

NSA_HEADS = GROUP_WIDTH // HEAD_DIM
NSA_KV_DIM = HEAD_DIM
CMP_BLOCK = 32
CMP_STRIDE = 16
SEL_BLOCK = 64
SEL_TOP_N = 16
WINDOW = 512
SEL_Q_BLOCK = 64
WIN_Q_BLOCK = 128
FORCE_SCORE = 1e4
NSA_PROJ = GROUP_WIDTH + 6 * NSA_KV_DIM + 3 * NSA_HEADS

POOL_WINDOWS = (2, 4, 8, 16)
POOL_GROUP = GROUP_WIDTH // len(POOL_WINDOWS)

RWKV_HEADS = GROUP_WIDTH // HEAD_DIM
DECAY_LORA = 64
AAA_LORA = 64
MV_LORA = 32
GATE_LORA = 160
RWKV_GN_EPS = 64e-5
RWKV_PROJ = 3 * GROUP_WIDTH + DECAY_LORA + AAA_LORA + GATE_LORA

DIL_HEADS = GROUP_WIDTH // HEAD_DIM
DIL_PATTERNS = ((128, 1), (512, 4), (2048, 16))
DIL_Q_BLOCK = 128
DIL_PROJ = 3 * GROUP_WIDTH

IN_WIDTH = NSA_PROJ + GROUP_WIDTH + RWKV_PROJ + DIL_PROJ

N_EXPERTS = 16
N_EXPERT_GROUPS = 4
EXPERTS_PER_GROUP = N_EXPERTS // N_EXPERT_GROUPS
TOP_K = 2
D_EXPERT = 256

MAX_POS_OFFSET = 4096
NEG_INF = -1e30
TINY = 1e-30
F32 = jnp.float32

kernel_name = 'hybrid_nsa_pool_rwkv7_dilated_moe'


def _split(z, sizes):
    out, off = [], 0
    for n in sizes:
        out.append(z[..., off:off + n])
        off += n
    return out


def _rms_norm(x, g):
    xf = x.astype(F32)
    y = xf * lax.rsqrt(jnp.mean(xf * xf, axis=-1, keepdims=True) + RMS_EPS)
    return (y * g.astype(F32)).astype(x.dtype)


def _rope_partial(x, pos):
    half = ROPE_DIM // 2
    inv_freq = ROPE_THETA ** (-2.0 * jnp.arange(half, dtype=F32) / ROPE_DIM)
    ang = pos.astype(F32)[..., None] * inv_freq
    cos, sin = jnp.cos(ang), jnp.sin(ang)
    xf = x.astype(F32)
    x1, x2, xp = xf[..., :half], xf[..., half:ROPE_DIM], xf[..., ROPE_DIM:]
    out = jnp.concatenate([x1 * cos - x2 * sin, x2 * cos + x1 * sin, xp], axis=-1)
    return out.astype(x.dtype)


def _masked_softmax(s, mask):
    s = jnp.where(mask, s, NEG_INF)
    m = jnp.max(s, axis=-1, keepdims=True)
    e = jnp.where(mask, jnp.exp(s - m), 0.0)
    den = jnp.maximum(jnp.sum(e, axis=-1, keepdims=True), TINY)
    return e / den, m + jnp.log(den)


def _nsa(q, kc, vc, ks, vs, kw, vw, gate_logit, pos, q_norm, k_norm, cmp_pe, cmp_w1, cmp_w2):
    B, S, H, hd = q.shape
    scale = hd ** -0.5
    t_idx = jnp.arange(S)
    q = _rope_partial(_rms_norm(q, q_norm), pos[:, :, None])

    ratio = CMP_BLOCK // CMP_STRIDE
    n_chunk = S // CMP_STRIDE
    n_cmp = n_chunk - ratio + 1
    c_end = jnp.arange(n_cmp) * CMP_STRIDE + CMP_BLOCK - 1

    def compress(z, j):
        zc = z.reshape(B, n_chunk, CMP_STRIDE, hd)
        blk = jnp.concatenate([zc[:, i:i + n_cmp] for i in range(ratio)], axis=2)
        blk = (blk + cmp_pe[j]).reshape(B, n_cmp, CMP_BLOCK * hd)
        return jax.nn.gelu(blk @ cmp_w1[j]) @ cmp_w2[j]

    k_cmp = _rope_partial(_rms_norm(compress(kc, 0), k_norm[0]), pos[:, c_end])
    v_cmp = compress(vc, 1).astype(F32)
    s = jnp.einsum('bshd,bcd->bhsc', q, k_cmp).astype(F32) * scale
    p_cmp, _ = _masked_softmax(s, c_end[None, :] <= t_idx[:, None])
    o_cmp = jnp.einsum('bhsc,bcd->bshd', p_cmp, v_cmp)

    n_blk = S // SEL_BLOCK
    n_sel = min(SEL_TOP_N, n_blk)
    b_start = jnp.arange(n_blk) * SEL_BLOCK
    cover = jnp.clip(jnp.minimum(c_end[:, None] + 1, b_start[None, :] + SEL_BLOCK)
                     - jnp.maximum(c_end[:, None] + 1 - CMP_BLOCK, b_start[None, :]), 0)
    cover = cover.astype(F32) / CMP_BLOCK
    imp = jnp.einsum('bhsc,cn->bsn', p_cmp, cover)
    cur = (t_idx // SEL_BLOCK)[:, None]
    blk = jnp.arange(n_blk)[None, :]
    forced = (blk == 0) | (blk == cur) | (blk == cur - 1)
    imp = jnp.where(blk > cur, -1.0, jnp.where(forced, FORCE_SCORE, imp))
    _, sel_idx = lax.top_k(imp, n_sel)

    ks = _rope_partial(_rms_norm(ks, k_norm[1]), pos)
    ks_blk = ks.reshape(B, n_blk, SEL_BLOCK, hd)
    vs_blk = vs.reshape(B, n_blk, SEL_BLOCK, hd)
    nq = S // SEL_Q_BLOCK
    q_b = q.reshape(B, nq, SEL_Q_BLOCK, H, hd).swapaxes(0, 1)
    idx_b = sel_idx.reshape(B, nq, SEL_Q_BLOCK, n_sel).swapaxes(0, 1)
    t_b = t_idx.reshape(nq, SEL_Q_BLOCK)
    b_ar = jnp.arange(B)[:, None, None]
    n_key = n_sel * SEL_BLOCK

    def sel_block(args):
        qq, ii, tt = args
        kg = ks_blk[b_ar, ii]
        vg = vs_blk[b_ar, ii].reshape(B, SEL_Q_BLOCK, n_key, hd).astype(F32)
        kpos = ii[..., None] * SEL_BLOCK + jnp.arange(SEL_BLOCK)
        mask = (kpos <= tt[None, :, None, None]).reshape(B, 1, SEL_Q_BLOCK, n_key)
        sc = jnp.einsum('bqhd,bqnkd->bhqnk', qq, kg).astype(F32).reshape(B, H, SEL_Q_BLOCK, n_key) * scale
        p, _ = _masked_softmax(sc, mask)
        return jnp.einsum('bhqk,bqkd->bqhd', p, vg)

    o_sel = lax.map(sel_block, (q_b, idx_b, t_b)).swapaxes(0, 1).reshape(B, S, H, hd)

    kw = _rope_partial(_rms_norm(kw, k_norm[2]), pos)
    nb = S // WIN_Q_BLOCK
    n_prev = WINDOW // WIN_Q_BLOCK

    def band(z):
        zp = jnp.pad(z, ((0, 0), (n_prev * WIN_Q_BLOCK, 0), (0, 0))).reshape(B, nb + n_prev, WIN_Q_BLOCK, hd)
        return jnp.concatenate([zp[:, j:j + nb] for j in range(n_prev + 1)], axis=2)

    kpos = (jnp.arange(nb)[:, None] - n_prev) * WIN_Q_BLOCK + jnp.arange((n_prev + 1) * WIN_Q_BLOCK)[None, :]
    dist = t_idx.reshape(nb, WIN_Q_BLOCK)[:, :, None] - kpos[:, None, :]
    mask = (kpos[:, None, :] >= 0) & (dist >= 0) & (dist < WINDOW)
    sc = jnp.einsum('bnqhd,bnkd->bhnqk', q.reshape(B, nb, WIN_Q_BLOCK, H, hd), band(kw)).astype(F32) * scale
    p, _ = _masked_softmax(sc, mask)
    o_win = jnp.einsum('bhnqk,bnkd->bnqhd', p, band(vw).astype(F32)).reshape(B, S, H, hd)

    g = jax.nn.sigmoid(gate_logit.astype(F32))
    o = g[..., 0:1] * o_cmp + g[..., 1:2] * o_sel + g[..., 2:3] * o_win
    return o.reshape(B, S, H * hd)


def _pool_mixer(u, pool_w, pool_scale):
    B, S, _ = u.shape
    count = jnp.arange(1, S + 1, dtype=F32)[None, :, None]
    outs = []
    for gi, w in enumerate(POOL_WINDOWS):
        ug = u[..., gi * POOL_GROUP:(gi + 1) * POOL_GROUP].astype(F32)
        cs = jnp.cumsum(ug, axis=1)
        cs_lag = jnp.pad(cs, ((0, 0), (w, 0), (0, 0)))[:, :S]
        mean = (cs - cs_lag) / jnp.minimum(count, float(w))
        outs.append((mean - ug) @ pool_w[gi])
    return jnp.concatenate(outs, axis=-1) * pool_scale


def _rwkv7(z, v_first, v_res, mu, w0, w2, a0, a2, g2, k_k, k_a, r_k, ln_w, ln_b):
    B, S, _ = z.shape
    H, N = RWKV_HEADS, HEAD_DIM
    zf = z.astype(F32)
    z_prev = jnp.pad(zf, ((0, 0), (1, 0), (0, 0)))[:, :-1]
    zs = zf + (z_prev - zf) * mu
    r, k, v, xw, xa, xg = _split(zs, (GROUP_WIDTH,) * 3 + (DECAY_LORA, AAA_LORA, GATE_LORA))
    w_log = -jax.nn.softplus(-(w0 + jnp.tanh(xw) @ w2)) - 0.5
    decay = jnp.exp(-jnp.exp(w_log))
    a = jax.nn.sigmoid(a0 + xa @ a2)
    g = jax.nn.sigmoid(xg) @ g2
    if v_res is None:
        v_first = v
    else:
        v0, v1, v2 = v_res
        v = v + (v_first - v) * jax.nn.sigmoid(v0 + (v @ v1) @ v2)

    def heads(t):
        return t.reshape(B, S, H, N)

    kk = heads(k * k_k)
    kk = kk / jnp.maximum(jnp.sqrt(jnp.sum(kk * kk, axis=-1, keepdims=True)), 1e-12)
    k = k * (1.0 + (a - 1.0) * k_a)
    rh, kh, vh, wh, ah = heads(r), heads(k), heads(v), heads(decay), heads(a)

    def step(state, inp):
        r_t, w_t, k_t, v_t, kk_t, a_t = inp
        sa = jnp.einsum('bhij,bhj->bhi', state, -kk_t)
        state = (state * w_t[:, :, None, :] + sa[..., None] * (kk_t * a_t)[:, :, None, :]
                 + v_t[..., None] * k_t[:, :, None, :])
        return state, jnp.einsum('bhij,bhj->bhi', state, r_t)

    def tmaj(t):
        return jnp.moveaxis(t, 1, 0)

    state0 = jnp.zeros((B, H, N, N), F32)
    _, o = lax.scan(step, state0, (tmaj(rh), tmaj(wh), tmaj(kh), tmaj(vh), tmaj(kk), tmaj(ah)))
    o = jnp.moveaxis(o, 0, 1)
    mean = jnp.mean(o, axis=-1, keepdims=True)
    var = jnp.mean(jnp.square(o - mean), axis=-1, keepdims=True)
    o = ((o - mean) * lax.rsqrt(var + RWKV_GN_EPS)).reshape(B, S, H * N) * ln_w + ln_b
    bonus = jnp.sum(rh * kh * r_k, axis=-1, keepdims=True) * vh
    o = (o + bonus.reshape(B, S, H * N)) * g
    return o, v_first


def _dilated_branch(q, k, v, window, dil):
    B, H, S, hd = q.shape
    L = S // dil
    n_back = window // dil
    nb = -(-L // DIL_Q_BLOCK)
    Lp = nb * DIL_Q_BLOCK
    n_prev = -(-n_back // DIL_Q_BLOCK)

    def strided(z):
        z = z.reshape(B, H, L, dil, hd).swapaxes(2, 3)
        return jnp.pad(z, ((0, 0), (0, 0), (0, 0), (0, Lp - L), (0, 0)))

    def band(z):
        zp = jnp.pad(z, ((0, 0), (0, 0), (0, 0), (n_prev * DIL_Q_BLOCK, 0), (0, 0)))
        zp = zp.reshape(B, H, dil, nb + n_prev, DIL_Q_BLOCK, hd)
        return jnp.concatenate([zp[:, :, :, j:j + nb] for j in range(n_prev + 1)], axis=4)

    qs = strided(q).reshape(B, H, dil, nb, DIL_Q_BLOCK, hd)
    kpos = (jnp.arange(nb)[:, None] - n_prev) * DIL_Q_BLOCK + jnp.arange((n_prev + 1) * DIL_Q_BLOCK)[None, :]
    dist = jnp.arange(Lp).reshape(nb, DIL_Q_BLOCK)[:, :, None] - kpos[:, None, :]
    mask = (kpos[:, None, :] >= 0) & (kpos[:, None, :] < L) & (dist >= 0) & (dist <= n_back)
    s = jnp.einsum('bhrnqd,bhrnkd->bhrnqk', qs, band(strided(k))).astype(F32) * (hd ** -0.5)
    p, lse = _masked_softmax(s, mask)
    o = jnp.einsum('bhrnqk,bhrnkd->bhrnqd', p, band(strided(v)).astype(F32))
    o = o.reshape(B, H, dil, Lp, hd)[:, :, :, :L].swapaxes(2, 3).reshape(B, H, S, hd)
    lse = lse[..., 0].reshape(B, H, dil, Lp)[..., :L].swapaxes(2, 3).reshape(B, H, S)
    return o, lse


def _dilated(q, k, v, pos, q_norm, k_norm):
    B, S, H, hd = q.shape
    q = _rope_partial(_rms_norm(q, q_norm), pos[:, :, None])
    k = _rope_partial(_rms_norm(k, k_norm), pos[:, :, None])
    qh, kh, vh = q.transpose(0, 2, 1, 3), k.transpose(0, 2, 1, 3), v.transpose(0, 2, 1, 3)
    outs, lses = [], []
    for window, dil in DIL_PATTERNS:
        o, lse = _dilated_branch(qh, kh, vh, window, dil)
        outs.append(o)
        lses.append(lse)
    wts = jax.nn.softmax(jnp.stack(lses), axis=0)
    o = jnp.sum(wts[..., None] * jnp.stack(outs), axis=0)
    return o.transpose(0, 2, 1, 3).reshape(B, S, H * hd)


def _moe(h, router_w, router_b, w_gate, w_up, w_down):
    B, S, _ = h.shape
    aff = jax.nn.sigmoid(jnp.einsum('bsd,de->bse', h, router_w).astype(F32))
    sel = (aff + router_b).reshape(B, S, N_EXPERT_GROUPS, EXPERTS_PER_GROUP)
    grp_score = jnp.sum(lax.top_k(sel, TOP_K)[0], axis=-1)
    grp = jnp.argmax(grp_score, axis=-1)
    sel_in = jnp.sum(sel * jax.nn.one_hot(grp, N_EXPERT_GROUPS, dtype=F32)[..., None], axis=2)
    _, loc = lax.top_k(sel_in, TOP_K)
    eidx = grp[..., None] * EXPERTS_PER_GROUP + loc
    a_sel = jnp.take_along_axis(aff, eidx, axis=-1)
    wts = a_sel / jnp.sum(a_sel, axis=-1, keepdims=True)
    gates = jnp.sum(jax.nn.one_hot(eidx, N_EXPERTS, dtype=F32) * wts[..., None], axis=-2)
    hg = jnp.einsum('bsd,edf->bsef', h, w_gate)
    hu = jnp.einsum('bsd,edf->bsef', h, w_up)
    act = jax.nn.silu(hg) * hu * gates[..., None].astype(h.dtype)
    return jnp.einsum('bsef,efd->bsd', act, w_down)


def setup_inputs(seed: int = 0) -> dict:
    key = jax.random.key(seed)
    keys = iter(jax.random.split(key, 64))
    L, D, hd, GW = DEPTH, D_MODEL, HEAD_DIM, GROUP_WIDTH

    def nrm(shape, scale):
        return jax.random.normal(next(keys), shape, F32) * scale

    def gain(shape):
        return 1.0 + nrm(shape, 0.05)

    x = nrm((BATCH, SEQ, D), 1.0)
    c = nrm((BATCH, D), 1.0)
    positions = (jnp.arange(SEQ, dtype=jnp.int32)[None, :]
                 + jax.random.randint(next(keys), (BATCH, 1), 0, MAX_POS_OFFSET, dtype=jnp.int32))
    return {
        'x': x,
        'c': c,
        'positions': positions,
        'ada_w': nrm((L, D, 6 * D), 0.5 * D ** -0.5),
        'ada_b': nrm((L, 6 * D), 0.02),
        'norm_mix_g': gain((L, D)),
        'norm_ffn_g': gain((L, D)),
        'w_in': nrm((L, D, IN_WIDTH), D ** -0.5),
        'w_out': nrm((L, D, D), D ** -0.5),
        'nsa_q_norm': gain((L, hd)),
        'nsa_k_norm': gain((L, 3, hd)),
        'nsa_cmp_pe': nrm((L, 2, CMP_BLOCK, hd), 0.1),
        'nsa_cmp_w1': nrm((L, 2, CMP_BLOCK * hd, hd), (CMP_BLOCK * hd) ** -0.5),
        'nsa_cmp_w2': nrm((L, 2, hd, hd), hd ** -0.5),
        'pool_w': nrm((L, len(POOL_WINDOWS), POOL_GROUP, POOL_GROUP), POOL_GROUP ** -0.5),
        'pool_scale': gain((L, GW)),
        'rwkv_mu': jax.random.uniform(next(keys), (L, RWKV_PROJ), F32),
        'rwkv_w0': nrm((L, GW), 1.0),
        'rwkv_w2': nrm((L, DECAY_LORA, GW), 0.5 * DECAY_LORA ** -0.5),
        'rwkv_a0': nrm((L, GW), 0.5),
        'rwkv_a2': nrm((L, AAA_LORA, GW), 0.5 * AAA_LORA ** -0.5),
        'rwkv_g2': nrm((L, GATE_LORA, GW), GATE_LORA ** -0.5),
        'rwkv_k_k': 0.85 + nrm((L, GW), 0.05),
        'rwkv_k_a': gain((L, GW)),
        'rwkv_r_k': nrm((L, RWKV_HEADS, hd), 0.1),
        'rwkv_ln_w': gain((L, GW)),
        'rwkv_ln_b': nrm((L, GW), 0.02),
        'rwkv_v0': nrm((L - 1, GW), 0.5),
        'rwkv_v1': nrm((L - 1, GW, MV_LORA), GW ** -0.5),
        'rwkv_v2': nrm((L - 1, MV_LORA, GW), 0.5 * MV_LORA ** -0.5),
        'dil_q_norm': gain((L, hd)),
        'dil_k_norm': gain((L, hd)),
        'router_w': nrm((D, N_EXPERTS), D ** -0.5),
        'router_b': nrm((N_EXPERTS,), 0.01),
        'moe_w_gate': nrm((L, N_EXPERTS, D, D_EXPERT), D ** -0.5),
        'moe_w_up': nrm((L, N_EXPERTS, D, D_EXPERT), D ** -0.5),
        'moe_w_down': nrm((L, N_EXPERTS, D_EXPERT, D), D_EXPERT ** -0.5),
    }


def reference(x, c, positions, ada_w, ada_b, norm_mix_g, norm_ffn_g, w_in, w_out,
              nsa_q_norm, nsa_k_norm, nsa_cmp_pe, nsa_cmp_w1, nsa_cmp_w2,
              pool_w, pool_scale,
              rwkv_mu, rwkv_w0, rwkv_w2, rwkv_a0, rwkv_a2, rwkv_g2, rwkv_k_k, rwkv_k_a,
              rwkv_r_k, rwkv_ln_w, rwkv_ln_b, rwkv_v0, rwkv_v1, rwkv_v2,
              dil_q_norm, dil_k_norm,
              router_w, router_b, moe_w_gate, moe_w_up, moe_w_down):
    B, S, D = x.shape
    mod_all = jnp.einsum('bd,lde->lbe', jax.nn.silu(c), ada_w) + ada_b[:, None, :]
    v_first = None
    for l in range(DEPTH):
        sh1, sc1, g1, sh2, sc2, g2 = jnp.split(mod_all[l][:, None, :], 6, axis=-1)

        h = _rms_norm(x, norm_mix_g[l]) * (1.0 + sc1) + sh1
        proj = h @ w_in[l]
        (q_a, kc, vc, ks, vs, kw, vw, gl, u_pool, z_rwkv, q_d, k_d, v_d) = _split(
            proj, (GROUP_WIDTH,) + (NSA_KV_DIM,) * 6 + (3 * NSA_HEADS, GROUP_WIDTH, RWKV_PROJ) + (GROUP_WIDTH,) * 3)

        y_nsa = _nsa(q_a.reshape(B, S, NSA_HEADS, HEAD_DIM), kc, vc, ks, vs, kw, vw,
                     gl.reshape(B, S, NSA_HEADS, 3), positions,
                     nsa_q_norm[l], nsa_k_norm[l], nsa_cmp_pe[l], nsa_cmp_w1[l], nsa_cmp_w2[l])
        y_pool = _pool_mixer(u_pool, pool_w[l], pool_scale[l])
        v_res = None if l == 0 else (rwkv_v0[l - 1], rwkv_v1[l - 1], rwkv_v2[l - 1])
        y_rwkv, v_first = _rwkv7(z_rwkv, v_first, v_res, rwkv_mu[l], rwkv_w0[l], rwkv_w2[l],
                                 rwkv_a0[l], rwkv_a2[l], rwkv_g2[l], rwkv_k_k[l], rwkv_k_a[l],
                                 rwkv_r_k[l], rwkv_ln_w[l], rwkv_ln_b[l])
        y_dil = _dilated(q_d.reshape(B, S, DIL_HEADS, HEAD_DIM), k_d.reshape(B, S, DIL_HEADS, HEAD_DIM),
                         v_d.reshape(B, S, DIL_HEADS, HEAD_DIM), positions, dil_q_norm[l], dil_k_norm[l])
        mix = jnp.concatenate([y_nsa, y_pool, y_rwkv, y_dil], axis=-1).astype(x.dtype)
        x = x + g1 * (mix @ w_out[l])

        h2 = _rms_norm(x, norm_ffn_g[l]) * (1.0 + sc2) + sh2
        x = x + g2 * _moe(h2, router_w, router_b, moe_w_gate[l], moe_w_up[l], moe_w_down[l])
    return x
```

```python
import numpy as np
import ml_dtypes
from contextlib import ExitStack
import concourse.bass as bass
import concourse.mybir as mybir
from concourse.bass_utils import run_bass_kernel_spmd

F32 = mybir.dt.float32
BF16 = mybir.dt.bfloat16
I32 = mybir.dt.int32
ALU = mybir.AluOpType
AF = mybir.ActivationFunctionType
AX = mybir.AxisListType
EPOCH = 30000

S = 2048
D = 1024
NT = 16
NB = 4
EPS = 1e-6


class V:
    __slots__ = ("t", "ap")

    def __init__(self, t, ap):
        self.t = t
        self.ap = ap

    def __getitem__(self, k):
        return V(self.t, self.ap[k])

    def re(self, s, **kw):
        return V(self.t, self.ap.rearrange(s, **kw))

    def bc(self, shape):
        return V(self.t, self.ap.to_broadcast(list(shape)))

    def cast(self, dt):
        return V(self.t, self.ap.bitcast(dt))


class T:
    def __init__(self, name, ap):
        self.name = name
        self.ap = ap
        self.last_w = None
        self.readers = {}
        self.dma_sem = None
        self.dma_cnt = 0

    def __getitem__(self, k):
        return V(self, self.ap[k])

    def view(self, name, ap):
        return T(name, ap)


class Sched:
    ENGS = ("pe", "act", "dve", "pool", "sp")

    def __init__(self, nc, stack):
        self.nc = nc
        self.stack = stack
        self.streams = {e: [] for e in self.ENGS}
        self.count = {e: 0 for e in self.ENGS}
        self.sems = {e: [] for e in self.ENGS}
        self.clock = {e: {} for e in self.ENGS}
        self.snap = {e: {} for e in self.ENGS}
        self.dma_known = {e: {} for e in self.ENGS}
        self.dma_tiles = []
        self.nsem = 0
        self.n_waits = 0

    def new_sem(self, name):
        self.nsem += 1
        return self.stack.enter_context(self.nc.semaphore(name))

    def eng_sem(self, e, n):
        idx = (n - 1) // EPOCH
        while len(self.sems[e]) <= idx:
            self.sems[e].append(self.new_sem(f"s_{e}_{len(self.sems[e])}"))
        return self.sems[e][idx], (n - 1) % EPOCH + 1

    def sbuf(self, name, shape, dtype):
        t = self.stack.enter_context(self.nc.sbuf_tensor("sb_" + name, list(shape), dtype))
        return T(name, t[:])

    def psum(self, name, shape, dtype):
        t = self.stack.enter_context(self.nc.psum_tensor("pp_" + name, list(shape), dtype))
        r = T(name, t[:])
        r.is_psum = True
        return r

    def _need(self, e, reads, writes):
        need = {}
        dneed = []

        def add(dep):
            if dep is None:
                return
            x, n = dep
            if x == e and e == "pe":
                return
            if need.get(x, 0) < n:
                need[x] = n
        for t in reads:
            add(t.last_w)
            if getattr(t, "is_psum", False):
                for x, n in t.readers.items():
                    if x != e:
                        add((x, n))
            if t.dma_cnt:
                dneed.append(t)
        for t in writes:
            add(t.last_w)
            for x, n in t.readers.items():
                if x == e:
                    continue
                add((x, n))
            if t.dma_cnt:
                dneed.append(t)
        return need, dneed

    def _emit_waits(self, e, need, dneed):
        waits = []
        ck = self.clock[e]
        for x, n in need.items():
            if ck.get(x, 0) >= n:
                continue
            waits.append(self.eng_sem(x, n))
            sn = self.snap[x].get(n)
            if sn:
                for y, m in sn.items():
                    if ck.get(y, 0) < m:
                        ck[y] = m
            if ck.get(x, 0) < n:
                ck[x] = n
        dk = self.dma_known[e]
        for t in dneed:
            if dk.get(id(t), 0) >= t.dma_cnt:
                continue
            waits.append((t.dma_sem, t.dma_cnt))
            dk[id(t)] = t.dma_cnt
        return waits

    limit = None
    limit_on = False
    opcount = 0

    def op(self, e, fn, reads=(), writes=()):
        if self.limit is not None and self.limit_on:
            self.opcount += 1
            if self.opcount > self.limit:
                return 0
        need, dneed = self._need(e, reads, writes)
        waits = self._emit_waits(e, need, dneed)
        self.count[e] += 1
        n = self.count[e]
        sem, _ = self.eng_sem(e, n)
        self.n_waits += len(waits)

        def run(h, fn=fn, waits=waits, sem=sem):
            for (s, v) in waits:
                h.wait_ge(s, v)
            fn(h).then_inc(sem, 1)
        self.streams[e].append(run)
        sn = dict(self.clock[e])
        sn[e] = n
        self.snap[e][n] = sn
        for t in reads:
            t.readers[e] = n
        for t in writes:
            t.last_w = (e, n)
            t.readers = {}
        return n

    def dma(self, e, out_ap, in_ap, tile, write, extra=()):
        reads, writes = ((), (tile,)) if write else ((tile,), ())
        need, dneed = self._need(e, reads, writes)
        waits = self._emit_waits(e, need, dneed)
        for xt in extra:
            if xt.dma_cnt:
                waits.append((xt.dma_sem, xt.dma_cnt))
        self.n_waits += len(waits)
        t = tile
        if t.dma_sem is None:
            t.dma_sem = self.new_sem(f"d{self.nsem}_" + t.name)
            self.dma_tiles.append(t)
        t.dma_cnt += 16
        dsem = t.dma_sem

        def run(h, waits=waits, dsem=dsem, out_ap=out_ap, in_ap=in_ap):
            for (s, v) in waits:
                h.wait_ge(s, v)
            h.dma_start(out=out_ap, in_=in_ap).then_inc(dsem, 16)
        self.streams[e].append(run)
        if write:
            t.last_w = None
            t.readers = {}

    def barrier(self, o, pstile, ones):
        if not hasattr(self, "_bs"):
            self._bs = {e: self.sbuf("bs_" + e, [128, 2], F32) for e in ("pe", "act", "dve", "pool")}
        bs = self._bs
        pap = pstile.ap[0:1, 0:1]
        oap = ones.ap[0:1, 0:1]
        self.op("pe", lambda h: h.matmul(pap, lhsT=oap, rhs=oap, start=True, stop=True),
                reads=[ones], writes=[pstile, bs["pe"]])
        self.op("act", lambda h: h.activation(out=bs["act"].ap[:, 0:1], in_=bs["act"].ap[:, 0:1], func=AF.Copy, scale=0.0), writes=[bs["act"]])
        self.op("dve", lambda h: h.memset(bs["dve"].ap[:, 0:1], 0.0), writes=[bs["dve"]])
        self.op("pool", lambda h: h.memset(bs["pool"].ap[:, 0:1], 0.0), writes=[bs["pool"]])
        allb = [bs[e] for e in ("pe", "act", "dve", "pool")]
        waits = [self.eng_sem(x, n) for x, n in (b.last_w for b in allb)]
        self.op("pe", lambda h: h.matmul(pap, lhsT=oap, rhs=oap, start=True, stop=True),
                reads=[ones] + allb, writes=[pstile])
        self.op("act", lambda h: h.activation(out=bs["act"].ap[:, 1:2], in_=bs["act"].ap[:, 0:1], func=AF.Copy), reads=allb, writes=[])
        self.op("dve", lambda h: h.memset(bs["dve"].ap[:, 1:2], 0.0), reads=allb, writes=[])
        self.op("pool", lambda h: h.memset(bs["pool"].ap[:, 1:2], 0.0), reads=allb, writes=[])

        def run(h, waits=waits):
            for (s_, v) in waits:
                h.wait_ge(s_, v)
        self.streams["sp"].append(run)

    def final_wait(self, e):
        waits = [(t.dma_sem, t.dma_cnt) for t in self.dma_tiles if t.dma_cnt]

        def run(h, waits=waits):
            for (s, v) in waits:
                h.wait_ge(s, v)
        self.streams[e].append(run)

    def emit(self):
        nc = self.nc
        with nc.Block() as block:
            @block.tensor
            def _(h):
                for r in self.streams["pe"]:
                    r(h)

            @block.scalar
            def _(h):
                for r in self.streams["act"]:
                    r(h)

            @block.vector
            def _(h):
                for r in self.streams["dve"]:
                    r(h)

            @block.gpsimd
            def _(h):
                for r in self.streams["pool"]:
                    r(h)

            @block.sync
            def _(h):
                for r in self.streams["sp"]:
                    r(h)


def _ap(x):
    return x.ap if isinstance(x, V) else x


def _ts(*xs):
    return [x.t for x in xs if isinstance(x, V)]


class Ops:
    def __init__(self, k):
        self.k = k

    def mm(self, out, lhsT, rhs, start=True, stop=True):
        self.k.op("pe", lambda h: h.matmul(out.ap, lhsT=lhsT.ap, rhs=rhs.ap, start=start, stop=stop),
                  reads=_ts(lhsT, rhs), writes=_ts(out))

    def tr(self, out, in_, ident):
        self.k.op("pe", lambda h: h.transpose(out.ap, in_.ap, ident.ap), reads=_ts(in_, ident), writes=_ts(out))

    def act(self, out, in_, func, bias=0.0, scale=1.0, accum=None):
        def f(h):
            kw = {}
            if accum is not None:
                kw["accum_out"] = accum.ap
            return h.activation(out=out.ap, in_=in_.ap, func=func, bias=_ap(bias), scale=_ap(scale), **kw)
        self.k.op("act", f, reads=_ts(in_, bias, scale), writes=_ts(out) + (_ts(accum) if accum is not None else []))

    def tt(self, e, out, a, b, op):
        self.k.op(e, lambda h: h.tensor_tensor(out=out.ap, in0=a.ap, in1=b.ap, op=op), reads=_ts(a, b), writes=_ts(out))

    def ts(self, e, out, a, s1, op0, s2=None, op1=None):
        def f(h):
            if op1 is None:
                return h.tensor_scalar(out=out.ap, in0=a.ap, scalar1=_ap(s1), scalar2=None, op0=op0)
            return h.tensor_scalar(out=out.ap, in0=a.ap, scalar1=_ap(s1), scalar2=_ap(s2), op0=op0, op1=op1)
        self.k.op(e, f, reads=_ts(a, s1, s2), writes=_ts(out))

    def stt(self, e, out, a, s, b, op0, op1):
        self.k.op(e, lambda h: h.scalar_tensor_tensor(out=out.ap, in0=a.ap, scalar=_ap(s), in1=b.ap, op0=op0, op1=op1),
                  reads=_ts(a, s, b), writes=_ts(out))

    def cp(self, e, out, in_):
        if e == "act":
            self.k.op("act", lambda h: h.activation(out=out.ap, in_=in_.ap, func=AF.Copy), reads=_ts(in_), writes=_ts(out))
        else:
            self.k.op(e, lambda h: h.tensor_copy(out=out.ap, in_=in_.ap), reads=_ts(in_), writes=_ts(out))

    def red(self, out, in_, op, axis=AX.X):
        self.k.op("dve", lambda h: h.tensor_reduce(out=out.ap, in_=in_.ap, axis=axis, op=op), reads=_ts(in_), writes=_ts(out))

    def recip(self, out, in_):
        self.k.op("dve", lambda h: h.reciprocal(out=out.ap, in_=in_.ap), reads=_ts(in_), writes=_ts(out))

    def memset(self, e, out, val):
        if e == "act_ms":
            self.k.op("act", lambda h: h.activation(out=out.ap, in_=out.ap, func=AF.Copy, scale=0.0), writes=_ts(out))
            return
        self.k.op(e, lambda h: h.memset(out.ap, val), writes=_ts(out))

    def load(self, e, out, src, extra=()):
        self.k.dma(e, out.ap, src, out.t, True, extra=extra)

    def store(self, e, dst, in_):
        self.k.dma(e, dst, in_.ap, in_.t, False)


def _cols(a, b):
    return list(range(a, b))


QK_COLS = (_cols(0, 256) + _cols(384, 448) * 2 + _cols(512, 576) * 2 + _cols(1964, 2220) + _cols(2220, 2476))
VN_COLS = _cols(448, 512) + _cols(576, 640)
FM_COLS = _cols(652, 908) + _cols(256, 384)
GL_COLS = _cols(640, 652)
VD_COLS = _cols(2476, 2732)
RW_COLS = _cols(908, 1964)


def _consts():
    c = {}
    c["ident_f"] = np.eye(128, dtype=np.float32)
    c["ident_b"] = np.eye(128, dtype=np.float32).astype(ml_dtypes.bfloat16)
    c["ones_f"] = np.ones((128, 128), np.float32)
    selE = np.zeros((16, 16, 128), np.float32)
    for e in range(16):
        selE[e, e, :] = 1.0
    c["selE"] = selE.reshape(16, 16 * 128)
    invf = (500000.0 ** (-2.0 * np.arange(8, dtype=np.float32) / 16.0)).astype(np.float32) / np.float32(2 * np.pi)
    c["invf"] = np.ascontiguousarray(np.broadcast_to(invf[None, :], (128, 8)), dtype=np.float32)
    p = np.arange(128)[:, None]
    q = np.arange(128)[None, :]
    c["trimask"] = np.concatenate([(p >= q), (p <= q)], axis=1).astype(np.float32).astype(ml_dtypes.bfloat16)
    bf = lambda a: np.ascontiguousarray(a, dtype=np.float32).astype(ml_dtypes.bfloat16)
    cc = np.arange(127)[:, None]
    tq = np.arange(2048)[None, :]
    c["cmpmask"] = bf(tq >= 16 * cc + 31)
    c_end = np.arange(127) * 16 + 31
    b_start = np.arange(32) * 64
    cover = np.clip(np.minimum(c_end[:, None] + 1, b_start[None, :] + 64) - np.maximum(c_end[:, None] + 1 - 32, b_start[None, :]), 0, None) / 32.0
    c["covones"] = bf(np.concatenate([cover, np.ones((127, 1))], axis=1))
    selG = np.zeros((12, 12, 128), np.float32)
    for e in range(12):
        selG[e, e, :] = 1.0
    c["selG"] = bf(selG.reshape(12, 12 * 128))
    E = np.zeros((32, 16, 128), np.float32)
    for j in range(16):
        for pp in range(128):
            E[2 * j + pp // 64, j, pp] = 1.0
    c["Esel"] = bf(E.reshape(32, 16 * 128))
    tt_ = np.arange(2048)
    cur = (tt_ // 64)[:, None]
    blk = np.arange(32)[None, :]
    forced = (blk == 0) | (blk == cur) | (blk == cur - 1)
    fut = blk > cur
    keep = (~forced & ~fut).astype(np.float32)
    add = np.where(fut, -1.0, np.where(forced, 1e4, 0.0)).astype(np.float32)
    c["selkeep"] = np.ascontiguousarray(keep.reshape(16, 128, 32).transpose(1, 0, 2))
    c["seladd"] = np.ascontiguousarray(add.reshape(16, 128, 32).transpose(1, 0, 2))
    c["trigt"] = bf(p > q)
    c["mU2"] = np.concatenate([(p < q), (p <= q)], axis=1).astype(np.float32)
    c["mL"] = (p > q).astype(np.float32)
    wwin = np.array([[2] * 64 + [4] * 64, [8] * 64 + [16] * 64], np.float32).T
    c["pool_rw"] = (1.0 / wwin).astype(np.float32)
    tcnt = np.arange(1, 17, dtype=np.float32)[None, None, :]
    c["pool_fix"] = (wwin[:, :, None] / np.minimum(tcnt, wwin[:, :, None])).astype(np.float32)
    return c


def _prep_inputs(inp):
    f = lambda a: np.ascontiguousarray(a, dtype=np.float32)
    sh = {}
    sh["ada_w"] = f(inp["ada_w"])
    sh["ada_b"] = f(inp["ada_b"].reshape(2, 48, 128).transpose(0, 2, 1))
    sh["g_mix"] = f(inp["norm_mix_g"].reshape(2, 8, 128).transpose(0, 2, 1))
    sh["g_ffn"] = f(inp["norm_ffn_g"].reshape(2, 8, 128).transpose(0, 2, 1))
    w_in = inp["w_in"]
    sh["w_qk"] = f(w_in[:, :, QK_COLS])
    sh["w_vn"] = f(w_in[:, :, VN_COLS])
    sh["w_fm"] = f(w_in[:, :, FM_COLS])
    sh["w_gl"] = f(w_in[:, :, GL_COLS])
    sh["w_vd"] = f(w_in[:, :, VD_COLS])
    sh["w_rw"] = f(w_in[:, :, RW_COLS])
    sh["w_dqk"] = f(w_in[:, :, _cols(1964, 2476)])
    sh["w_nqk"] = f(w_in[:, :, _cols(0, 256) + _cols(384, 448) * 2 + _cols(512, 576) * 2])
    gd = np.concatenate([np.tile(inp["dil_q_norm"], (1, 4)), np.tile(inp["dil_k_norm"], (1, 4))], axis=1)
    sh["g_dqk"] = f(np.broadcast_to(gd[:, None, :], (2, 128, 512)))
    gn = np.concatenate([np.tile(inp["nsa_q_norm"], (1, 4)), np.tile(inp["nsa_k_norm"][:, 1], (1, 2)),
                         np.tile(inp["nsa_k_norm"][:, 2], (1, 2))], axis=1)
    sh["g_nqk"] = f(np.broadcast_to(gn[:, None, :], (2, 128, 512)))
    sh["cmp_w1"] = f(inp["nsa_cmp_w1"])
    sh["cmp_w2"] = f(inp["nsa_cmp_w2"])
    sh["cmp_peT"] = f(inp["nsa_cmp_pe"].transpose(0, 1, 3, 2).reshape(2, 128, 32))
    gk = np.tile(inp["nsa_k_norm"][:, 0], (1, 2))
    sh["g_kcmp"] = f(np.broadcast_to(gk[:, None, :], (2, 128, 128)))
    rowb = lambda a: f(np.broadcast_to(a[:, None, :], (a.shape[0], 128, a.shape[1])))
    sh["rw_mu"] = rowb(inp["rwkv_mu"])
    sh["rw_w0"] = rowb(inp["rwkv_w0"])
    sh["rw_a0"] = rowb(inp["rwkv_a0"])
    sh["rw_kk"] = rowb(inp["rwkv_k_k"])
    sh["rw_ka"] = rowb(inp["rwkv_k_a"])
    sh["rw_rk"] = rowb(inp["rwkv_r_k"].reshape(2, 256))
    sh["rw_lnw"] = rowb(inp["rwkv_ln_w"])
    sh["rw_lnb"] = rowb(inp["rwkv_ln_b"])
    sh["rw_v0"] = rowb(np.concatenate([np.zeros_like(inp["rwkv_v0"]), inp["rwkv_v0"]], axis=0))
    sh["rw_w2"] = f(inp["rwkv_w2"])
    sh["rw_a2"] = f(inp["rwkv_a2"])
    sh["rw_g2"] = f(inp["rwkv_g2"])
    sh["rw_v1"] = f(inp["rwkv_v1"])
    sh["rw_v2"] = f(inp["rwkv_v2"])
    sh["pool_w"] = f(inp["pool_w"])
    sh["pool_scale"] = f(inp["pool_scale"].reshape(2, 2, 128).transpose(0, 2, 1))
    sh["w_out"] = f(inp["w_out"])
    sh["router_w"] = f(inp["router_w"])
    sh["router_b"] = f(np.broadcast_to(inp["router_b"][None, :], (128, 16)))
    sh["moe_wg"] = f(inp["moe_w_gate"])
    sh["moe_wu"] = f(inp["moe_w_up"])
    sh["moe_wd"] = f(inp["moe_w_down"])
    sh.update(_consts())
    per = []
    for b in range(8):
        d = {}
        d["xT"] = f(inp["x"][b].T)
        d["cvec"] = f(inp["c"][b].reshape(8, 128).T)
        d["pos_tm"] = np.ascontiguousarray(inp["positions"][b].reshape(16, 128).T.astype(np.int32))
        d["pos_ce"] = np.ascontiguousarray(inp["positions"][b][31::16][:127].reshape(127, 1).astype(np.int32))
        per.append(d)
    return sh, per


def build(shapes, dbg=None, n_layers=2):
    dbg = dbg or {}
    nc = bass.Bass("TRN2", target_bir_lowering=False)
    dram = {}
    for name, (shape, dt) in shapes.items():
        dram[name] = nc.dram_tensor(name, list(shape), dt, kind="ExternalInput").ap()
    outT = nc.dram_tensor("outT", [D, S], F32, kind="ExternalOutput").ap()
    dbg_out = {}
    for name, shape in dbg.get("outs", {}).items():
        dbg_out[name] = nc.dram_tensor(name, list(shape), F32, kind="ExternalOutput").ap()

    with ExitStack() as st:
        k = Sched(nc, st)
        o = Ops(k)
        xT = k.sbuf("xT", [128, 8, S], F32)
        hT = k.sbuf("hT", [128, 8, S + 1], BF16)
        mixT = k.sbuf("mixT", [128, 2, S], BF16)
        wA = k.sbuf("wA", [128, 8704], BF16)
        wB = k.sbuf("wB", [128, 8704], BF16)
        wo = k.sbuf("wo", [128, 2, D], BF16)
        ident_f = k.sbuf("ident_f", [128, 128], F32)
        ident_b = k.sbuf("ident_b", [128, 128], BF16)
        ones_f = k.sbuf("ones_f", [128, 128], F32)
        cvec = k.sbuf("cvec", [128, 8], F32)
        scv = k.sbuf("scv", [128, 8], F32)
        mods = [k.sbuf(f"mod{l}", [128, 48], F32) for l in range(2)]
        adab = [k.sbuf(f"adab{l}", [128, 48], F32) for l in range(2)]
        gs1 = [k.sbuf(f"gs1_{l}", [128, 8], F32) for l in range(2)]
        gs2 = [k.sbuf(f"gs2_{l}", [128, 8], F32) for l in range(2)]
        gmx = [k.sbuf(f"gmx{l}", [128, 8], F32) for l in range(2)]
        gff = [k.sbuf(f"gff{l}", [128, 8], F32) for l in range(2)]
        rw_sb = k.sbuf("rw_sb", [128, 8, 16], F32)
        rb_sb = k.sbuf("rb_sb", [128, 16], F32)
        rbias = k.sbuf("rbias", [16, 1], F32)
        sq = [k.sbuf(f"sq{i}", [128, 512], F32) for i in range(2)]
        tmpf = [k.sbuf(f"tmpf{i}", [128, 512], F32) for i in range(2)]
        rstd = k.sbuf("rstd", [128, 512], F32)
        ps = [k.psum(f"ps{i}", [128, 512], F32) for i in range(7)]
        pT = k.psum("pT", [128, 1024], BF16)
        cosT = k.sbuf("cosT", [128, NT, 8], F32)
        sinT = k.sbuf("sinT", [128, NT, 8], F32)
        trimask = k.sbuf("trimask", [128, 256], BF16)
        ARENA = 48 * 1024
        arena = k.sbuf("arena", [128, ARENA // 2], BF16)
        ar_state = {"off": 0}

        def ar_reset():
            k.barrier(o, ps[6], ones_f)
            ar_state["off"] = 0
            ar_state["offA"] = 0
            ar_state["offB"] = 0

        def ar_rewind(off):
            k.barrier(o, ps[6], ones_f)
            ar_state["off"] = off

        def ar(name, shape, dt, pool="main"):
            esz = 4 if dt in (F32, I32) else 2
            n = int(np.prod(shape[1:]))
            nb = (n * esz + 3) // 4 * 4
            key = "off" if pool == "main" else "off" + pool
            base_ap, cap = {"main": (arena.ap, ARENA), "A": (wA.ap, 17408), "B": (wB.ap, 8192)}[pool]
            off = ar_state.get(key, 0)
            assert off + nb <= cap, (name, pool, off, nb)
            ar_state[key] = off + nb
            ap = base_ap[0:shape[0], off // 2:(off + nb) // 2]
            if dt != BF16:
                ap = ap.bitcast(dt)
            ap = ap[:, 0:n]
            if len(shape) == 3:
                ap = ap.rearrange("p (a b) -> p a b", a=shape[1])
            elif len(shape) == 4:
                ap = ap.rearrange("p (a b c) -> p a b c", a=shape[1], b=shape[2])
            return T(name, ap)

        o.load("sp", ident_f[:], dram["ident_f"])
        o.load("pool", ident_b[:], dram["ident_b"])
        o.load("sp", ones_f[:], dram["ones_f"])
        o.load("sp", cvec[:], dram["cvec"])
        o.load("sp", xT[:], dram["xT"].rearrange("(c p) t -> p c t", p=128))
        o.load("sp", rw_sb[:], dram["router_w"].rearrange("(c p) e -> p c e", p=128))
        o.load("sp", rb_sb[:], dram["router_b"])
        for l in range(2):
            o.load("sp", adab[l][:], dram["ada_b"][l])
            o.load("sp", gmx[l][:], dram["g_mix"][l])
            o.load("sp", gff[l][:], dram["g_ffn"][l])
        o.memset("pool", hT[:, :, 0:1], 0.0)

        o.act(scv[:], cvec[:], AF.Silu)
        stage = [wA[:, 0:4096].cast(F32).re("p (c e) -> p c e", c=8), wB[:, 0:4096].cast(F32).re("p (c e) -> p c e", c=8)]
        for l in range(0 if dbg.get("only") else n_layers):
            for blk in range(24):
                sg = stage[blk % 2]
                o.load("sp", sg, dram["ada_w"][l][:, blk * 256:(blk + 1) * 256].rearrange("(c p) e -> p c e", p=128))
                for jj in range(2):
                    j = blk * 2 + jj
                    for c in range(8):
                        o.mm(ps[0][:, j:j + 1], sg[:, c, jj * 128:(jj + 1) * 128], scv[:, c:c + 1], start=(c == 0), stop=(c == 7))
            o.tt("dve", mods[l][:], ps[0][:, 0:48], adab[l][:], ALU.add)
            o.stt("dve", gs1[l][:], mods[l][:, 8:16], 1.0, gmx[l][:], ALU.add, ALU.mult)
            o.stt("dve", gs2[l][:], mods[l][:, 32:40], 1.0, gff[l][:], ALU.add, ALU.mult)
        if "mods" in dbg_out:
            o.store("sp", dbg_out["mods"][0], mods[0][:])
            o.store("sp", dbg_out["mods"][1], mods[1][:])

        def norm(l, gs, sh_off, router=False):
            sh = mods[l]
            if router:
                for c in range(8):
                    o.mm(ps[5][0:16, 0:1], rw_sb[:, c, :], sh[:, sh_off + c:sh_off + c + 1], start=(c == 0), stop=(c == 7))
                o.cp("dve", rbias[:], ps[5][0:16, 0:1])
            for tb in range(NB):
                tsl = slice(tb * 512, (tb + 1) * 512)
                for c in range(8):
                    s_ = sq[c % 2]
                    o.act(s_[:], xT[:, c, tsl], AF.Square)
                    o.mm(ps[0][:], ones_f[:], s_[:], start=(c == 0), stop=(c == 7))
                o.act(rstd[:], ps[0][:], AF.Sqrt, bias=EPS, scale=1.0 / D)
                o.recip(rstd[:], rstd[:])
                for c in range(8):
                    t_ = tmpf[c % 2]
                    o.stt("dve", t_[:], xT[:, c, tsl], gs[:, c:c + 1], rstd[:], ALU.mult, ALU.mult)
                    o.act(hT[:, c, 1 + tb * 512:1 + (tb + 1) * 512], t_[:], AF.Identity, bias=sh[:, sh_off + c:sh_off + c + 1])
                    if router:
                        o.mm(ps[1][0:16, :], rw_sb[:, c, :], t_[:], start=(c == 0), stop=(c == 7))
                if router:
                    o.act(F["affT"][:, tsl], ps[1][0:16, :], AF.Sigmoid, bias=rbias[:])

        def out_proj(l, grp):
            o.load("pool", wo[:], dram["w_out"][l][grp * 256:(grp + 1) * 256, :].rearrange("(c p) e -> p c e", p=128))
            g1 = mods[l][:, 16:24]
            for tb in range(NB):
                tsl = slice(tb * 512, (tb + 1) * 512)
                for dc in range(8):
                    p_ = ps[2 + (dc % 2)]
                    for kc in range(2):
                        o.mm(p_[:], wo[:, kc, dc * 128:(dc + 1) * 128], mixT[:, kc, tsl], start=(kc == 0), stop=(kc == 1))
                    o.stt("dve", xT[:, dc, tsl], p_[:], g1[:, dc:dc + 1], xT[:, dc, tsl], ALU.mult, ALU.add)

        rt_tiles = {}
        moe_tiles = {}
        F = {}

        def ffn_alloc():
            ar_reset()
            F["affT"] = ar("affT", [16, S], F32)
            F["gatesT"] = ar("gatesT", [16, S], F32)
            F["selE"] = ar("selE", [16, 16, 128], F32)
            o.load("sp", F["selE"][:], dram["selE"].rearrange("k (e m) -> k e m", e=16))
            rt_tiles.update(dict(
                aff=ar("r_aff", [128, NT, 16], F32), sel=ar("r_sel", [128, NT, 16], F32),
                sel2=ar("r_sel2", [128, NT, 16], F32), m1=ar("r_m1", [128, NT * 4], F32),
                m2=ar("r_m2", [128, NT * 4], F32), gsc=ar("r_gsc", [128, NT, 4], F32),
                gm=ar("r_gm", [128, NT], F32), den=ar("r_den", [128, NT], F32)))
            moe_tiles["gb"] = [ar(f"m_gb{i}", [128, 512], F32) for i in range(2)]
            moe_tiles["sg"] = [ar(f"m_sg{i}", [128, 512], BF16) for i in range(2)]
            moe_tiles["u2"] = [ar(f"m_u2{i}", [128, 512], BF16) for i in range(2)]
            moe_tiles["aT"] = [ar(f"m_aT{i}", [128, 2, 512], BF16) for i in range(2)]

        def routing():
            aff, sel, sel2, m1, m2, gsc, gm, den = (rt_tiles[n] for n in ("aff", "sel", "sel2", "m1", "m2", "gsc", "gm", "den"))
            for t in range(NT):
                o.tr(ps[4][:, t * 16:(t + 1) * 16], F["affT"][:, t * 128:(t + 1) * 128], ident_f[0:16, 0:16])
            o.cp("dve", aff[:].re("p t e -> p (t e)"), ps[4][:, 0:256])
            o.tt("dve", sel[:], aff[:], rb_sb[:].re("p (o e) -> p o e", o=1).bc([128, NT, 16]), ALU.add)
            s4 = sel[:].re("p t (g j) -> p (t g) j", g=4)
            o.red(m1[:], s4, ALU.max)
            o.tt("dve", sel2[:].re("p t (g j) -> p (t g) j", g=4), s4, m1[:].re("p (a o) -> p a o", o=1).bc([128, NT * 4, 4]), ALU.is_ge)
            o.stt("dve", sel2[:], sel2[:], -1e9, sel[:], ALU.mult, ALU.add)
            o.red(m2[:], sel2[:].re("p t (g j) -> p (t g) j", g=4), ALU.max)
            o.tt("dve", gsc[:].re("p t g -> p (t g)"), m1[:], m2[:], ALU.add)
            o.red(gm[:], gsc[:], ALU.max)
            o.tt("dve", gsc[:], gsc[:], gm[:].re("p (t o) -> p t o", o=1).bc([128, NT, 4]), ALU.is_ge)
            o.tt("dve", sel2[:].re("p t (g j) -> p (t g) j", g=4), s4, m2[:].re("p (a o) -> p a o", o=1).bc([128, NT * 4, 4]), ALU.is_ge)
            o.tt("dve", sel2[:].re("p t (g j) -> p (t g) j", g=4), sel2[:].re("p t (g j) -> p (t g) j", g=4),
                 gsc[:].re("p t (g o) -> p (t g) o", o=1).bc([128, NT * 4, 4]), ALU.mult)
            o.tt("dve", sel2[:], sel2[:], aff[:], ALU.mult)
            o.red(den[:], sel2[:], ALU.add)
            o.recip(den[:], den[:])
            o.tt("dve", sel2[:], sel2[:], den[:].re("p (t o) -> p t o", o=1).bc([128, NT, 16]), ALU.mult)
            for g4 in range(4):
                for t4 in range(4):
                    t = g4 * 4 + t4
                    o.tr(ps[5][0:16, t4 * 128:(t4 + 1) * 128], sel2[:, t, :], ident_f[:])
                o.cp("act", F["gatesT"][:, g4 * 512:(g4 + 1) * 512], ps[5][0:16, :])


        def moe(l):
            g2 = mods[l][:, 40:48]

            def wviews(e):
                wb = (wA, wB)[e % 2]
                return (wb[:, 0:2048].re("p (c f) -> p c f", c=8), wb[:, 2048:4096].re("p (c f) -> p c f", c=8),
                        wb[:, 4096:6144].re("p (c d) -> p c d", c=2))

            def load_w(e):
                wg, wu, wd = wviews(e)
                o.load("pool", wg, dram["moe_wg"][l, e].rearrange("(c p) f -> p c f", p=128))
                o.load("pool", wu, dram["moe_wu"][l, e].rearrange("(c p) f -> p c f", p=128))
                o.load("pool", wd, dram["moe_wd"][l, e].rearrange("(c p) d -> p c d", p=128))

            def down(wd, aT, tsl):
                for dc in range(8):
                    pd = ps[4 + (dc % 2)]
                    for fc in range(2):
                        o.mm(pd[:], wd[:, fc, dc * 128:(dc + 1) * 128], aT[:, fc, :], start=(fc == 0), stop=(fc == 1))
                    o.stt("dve", xT[:, dc, tsl], pd[:], g2[:, dc:dc + 1], xT[:, dc, tsl], ALU.mult, ALU.add)

            load_w(0)
            pending = None
            it = 0
            for e in range(16):
                wg, wu, wd = wviews(e)
                for tb in range(NB):
                    tsl = slice(tb * 512, (tb + 1) * 512)
                    hsl = slice(1 + tb * 512, 1 + (tb + 1) * 512)
                    gb = moe_tiles["gb"][it % 2]
                    aT = moe_tiles["aT"][it % 2]
                    it += 1
                    o.mm(ps[6][:], F["selE"][:, e, :], F["gatesT"][:, tsl])
                    o.cp("act", gb[:], ps[6][:])
                    for fc in range(2):
                        pg, pu = ps[0 + fc], ps[2 + fc]
                        for c in range(8):
                            o.mm(pg[:], wg[:, c, fc * 128:(fc + 1) * 128], hT[:, c, hsl], start=(c == 0), stop=(c == 7))
                        for c in range(8):
                            o.mm(pu[:], wu[:, c, fc * 128:(fc + 1) * 128], hT[:, c, hsl], start=(c == 0), stop=(c == 7))
                        sg = moe_tiles["sg"][fc]
                        u2 = moe_tiles["u2"][fc]
                        o.act(sg[:], pg[:], AF.Silu)
                        o.tt("dve", u2[:], pu[:], gb[:], ALU.mult)
                        o.tt("pool", aT[:, fc, :], sg[:], u2[:], ALU.mult)
                    if pending is not None:
                        pending()
                    if tb == 0 and e + 1 < 16:
                        load_w(e + 1)
                    pending = (lambda wd=wd, aT=aT, tsl=tsl: down(wd, aT, tsl))
            pending()

        MAGIC = 12582912.0

        def rope_table(dst_cos, dst_sin, pos_i, nparts, ncol, tmp_y, tmp_r, posf, invf_t):
            o.cp("dve", posf, pos_i)
            o.tt("dve", tmp_y, posf.re("p (t o) -> p t o", o=1).bc([nparts, ncol, 8]),
                 invf_t.re("p (o f) -> p o f", o=1).bc([nparts, ncol, 8]), ALU.mult)
            for dst, shift in ((dst_sin, 0.0), (dst_cos, 0.25)):
                o.ts("dve", tmp_r, tmp_y, shift, ALU.add, MAGIC, ALU.add)
                o.ts("dve", tmp_r, tmp_r, MAGIC, ALU.subtract)
                o.stt("dve", tmp_r, tmp_y, shift, tmp_r, ALU.add, ALU.subtract)
                o.act(dst, tmp_r, AF.Sin, scale=6.28318)

        invf_t = k.sbuf("invf", [128, 8], F32)
        posi = k.sbuf("posi", [128, NT], I32)
        posf = k.sbuf("posf", [128, NT], F32)
        rt_y = k.sbuf("rt_y", [128, NT, 8], F32)
        rt_r = k.sbuf("rt_r", [128, NT, 8], F32)
        o.load("sp", invf_t[:], dram["invf"])
        o.load("sp", posi[:], dram["pos_tm"])
        o.load("pool", trimask[:], dram["trimask"])
        rope_table(cosT[:], sinT[:], posi[:], 128, NT, rt_y[:], rt_r[:], posf[:], invf_t[:])

        def proj_qk(l, wname, gname, qkT, wbuf, sc):
            o.load("pool", wbuf, dram[wname][l].rearrange("(c p) e -> p c e", p=128))
            o.load("sp", sc["gain"][:], dram[gname][l])
            t1, t2, yb, ss = sc["t1"], sc["t2"], sc["yb"], sc["ss"]
            ra, rb, rc, rd = sc["ra"], sc["rb"], sc["rc"], sc["rd"]
            for tt in range(NT):
                hs = slice(1 + tt * 128, 1 + (tt + 1) * 128)
                for c in range(8):
                    o.mm(ps[0][:], hT[:, c, hs], wbuf[:, c, :], start=(c == 0), stop=(c == 7))
                o.act(t1[:], ps[0][:], AF.Square)
                o.red(ss[:], t1[:].re("p (h d) -> p h d", h=8), ALU.add)
                o.act(ss[:], ss[:], AF.Sqrt, bias=EPS, scale=1.0 / 64)
                o.recip(ss[:], ss[:])
                o.tt("dve", t2[:].re("p (h d) -> p h d", h=8), ps[0][:].re("p (h d) -> p h d", h=8),
                     ss[:].re("p (h o) -> p h o", o=1).bc([128, 8, 64]), ALU.mult)
                o.tt("pool", t2[:], t2[:], sc["gain"][:], ALU.mult)
                y3 = t2[:].re("p (h d) -> p h d", h=8)
                yb3 = yb[:].re("p (h d) -> p h d", h=8)
                cs = cosT[:, tt, :].re("p (o f) -> p o f", o=1).bc([128, 8, 8])
                sn = sinT[:, tt, :].re("p (o f) -> p o f", o=1).bc([128, 8, 8])
                o.tt("dve", ra[:], y3[:, :, 0:8], cs, ALU.mult)
                o.tt("pool", rb[:], y3[:, :, 8:16], sn, ALU.mult)
                o.tt("dve", yb3[:, :, 0:8], ra[:], rb[:], ALU.subtract)
                o.tt("pool", rc[:], y3[:, :, 8:16], cs, ALU.mult)
                o.tt("dve", rd[:], y3[:, :, 0:8], sn, ALU.mult)
                o.tt("pool", yb3[:, :, 8:16], rc[:], rd[:], ALU.add)
                o.cp("act", yb3[:, :, 16:64], y3[:, :, 16:64])
                for j in range(4):
                    o.tr(pT[:, j * 128:(j + 1) * 128], yb[:, j * 128:(j + 1) * 128], ident_b[:])
                o.cp("act", qkT[:, :, tt * 128:(tt + 1) * 128], pT[:, 0:512].re("p (j t) -> p j t", j=4))

        def qk_scratch():
            return dict(gain=ar("gain", [128, 512], F32), t1=ar("t1", [128, 512], F32), t2=ar("t2", [128, 512], F32),
                        yb=ar("yb", [128, 512], BF16), ss=ar("ss", [128, 8], F32),
                        ra=ar("ra", [128, 8, 8], F32), rb=ar("rb", [128, 8, 8], F32),
                        rc=ar("rc", [128, 8, 8], F32), rd=ar("rd", [128, 8, 8], F32))

        def dilated(l):
            ar_reset()
            qkT = ar("qkT_d", [128, 4, S], BF16)
            mark = ar_state["off"]
            sc = qk_scratch()
            wq = wB[:, 0:4096].re("p (c e) -> p c e", c=8)
            wv = wB[:, 4096:6144].re("p (c e) -> p c e", c=8)
            proj_qk(l, "w_dqk", "g_dqk", qkT, wq, sc)
            ar_rewind(mark)
            Vd = ar("Vd", [128, NT, 256], BF16)
            acc = [ar(f"acc{i}", [128, S], F32) for i in range(2)]
            pt = [ar(f"pt{i}", [128, 256], BF16) for i in range(2)]
            dtmp = sq[0]
            o.load("pool", wv, dram["w_vd"][l].rearrange("(c p) e -> p c e", p=128))
            o.memset("pool", Vd[:, :, 64:192], 1.0)
            it = 0
            for hp in range(2):
                for pi, dil in enumerate((1, 4, 16)):
                    nblk = (S // dil) // 128
                    for r in range(dil):
                        for m in range(nblk):
                            oi = r * nblk + m
                            st_ = 1 + r + dil * 128 * m
                            for c in range(8):
                                o.mm(ps[4][:, 0:128], hT[:, c, st_:st_ + dil * 127 + 1:dil], wv[:, c, hp * 128:(hp + 1) * 128], start=(c == 0), stop=(c == 7))
                            o.cp("act", Vd[:, oi, :].re("p (a b) -> p a b", b=64)[:, 0:4:3, :], ps[4][:, 0:128].re("p (a b) -> p a b", a=2))
                    items = []
                    for hh in range(2):
                        h = hp * 2 + hh
                        base = hh * 64
                        a_ = acc[hh]
                        for r in range(dil):
                            for n in range(nblk):
                                ms = [m for m in (n - 1, n) if m >= 0]
                                ncols = 128 * len(ms)
                                qcols = slice(r + dil * 128 * n, r + dil * 128 * n + dil * 127 + 1, dil)
                                pss = ps[it % 2]
                                pso = ps[2 + it % 2]
                                p_ = pt[it % 2]
                                it += 1

                                def A(ms=ms, ncols=ncols, qcols=qcols, pss=pss, p_=p_, base=base, r=r):
                                    for j, m in enumerate(ms):
                                        kcols = slice(r + dil * 128 * m, r + dil * 128 * m + dil * 127 + 1, dil)
                                        o.mm(pss[:, j * 128:(j + 1) * 128], qkT[base:base + 64, 2 + hp, kcols], qkT[base:base + 64, hp, qcols])
                                    o.act(p_[:, 0:ncols], pss[:, 0:ncols], AF.Exp, scale=0.125)
                                    o.tt("pool", p_[:, 0:ncols], p_[:, 0:ncols], trimask[:, 256 - ncols:256], ALU.mult)

                                def B(ms=ms, qcols=qcols, pso=pso, p_=p_, hh=hh, a_=a_, r=r):
                                    for j, m in enumerate(ms):
                                        oi = r * nblk + m
                                        lv = Vd[:, oi, hh * 128:(hh + 1) * 128]
                                        o.mm(pso[:, 0:128], lv, p_[:, j * 128:(j + 1) * 128], start=(j == 0), stop=(j == len(ms) - 1))
                                    if pi == 0:
                                        o.cp("dve", a_[:, qcols], pso[:, 0:128])
                                    else:
                                        o.tt("dve", a_[:, qcols], pso[:, 0:128], a_[:, qcols], ALU.add)
                                items.append((A, B))
                    prevB = None
                    for A, B in items:
                        A()
                        if prevB is not None:
                            prevB()
                        prevB = B
                    prevB()
                for hh in range(2):
                    a_ = acc[hh]
                    ob, db = (0, 64) if hh == 0 else (64, 0)
                    for tb in range(NB):
                        tsl = slice(tb * 512, (tb + 1) * 512)
                        o.cp("act", dtmp[ob:ob + 64, :], a_[db:db + 64, tsl])
                        o.recip(dtmp[ob:ob + 64, :], dtmp[ob:ob + 64, :])
                        o.tt("dve", mixT[ob:ob + 64, hp, tsl], a_[ob:ob + 64, tsl], dtmp[ob:ob + 64, :], ALU.mult)


        def nsa(l):
            ar_reset()
            qkT = ar("qkT_n", [128, 4, S], BF16)
            mark = ar_state["off"]
            sc = qk_scratch()
            wq = wB[:, 0:4096].re("p (c e) -> p c e", c=8)
            wv = wB[:, 4096:5120].re("p (c e) -> p c e", c=8)
            wf = wB[:, 5120:6144].re("p (c e) -> p c e", c=8)
            wg = wB[:, 6144:6240].re("p (c e) -> p c e", c=8)
            proj_qk(l, "w_nqk", "g_nqk", qkT, wq, sc)
            ar_rewind(mark)
            Vn = ar("Vn", [128, NT, 4, 128], BF16)
            kcvc = ar("kcvc", [128, S], BF16, "A")
            sgT = ar("sgT", [12, S], BF16, "B")
            selT = ar("selT", [32, S], BF16, "B")
            cmpmask = ar("cmpmask", [127, 512], BF16)
            o.load("pool", wv, dram["w_vn"][l].rearrange("(c p) e -> p c e", p=128))
            o.load("pool", wf, dram["w_fm"][l][:, 256:384].rearrange("(c p) e -> p c e", p=128))
            o.load("pool", wg, dram["w_gl"][l].rearrange("(c p) e -> p c e", p=128))
            small = {}
            for nm, shp, dt, pl in (("covones", [127, 33], BF16, "main"), ("selG", [12, 12, 128], BF16, "main"), ("Esel", [32, 16, 128], BF16, "A"),
                                    ("selkeep", [128, 32], F32, "main"), ("seladd", [128, 32], F32, "main"), ("trigt", [128, 128], BF16, "main")):
                small[nm] = ar("c_" + nm, shp, dt, pl)
            o.load("pool", small["covones"][:], dram["covones"])
            o.load("pool", small["selG"][:], dram["selG"].rearrange("k (e m) -> k e m", e=12))
            o.load("pool", small["Esel"][:], dram["Esel"].rearrange("k (e m) -> k e m", e=16))
            o.load("pool", small["trigt"][:], dram["trigt"])
            o.memset("pool", Vn[:], 1.0)
            for tt in range(NT):
                hs = slice(1 + tt * 128, 1 + (tt + 1) * 128)
                for c in range(8):
                    o.mm(ps[4][:, 0:128], hT[:, c, hs], wv[:, c, :], start=(c == 0), stop=(c == 7))
                vflat = Vn[:, tt, :, :].re("p a b -> p (a b)")
                o.cp("act", vflat[:, 0:64], ps[4][:, 0:64])
                o.cp("act", vflat[:, 192:256], ps[4][:, 0:64])
                o.cp("dve", vflat[:, 256:320], ps[4][:, 64:128])
                o.cp("dve", vflat[:, 448:512], ps[4][:, 64:128])
            for tb in range(NB):
                hsl = slice(1 + tb * 512, 1 + (tb + 1) * 512)
                tsl = slice(tb * 512, (tb + 1) * 512)
                for c in range(8):
                    o.mm(ps[0][:], wf[:, c, :], hT[:, c, hsl], start=(c == 0), stop=(c == 7))
                o.cp("act", kcvc[:, tsl], ps[0][:])
                for c in range(8):
                    o.mm(ps[1][0:12, :], wg[:, c, :], hT[:, c, hsl], start=(c == 0), stop=(c == 7))
                o.act(sgT[:, tsl], ps[1][0:12, :], AF.Sigmoid)
            w1t = ar("w1t", [128, 32, 64], BF16, "A")
            peT = ar("peT", [128, 32], BF16, "A")
            w2t = ar("w2t", [64, 2, 64], BF16, "A")
            gel = ar("gel", [64, 2, 128], BF16, "A")
            for j in range(2):
                o.load("pool", w1t[j * 64:(j + 1) * 64, :, :], dram["cmp_w1"][l, j].rearrange("(i d) f -> d i f", d=64))
                o.load("pool", w2t[:, j, :], dram["cmp_w2"][l, j])
            o.load("pool", peT[:], dram["cmp_peT"][l])
            for j in range(2):
                b0 = j * 64
                for i in range(32):
                    o.mm(ps[2][0:64, 0:127], w1t[b0:b0 + 64, i, :], kcvc[b0:b0 + 64, i:i + 16 * 126 + 1:16], start=(i == 0), stop=False)
                for i in range(32):
                    o.mm(ps[2][0:64, 0:127], w1t[b0:b0 + 64, i, :], peT[b0:b0 + 64, i:i + 1].bc([64, 127]), start=False, stop=(i == 31))
                o.act(gel[:, j, 0:127], ps[2][0:64, 0:127], AF.Gelu_apprx_tanh)
            kc_f = ar("kc_f", [128, 128], F32, "A")
            kc_b = ar("kc_b", [128, 128], BF16, "A")
            kc_sq = ar("kc_sq", [128, 128], F32, "A")
            gkc = ar("gkc", [128, 128], F32, "A")
            kss = ar("kss", [128, 2], F32, "A")
            kcT = ar("kcT", [128, 128], BF16, "A")
            vc_aug = ar("vc_aug", [128, 2, 128], BF16)
            pce_i = ar("pce_i", [128, 1], I32, "A")
            pce_f = ar("pce_f", [128, 1], F32, "A")
            cy = ar("cy", [128, 1, 8], F32, "A")
            cr = ar("cr", [128, 1, 8], F32, "A")
            ccos = ar("ccos", [128, 1, 8], F32, "A")
            csin = ar("csin", [128, 1, 8], F32, "A")
            cra = ar("cra", [128, 2, 8], F32, "A")
            crb = ar("crb", [128, 2, 8], F32, "A")
            o.load("sp", gkc[:], dram["g_kcmp"][l])
            o.load("sp", pce_i[0:127, :], dram["pos_ce"])
            rope_table(ccos[0:127], csin[0:127], pce_i[0:127, :], 127, 1, cy[0:127], cr[0:127], pce_f[0:127, :], invf_t[0:127, :])
            for d2 in range(2):
                o.mm(ps[3][0:127, d2 * 64:(d2 + 1) * 64], gel[:, 0, 0:127], w2t[:, 0, :])
            o.mm(ps[3][0:127, 128:192], gel[:, 1, 0:127], w2t[:, 1, :])
            R = slice(0, 127)
            o.act(kc_sq[R, :], ps[3][R, 0:128], AF.Square)
            o.red(kss[R, :], kc_sq[R, :].re("p (h d) -> p h d", h=2), ALU.add)
            o.act(kss[R, :], kss[R, :], AF.Sqrt, bias=EPS, scale=1.0 / 64)
            o.recip(kss[R, :], kss[R, :])
            o.tt("dve", kc_f[R, :].re("p (h d) -> p h d", h=2), ps[3][R, 0:128].re("p (h d) -> p h d", h=2),
                 kss[R, :].re("p (h o) -> p h o", o=1).bc([127, 2, 64]), ALU.mult)
            o.tt("dve", kc_f[R, :], kc_f[R, :], gkc[R, :], ALU.mult)
            y3 = kc_f[R, :].re("p (h d) -> p h d", h=2)
            yb3 = kc_b[R, :].re("p (h d) -> p h d", h=2)
            cs = ccos[R, :, :].bc([127, 2, 8])
            sn = csin[R, :, :].bc([127, 2, 8])
            o.tt("dve", cra[R], y3[:, :, 0:8], cs, ALU.mult)
            o.tt("dve", crb[R], y3[:, :, 8:16], sn, ALU.mult)
            o.tt("dve", yb3[:, :, 0:8], cra[R], crb[R], ALU.subtract)
            o.tt("dve", cra[R], y3[:, :, 8:16], cs, ALU.mult)
            o.tt("dve", crb[R], y3[:, :, 0:8], sn, ALU.mult)
            o.tt("dve", yb3[:, :, 8:16], cra[R], crb[R], ALU.add)
            o.cp("act", yb3[:, :, 16:64], y3[:, :, 16:64])
            o.tr(pT[:, 0:127], kc_b[R, :], ident_b[0:127, 0:127])
            o.cp("act", kcT[:, 0:127], pT[:, 0:127])
            o.memset("pool", vc_aug[:], 1.0)
            o.cp("act", vc_aug[R, 0, 0:64], ps[3][R, 128:192])
            o.cp("act", vc_aug[R, 1, 64:128], ps[3][R, 128:192])

            wgt = ar("wgt", [128, 512], F32)
            ctr = ar("ctr", [128, 512], BF16)
            first = {}

            def epilogue(pso, ncol, h, br, tcol0, clamp=False):
                ob, db = (0, 64) if h % 2 == 0 else (64, 0)
                tsl_ = slice(tcol0, tcol0 + ncol)
                o.cp("act", wgt[ob:ob + 64, 0:ncol], pso[db:db + 64, 0:ncol])
                if clamp:
                    o.ts("dve", wgt[ob:ob + 64, 0:ncol], wgt[ob:ob + 64, 0:ncol], 1e-30, ALU.max)
                o.recip(wgt[ob:ob + 64, 0:ncol], wgt[ob:ob + 64, 0:ncol])
                o.mm(ps[6][:, 0:ncol], small["selG"][:, h * 3 + br, :], sgT[:, tsl_])
                o.tt("dve", wgt[ob:ob + 64, 0:ncol], wgt[ob:ob + 64, 0:ncol], ps[6][ob:ob + 64, 0:ncol], ALU.mult)
                dst = mixT[ob:ob + 64, h // 2, tsl_]
                if br == 0:
                    o.tt("dve", dst, pso[ob:ob + 64, 0:ncol], wgt[ob:ob + 64, 0:ncol], ALU.mult)
                else:
                    o.tt("dve", ctr[ob:ob + 64, 0:ncol], pso[ob:ob + 64, 0:ncol], wgt[ob:ob + 64, 0:ncol], ALU.mult)
                    o.tt("pool", dst, dst, ctr[ob:ob + 64, 0:ncol], ALU.add)

            eT = [ar(f"eT{i}", [128, 512], BF16) for i in range(2)]
            imp = ar("imp", [128, 32], F32)
            imp2 = ar("imp2", [128, 32], F32)
            mx8 = ar("mx8", [128, 8], F32)
            rdn = ar("rdn", [128, 4], F32)
            selm = ar("selm", [128, 32], F32)
            it = 0
            for tb in range(NB):
                tsl = slice(tb * 512, (tb + 1) * 512)
                o.load("pool", cmpmask[:], dram["cmpmask"][:, tsl])
                for h in range(4):
                    base = (h % 2) * 64
                    e_ = eT[it % 2]
                    it += 1
                    o.mm(ps[0][0:127, :], kcT[base:base + 64, 0:127], qkT[base:base + 64, h // 2, tsl])
                    o.act(e_[0:127, :], ps[0][0:127, :], AF.Exp, scale=0.125)
                    o.tt("pool", e_[0:127, :], e_[0:127, :], cmpmask[:, :], ALU.mult)
                    o.mm(ps[1][:], vc_aug[0:127, h % 2, :], e_[0:127, :])
                    for q4 in range(4):
                        o.mm((ps[2], ps[5])[q4 // 2][:, ((q4 % 2) * 4 + h) * 33:((q4 % 2) * 4 + h + 1) * 33], e_[0:127, q4 * 128:(q4 + 1) * 128], small["covones"][:, :])
                    epilogue(ps[1], 512, h, 0, tb * 512, clamp=True)
                for q4 in range(4):
                    tt = tb * 4 + q4
                    pv = (ps[2], ps[5])[q4 // 2][:, (q4 % 2) * 132:(q4 % 2 + 1) * 132].re("p (h c) -> p h c", h=4)
                    o.load("sp", small["selkeep"][:], dram["selkeep"][:, tt, :])
                    o.load("sp", small["seladd"][:], dram["seladd"][:, tt, :])
                    o.ts("dve", rdn[:], pv[:, :, 32], 1e-30, ALU.max)
                    o.recip(rdn[:], rdn[:])
                    o.ts("dve", imp[:], pv[:, 0, 0:32], rdn[:, 0:1], ALU.mult)
                    for h in range(1, 4):
                        o.stt("dve", imp[:], pv[:, h, 0:32], rdn[:, h:h + 1], imp[:], ALU.mult, ALU.add)
                    o.tt("dve", imp[:], imp[:], small["selkeep"][:, :], ALU.mult)
                    o.tt("dve", imp[:], imp[:], small["seladd"][:, :], ALU.add)
                    k.op("dve", lambda h_, a=mx8, b=imp: h_.max(out=a.ap, in_=b.ap), reads=[imp], writes=[mx8])
                    k.op("dve", lambda h_, a=imp2, b=mx8, c_=imp: h_.match_replace(out=a.ap, in_to_replace=b.ap, in_values=c_.ap, imm_value=-1e30),
                         reads=[imp, mx8], writes=[imp2])
                    k.op("dve", lambda h_, a=mx8, b=imp2: h_.max(out=a.ap, in_=b.ap), reads=[imp2], writes=[mx8])
                    o.ts("dve", selm[:], imp[:], mx8[:, 7:8], ALU.is_ge)
                    o.tr(ps[3][0:32, 0:128], selm[:], ident_f[:])
                    o.cp("act", selT[:, tt * 128:(tt + 1) * 128], ps[3][0:32, 0:128])

            mk = [ar(f"mk{i}", [128, 128], BF16) for i in range(2)]
            pp = [ar(f"pp{i}", [128, 256], BF16) for i in range(2)]
            it = 0
            items = []
            for br, kpair, vbase, span in ((1, 2, 0, 16), (2, 3, 2, 4)):
                for n in range(NT):
                    qsl = slice(n * 128, (n + 1) * 128)
                    js = [j for j in range(max(0, n - span), n + 1)]
                    for par in range(2):
                        base = par * 64
                        pso = ps[2 + par]
                        for ji, j in enumerate(js):
                            ksl = slice(j * 128, (j + 1) * 128)
                            p_ = pp[it % 2]
                            m_ = mk[it % 2]
                            pss = ps[it % 2]
                            it += 1

                            def A(br=br, kpair=kpair, span=span, n=n, j=j, qsl=qsl, ksl=ksl, base=base, p_=p_, m_=m_, pss=pss):
                                for hh in range(2):
                                    o.mm(pss[:, hh * 128:(hh + 1) * 128], qkT[base:base + 64, kpair, ksl], qkT[base:base + 64, hh, qsl])
                                o.act(p_[:], pss[:, 0:256], AF.Exp, scale=0.125)
                                msk = None
                                if br == 1:
                                    o.mm(ps[5][:, 0:128], small["Esel"][:, j, :], selT[:, qsl])
                                    if j == n:
                                        o.tt("dve", m_[:], ps[5][:, 0:128], trimask[:, 128:256], ALU.mult)
                                    else:
                                        o.cp("act", m_[:], ps[5][:, 0:128])
                                    msk = m_[:]
                                else:
                                    if j == n:
                                        msk = trimask[:, 128:256]
                                    elif j == n - span:
                                        msk = small["trigt"][:]
                                if msk is not None:
                                    o.tt("pool", p_[:].re("p (h q) -> p h q", h=2), p_[:].re("p (h q) -> p h q", h=2),
                                         msk.re("p (o q) -> p o q", o=1).bc([128, 2, 128]), ALU.mult)

                            def B(br=br, vbase=vbase, par=par, n=n, j=j, ji=ji, nj=len(js), p_=p_, pso=pso):
                                o.mm(pso[:, 0:256], Vn[:, j, vbase + par, :], p_[:], start=(ji == 0), stop=(ji == nj - 1))
                                if ji == nj - 1:
                                    for hh in range(2):
                                        h = hh * 2 + par
                                        epilogue(pso[:, hh * 128:(hh + 1) * 128], 128, h, br, n * 128)
                            items.append((A, B))
            prevB = None
            for A, B in items:
                A()
                if prevB is not None:
                    prevB()
                prevB = B
            prevB()

        vfirst = None if dbg.get("no_vstore") else nc.dram_tensor("vfirst_scratch", [S, 256], F32).ap()
        vstore_tiles = []

        def rwkv(l):
            ar_reset()
            WA = wA[:, 0:8448].re("p (c e) -> p c e", c=8)
            WB = wB[:, 0:8448].re("p (c e) -> p c e", c=8)
            mark0 = ar_state["off"]
            mu_t = ar("mu_t", [128, 1056], F32)
            omu = ar("omu", [128, 1056], F32)
            stg = [ar(f"stg{i}", [128, 1056], F32) for i in range(2)]
            o.load("sp", mu_t[:], dram["rw_mu"][l])
            o.ts("dve", omu[:], mu_t[:], -1.0, ALU.mult, 1.0, ALU.add)
            for c in range(8):
                sg = stg[c % 2]
                o.load("sp", sg[:], dram["w_rw"][l][c * 128:(c + 1) * 128, :])
                o.tt("dve", WA[:, c, :], sg[:], omu[:], ALU.mult)
                o.tt("pool", WB[:, c, :], sg[:], mu_t[:], ALU.mult)
            ar_rewind(mark0)
            if dbg.get("rwkv_stage", 9) <= -3:
                return
            P = {}
            for nm in ("w0", "a0", "kk", "ka", "rk", "lnw", "lnb") + (("v0",) if l > 0 else ()):
                P[nm] = ar("p_" + nm, [128, 256], F32)
                o.load("sp", P[nm][:], dram["rw_" + nm][l])
            wa2 = ar("wa2", [64, 2, 256], F32)
            g2a = ar("g2a", [128, 256], F32)
            g2b = ar("g2b", [32, 256], F32)
            o.load("sp", wa2[:, 0, :], dram["rw_w2"][l])
            o.load("sp", wa2[:, 1, :], dram["rw_a2"][l])
            o.load("sp", g2a[:], dram["rw_g2"][l][0:128, :])
            o.load("sp", g2b[:], dram["rw_g2"][l][128:160, :])
            if l > 0:
                v1 = ar("v1", [128, 2, 32], F32)
                v2 = ar("v2", [32, 256], F32)
                o.load("sp", v1[:], dram["rw_v1"][0].rearrange("(c p) r -> p c r", p=128))
                o.load("sp", v2[:], dram["rw_v2"][0])
            mU2 = ar("mU2", [128, 256], F32)
            mL = ar("mL", [128, 128], F32)
            o.load("sp", mU2[:], dram["mU2"])
            o.load("sp", mL[:], dram["mL"])
            Hs = ar("Hs", [64, 4, 64], F32)
            o.memset("dve", Hs[:], 0.0)
            if dbg.get("rwkv_stage", 9) <= -2:
                return
            nm256 = ("t0", "t1", "lw", "a_", "kk", "kp", "b_", "vv", "gi", "ginv", "ge", "gC", "gsb")
            W = {n_: ar("rw_" + n_, [128, 256], F32) for n_ in nm256}
            t0, t1, lw, a_, kk, kp, b_, vv, gi, ginv, ge, gC, gsb = (W[n_] for n_ in nm256)
            vf = gC
            vT = gi[:].re("p (a t) -> p a t", a=2)
            vv1T = ge[0:32, 0:128]
            XTkr = ar("XTkr", [64, 4, 2, 128], F32)
            XTbk = ar("XTbk", [64, 4, 2, 128], F32)
            P1 = ar("P1", [128, 256], F32)
            P2 = ar("P2", [128, 256], F32)
            Lc0 = ar("Lc0", [128, 128], F32)
            Lb = [ar(f"Lb{i}", [128, 128], F32) for i in range(2)]
            Ub = [ar(f"Ub{i}", [128, 128], F32) for i in range(2)]
            Y = [ar(f"Y{i}", [128, 128], F32) for i in range(2)]
            ILs = [ar(f"IL{i}", [128, 128], F32) for i in range(2)]
            RH = ar("RH", [128, 128], F32)
            nWU = ar("nWU", [128, 128], F32)
            TT = ar("TT", [64, 64], F32)
            QT = ar("QT", [64, 128], F32)
            s4 = ar("s4", [128, 4], F32)
            s4b = ar("s4b", [128, 4], F32)
            bc4 = ar("bc4", [128, 4], F32)
            gcol = ar("gcol", [64, 4], F32)
            lxw = ar("lxw", [64, 2, 128], F32)
            lxg = ar("lxg", [128, 128], F32)
            lxg2 = ar("lxg2", [32, 128], F32)
            yb_ = ar("yb_", [128, 256], BF16)
            h3 = lambda v_: v_.re("p (h d) -> p h d", h=4)
            b3 = lambda v_: v_.re("p (h o) -> p h o", o=1).bc([128, 4, 64])

            for tt in range(dbg.get("rwkv_tiles", NT)):
                cur = slice(1 + tt * 128, 1 + (tt + 1) * 128)
                prv = slice(tt * 128, (tt + 1) * 128)
                for (pb, c0, c1) in ((ps[0][:, 0:512], 0, 512), (ps[1][:, 0:256], 512, 768)):
                    for c in range(8):
                        o.mm(pb, hT[:, c, cur], WA[:, c, c0:c1], start=(c == 0), stop=False)
                    for c in range(8):
                        o.mm(pb, hT[:, c, prv], WB[:, c, c0:c1], start=False, stop=(c == 7))
                for (pb, c0, c1) in ((ps[2][:, 0:128], 768, 896), (ps[2][:, 128:256], 896, 1024), (ps[2][0:32, 256:384], 1024, 1056)):
                    for c in range(8):
                        o.mm(pb, WA[:, c, c0:c1], hT[:, c, cur], start=(c == 0), stop=False)
                    for c in range(8):
                        o.mm(pb, WB[:, c, c0:c1], hT[:, c, prv], start=False, stop=(c == 7))
                if dbg.get("rwkv_stage", 9) <= -1:
                    continue
                sub = dbg.get("rwkv_sub", 99)
                if sub > 0:
                    o.act(lxw[:, 0, :], ps[2][0:64, 0:128], AF.Tanh)
                if sub > 1:
                    o.cp("act", lxw[:, 1, :], ps[2][64:128, 0:128])
                if sub > 2:
                    o.act(lxg[:], ps[2][:, 128:256], AF.Sigmoid)
                if sub > 3:
                    o.act(lxg2[:], ps[2][0:32, 256:384], AF.Sigmoid)
                if sub > 4:
                    o.mm(ps[3][:, 0:256], lxw[:, 0, :], wa2[:, 0, :])
                if sub > 5:
                    o.mm(ps[3][:, 256:512], lxw[:, 1, :], wa2[:, 1, :])
                if sub > 6:
                    o.mm(ps[4][:, 0:256], lxg[:], g2a[:], start=True, stop=False)
                if sub > 7:
                    o.mm(ps[4][:, 0:256], lxg2[:], g2b[:], start=False, stop=True)
                if sub > 8:
                    o.cp("act", gsb[:], ps[4][:, 0:256])
                if dbg.get("rwkv_stage", 9) < 1:
                    continue
                o.tt("dve", t0[:], ps[3][:, 0:256], P["w0"][:], ALU.add)
                o.act(lw[:], t0[:], AF.Sigmoid)
                o.ts("pool", lw[:], lw[:], -0.6065306597126334, ALU.mult)
                o.tt("dve", t0[:], ps[3][:, 256:512], P["a0"][:], ALU.add)
                o.act(a_[:], t0[:], AF.Sigmoid)
                o.cp("act", vv[:], ps[1][:, 0:256])
                if l == 0:
                    if not dbg.get("no_vstore"):
                        o.store("sp", vfirst[tt * 128:(tt + 1) * 128, :], vv[:])
                        if vv not in vstore_tiles:
                            vstore_tiles.append(vv)
                else:
                    o.load("sp", vf[:], vfirst[tt * 128:(tt + 1) * 128, :], extra=vstore_tiles)
                    for c2 in range(2):
                        o.tr(ps[6][:, c2 * 128:(c2 + 1) * 128], vv[:, c2 * 128:(c2 + 1) * 128], ident_f[:])
                    o.cp("act", vT, ps[6][:, 0:256].re("p (a t) -> p a t", a=2))
                    for c2 in range(2):
                        o.mm(ps[6][0:32, 256:384], v1[:, c2, :], vT[:, c2, :], start=(c2 == 0), stop=(c2 == 1))
                    o.cp("act", vv1T, ps[6][0:32, 256:384])
                    o.mm(ps[3][:, 0:256], vv1T, v2[:])
                    o.tt("dve", t0[:], ps[3][:, 0:256], P["v0"][:], ALU.add)
                    o.act(t0[:], t0[:], AF.Sigmoid)
                    o.tt("dve", t1[:], vf[:], vv[:], ALU.subtract)
                    o.tt("dve", t1[:], t1[:], t0[:], ALU.mult)
                    o.tt("dve", vv[:], vv[:], t1[:], ALU.add)
                kps = ps[0][:, 256:512]
                rps = ps[0][:, 0:256]
                o.tt("dve", kk[:], kps, P["kk"][:], ALU.mult)
                o.tt("pool", t0[:], kk[:], kk[:], ALU.mult)
                o.red(s4[:], h3(t0[:]), ALU.add)
                o.act(s4[:], s4[:], AF.Sqrt)
                o.ts("dve", s4[:], s4[:], 1e-12, ALU.max)
                o.recip(s4[:], s4[:])
                o.tt("dve", h3(kk[:]), h3(kk[:]), b3(s4[:]), ALU.mult)
                o.stt("dve", t0[:], a_[:], -1.0, P["ka"][:], ALU.add, ALU.mult)
                o.stt("dve", kp[:], t0[:], 1.0, kps, ALU.add, ALU.mult)
                o.tt("pool", b_[:], kk[:], a_[:], ALU.mult)
                o.tt("dve", t1[:], rps, kp[:], ALU.mult)
                o.tt("pool", t1[:], t1[:], P["rk"][:], ALU.mult)
                o.red(bc4[:], h3(t1[:]), ALU.add)
                if dbg.get("rwkv_stage", 9) < 2:
                    continue
                o.mm(ps[5][:, 0:256], mU2[:, 128:256], lw[:])
                o.mm(ps[5][:, 256:512], ones_f[:], lw[:])
                for h in range(4):
                    o.mm(ps[4][0:64, 256 + h:257 + h], lw[:, h * 64:(h + 1) * 64], ones_f[:, 0:1])
                o.act(gcol[:], ps[4][0:64, 256:260], AF.Exp)
                cum = ps[5][:, 0:256]
                o.act(gi[:], cum, AF.Exp)
                o.act(ginv[:], cum, AF.Exp, scale=-1.0)
                o.tt("dve", t0[:], cum, lw[:], ALU.subtract)
                o.act(ge[:], t0[:], AF.Exp)
                o.cp("act", t1[:], ps[5][:, 256:512])
                o.tt("dve", t1[:], t1[:], cum, ALU.subtract)
                o.act(gC[:], t1[:], AF.Exp)
                o.tt("pool", ge[:], kk[:], ge[:], ALU.mult)
                o.tt("dve", gi[:], rps, gi[:], ALU.mult)
                o.tt("pool", a_[:], b_[:], ginv[:], ALU.mult)
                o.tt("pool", ginv[:], kp[:], ginv[:], ALU.mult)
                o.tt("pool", b_[:], b_[:], gC[:], ALU.mult)
                o.tt("pool", gC[:], kp[:], gC[:], ALU.mult)
                KKt, Rt, Bh, Kh, BC, KC = ge, gi, a_, ginv, b_, gC
                for qi, (src, dst, idx) in enumerate(((KKt, XTkr, 0), (Rt, XTkr, 1), (Bh, XTbk, 0), (Kh, XTbk, 1))):
                    for h in range(4):
                        o.tr(ps[6][0:64, h * 128:(h + 1) * 128], src[:, h * 64:(h + 1) * 64], ident_f[:])
                    o.cp("act" if qi % 2 == 0 else "dve", dst[:, :, idx, :], ps[6][0:64, 0:512].re("p (a t) -> p a t", a=4))
                if dbg.get("rwkv_stage", 9) < 3:
                    continue
                k.limit = dbg.get("oplimit")
                k.limit_on = True
                for h in range(4):
                    hc = slice(h * 64, (h + 1) * 64)
                    BhT = XTbk[:, h, 0, :]
                    KhT = XTbk[:, h, 1, :]
                    KRT = XTkr[:, h, :, :].re("p a t -> p (a t)")
                    KKtT = XTkr[:, h, 0, :]
                    RtT = XTkr[:, h, 1, :]
                    o.mm(ps[0][:, 0:256], BhT, KRT)
                    o.mm(ps[1][:, 0:256], KhT, KRT)
                    o.mm(ps[1][:, 256:384], KKtT, BhT)
                    o.tt("dve", P1[:], ps[0][:, 0:256], mU2[:], ALU.mult)
                    o.tt("dve", P2[:], ps[1][:, 0:256], mU2[:], ALU.mult)
                    o.tt("dve", Lc0[:], ps[1][:, 256:384], mL[:], ALU.mult)
                    o.tt("pool", Y[0][:], ident_f[:], P1[:, 0:128], ALU.subtract)
                    Uc, Lcur = P1[:, 0:128], Lc0[:]

                    def sq_step(ks, Uc, Lcur):
                        last = ks == 5
                        o.mm(ps[2][:, 0:128], Uc, Lcur)
                        if not last:
                            o.mm(ps[6][:, 0:128], Lcur, Uc)
                        o.cp("act", Lb[ks % 2][:], ps[2][:, 0:128])
                        if not last:
                            o.cp("dve", Ub[ks % 2][:], ps[6][:, 0:128])
                        o.tt("pool", ILs[ks % 2][:], Lb[ks % 2][:], ident_f[:], ALU.add)
                        return Ub[ks % 2][:], Lb[ks % 2][:]

                    def y_step(ks):
                        o.mm(ps[0][:, 256:384], ILs[ks % 2][:], Y[ks % 2][:])
                        o.cp("act", Y[(ks + 1) % 2][:], ps[0][:, 256:384])

                    Uc, Lcur = sq_step(0, Uc, Lcur)
                    for ks in range(1, 6):
                        Uc, Lcur = sq_step(ks, Uc, Lcur)
                        y_step(ks - 1)
                    y_step(5)
                    Yf = Y[0]
                    o.mm(ps[3][:, 0:64], P2[:, 0:128], vv[:, hc])
                    o.cp("act", RH[:, 64:128], ps[3][:, 0:64])
                    o.cp("pool", RH[:, 0:64], KKt[:, hc])
                    o.mm(ps[3][:, 64:192], Yf[:], RH[:])
                    o.ts("dve", nWU[:], ps[3][:, 64:192], -1.0, ALU.mult)
                    o.mm(ps[3][0:64, 192:256], nWU[:, 0:64], BC[:, hc])
                    o.cp("act", TT[:], ps[3][0:64, 192:256])
                    o.mm(ps[3][0:64, 256:384], nWU[:, 0:64], P1[:, 128:256], start=True, stop=False)
                    o.mm(ps[3][0:64, 256:384], ident_f[0:64, 0:64], RtT, start=False, stop=True)
                    o.cp("act", QT[:], ps[3][0:64, 256:384])
                    o.mm(ps[5][:, hc], P2[:, 128:256], vv[:, hc], start=True, stop=False)
                    o.mm(ps[5][:, hc], P1[:, 128:256], nWU[:, 64:128], start=False, stop=False)
                    o.mm(ps[5][:, hc], QT[:], Hs[:, h, :], start=False, stop=True)
                    o.mm(ps[4][0:64, hc], KC[:, hc], vv[:, hc], start=True, stop=False)
                    o.mm(ps[4][0:64, hc], BC[:, hc], nWU[:, 64:128], start=False, stop=False)
                    o.mm(ps[4][0:64, hc], TT[:], Hs[:, h, :], start=False, stop=True)
                    o.stt("dve", Hs[:, h, :], Hs[:, h, :], gcol[:, h:h + 1], ps[4][0:64, hc], ALU.mult, ALU.add)
                if dbg.get("rwkv_stage", 9) < 4:
                    continue
                k.limit_on = False
                if "dumpP1" in dbg_out:
                    o.store("sp", dbg_out["dumpP1"], P1[:])
                    o.store("sp", dbg_out["dumpP2"], P2[:])
                    o.store("sp", dbg_out["dumpL"], Lc0[:])
                    o.store("sp", dbg_out["dumpY"], Y[0][:])
                    o.store("sp", dbg_out["dumpX"], XTkr[:].re("p a b t -> p (a b t)"))
                O_ = ps[5][:, 0:256]
                xc = kk
                o.red(s4[:], h3(O_), ALU.add)
                o.ts("dve", s4[:], s4[:], 1.0 / 64, ALU.mult)
                o.tt("dve", h3(xc[:]), h3(O_), b3(s4[:]), ALU.subtract)
                o.tt("pool", t0[:], xc[:], xc[:], ALU.mult)
                o.red(s4b[:], h3(t0[:]), ALU.add)
                o.act(s4b[:], s4b[:], AF.Sqrt, bias=64e-5, scale=1.0 / 64)
                o.recip(s4b[:], s4b[:])
                o.tt("dve", h3(xc[:]), h3(xc[:]), b3(s4b[:]), ALU.mult)
                o.tt("pool", xc[:], xc[:], P["lnw"][:], ALU.mult)
                o.tt("pool", xc[:], xc[:], P["lnb"][:], ALU.add)
                o.tt("dve", h3(t0[:]), h3(vv[:]), b3(bc4[:]), ALU.mult)
                o.tt("dve", xc[:], xc[:], t0[:], ALU.add)
                o.tt("dve", yb_[:], xc[:], gsb[:], ALU.mult)
                for c2 in range(2):
                    o.tr(pT[:, c2 * 128:(c2 + 1) * 128], yb_[:, c2 * 128:(c2 + 1) * 128], ident_b[:])
                o.cp("act", mixT[:, :, tt * 128:(tt + 1) * 128], pT[:, 0:256].re("p (a t) -> p a t", a=2))

        pool_c = {}

        def pool_mixer(l):
            ar_reset()
            if not pool_c:
                pool_c["rw"] = k.sbuf("pool_rw", [128, 2], F32)
                pool_c["fix"] = k.sbuf("pool_fix", [128, 2, 16], F32)
                pool_c["sc"] = k.sbuf("pool_sc", [128, 2], F32)
                o.load("sp", pool_c["rw"][:], dram["pool_rw"])
                o.load("sp", pool_c["fix"][:], dram["pool_fix"])
            o.load("sp", pool_c["sc"][:], dram["pool_scale"][l])
            wu_ = wB[:, 0:2048].re("p (c e) -> p c e", c=8)
            o.load("pool", wu_, dram["w_fm"][l][:, 0:256].rearrange("(c p) e -> p c e", p=128))
            pw = ar("pw", [128, 2, 128], BF16)
            pwf = ar("pwf", [128, 2, 128], F32)
            o.memset("pool", pwf[:], 0.0)
            for gi in range(4):
                o.load("sp", pwf[(gi % 2) * 64:(gi % 2) * 64 + 64, gi // 2, (gi % 2) * 64:(gi % 2) * 64 + 64], dram["pool_w"][l, gi])
            o.cp("dve", pw[:], pwf[:])
            PADW = 16
            u = [ar(f"pu{i}", [128, PADW + S], F32) for i in range(1)][0]
            sa = ar("psa", [128, PADW + S], F32)
            sb_ = ar("psb", [128, PADW + S], F32)
            dm = ar("pdm", [128, S], BF16)
            for mt in range(2):
                o.memset("pool", u[:, 0:PADW], 0.0)
                o.memset("pool", sa[:, 0:PADW], 0.0)
                o.memset("pool", sb_[:, 0:PADW], 0.0)
                for tb in range(NB):
                    for c in range(8):
                        o.mm(ps[0][:], wu_[:, c, mt * 128:(mt + 1) * 128], hT[:, c, 1 + tb * 512:1 + (tb + 1) * 512], start=(c == 0), stop=(c == 7))
                    o.cp("act", u[:, PADW + tb * 512:PADW + (tb + 1) * 512], ps[0][:])
                wl, wh = ((2, 4), (8, 16))[mt]
                full = slice(PADW, PADW + S)

                def sh(t, d):
                    return t[:, PADW - d:PADW + S - d]
                o.tt("dve", sa[:, full], u[:, full], sh(u, 1), ALU.add)
                cur, oth, w = sa, sb_, 2
                res = {}
                res[2] = cur
                while w < wh:
                    o.tt("dve", oth[:, full], cur[:, full], sh(cur, w), ALU.add)
                    w *= 2
                    if w == wl:
                        pass
                    res[w] = oth
                    cur, oth = oth, cur
                lo_t, hi_t = sa, sb_
                for (pr, src) in ((slice(0, 64), lo_t), (slice(64, 128), hi_t)):
                    o.ts("dve", src[pr, full], src[pr, full], pool_c["rw"][pr, mt:mt + 1], ALU.mult)
                    o.tt("dve", src[pr, PADW:PADW + 16], src[pr, PADW:PADW + 16], pool_c["fix"][pr, mt, :], ALU.mult)
                    o.tt("dve", dm[pr, :], src[pr, full], u[pr, full], ALU.subtract)
                for tb in range(NB):
                    tsl = slice(tb * 512, (tb + 1) * 512)
                    o.mm(ps[1][:], pw[:, mt, :], dm[:, tsl])
                    o.act(mixT[:, mt, tsl], ps[1][:], AF.Identity, scale=pool_c["sc"][:, mt:mt + 1])

        if dbg.get("only"):
            o.load("pool", hT[:], dram["hT_in"].rearrange("(c p) t -> p c t", p=128))
            {"rwkv": rwkv, "dil": dilated, "nsa": nsa, "pool": pool_mixer}[dbg["only"]](dbg.get("layer", 0))
            o.store("pool", dbg_out["mix_only"].rearrange("(c p) t -> p c t", p=128), mixT[:])
        for l in range(0 if dbg.get("only") else n_layers):
            norm(l, gs1[l], 0)
            if "hT" in dbg_out and l == dbg.get("layer", 0):
                pass
            comp = dbg.get("compute", ("nsa", "pool", "rwkv", "dil"))
            for grp, nm, fn in ((3, "dil", dilated), (1, "pool", pool_mixer), (0, "nsa", nsa), (2, "rwkv", rwkv)):
                if nm in comp:
                    fn(l)
                    if f"mix{grp}_{l}" in dbg_out:
                        o.store("pool", dbg_out[f"mix{grp}_{l}"].rearrange("(c p) t -> p c t", p=128), mixT[:])
                elif dbg.get("mix_in"):
                    o.load("pool", mixT[:], dram[f"mix_in{l}"][grp * 256:(grp + 1) * 256, :].rearrange("(c p) t -> p c t", p=128))
                else:
                    continue
                out_proj(l, grp)
            if f"x1_{l}" in dbg_out:
                o.store("sp", dbg_out[f"x1_{l}"].rearrange("(c p) t -> p c t", p=128), xT[:])
            ffn_alloc()
            norm(l, gs2[l], 24, router=True)
            routing()
            if f"gates_{l}" in dbg_out:
                o.store("sp", dbg_out[f"gates_{l}"], F["gatesT"][:])
            moe(l)

        o.store("sp", outT.rearrange("(c p) t -> p c t", p=128), xT[:])
        k.final_wait("sp")
        k.emit()
        print("instr counts", k.count, "waits", k.n_waits, "sems", k.nsem, "sbuf left", nc.sbuf_bytes_remaining)
    return nc


_NP2DT = {np.dtype(np.float32): F32, np.dtype(np.int32): I32, np.dtype(ml_dtypes.bfloat16): BF16}


def run(inp, dbg=None, n_layers=2, extra_per=None):
    sh, per = _prep_inputs(inp)
    if extra_per:
        for b in range(8):
            per[b].update(extra_per[b])
    in_maps = []
    for b in range(8):
        d = dict(sh)
        d.update(per[b])
        in_maps.append(d)
    shapes = {n: (a.shape, _NP2DT[a.dtype]) for n, a in in_maps[0].items()}
    nc = build(shapes, dbg=dbg, n_layers=n_layers)
    ncore = dbg.get("ncores", 8) if dbg else 8
    res = run_bass_kernel_spmd(nc, in_maps[:ncore], core_ids=list(range(ncore)), **({"trace": True} if (dbg and dbg.get("trace")) else {}))
    return res


def kernel(**inputs):
    inp = {k_: np.asarray(v) for k_, v in inputs.items()}
    res = run(inp)
    out = np.stack([np.ascontiguousarray(res.results[b]["outT"].T) for b in range(8)], axis=0)
    return out.astype(np.float32)
```

```python
import numpy as np
import ml_dtypes
from contextlib import ExitStack
import concourse.bass as bass
import concourse.mybir as mybir
from concourse.bass_utils import run_bass_kernel_spmd

F32 = mybir.dt.float32
BF16 = mybir.dt.bfloat16
I32 = mybir.dt.int32
ALU = mybir.AluOpType
AF = mybir.ActivationFunctionType
AX = mybir.AxisListType
EPOCH = 30000

S = 2048
D = 1024
NT = 16
NB = 4
EPS = 1e-6


class V:
    __slots__ = ("t", "ap")

    def __init__(self, t, ap):
        self.t = t
        self.ap = ap

    def __getitem__(self, k):
        return V(self.t, self.ap[k])

    def re(self, s, **kw):
        return V(self.t, self.ap.rearrange(s, **kw))

    def bc(self, shape):
        return V(self.t, self.ap.to_broadcast(list(shape)))

    def cast(self, dt):
        return V(self.t, self.ap.bitcast(dt))


class T:
    def __init__(self, name, ap):
        self.name = name
        self.ap = ap
        self.last_w = None
        self.readers = {}
        self.dma_sem = None
        self.dma_cnt = 0

    def __getitem__(self, k):
        return V(self, self.ap[k])

    def view(self, name, ap):
        return T(name, ap)


class Sched:
    ENGS = ("pe", "act", "dve", "pool", "sp")

    def __init__(self, nc, stack):
        self.nc = nc
        self.stack = stack
        self.streams = {e: [] for e in self.ENGS}
        self.count = {e: 0 for e in self.ENGS}
        self.sems = {e: [] for e in self.ENGS}
        self.clock = {e: {} for e in self.ENGS}
        self.snap = {e: {} for e in self.ENGS}
        self.dma_known = {e: {} for e in self.ENGS}
        self.dma_tiles = []
        self.nsem = 0
        self.n_waits = 0

    def new_sem(self, name):
        self.nsem += 1
        return self.stack.enter_context(self.nc.semaphore(name))

    def eng_sem(self, e, n):
        idx = (n - 1) // EPOCH
        while len(self.sems[e]) <= idx:
            self.sems[e].append(self.new_sem(f"s_{e}_{len(self.sems[e])}"))
        return self.sems[e][idx], (n - 1) % EPOCH + 1

    def sbuf(self, name, shape, dtype):
        t = self.stack.enter_context(self.nc.sbuf_tensor("sb_" + name, list(shape), dtype))
        return T(name, t[:])

    def psum(self, name, shape, dtype):
        t = self.stack.enter_context(self.nc.psum_tensor("pp_" + name, list(shape), dtype))
        r = T(name, t[:])
        r.is_psum = True
        return r

    def _need(self, e, reads, writes):
        need = {}
        dneed = []

        def add(dep):
            if dep is None:
                return
            x, n = dep
            if x == e and e == "pe":
                return
            if need.get(x, 0) < n:
                need[x] = n
        for t in reads:
            add(t.last_w)
            if getattr(t, "is_psum", False):
                for x, n in t.readers.items():
                    if x != e:
                        add((x, n))
            if t.dma_cnt:
                dneed.append(t)
        for t in writes:
            add(t.last_w)
            for x, n in t.readers.items():
                if x == e:
                    continue
                add((x, n))
            if t.dma_cnt:
                dneed.append(t)
        return need, dneed

    def _emit_waits(self, e, need, dneed):
        waits = []
        ck = self.clock[e]
        for x, n in need.items():
            if ck.get(x, 0) >= n:
                continue
            waits.append(self.eng_sem(x, n))
            sn = self.snap[x].get(n)
            if sn:
                for y, m in sn.items():
                    if ck.get(y, 0) < m:
                        ck[y] = m
            if ck.get(x, 0) < n:
                ck[x] = n
        dk = self.dma_known[e]
        for t in dneed:
            if dk.get(id(t), 0) >= t.dma_cnt:
                continue
            waits.append((t.dma_sem, t.dma_cnt))
            dk[id(t)] = t.dma_cnt
        return waits

    limit = None
    limit_on = False
    phase = "init"
    annotate = False
    opcount = 0

    def op(self, e, fn, reads=(), writes=()):
        if self.limit is not None and self.limit_on:
            self.opcount += 1
            if self.opcount > self.limit:
                return 0
        need, dneed = self._need(e, reads, writes)
        waits = self._emit_waits(e, need, dneed)
        self.count[e] += 1
        n = self.count[e]
        sem, _ = self.eng_sem(e, n)
        self.n_waits += len(waits)

        ph = self.phase if self.annotate else None

        def run(h, fn=fn, waits=waits, sem=sem, ph=ph):
            for (s, v) in waits:
                h.wait_ge(s, v)
            ins = fn(h)
            if ph is not None:
                ins = ins.annotate(ph)
            ins.then_inc(sem, 1)
        self.streams[e].append(run)
        sn = dict(self.clock[e])
        sn[e] = n
        self.snap[e][n] = sn
        for t in reads:
            t.readers[e] = n
        for t in writes:
            t.last_w = (e, n)
            t.readers = {}
        return n

    def dma(self, e, out_ap, in_ap, tile, write, extra=()):
        reads, writes = ((), (tile,)) if write else ((tile,), ())
        need, dneed = self._need(e, reads, writes)
        waits = self._emit_waits(e, need, dneed)
        for xt in extra:
            if xt.dma_cnt:
                waits.append((xt.dma_sem, xt.dma_cnt))
        self.n_waits += len(waits)
        t = tile
        if t.dma_sem is None:
            t.dma_sem = self.new_sem(f"d{self.nsem}_" + t.name)
            self.dma_tiles.append(t)
        t.dma_cnt += 16
        dsem = t.dma_sem

        def run(h, waits=waits, dsem=dsem, out_ap=out_ap, in_ap=in_ap):
            for (s, v) in waits:
                h.wait_ge(s, v)
            h.dma_start(out=out_ap, in_=in_ap).then_inc(dsem, 16)
        self.streams[e].append(run)
        if write:
            t.last_w = None
            t.readers = {}

    def barrier(self, o, pstile, ones):
        if not hasattr(self, "_bs"):
            self._bs = {e: self.sbuf("bs_" + e, [128, 2], F32) for e in ("pe", "act", "dve", "pool")}
        bs = self._bs
        pap = pstile.ap[0:1, 0:1]
        oap = ones.ap[0:1, 0:1]
        self.op("pe", lambda h: h.matmul(pap, lhsT=oap, rhs=oap, start=True, stop=True),
                reads=[ones], writes=[pstile, bs["pe"]])
        self.op("act", lambda h: h.activation(out=bs["act"].ap[:, 0:1], in_=bs["act"].ap[:, 0:1], func=AF.Copy, scale=0.0), writes=[bs["act"]])
        self.op("dve", lambda h: h.memset(bs["dve"].ap[:, 0:1], 0.0), writes=[bs["dve"]])
        self.op("pool", lambda h: h.memset(bs["pool"].ap[:, 0:1], 0.0), writes=[bs["pool"]])
        allb = [bs[e] for e in ("pe", "act", "dve", "pool")]
        waits = [self.eng_sem(x, n) for x, n in (b.last_w for b in allb)]
        self.op("pe", lambda h: h.matmul(pap, lhsT=oap, rhs=oap, start=True, stop=True),
                reads=[ones] + allb, writes=[pstile])
        self.op("act", lambda h: h.activation(out=bs["act"].ap[:, 1:2], in_=bs["act"].ap[:, 0:1], func=AF.Copy), reads=allb, writes=[])
        self.op("dve", lambda h: h.memset(bs["dve"].ap[:, 1:2], 0.0), reads=allb, writes=[])
        self.op("pool", lambda h: h.memset(bs["pool"].ap[:, 1:2], 0.0), reads=allb, writes=[])

        def run(h, waits=waits):
            for (s_, v) in waits:
                h.wait_ge(s_, v)
        self.streams["sp"].append(run)

    def final_wait(self, e):
        waits = [(t.dma_sem, t.dma_cnt) for t in self.dma_tiles if t.dma_cnt]

        def run(h, waits=waits):
            for (s, v) in waits:
                h.wait_ge(s, v)
        self.streams[e].append(run)

    def emit(self):
        nc = self.nc
        with nc.Block() as block:
            @block.tensor
            def _(h):
                for r in self.streams["pe"]:
                    r(h)

            @block.scalar
            def _(h):
                for r in self.streams["act"]:
                    r(h)

            @block.vector
            def _(h):
                for r in self.streams["dve"]:
                    r(h)

            @block.gpsimd
            def _(h):
                for r in self.streams["pool"]:
                    r(h)

            @block.sync
            def _(h):
                for r in self.streams["sp"]:
                    r(h)


def _ap(x):
    return x.ap if isinstance(x, V) else x


def _ts(*xs):
    return [x.t for x in xs if isinstance(x, V)]


class Ops:
    def __init__(self, k):
        self.k = k

    def mm(self, out, lhsT, rhs, start=True, stop=True):
        self.k.op("pe", lambda h: h.matmul(out.ap, lhsT=lhsT.ap, rhs=rhs.ap, start=start, stop=stop),
                  reads=_ts(lhsT, rhs), writes=_ts(out))

    def tr(self, out, in_, ident):
        self.k.op("pe", lambda h: h.transpose(out.ap, in_.ap, ident.ap), reads=_ts(in_, ident), writes=_ts(out))

    def act(self, out, in_, func, bias=0.0, scale=1.0, accum=None):
        def f(h):
            kw = {}
            if accum is not None:
                kw["accum_out"] = accum.ap
            return h.activation(out=out.ap, in_=in_.ap, func=func, bias=_ap(bias), scale=_ap(scale), **kw)
        self.k.op("act", f, reads=_ts(in_, bias, scale), writes=_ts(out) + (_ts(accum) if accum is not None else []))

    def tt(self, e, out, a, b, op):
        self.k.op(e, lambda h: h.tensor_tensor(out=out.ap, in0=a.ap, in1=b.ap, op=op), reads=_ts(a, b), writes=_ts(out))

    def ts(self, e, out, a, s1, op0, s2=None, op1=None):
        def f(h):
            if op1 is None:
                return h.tensor_scalar(out=out.ap, in0=a.ap, scalar1=_ap(s1), scalar2=None, op0=op0)
            return h.tensor_scalar(out=out.ap, in0=a.ap, scalar1=_ap(s1), scalar2=_ap(s2), op0=op0, op1=op1)
        self.k.op(e, f, reads=_ts(a, s1, s2), writes=_ts(out))

    def stt(self, e, out, a, s, b, op0, op1):
        self.k.op(e, lambda h: h.scalar_tensor_tensor(out=out.ap, in0=a.ap, scalar=_ap(s), in1=b.ap, op0=op0, op1=op1),
                  reads=_ts(a, s, b), writes=_ts(out))

    def cp(self, e, out, in_):
        if e == "act":
            self.k.op("act", lambda h: h.activation(out=out.ap, in_=in_.ap, func=AF.Copy), reads=_ts(in_), writes=_ts(out))
        else:
            self.k.op(e, lambda h: h.tensor_copy(out=out.ap, in_=in_.ap), reads=_ts(in_), writes=_ts(out))

    def red(self, out, in_, op, axis=AX.X):
        self.k.op("dve", lambda h: h.tensor_reduce(out=out.ap, in_=in_.ap, axis=axis, op=op), reads=_ts(in_), writes=_ts(out))

    def recip(self, out, in_):
        self.k.op("dve", lambda h: h.reciprocal(out=out.ap, in_=in_.ap), reads=_ts(in_), writes=_ts(out))

    def memset(self, e, out, val):
        if e == "act_ms":
            self.k.op("act", lambda h: h.activation(out=out.ap, in_=out.ap, func=AF.Copy, scale=0.0), writes=_ts(out))
            return
        self.k.op(e, lambda h: h.memset(out.ap, val), writes=_ts(out))

    def load(self, e, out, src, extra=()):
        self.k.dma(e, out.ap, src, out.t, True, extra=extra)

    def store(self, e, dst, in_):
        self.k.dma(e, dst, in_.ap, in_.t, False)


def _cols(a, b):
    return list(range(a, b))


QK_COLS = (_cols(0, 256) + _cols(384, 448) * 2 + _cols(512, 576) * 2 + _cols(1964, 2220) + _cols(2220, 2476))
VN_COLS = _cols(448, 512) + _cols(576, 640)
FM_COLS = _cols(652, 908) + _cols(256, 384)
GL_COLS = _cols(640, 652)
VD_COLS = _cols(2476, 2732)
RW_COLS = _cols(908, 1964)


def _consts():
    c = {}
    c["ident_f"] = np.eye(128, dtype=np.float32)
    c["ident_b"] = np.eye(128, dtype=np.float32).astype(ml_dtypes.bfloat16)
    c["ones_f"] = np.ones((128, 128), np.float32)
    selE = np.zeros((16, 16, 128), np.float32)
    for e in range(16):
        selE[e, e, :] = 1.0
    c["selE"] = selE.reshape(16, 16 * 128)
    invf = (500000.0 ** (-2.0 * np.arange(8, dtype=np.float32) / 16.0)).astype(np.float32) / np.float32(2 * np.pi)
    c["invf"] = np.ascontiguousarray(np.broadcast_to(invf[None, :], (128, 8)), dtype=np.float32)
    p = np.arange(128)[:, None]
    q = np.arange(128)[None, :]
    c["trimask"] = np.concatenate([(p >= q), (p <= q)], axis=1).astype(np.float32).astype(ml_dtypes.bfloat16)
    bf = lambda a: np.ascontiguousarray(a, dtype=np.float32).astype(ml_dtypes.bfloat16)
    cc = np.arange(127)[:, None]
    tq = np.arange(2048)[None, :]
    c["cmpmask"] = bf(tq >= 16 * cc + 31)
    c_end = np.arange(127) * 16 + 31
    b_start = np.arange(32) * 64
    cover = np.clip(np.minimum(c_end[:, None] + 1, b_start[None, :] + 64) - np.maximum(c_end[:, None] + 1 - 32, b_start[None, :]), 0, None) / 32.0
    c["covones"] = bf(np.concatenate([cover, np.ones((127, 1))], axis=1))
    selG = np.zeros((12, 12, 128), np.float32)
    for e in range(12):
        selG[e, e, :] = 1.0
    c["selG"] = bf(selG.reshape(12, 12 * 128))
    E = np.zeros((32, 16, 128), np.float32)
    for j in range(16):
        for pp in range(128):
            E[2 * j + pp // 64, j, pp] = 1.0
    c["Esel"] = bf(E.reshape(32, 16 * 128))
    tt_ = np.arange(2048)
    cur = (tt_ // 64)[:, None]
    blk = np.arange(32)[None, :]
    forced = (blk == 0) | (blk == cur) | (blk == cur - 1)
    fut = blk > cur
    keep = (~forced & ~fut).astype(np.float32)
    add = np.where(fut, -1.0, np.where(forced, 1e4, 0.0)).astype(np.float32)
    c["selkeep"] = np.ascontiguousarray(keep.reshape(16, 128, 32).transpose(1, 0, 2))
    c["seladd"] = np.ascontiguousarray(add.reshape(16, 128, 32).transpose(1, 0, 2))
    c["trigt"] = bf(p > q)
    c["mU2"] = np.concatenate([(p < q), (p <= q)], axis=1).astype(np.float32)
    c["mL"] = (p > q).astype(np.float32)
    wwin = np.array([[2] * 64 + [4] * 64, [8] * 64 + [16] * 64], np.float32).T
    c["pool_rw"] = (1.0 / wwin).astype(np.float32)
    tcnt = np.arange(1, 17, dtype=np.float32)[None, None, :]
    c["pool_fix"] = (wwin[:, :, None] / np.minimum(tcnt, wwin[:, :, None])).astype(np.float32)
    return c


def _prep_inputs(inp):
    f = lambda a: np.ascontiguousarray(a, dtype=np.float32)
    sh = {}
    sh["ada_w"] = f(inp["ada_w"])
    sh["ada_b"] = f(inp["ada_b"].reshape(2, 48, 128).transpose(0, 2, 1))
    sh["g_mix"] = f(inp["norm_mix_g"].reshape(2, 8, 128).transpose(0, 2, 1))
    sh["g_ffn"] = f(inp["norm_ffn_g"].reshape(2, 8, 128).transpose(0, 2, 1))
    w_in = inp["w_in"]
    sh["w_qk"] = f(w_in[:, :, QK_COLS])
    sh["w_vn"] = f(w_in[:, :, VN_COLS])
    sh["w_fm"] = f(w_in[:, :, FM_COLS])
    sh["w_gl"] = f(w_in[:, :, GL_COLS])
    sh["w_vd"] = f(w_in[:, :, VD_COLS])
    sh["w_rw"] = f(w_in[:, :, RW_COLS])
    sh["w_dqk"] = f(w_in[:, :, _cols(1964, 2476)])
    sh["w_nqk"] = f(w_in[:, :, _cols(0, 256) + _cols(384, 448) * 2 + _cols(512, 576) * 2])
    gd = np.concatenate([np.tile(inp["dil_q_norm"], (1, 4)), np.tile(inp["dil_k_norm"], (1, 4))], axis=1)
    sh["g_dqk"] = f(np.broadcast_to(gd[:, None, :], (2, 128, 512)))
    gn = np.concatenate([np.tile(inp["nsa_q_norm"], (1, 4)), np.tile(inp["nsa_k_norm"][:, 1], (1, 2)),
                         np.tile(inp["nsa_k_norm"][:, 2], (1, 2))], axis=1)
    sh["g_nqk"] = f(np.broadcast_to(gn[:, None, :], (2, 128, 512)))
    sh["cmp_w1"] = f(inp["nsa_cmp_w1"])
    sh["cmp_w2"] = f(inp["nsa_cmp_w2"])
    sh["cmp_peT"] = f(inp["nsa_cmp_pe"].transpose(0, 1, 3, 2).reshape(2, 128, 32))
    gk = np.tile(inp["nsa_k_norm"][:, 0], (1, 2))
    sh["g_kcmp"] = f(np.broadcast_to(gk[:, None, :], (2, 128, 128)))
    rowb = lambda a: f(np.broadcast_to(a[:, None, :], (a.shape[0], 128, a.shape[1])))
    sh["rw_mu"] = rowb(inp["rwkv_mu"])
    sh["rw_w0"] = rowb(inp["rwkv_w0"])
    sh["rw_a0"] = rowb(inp["rwkv_a0"])
    sh["rw_kk"] = rowb(inp["rwkv_k_k"])
    sh["rw_ka"] = rowb(inp["rwkv_k_a"])
    sh["rw_rk"] = rowb(inp["rwkv_r_k"].reshape(2, 256))
    sh["rw_lnw"] = rowb(inp["rwkv_ln_w"])
    sh["rw_lnb"] = rowb(inp["rwkv_ln_b"])
    sh["rw_v0"] = rowb(np.concatenate([np.zeros_like(inp["rwkv_v0"]), inp["rwkv_v0"]], axis=0))
    sh["rw_w2"] = f(inp["rwkv_w2"])
    sh["rw_a2"] = f(inp["rwkv_a2"])
    sh["rw_g2"] = f(inp["rwkv_g2"])
    sh["rw_v1"] = f(inp["rwkv_v1"])
    sh["rw_v2"] = f(inp["rwkv_v2"])
    sh["pool_w"] = f(inp["pool_w"])
    sh["pool_scale"] = f(inp["pool_scale"].reshape(2, 2, 128).transpose(0, 2, 1))
    sh["w_out"] = f(inp["w_out"])
    sh["router_w"] = f(inp["router_w"])
    sh["router_b"] = f(np.broadcast_to(inp["router_b"][None, :], (128, 16)))
    sh["moe_wg"] = f(inp["moe_w_gate"])
    sh["moe_wu"] = f(inp["moe_w_up"])
    sh["moe_wd"] = f(inp["moe_w_down"])
    sh.update(_consts())
    per = []
    for b in range(8):
        d = {}
        d["xT"] = f(inp["x"][b].T)
        d["cvec"] = f(inp["c"][b].reshape(8, 128).T)
        d["pos_tm"] = np.ascontiguousarray(inp["positions"][b].reshape(16, 128).T.astype(np.int32))
        d["pos_ce"] = np.ascontiguousarray(inp["positions"][b][31::16][:127].reshape(127, 1).astype(np.int32))
        per.append(d)
    return sh, per


def build(shapes, dbg=None, n_layers=2):
    dbg = dbg or {}
    nc = bass.Bass("TRN2", target_bir_lowering=False)
    dram = {}
    for name, (shape, dt) in shapes.items():
        dram[name] = nc.dram_tensor(name, list(shape), dt, kind="ExternalInput").ap()
    outT = nc.dram_tensor("outT", [D, S], F32, kind="ExternalOutput").ap()
    dbg_out = {}
    for name, shape in dbg.get("outs", {}).items():
        dbg_out[name] = nc.dram_tensor(name, list(shape), F32, kind="ExternalOutput").ap()

    with ExitStack() as st:
        k = Sched(nc, st)
        k.annotate = bool(dbg.get("trace"))
        o = Ops(k)
        xT = k.sbuf("xT", [128, 8, S], F32)
        hT = k.sbuf("hT", [128, 8, S + 1], BF16)
        mixT = k.sbuf("mixT", [128, 2, S], BF16)
        wA = k.sbuf("wA", [128, 8704], BF16)
        wB = k.sbuf("wB", [128, 8704], BF16)
        wo = k.sbuf("wo", [128, 2, D], BF16)
        ident_f = k.sbuf("ident_f", [128, 128], F32)
        ident_b = k.sbuf("ident_b", [128, 128], BF16)
        ones_f = k.sbuf("ones_f", [128, 128], F32)
        cvec = k.sbuf("cvec", [128, 8], F32)
        scv = k.sbuf("scv", [128, 8], F32)
        mods = [k.sbuf(f"mod{l}", [128, 48], F32) for l in range(2)]
        adab = [k.sbuf(f"adab{l}", [128, 48], F32) for l in range(2)]
        gs1 = [k.sbuf(f"gs1_{l}", [128, 8], F32) for l in range(2)]
        gs2 = [k.sbuf(f"gs2_{l}", [128, 8], F32) for l in range(2)]
        gmx = [k.sbuf(f"gmx{l}", [128, 8], F32) for l in range(2)]
        gff = [k.sbuf(f"gff{l}", [128, 8], F32) for l in range(2)]
        rw_sb = k.sbuf("rw_sb", [128, 8, 16], F32)
        rb_sb = k.sbuf("rb_sb", [128, 16], F32)
        rbias = k.sbuf("rbias", [16, 1], F32)
        sq = [k.sbuf(f"sq{i}", [128, 512], F32) for i in range(2)]
        tmpf = [k.sbuf(f"tmpf{i}", [128, 512], F32) for i in range(2)]
        rstd = k.sbuf("rstd", [128, 512], F32)
        ps = [k.psum(f"ps{i}", [128, 512], F32) for i in range(7)]
        pT = k.psum("pT", [128, 1024], BF16)
        cosT = k.sbuf("cosT", [128, NT, 8], F32)
        sinT = k.sbuf("sinT", [128, NT, 8], F32)
        trimask = k.sbuf("trimask", [128, 256], BF16)
        ARENA = 48 * 1024
        arena = k.sbuf("arena", [128, ARENA // 2], BF16)
        ar_state = {"off": 0}

        def ar_reset():
            k.barrier(o, ps[6], ones_f)
            ar_state["off"] = 0
            ar_state["offA"] = 0
            ar_state["offB"] = 0

        def ar_rewind(off):
            k.barrier(o, ps[6], ones_f)
            ar_state["off"] = off

        def ar(name, shape, dt, pool="main"):
            esz = 4 if dt in (F32, I32) else 2
            n = int(np.prod(shape[1:]))
            nb = (n * esz + 3) // 4 * 4
            key = "off" if pool == "main" else "off" + pool
            base_ap, cap = {"main": (arena.ap, ARENA), "A": (wA.ap, 17408), "B": (wB.ap, 8192)}[pool]
            off = ar_state.get(key, 0)
            assert off + nb <= cap, (name, pool, off, nb)
            ar_state[key] = off + nb
            ap = base_ap[0:shape[0], off // 2:(off + nb) // 2]
            if dt != BF16:
                ap = ap.bitcast(dt)
            ap = ap[:, 0:n]
            if len(shape) == 3:
                ap = ap.rearrange("p (a b) -> p a b", a=shape[1])
            elif len(shape) == 4:
                ap = ap.rearrange("p (a b c) -> p a b c", a=shape[1], b=shape[2])
            return T(name, ap)

        o.load("sp", ident_f[:], dram["ident_f"])
        o.load("pool", ident_b[:], dram["ident_b"])
        o.load("sp", ones_f[:], dram["ones_f"])
        o.load("sp", cvec[:], dram["cvec"])
        o.load("sp", xT[:], dram["xT"].rearrange("(c p) t -> p c t", p=128))
        o.load("sp", rw_sb[:], dram["router_w"].rearrange("(c p) e -> p c e", p=128))
        o.load("sp", rb_sb[:], dram["router_b"])
        for l in range(2):
            o.load("sp", adab[l][:], dram["ada_b"][l])
            o.load("sp", gmx[l][:], dram["g_mix"][l])
            o.load("sp", gff[l][:], dram["g_ffn"][l])
        o.memset("pool", hT[:, :, 0:1], 0.0)

        o.act(scv[:], cvec[:], AF.Silu)
        stage = [wA[:, 0:4096].cast(F32).re("p (c e) -> p c e", c=8), wB[:, 0:4096].cast(F32).re("p (c e) -> p c e", c=8)]
        for l in range(0 if dbg.get("only") else n_layers):
            for blk in range(24):
                sg = stage[blk % 2]
                o.load("sp", sg, dram["ada_w"][l][:, blk * 256:(blk + 1) * 256].rearrange("(c p) e -> p c e", p=128))
                for jj in range(2):
                    j = blk * 2 + jj
                    for c in range(8):
                        o.mm(ps[0][:, j:j + 1], sg[:, c, jj * 128:(jj + 1) * 128], scv[:, c:c + 1], start=(c == 0), stop=(c == 7))
            o.tt("dve", mods[l][:], ps[0][:, 0:48], adab[l][:], ALU.add)
            o.stt("dve", gs1[l][:], mods[l][:, 8:16], 1.0, gmx[l][:], ALU.add, ALU.mult)
            o.stt("dve", gs2[l][:], mods[l][:, 32:40], 1.0, gff[l][:], ALU.add, ALU.mult)
        if "mods" in dbg_out:
            o.store("sp", dbg_out["mods"][0], mods[0][:])
            o.store("sp", dbg_out["mods"][1], mods[1][:])

        def norm(l, gs, sh_off, router=False):
            k.phase = "norm"
            sh = mods[l]
            if router:
                for c in range(8):
                    o.mm(ps[5][0:16, 0:1], rw_sb[:, c, :], sh[:, sh_off + c:sh_off + c + 1], start=(c == 0), stop=(c == 7))
                o.cp("dve", rbias[:], ps[5][0:16, 0:1])
            for tb in range(NB):
                tsl = slice(tb * 512, (tb + 1) * 512)
                for c in range(8):
                    s_ = sq[c % 2]
                    o.act(s_[:], xT[:, c, tsl], AF.Square)
                    o.mm(ps[0][:], ones_f[:], s_[:], start=(c == 0), stop=(c == 7))
                o.act(rstd[:], ps[0][:], AF.Sqrt, bias=EPS, scale=1.0 / D)
                o.recip(rstd[:], rstd[:])
                for c in range(8):
                    t_ = tmpf[c % 2]
                    o.stt("dve", t_[:], xT[:, c, tsl], gs[:, c:c + 1], rstd[:], ALU.mult, ALU.mult)
                    o.act(hT[:, c, 1 + tb * 512:1 + (tb + 1) * 512], t_[:], AF.Identity, bias=sh[:, sh_off + c:sh_off + c + 1])
                    if router:
                        o.mm(ps[1][0:16, :], rw_sb[:, c, :], t_[:], start=(c == 0), stop=(c == 7))
                if router:
                    o.act(F["affT"][:, tsl], ps[1][0:16, :], AF.Sigmoid, bias=rbias[:])

        def out_proj(l, grp):
            k.phase = "out_proj"
            o.load("pool", wo[:], dram["w_out"][l][grp * 256:(grp + 1) * 256, :].rearrange("(c p) e -> p c e", p=128))
            g1 = mods[l][:, 16:24]
            for tb in range(NB):
                tsl = slice(tb * 512, (tb + 1) * 512)
                for dc in range(8):
                    p_ = ps[2 + (dc % 2)]
                    for kc in range(2):
                        o.mm(p_[:], wo[:, kc, dc * 128:(dc + 1) * 128], mixT[:, kc, tsl], start=(kc == 0), stop=(kc == 1))
                    o.stt("dve", xT[:, dc, tsl], p_[:], g1[:, dc:dc + 1], xT[:, dc, tsl], ALU.mult, ALU.add)

        rt_tiles = {}
        moe_tiles = {}
        F = {}

        def ffn_alloc():
            ar_reset()
            F["affT"] = ar("affT", [16, S], F32)
            F["gatesT"] = ar("gatesT", [16, S], F32)
            F["selE"] = ar("selE", [16, 16, 128], F32)
            o.load("sp", F["selE"][:], dram["selE"].rearrange("k (e m) -> k e m", e=16))
            rt_tiles.update(dict(
                aff=ar("r_aff", [128, NT, 16], F32), sel=ar("r_sel", [128, NT, 16], F32),
                sel2=ar("r_sel2", [128, NT, 16], F32), m1=ar("r_m1", [128, NT * 4], F32),
                m2=ar("r_m2", [128, NT * 4], F32), gsc=ar("r_gsc", [128, NT, 4], F32),
                gm=ar("r_gm", [128, NT], F32), den=ar("r_den", [128, NT], F32)))
            moe_tiles["gb"] = [ar(f"m_gb{i}", [128, 512], F32) for i in range(2)]
            moe_tiles["sg"] = [ar(f"m_sg{i}", [128, 512], BF16) for i in range(2)]
            moe_tiles["u2"] = [ar(f"m_u2{i}", [128, 512], BF16) for i in range(2)]
            moe_tiles["aT"] = [ar(f"m_aT{i}", [128, 2, 512], BF16) for i in range(2)]

        def routing():
            k.phase = "routing"
            aff, sel, sel2, m1, m2, gsc, gm, den = (rt_tiles[n] for n in ("aff", "sel", "sel2", "m1", "m2", "gsc", "gm", "den"))
            for t in range(NT):
                o.tr(ps[4][:, t * 16:(t + 1) * 16], F["affT"][:, t * 128:(t + 1) * 128], ident_f[0:16, 0:16])
            o.cp("dve", aff[:].re("p t e -> p (t e)"), ps[4][:, 0:256])
            o.tt("dve", sel[:], aff[:], rb_sb[:].re("p (o e) -> p o e", o=1).bc([128, NT, 16]), ALU.add)
            s4 = sel[:].re("p t (g j) -> p (t g) j", g=4)
            o.red(m1[:], s4, ALU.max)
            o.tt("dve", sel2[:].re("p t (g j) -> p (t g) j", g=4), s4, m1[:].re("p (a o) -> p a o", o=1).bc([128, NT * 4, 4]), ALU.is_ge)
            o.stt("dve", sel2[:], sel2[:], -1e9, sel[:], ALU.mult, ALU.add)
            o.red(m2[:], sel2[:].re("p t (g j) -> p (t g) j", g=4), ALU.max)
            o.tt("dve", gsc[:].re("p t g -> p (t g)"), m1[:], m2[:], ALU.add)
            o.red(gm[:], gsc[:], ALU.max)
            o.tt("dve", gsc[:], gsc[:], gm[:].re("p (t o) -> p t o", o=1).bc([128, NT, 4]), ALU.is_ge)
            o.tt("dve", sel2[:].re("p t (g j) -> p (t g) j", g=4), s4, m2[:].re("p (a o) -> p a o", o=1).bc([128, NT * 4, 4]), ALU.is_ge)
            o.tt("dve", sel2[:].re("p t (g j) -> p (t g) j", g=4), sel2[:].re("p t (g j) -> p (t g) j", g=4),
                 gsc[:].re("p t (g o) -> p (t g) o", o=1).bc([128, NT * 4, 4]), ALU.mult)
            o.tt("dve", sel2[:], sel2[:], aff[:], ALU.mult)
            o.red(den[:], sel2[:], ALU.add)
            o.recip(den[:], den[:])
            o.tt("dve", sel2[:], sel2[:], den[:].re("p (t o) -> p t o", o=1).bc([128, NT, 16]), ALU.mult)
            for g4 in range(4):
                for t4 in range(4):
                    t = g4 * 4 + t4
                    o.tr(ps[5][0:16, t4 * 128:(t4 + 1) * 128], sel2[:, t, :], ident_f[:])
                o.cp("act", F["gatesT"][:, g4 * 512:(g4 + 1) * 512], ps[5][0:16, :])


        def moe(l):
            k.phase = "moe"
            g2 = mods[l][:, 40:48]

            def wviews(e):
                wb = (wA, wB)[e % 2]
                return (wb[:, 0:2048].re("p (c f) -> p c f", c=8), wb[:, 2048:4096].re("p (c f) -> p c f", c=8),
                        wb[:, 4096:6144].re("p (c d) -> p c d", c=2))

            def load_w(e):
                wg, wu, wd = wviews(e)
                o.load("pool", wg, dram["moe_wg"][l, e].rearrange("(c p) f -> p c f", p=128))
                o.load("pool", wu, dram["moe_wu"][l, e].rearrange("(c p) f -> p c f", p=128))
                o.load("pool", wd, dram["moe_wd"][l, e].rearrange("(c p) d -> p c d", p=128))

            def down(wd, aT, tsl):
                for dc in range(8):
                    pd = ps[4 + (dc % 2)]
                    for fc in range(2):
                        o.mm(pd[:], wd[:, fc, dc * 128:(dc + 1) * 128], aT[:, fc, :], start=(fc == 0), stop=(fc == 1))
                    o.stt("dve", xT[:, dc, tsl], pd[:], g2[:, dc:dc + 1], xT[:, dc, tsl], ALU.mult, ALU.add)

            load_w(0)
            pending = None
            it = 0
            for e in range(16):
                wg, wu, wd = wviews(e)
                for tb in range(NB):
                    tsl = slice(tb * 512, (tb + 1) * 512)
                    hsl = slice(1 + tb * 512, 1 + (tb + 1) * 512)
                    gb = moe_tiles["gb"][it % 2]
                    aT = moe_tiles["aT"][it % 2]
                    it += 1
                    o.mm(ps[6][:], F["selE"][:, e, :], F["gatesT"][:, tsl])
                    o.cp("act", gb[:], ps[6][:])
                    for fc in range(2):
                        pg, pu = ps[0 + fc], ps[2 + fc]
                        for c in range(8):
                            o.mm(pg[:], wg[:, c, fc * 128:(fc + 1) * 128], hT[:, c, hsl], start=(c == 0), stop=(c == 7))
                        for c in range(8):
                            o.mm(pu[:], wu[:, c, fc * 128:(fc + 1) * 128], hT[:, c, hsl], start=(c == 0), stop=(c == 7))
                        sg = moe_tiles["sg"][fc]
                        u2 = moe_tiles["u2"][fc]
                        o.act(sg[:], pg[:], AF.Silu)
                        o.tt("dve", u2[:], pu[:], gb[:], ALU.mult)
                        o.tt("pool", aT[:, fc, :], sg[:], u2[:], ALU.mult)
                    if pending is not None:
                        pending()
                    if tb == 0 and e + 1 < 16:
                        load_w(e + 1)
                    pending = (lambda wd=wd, aT=aT, tsl=tsl: down(wd, aT, tsl))
            pending()

        MAGIC = 12582912.0

        def rope_table(dst_cos, dst_sin, pos_i, nparts, ncol, tmp_y, tmp_r, posf, invf_t):
            o.cp("dve", posf, pos_i)
            o.tt("dve", tmp_y, posf.re("p (t o) -> p t o", o=1).bc([nparts, ncol, 8]),
                 invf_t.re("p (o f) -> p o f", o=1).bc([nparts, ncol, 8]), ALU.mult)
            for dst, shift in ((dst_sin, 0.0), (dst_cos, 0.25)):
                o.ts("dve", tmp_r, tmp_y, shift, ALU.add, MAGIC, ALU.add)
                o.ts("dve", tmp_r, tmp_r, MAGIC, ALU.subtract)
                o.stt("dve", tmp_r, tmp_y, shift, tmp_r, ALU.add, ALU.subtract)
                o.act(dst, tmp_r, AF.Sin, scale=6.28318)

        invf_t = k.sbuf("invf", [128, 8], F32)
        posi = k.sbuf("posi", [128, NT], I32)
        posf = k.sbuf("posf", [128, NT], F32)
        rt_y = k.sbuf("rt_y", [128, NT, 8], F32)
        rt_r = k.sbuf("rt_r", [128, NT, 8], F32)
        o.load("sp", invf_t[:], dram["invf"])
        o.load("sp", posi[:], dram["pos_tm"])
        o.load("pool", trimask[:], dram["trimask"])
        rope_table(cosT[:], sinT[:], posi[:], 128, NT, rt_y[:], rt_r[:], posf[:], invf_t[:])

        def proj_qk(l, wname, gname, qkT, wbuf, sc):
            k.phase = k.phase.split("/")[0] + "/projqk"
            o.load("pool", wbuf, dram[wname][l].rearrange("(c p) e -> p c e", p=128))
            o.load("sp", sc["gain"][:], dram[gname][l])
            for tt in range(NT):
                b = tt % 2
                t1, t2, yb, ss = sc["t1"][b], sc["t2"][b], sc["yb"][b], sc["ss"][b]
                ra, rb, rc, rd = sc["ra"][b], sc["rb"][b], sc["rc"][b], sc["rd"][b]
                pq = ps[b]
                pTh = pT[:, b * 512:(b + 1) * 512]
                hs = slice(1 + tt * 128, 1 + (tt + 1) * 128)
                for c in range(8):
                    o.mm(pq[:], hT[:, c, hs], wbuf[:, c, :], start=(c == 0), stop=(c == 7))
                o.act(t1[:], pq[:], AF.Square)
                o.red(ss[:], t1[:].re("p (h d) -> p h d", h=8), ALU.add)
                o.act(ss[:], ss[:], AF.Sqrt, bias=EPS, scale=1.0 / 64)
                o.recip(ss[:], ss[:])
                o.tt("dve", t2[:].re("p (h d) -> p h d", h=8), pq[:].re("p (h d) -> p h d", h=8),
                     ss[:].re("p (h o) -> p h o", o=1).bc([128, 8, 64]), ALU.mult)
                o.tt("pool", t2[:], t2[:], sc["gain"][:], ALU.mult)
                y3 = t2[:].re("p (h d) -> p h d", h=8)
                yb3 = yb[:].re("p (h d) -> p h d", h=8)
                cs = cosT[:, tt, :].re("p (o f) -> p o f", o=1).bc([128, 8, 8])
                sn = sinT[:, tt, :].re("p (o f) -> p o f", o=1).bc([128, 8, 8])
                o.tt("dve", ra[:], y3[:, :, 0:8], cs, ALU.mult)
                o.tt("pool", rb[:], y3[:, :, 8:16], sn, ALU.mult)
                o.tt("dve", yb3[:, :, 0:8], ra[:], rb[:], ALU.subtract)
                o.tt("pool", rc[:], y3[:, :, 8:16], cs, ALU.mult)
                o.tt("dve", rd[:], y3[:, :, 0:8], sn, ALU.mult)
                o.tt("pool", yb3[:, :, 8:16], rc[:], rd[:], ALU.add)
                o.cp("act", yb3[:, :, 16:64], y3[:, :, 16:64])
                for j_ in range(4):
                    o.tr(pTh[:, j_ * 128:(j_ + 1) * 128], yb[:, j_ * 128:(j_ + 1) * 128], ident_b[:])
                o.cp("act", qkT[:, :, tt * 128:(tt + 1) * 128], pTh.re("p (j t) -> p j t", j=4))

        def qk_scratch():
            two = lambda nm, shp, dt: [ar(f"{nm}{i}", shp, dt) for i in range(2)]
            return dict(gain=ar("gain", [128, 512], F32), t1=two("t1", [128, 512], F32), t2=two("t2", [128, 512], F32),
                        yb=two("yb", [128, 512], BF16), ss=two("ss", [128, 8], F32),
                        ra=two("ra", [128, 8, 8], F32), rb=two("rb", [128, 8, 8], F32),
                        rc=two("rc", [128, 8, 8], F32), rd=two("rd", [128, 8, 8], F32))

        def dilated(l):
            k.phase = "dilated"
            ar_reset()
            qkT = ar("qkT_d", [128, 4, S], BF16)
            mark = ar_state["off"]
            sc = qk_scratch()
            wq = wB[:, 0:4096].re("p (c e) -> p c e", c=8)
            wv = wB[:, 4096:6144].re("p (c e) -> p c e", c=8)
            proj_qk(l, "w_dqk", "g_dqk", qkT, wq, sc)
            ar_rewind(mark)
            k.phase = "dilated/attn"
            Vd = ar("Vd", [128, NT, 256], BF16)
            acc = [ar(f"acc{i}", [128, S], F32) for i in range(2)]
            pt = [ar(f"pt{i}", [128, 256], BF16) for i in range(3)]
            dtmp = sq[0]
            o.load("pool", wv, dram["w_vd"][l].rearrange("(c p) e -> p c e", p=128))
            o.memset("pool", Vd[:, :, 64:192], 1.0)
            it = 0
            for hp in range(2):
                for pi, dil in enumerate((1, 4, 16)):
                    nblk = (S // dil) // 128
                    for r in range(dil):
                        for m in range(nblk):
                            oi = r * nblk + m
                            st_ = 1 + r + dil * 128 * m
                            for c in range(8):
                                o.mm(ps[4][:, 0:128], hT[:, c, st_:st_ + dil * 127 + 1:dil], wv[:, c, hp * 128:(hp + 1) * 128], start=(c == 0), stop=(c == 7))
                            o.cp("act", Vd[:, oi, :].re("p (a b) -> p a b", b=64)[:, 0:4:3, :], ps[4][:, 0:128].re("p (a b) -> p a b", a=2))
                    items = []
                    for hh in range(2):
                        h = hp * 2 + hh
                        base = hh * 64
                        a_ = acc[hh]
                        for r in range(dil):
                            for n in range(nblk):
                                ms = [m for m in (n - 1, n) if m >= 0]
                                ncols = 128 * len(ms)
                                qcols = slice(r + dil * 128 * n, r + dil * 128 * n + dil * 127 + 1, dil)
                                pss = (ps[0], ps[1], ps[5])[it % 3]
                                pso = ps[2 + it % 2]
                                p_ = pt[it % 3]
                                it += 1

                                def A(ms=ms, ncols=ncols, qcols=qcols, pss=pss, p_=p_, base=base, r=r):
                                    for j, m in enumerate(ms):
                                        kcols = slice(r + dil * 128 * m, r + dil * 128 * m + dil * 127 + 1, dil)
                                        o.mm(pss[:, j * 128:(j + 1) * 128], qkT[base:base + 64, 2 + hp, kcols], qkT[base:base + 64, hp, qcols])
                                    o.act(p_[:, 0:ncols], pss[:, 0:ncols], AF.Exp, scale=0.125)
                                    o.tt("pool", p_[:, 0:ncols], p_[:, 0:ncols], trimask[:, 256 - ncols:256], ALU.mult)

                                def B(ms=ms, qcols=qcols, pso=pso, p_=p_, hh=hh, a_=a_, r=r):
                                    for j, m in enumerate(ms):
                                        oi = r * nblk + m
                                        lv = Vd[:, oi, hh * 128:(hh + 1) * 128]
                                        o.mm(pso[:, 0:128], lv, p_[:, j * 128:(j + 1) * 128], start=(j == 0), stop=(j == len(ms) - 1))
                                    if pi == 0:
                                        o.cp("dve", a_[:, qcols], pso[:, 0:128])
                                    else:
                                        o.tt("dve", a_[:, qcols], pso[:, 0:128], a_[:, qcols], ALU.add)
                                items.append((A, B))
                    LOOK = 2
                    for idx, (A, B) in enumerate(items):
                        A()
                        if idx >= LOOK:
                            items[idx - LOOK][1]()
                    for idx in range(max(0, len(items) - LOOK), len(items)):
                        items[idx][1]()
                for hh in range(2):
                    a_ = acc[hh]
                    ob, db = (0, 64) if hh == 0 else (64, 0)
                    for tb in range(NB):
                        tsl = slice(tb * 512, (tb + 1) * 512)
                        o.cp("act", dtmp[ob:ob + 64, :], a_[db:db + 64, tsl])
                        o.recip(dtmp[ob:ob + 64, :], dtmp[ob:ob + 64, :])
                        o.tt("dve", mixT[ob:ob + 64, hp, tsl], a_[ob:ob + 64, tsl], dtmp[ob:ob + 64, :], ALU.mult)


        def nsa(l):
            k.phase = "nsa"
            ar_reset()
            qkT = ar("qkT_n", [128, 4, S], BF16)
            mark = ar_state["off"]
            sc = qk_scratch()
            wq = wB[:, 0:4096].re("p (c e) -> p c e", c=8)
            wv = wB[:, 4096:5120].re("p (c e) -> p c e", c=8)
            wf = wB[:, 5120:6144].re("p (c e) -> p c e", c=8)
            wg = wB[:, 6144:6240].re("p (c e) -> p c e", c=8)
            proj_qk(l, "w_nqk", "g_nqk", qkT, wq, sc)
            ar_rewind(mark)
            Vn = ar("Vn", [128, NT, 4, 128], BF16)
            kcvc = ar("kcvc", [128, S], BF16, "A")
            sgT = ar("sgT", [12, S], BF16, "B")
            selT = ar("selT", [32, S], BF16, "B")
            cmpmask = ar("cmpmask", [127, 512], BF16)
            o.load("pool", wv, dram["w_vn"][l].rearrange("(c p) e -> p c e", p=128))
            o.load("pool", wf, dram["w_fm"][l][:, 256:384].rearrange("(c p) e -> p c e", p=128))
            o.load("pool", wg, dram["w_gl"][l].rearrange("(c p) e -> p c e", p=128))
            small = {}
            for nm, shp, dt, pl in (("covones", [127, 33], BF16, "main"), ("selG", [12, 12, 128], BF16, "main"), ("Esel", [32, 16, 128], BF16, "A"),
                                    ("selkeep", [128, 32], F32, "main"), ("seladd", [128, 32], F32, "main"), ("trigt", [128, 128], BF16, "main")):
                small[nm] = ar("c_" + nm, shp, dt, pl)
            o.load("pool", small["covones"][:], dram["covones"])
            o.load("pool", small["selG"][:], dram["selG"].rearrange("k (e m) -> k e m", e=12))
            o.load("pool", small["Esel"][:], dram["Esel"].rearrange("k (e m) -> k e m", e=16))
            o.load("pool", small["trigt"][:], dram["trigt"])
            k.phase = "nsa/vproj"
            o.memset("pool", Vn[:], 1.0)
            for tt in range(NT):
                hs = slice(1 + tt * 128, 1 + (tt + 1) * 128)
                for c in range(8):
                    o.mm(ps[4][:, 0:128], hT[:, c, hs], wv[:, c, :], start=(c == 0), stop=(c == 7))
                vflat = Vn[:, tt, :, :].re("p a b -> p (a b)")
                o.cp("act", vflat[:, 0:64], ps[4][:, 0:64])
                o.cp("act", vflat[:, 192:256], ps[4][:, 0:64])
                o.cp("dve", vflat[:, 256:320], ps[4][:, 64:128])
                o.cp("dve", vflat[:, 448:512], ps[4][:, 64:128])
            for tb in range(NB):
                hsl = slice(1 + tb * 512, 1 + (tb + 1) * 512)
                tsl = slice(tb * 512, (tb + 1) * 512)
                for c in range(8):
                    o.mm(ps[0][:], wf[:, c, :], hT[:, c, hsl], start=(c == 0), stop=(c == 7))
                o.cp("act", kcvc[:, tsl], ps[0][:])
                for c in range(8):
                    o.mm(ps[1][0:12, :], wg[:, c, :], hT[:, c, hsl], start=(c == 0), stop=(c == 7))
                o.act(sgT[:, tsl], ps[1][0:12, :], AF.Sigmoid)
            k.phase = "nsa/cmp"
            w1t = ar("w1t", [128, 32, 64], BF16, "A")
            peT = ar("peT", [128, 32], BF16, "A")
            w2t = ar("w2t", [64, 2, 64], BF16, "A")
            gel = ar("gel", [64, 2, 128], BF16, "A")
            for j in range(2):
                o.load("pool", w1t[j * 64:(j + 1) * 64, :, :], dram["cmp_w1"][l, j].rearrange("(i d) f -> d i f", d=64))
                o.load("pool", w2t[:, j, :], dram["cmp_w2"][l, j])
            o.load("pool", peT[:], dram["cmp_peT"][l])
            for j in range(2):
                b0 = j * 64
                for i in range(32):
                    o.mm(ps[2][0:64, 0:127], w1t[b0:b0 + 64, i, :], kcvc[b0:b0 + 64, i:i + 16 * 126 + 1:16], start=(i == 0), stop=False)
                for i in range(32):
                    o.mm(ps[2][0:64, 0:127], w1t[b0:b0 + 64, i, :], peT[b0:b0 + 64, i:i + 1].bc([64, 127]), start=False, stop=(i == 31))
                o.act(gel[:, j, 0:127], ps[2][0:64, 0:127], AF.Gelu_apprx_tanh)
            kc_f = ar("kc_f", [128, 128], F32, "A")
            kc_b = ar("kc_b", [128, 128], BF16, "A")
            kc_sq = ar("kc_sq", [128, 128], F32, "A")
            gkc = ar("gkc", [128, 128], F32, "A")
            kss = ar("kss", [128, 2], F32, "A")
            kcT = ar("kcT", [128, 128], BF16, "A")
            vc_aug = ar("vc_aug", [128, 2, 128], BF16)
            pce_i = ar("pce_i", [128, 1], I32, "A")
            pce_f = ar("pce_f", [128, 1], F32, "A")
            cy = ar("cy", [128, 1, 8], F32, "A")
            cr = ar("cr", [128, 1, 8], F32, "A")
            ccos = ar("ccos", [128, 1, 8], F32, "A")
            csin = ar("csin", [128, 1, 8], F32, "A")
            cra = ar("cra", [128, 2, 8], F32, "A")
            crb = ar("crb", [128, 2, 8], F32, "A")
            o.load("sp", gkc[:], dram["g_kcmp"][l])
            o.load("sp", pce_i[0:127, :], dram["pos_ce"])
            rope_table(ccos[0:127], csin[0:127], pce_i[0:127, :], 127, 1, cy[0:127], cr[0:127], pce_f[0:127, :], invf_t[0:127, :])
            for d2 in range(2):
                o.mm(ps[3][0:127, d2 * 64:(d2 + 1) * 64], gel[:, 0, 0:127], w2t[:, 0, :])
            o.mm(ps[3][0:127, 128:192], gel[:, 1, 0:127], w2t[:, 1, :])
            R = slice(0, 127)
            o.act(kc_sq[R, :], ps[3][R, 0:128], AF.Square)
            o.red(kss[R, :], kc_sq[R, :].re("p (h d) -> p h d", h=2), ALU.add)
            o.act(kss[R, :], kss[R, :], AF.Sqrt, bias=EPS, scale=1.0 / 64)
            o.recip(kss[R, :], kss[R, :])
            o.tt("dve", kc_f[R, :].re("p (h d) -> p h d", h=2), ps[3][R, 0:128].re("p (h d) -> p h d", h=2),
                 kss[R, :].re("p (h o) -> p h o", o=1).bc([127, 2, 64]), ALU.mult)
            o.tt("dve", kc_f[R, :], kc_f[R, :], gkc[R, :], ALU.mult)
            y3 = kc_f[R, :].re("p (h d) -> p h d", h=2)
            yb3 = kc_b[R, :].re("p (h d) -> p h d", h=2)
            cs = ccos[R, :, :].bc([127, 2, 8])
            sn = csin[R, :, :].bc([127, 2, 8])
            o.tt("dve", cra[R], y3[:, :, 0:8], cs, ALU.mult)
            o.tt("dve", crb[R], y3[:, :, 8:16], sn, ALU.mult)
            o.tt("dve", yb3[:, :, 0:8], cra[R], crb[R], ALU.subtract)
            o.tt("dve", cra[R], y3[:, :, 8:16], cs, ALU.mult)
            o.tt("dve", crb[R], y3[:, :, 0:8], sn, ALU.mult)
            o.tt("dve", yb3[:, :, 8:16], cra[R], crb[R], ALU.add)
            o.cp("act", yb3[:, :, 16:64], y3[:, :, 16:64])
            o.tr(pT[:, 0:127], kc_b[R, :], ident_b[0:127, 0:127])
            o.cp("act", kcT[:, 0:127], pT[:, 0:127])
            o.memset("pool", vc_aug[:], 1.0)
            o.cp("act", vc_aug[R, 0, 0:64], ps[3][R, 128:192])
            o.cp("act", vc_aug[R, 1, 64:128], ps[3][R, 128:192])

            wgt = ar("wgt", [128, 512], F32)
            ctr = ar("ctr", [128, 512], BF16)
            first = {}

            def epilogue(pso, ncol, h, br, tcol0, clamp=False):
                ob, db = (0, 64) if h % 2 == 0 else (64, 0)
                tsl_ = slice(tcol0, tcol0 + ncol)
                o.cp("act", wgt[ob:ob + 64, 0:ncol], pso[db:db + 64, 0:ncol])
                if clamp:
                    o.ts("dve", wgt[ob:ob + 64, 0:ncol], wgt[ob:ob + 64, 0:ncol], 1e-30, ALU.max)
                o.recip(wgt[ob:ob + 64, 0:ncol], wgt[ob:ob + 64, 0:ncol])
                o.mm(ps[6][:, 0:ncol], small["selG"][:, h * 3 + br, :], sgT[:, tsl_])
                o.tt("dve", wgt[ob:ob + 64, 0:ncol], wgt[ob:ob + 64, 0:ncol], ps[6][ob:ob + 64, 0:ncol], ALU.mult)
                dst = mixT[ob:ob + 64, h // 2, tsl_]
                if br == 0:
                    o.tt("dve", dst, pso[ob:ob + 64, 0:ncol], wgt[ob:ob + 64, 0:ncol], ALU.mult)
                else:
                    o.tt("dve", ctr[ob:ob + 64, 0:ncol], pso[ob:ob + 64, 0:ncol], wgt[ob:ob + 64, 0:ncol], ALU.mult)
                    o.tt("pool", dst, dst, ctr[ob:ob + 64, 0:ncol], ALU.add)

            eT = [ar(f"eT{i}", [128, 512], BF16) for i in range(2)]
            imp = ar("imp", [128, 32], F32)
            imp2 = ar("imp2", [128, 32], F32)
            mx8 = ar("mx8", [128, 8], F32)
            rdn = ar("rdn", [128, 4], F32)
            selm = ar("selm", [128, 32], F32)
            it = 0
            for tb in range(NB):
                tsl = slice(tb * 512, (tb + 1) * 512)
                o.load("pool", cmpmask[:], dram["cmpmask"][:, tsl])
                for h in range(4):
                    base = (h % 2) * 64
                    e_ = eT[it % 2]
                    it += 1
                    o.mm(ps[0][0:127, :], kcT[base:base + 64, 0:127], qkT[base:base + 64, h // 2, tsl])
                    o.act(e_[0:127, :], ps[0][0:127, :], AF.Exp, scale=0.125)
                    o.tt("pool", e_[0:127, :], e_[0:127, :], cmpmask[:, :], ALU.mult)
                    o.mm(ps[1][:], vc_aug[0:127, h % 2, :], e_[0:127, :])
                    for q4 in range(4):
                        o.mm((ps[2], ps[5])[q4 // 2][:, ((q4 % 2) * 4 + h) * 33:((q4 % 2) * 4 + h + 1) * 33], e_[0:127, q4 * 128:(q4 + 1) * 128], small["covones"][:, :])
                    epilogue(ps[1], 512, h, 0, tb * 512, clamp=True)
                for q4 in range(4):
                    tt = tb * 4 + q4
                    pv = (ps[2], ps[5])[q4 // 2][:, (q4 % 2) * 132:(q4 % 2 + 1) * 132].re("p (h c) -> p h c", h=4)
                    o.load("sp", small["selkeep"][:], dram["selkeep"][:, tt, :])
                    o.load("sp", small["seladd"][:], dram["seladd"][:, tt, :])
                    o.ts("dve", rdn[:], pv[:, :, 32], 1e-30, ALU.max)
                    o.recip(rdn[:], rdn[:])
                    o.ts("dve", imp[:], pv[:, 0, 0:32], rdn[:, 0:1], ALU.mult)
                    for h in range(1, 4):
                        o.stt("dve", imp[:], pv[:, h, 0:32], rdn[:, h:h + 1], imp[:], ALU.mult, ALU.add)
                    o.tt("dve", imp[:], imp[:], small["selkeep"][:, :], ALU.mult)
                    o.tt("dve", imp[:], imp[:], small["seladd"][:, :], ALU.add)
                    k.op("dve", lambda h_, a=mx8, b=imp: h_.max(out=a.ap, in_=b.ap), reads=[imp], writes=[mx8])
                    k.op("dve", lambda h_, a=imp2, b=mx8, c_=imp: h_.match_replace(out=a.ap, in_to_replace=b.ap, in_values=c_.ap, imm_value=-1e30),
                         reads=[imp, mx8], writes=[imp2])
                    k.op("dve", lambda h_, a=mx8, b=imp2: h_.max(out=a.ap, in_=b.ap), reads=[imp2], writes=[mx8])
                    o.ts("dve", selm[:], imp[:], mx8[:, 7:8], ALU.is_ge)
                    o.tr(ps[3][0:32, 0:128], selm[:], ident_f[:])
                    o.cp("act", selT[:, tt * 128:(tt + 1) * 128], ps[3][0:32, 0:128])

            k.phase = "nsa/selwin"
            mk = [ar(f"mk{i}", [128, 128], BF16) for i in range(3)]
            pp = [ar(f"pp{i}", [128, 256], BF16) for i in range(3)]
            it = 0
            items = []
            for br, kpair, vbase, span in ((1, 2, 0, 16), (2, 3, 2, 4)):
                for n in range(NT):
                    qsl = slice(n * 128, (n + 1) * 128)
                    js = [j for j in range(max(0, n - span), n + 1)]
                    for par in range(2):
                        base = par * 64
                        pso = ps[2 + par]
                        for ji, j in enumerate(js):
                            ksl = slice(j * 128, (j + 1) * 128)
                            p_ = pp[it % 3]
                            m_ = mk[it % 3]
                            pss = (ps[0], ps[1], ps[4])[it % 3]
                            it += 1

                            def A(br=br, kpair=kpair, span=span, n=n, j=j, qsl=qsl, ksl=ksl, base=base, p_=p_, m_=m_, pss=pss):
                                for hh in range(2):
                                    o.mm(pss[:, hh * 128:(hh + 1) * 128], qkT[base:base + 64, kpair, ksl], qkT[base:base + 64, hh, qsl])
                                o.act(p_[:], pss[:, 0:256], AF.Exp, scale=0.125)
                                msk = None
                                if br == 1:
                                    o.mm(ps[5][:, 0:128], small["Esel"][:, j, :], selT[:, qsl])
                                    if j == n:
                                        o.tt("dve", m_[:], ps[5][:, 0:128], trimask[:, 128:256], ALU.mult)
                                    else:
                                        o.cp("act", m_[:], ps[5][:, 0:128])
                                    msk = m_[:]
                                else:
                                    if j == n:
                                        msk = trimask[:, 128:256]
                                    elif j == n - span:
                                        msk = small["trigt"][:]
                                if msk is not None:
                                    o.tt("pool", p_[:].re("p (h q) -> p h q", h=2), p_[:].re("p (h q) -> p h q", h=2),
                                         msk.re("p (o q) -> p o q", o=1).bc([128, 2, 128]), ALU.mult)

                            def B(br=br, vbase=vbase, par=par, n=n, j=j, ji=ji, nj=len(js), p_=p_, pso=pso):
                                o.mm(pso[:, 0:256], Vn[:, j, vbase + par, :], p_[:], start=(ji == 0), stop=(ji == nj - 1))
                                if ji == nj - 1:
                                    for hh in range(2):
                                        h = hh * 2 + par
                                        epilogue(pso[:, hh * 128:(hh + 1) * 128], 128, h, br, n * 128)
                            items.append((A, B))
            LOOK = 2
            for idx, (A, B) in enumerate(items):
                A()
                if idx >= LOOK:
                    items[idx - LOOK][1]()
            for idx in range(max(0, len(items) - LOOK), len(items)):
                items[idx][1]()

        vfirst = None if dbg.get("no_vstore") else nc.dram_tensor("vfirst_scratch", [S, 256], F32).ap()
        vstore_tiles = []

        def rwkv(l):
            k.phase = "rwkv"
            ar_reset()
            WA = wA[:, 0:8448].re("p (c e) -> p c e", c=8)
            WB = wB[:, 0:8448].re("p (c e) -> p c e", c=8)
            mark0 = ar_state["off"]
            mu_t = ar("mu_t", [128, 1056], F32)
            omu = ar("omu", [128, 1056], F32)
            stg = [ar(f"stg{i}", [128, 1056], F32) for i in range(2)]
            o.load("sp", mu_t[:], dram["rw_mu"][l])
            o.ts("dve", omu[:], mu_t[:], -1.0, ALU.mult, 1.0, ALU.add)
            for c in range(8):
                sg = stg[c % 2]
                o.load("sp", sg[:], dram["w_rw"][l][c * 128:(c + 1) * 128, :])
                o.tt("dve", WA[:, c, :], sg[:], omu[:], ALU.mult)
                o.tt("pool", WB[:, c, :], sg[:], mu_t[:], ALU.mult)
            ar_rewind(mark0)
            if dbg.get("rwkv_stage", 9) <= -3:
                return
            P = {}
            for nm in ("w0", "a0", "kk", "ka", "rk", "lnw", "lnb") + (("v0",) if l > 0 else ()):
                P[nm] = ar("p_" + nm, [128, 256], F32)
                o.load("sp", P[nm][:], dram["rw_" + nm][l])
            wa2 = ar("wa2", [64, 2, 256], F32)
            g2a = ar("g2a", [128, 256], F32)
            g2b = ar("g2b", [32, 256], F32)
            o.load("sp", wa2[:, 0, :], dram["rw_w2"][l])
            o.load("sp", wa2[:, 1, :], dram["rw_a2"][l])
            o.load("sp", g2a[:], dram["rw_g2"][l][0:128, :])
            o.load("sp", g2b[:], dram["rw_g2"][l][128:160, :])
            if l > 0:
                v1 = ar("v1", [128, 2, 32], F32)
                v2 = ar("v2", [32, 256], F32)
                o.load("sp", v1[:], dram["rw_v1"][0].rearrange("(c p) r -> p c r", p=128))
                o.load("sp", v2[:], dram["rw_v2"][0])
            mU2 = ar("mU2", [128, 256], F32)
            mL = ar("mL", [128, 128], F32)
            o.load("sp", mU2[:], dram["mU2"])
            o.load("sp", mL[:], dram["mL"])
            Hs = ar("Hs", [64, 4, 64], F32)
            o.memset("dve", Hs[:], 0.0)
            if dbg.get("rwkv_stage", 9) <= -2:
                return
            nm256 = ("t0", "t1", "lw", "a_", "kk", "kp", "b_", "vv", "gi", "ginv", "ge", "gC", "gsb")
            W = {n_: ar("rw_" + n_, [128, 256], F32) for n_ in nm256}
            t0, t1, lw, a_, kk, kp, b_, vv, gi, ginv, ge, gC, gsb = (W[n_] for n_ in nm256)
            vf = gC
            vT = gi[:].re("p (a t) -> p a t", a=2)
            vv1T = ge[0:32, 0:128]
            XTkr = ar("XTkr", [64, 4, 2, 128], F32)
            XTbk = ar("XTbk", [64, 4, 2, 128], F32)
            P1 = ar("P1", [128, 256], F32)
            P2 = ar("P2", [128, 256], F32)
            Lc0 = ar("Lc0", [128, 128], F32)
            Lb = [ar(f"Lb{i}", [128, 128], F32) for i in range(2)]
            Ub = [ar(f"Ub{i}", [128, 128], F32) for i in range(2)]
            Y = [ar(f"Y{i}", [128, 128], F32) for i in range(2)]
            ILs = [ar(f"IL{i}", [128, 128], F32) for i in range(2)]
            RH = ar("RH", [128, 128], F32)
            nWU = ar("nWU", [128, 128], F32)
            TT = ar("TT", [64, 64], F32)
            QT = ar("QT", [64, 128], F32)
            s4 = ar("s4", [128, 4], F32)
            s4b = ar("s4b", [128, 4], F32)
            bc4 = ar("bc4", [128, 4], F32)
            gcol = ar("gcol", [64, 4], F32)
            lxw = ar("lxw", [64, 2, 128], F32)
            lxg = ar("lxg", [128, 128], F32)
            lxg2 = ar("lxg2", [32, 128], F32)
            yb_ = ar("yb_", [128, 256], BF16)
            h3 = lambda v_: v_.re("p (h d) -> p h d", h=4)
            b3 = lambda v_: v_.re("p (h o) -> p h o", o=1).bc([128, 4, 64])

            for tt in range(dbg.get("rwkv_tiles", NT)):
                cur = slice(1 + tt * 128, 1 + (tt + 1) * 128)
                prv = slice(tt * 128, (tt + 1) * 128)
                k.phase = "rwkv/front"
                for (pb, c0, c1) in ((ps[0][:, 0:512], 0, 512), (ps[1][:, 0:256], 512, 768)):
                    for c in range(8):
                        o.mm(pb, hT[:, c, cur], WA[:, c, c0:c1], start=(c == 0), stop=False)
                    for c in range(8):
                        o.mm(pb, hT[:, c, prv], WB[:, c, c0:c1], start=False, stop=(c == 7))
                for (pb, c0, c1) in ((ps[2][:, 0:128], 768, 896), (ps[2][:, 128:256], 896, 1024), (ps[2][0:32, 256:384], 1024, 1056)):
                    for c in range(8):
                        o.mm(pb, WA[:, c, c0:c1], hT[:, c, cur], start=(c == 0), stop=False)
                    for c in range(8):
                        o.mm(pb, WB[:, c, c0:c1], hT[:, c, prv], start=False, stop=(c == 7))
                if dbg.get("rwkv_stage", 9) <= -1:
                    continue
                sub = dbg.get("rwkv_sub", 99)
                if sub > 0:
                    o.act(lxw[:, 0, :], ps[2][0:64, 0:128], AF.Tanh)
                if sub > 1:
                    o.cp("act", lxw[:, 1, :], ps[2][64:128, 0:128])
                if sub > 2:
                    o.act(lxg[:], ps[2][:, 128:256], AF.Sigmoid)
                if sub > 3:
                    o.act(lxg2[:], ps[2][0:32, 256:384], AF.Sigmoid)
                if sub > 4:
                    o.mm(ps[3][:, 0:256], lxw[:, 0, :], wa2[:, 0, :])
                if sub > 5:
                    o.mm(ps[3][:, 256:512], lxw[:, 1, :], wa2[:, 1, :])
                if sub > 6:
                    o.mm(ps[4][:, 0:256], lxg[:], g2a[:], start=True, stop=False)
                if sub > 7:
                    o.mm(ps[4][:, 0:256], lxg2[:], g2b[:], start=False, stop=True)
                if sub > 8:
                    o.cp("act", gsb[:], ps[4][:, 0:256])
                if dbg.get("rwkv_stage", 9) < 1:
                    continue
                o.tt("dve", t0[:], ps[3][:, 0:256], P["w0"][:], ALU.add)
                o.act(lw[:], t0[:], AF.Sigmoid)
                o.ts("pool", lw[:], lw[:], -0.6065306597126334, ALU.mult)
                o.tt("dve", t0[:], ps[3][:, 256:512], P["a0"][:], ALU.add)
                o.act(a_[:], t0[:], AF.Sigmoid)
                o.cp("act", vv[:], ps[1][:, 0:256])
                if l == 0:
                    if not dbg.get("no_vstore"):
                        o.store("sp", vfirst[tt * 128:(tt + 1) * 128, :], vv[:])
                        if vv not in vstore_tiles:
                            vstore_tiles.append(vv)
                else:
                    o.load("sp", vf[:], vfirst[tt * 128:(tt + 1) * 128, :], extra=vstore_tiles)
                    for c2 in range(2):
                        o.tr(ps[6][:, c2 * 128:(c2 + 1) * 128], vv[:, c2 * 128:(c2 + 1) * 128], ident_f[:])
                    o.cp("act", vT, ps[6][:, 0:256].re("p (a t) -> p a t", a=2))
                    for c2 in range(2):
                        o.mm(ps[6][0:32, 256:384], v1[:, c2, :], vT[:, c2, :], start=(c2 == 0), stop=(c2 == 1))
                    o.cp("act", vv1T, ps[6][0:32, 256:384])
                    o.mm(ps[3][:, 0:256], vv1T, v2[:])
                    o.tt("dve", t0[:], ps[3][:, 0:256], P["v0"][:], ALU.add)
                    o.act(t0[:], t0[:], AF.Sigmoid)
                    o.tt("dve", t1[:], vf[:], vv[:], ALU.subtract)
                    o.tt("dve", t1[:], t1[:], t0[:], ALU.mult)
                    o.tt("dve", vv[:], vv[:], t1[:], ALU.add)
                kps = ps[0][:, 256:512]
                rps = ps[0][:, 0:256]
                o.tt("dve", kk[:], kps, P["kk"][:], ALU.mult)
                o.tt("pool", t0[:], kk[:], kk[:], ALU.mult)
                o.red(s4[:], h3(t0[:]), ALU.add)
                o.act(s4[:], s4[:], AF.Sqrt)
                o.ts("dve", s4[:], s4[:], 1e-12, ALU.max)
                o.recip(s4[:], s4[:])
                o.tt("dve", h3(kk[:]), h3(kk[:]), b3(s4[:]), ALU.mult)
                o.stt("dve", t0[:], a_[:], -1.0, P["ka"][:], ALU.add, ALU.mult)
                o.stt("dve", kp[:], t0[:], 1.0, kps, ALU.add, ALU.mult)
                o.tt("pool", b_[:], kk[:], a_[:], ALU.mult)
                o.tt("dve", t1[:], rps, kp[:], ALU.mult)
                o.tt("pool", t1[:], t1[:], P["rk"][:], ALU.mult)
                o.red(bc4[:], h3(t1[:]), ALU.add)
                if dbg.get("rwkv_stage", 9) < 2:
                    continue
                o.mm(ps[5][:, 0:256], mU2[:, 128:256], lw[:])
                o.mm(ps[5][:, 256:512], ones_f[:], lw[:])
                for h in range(4):
                    o.mm(ps[4][0:64, 256 + h:257 + h], lw[:, h * 64:(h + 1) * 64], ones_f[:, 0:1])
                o.act(gcol[:], ps[4][0:64, 256:260], AF.Exp)
                cum = ps[5][:, 0:256]
                o.act(gi[:], cum, AF.Exp)
                o.act(ginv[:], cum, AF.Exp, scale=-1.0)
                o.tt("dve", t0[:], cum, lw[:], ALU.subtract)
                o.act(ge[:], t0[:], AF.Exp)
                o.cp("act", t1[:], ps[5][:, 256:512])
                o.tt("dve", t1[:], t1[:], cum, ALU.subtract)
                o.act(gC[:], t1[:], AF.Exp)
                o.tt("pool", ge[:], kk[:], ge[:], ALU.mult)
                o.tt("dve", gi[:], rps, gi[:], ALU.mult)
                o.tt("pool", a_[:], b_[:], ginv[:], ALU.mult)
                o.tt("pool", ginv[:], kp[:], ginv[:], ALU.mult)
                o.tt("pool", b_[:], b_[:], gC[:], ALU.mult)
                o.tt("pool", gC[:], kp[:], gC[:], ALU.mult)
                KKt, Rt, Bh, Kh, BC, KC = ge, gi, a_, ginv, b_, gC
                for qi, (src, dst, idx) in enumerate(((KKt, XTkr, 0), (Rt, XTkr, 1), (Bh, XTbk, 0), (Kh, XTbk, 1))):
                    for h in range(4):
                        o.tr(ps[6][0:64, h * 128:(h + 1) * 128], src[:, h * 64:(h + 1) * 64], ident_f[:])
                    o.cp("act" if qi % 2 == 0 else "dve", dst[:, :, idx, :], ps[6][0:64, 0:512].re("p (a t) -> p a t", a=4))
                if dbg.get("rwkv_stage", 9) < 3:
                    continue
                k.phase = "rwkv/heads"
                k.limit = dbg.get("oplimit")
                k.limit_on = True
                for h in range(4):
                    hc = slice(h * 64, (h + 1) * 64)
                    BhT = XTbk[:, h, 0, :]
                    KhT = XTbk[:, h, 1, :]
                    KRT = XTkr[:, h, :, :].re("p a t -> p (a t)")
                    KKtT = XTkr[:, h, 0, :]
                    RtT = XTkr[:, h, 1, :]
                    o.mm(ps[0][:, 0:256], BhT, KRT)
                    o.mm(ps[1][:, 0:256], KhT, KRT)
                    o.mm(ps[1][:, 256:384], KKtT, BhT)
                    o.tt("dve", P1[:], ps[0][:, 0:256], mU2[:], ALU.mult)
                    o.tt("dve", P2[:], ps[1][:, 0:256], mU2[:], ALU.mult)
                    o.tt("dve", Lc0[:], ps[1][:, 256:384], mL[:], ALU.mult)
                    o.tt("pool", Y[0][:], ident_f[:], P1[:, 0:128], ALU.subtract)
                    Uc, Lcur = P1[:, 0:128], Lc0[:]

                    def sq_step(ks, Uc, Lcur):
                        last = ks == 5
                        o.mm(ps[2][:, 0:128], Uc, Lcur)
                        if not last:
                            o.mm(ps[6][:, 0:128], Lcur, Uc)
                        o.cp("act", Lb[ks % 2][:], ps[2][:, 0:128])
                        if not last:
                            o.cp("dve", Ub[ks % 2][:], ps[6][:, 0:128])
                        o.tt("pool", ILs[ks % 2][:], Lb[ks % 2][:], ident_f[:], ALU.add)
                        return Ub[ks % 2][:], Lb[ks % 2][:]

                    def y_step(ks):
                        o.mm(ps[0][:, 256:384], ILs[ks % 2][:], Y[ks % 2][:])
                        o.cp("act", Y[(ks + 1) % 2][:], ps[0][:, 256:384])

                    Uc, Lcur = sq_step(0, Uc, Lcur)
                    for ks in range(1, 6):
                        Uc, Lcur = sq_step(ks, Uc, Lcur)
                        y_step(ks - 1)
                    y_step(5)
                    Yf = Y[0]
                    o.mm(ps[3][:, 0:64], P2[:, 0:128], vv[:, hc])
                    o.cp("act", RH[:, 64:128], ps[3][:, 0:64])
                    o.cp("pool", RH[:, 0:64], KKt[:, hc])
                    o.mm(ps[3][:, 64:192], Yf[:], RH[:])
                    o.ts("dve", nWU[:], ps[3][:, 64:192], -1.0, ALU.mult)
                    o.mm(ps[3][0:64, 192:256], nWU[:, 0:64], BC[:, hc])
                    o.cp("act", TT[:], ps[3][0:64, 192:256])
                    o.mm(ps[3][0:64, 256:384], nWU[:, 0:64], P1[:, 128:256], start=True, stop=False)
                    o.mm(ps[3][0:64, 256:384], ident_f[0:64, 0:64], RtT, start=False, stop=True)
                    o.cp("act", QT[:], ps[3][0:64, 256:384])
                    o.mm(ps[5][:, hc], P2[:, 128:256], vv[:, hc], start=True, stop=False)
                    o.mm(ps[5][:, hc], P1[:, 128:256], nWU[:, 64:128], start=False, stop=False)
                    o.mm(ps[5][:, hc], QT[:], Hs[:, h, :], start=False, stop=True)
                    o.mm(ps[4][0:64, hc], KC[:, hc], vv[:, hc], start=True, stop=False)
                    o.mm(ps[4][0:64, hc], BC[:, hc], nWU[:, 64:128], start=False, stop=False)
                    o.mm(ps[4][0:64, hc], TT[:], Hs[:, h, :], start=False, stop=True)
                    o.stt("dve", Hs[:, h, :], Hs[:, h, :], gcol[:, h:h + 1], ps[4][0:64, hc], ALU.mult, ALU.add)
                if dbg.get("rwkv_stage", 9) < 4:
                    continue
                k.limit_on = False
                if "dumpP1" in dbg_out:
                    o.store("sp", dbg_out["dumpP1"], P1[:])
                    o.store("sp", dbg_out["dumpP2"], P2[:])
                    o.store("sp", dbg_out["dumpL"], Lc0[:])
                    o.store("sp", dbg_out["dumpY"], Y[0][:])
                    o.store("sp", dbg_out["dumpX"], XTkr[:].re("p a b t -> p (a b t)"))
                k.phase = "rwkv/post"
                O_ = ps[5][:, 0:256]
                xc = kk
                o.red(s4[:], h3(O_), ALU.add)
                o.ts("dve", s4[:], s4[:], 1.0 / 64, ALU.mult)
                o.tt("dve", h3(xc[:]), h3(O_), b3(s4[:]), ALU.subtract)
                o.tt("pool", t0[:], xc[:], xc[:], ALU.mult)
                o.red(s4b[:], h3(t0[:]), ALU.add)
                o.act(s4b[:], s4b[:], AF.Sqrt, bias=64e-5, scale=1.0 / 64)
                o.recip(s4b[:], s4b[:])
                o.tt("dve", h3(xc[:]), h3(xc[:]), b3(s4b[:]), ALU.mult)
                o.tt("pool", xc[:], xc[:], P["lnw"][:], ALU.mult)
                o.tt("pool", xc[:], xc[:], P["lnb"][:], ALU.add)
                o.tt("dve", h3(t0[:]), h3(vv[:]), b3(bc4[:]), ALU.mult)
                o.tt("dve", xc[:], xc[:], t0[:], ALU.add)
                o.tt("dve", yb_[:], xc[:], gsb[:], ALU.mult)
                for c2 in range(2):
                    o.tr(pT[:, c2 * 128:(c2 + 1) * 128], yb_[:, c2 * 128:(c2 + 1) * 128], ident_b[:])
                o.cp("act", mixT[:, :, tt * 128:(tt + 1) * 128], pT[:, 0:256].re("p (a t) -> p a t", a=2))

        pool_c = {}

        def pool_mixer(l):
            k.phase = "pool"
            ar_reset()
            if not pool_c:
                pool_c["rw"] = k.sbuf("pool_rw", [128, 2], F32)
                pool_c["fix"] = k.sbuf("pool_fix", [128, 2, 16], F32)
                pool_c["sc"] = k.sbuf("pool_sc", [128, 2], F32)
                o.load("sp", pool_c["rw"][:], dram["pool_rw"])
                o.load("sp", pool_c["fix"][:], dram["pool_fix"])
            o.load("sp", pool_c["sc"][:], dram["pool_scale"][l])
            wu_ = wB[:, 0:2048].re("p (c e) -> p c e", c=8)
            o.load("pool", wu_, dram["w_fm"][l][:, 0:256].rearrange("(c p) e -> p c e", p=128))
            pw = ar("pw", [128, 2, 128], BF16)
            pwf = ar("pwf", [128, 2, 128], F32)
            o.memset("pool", pwf[:], 0.0)
            for gi in range(4):
                o.load("sp", pwf[(gi % 2) * 64:(gi % 2) * 64 + 64, gi // 2, (gi % 2) * 64:(gi % 2) * 64 + 64], dram["pool_w"][l, gi])
            o.cp("dve", pw[:], pwf[:])
            PADW = 16
            u = [ar(f"pu{i}", [128, PADW + S], F32) for i in range(1)][0]
            sa = ar("psa", [128, PADW + S], F32)
            sb_ = ar("psb", [128, PADW + S], F32)
            dm = ar("pdm", [128, S], BF16)
            for mt in range(2):
                o.memset("pool", u[:, 0:PADW], 0.0)
                o.memset("pool", sa[:, 0:PADW], 0.0)
                o.memset("pool", sb_[:, 0:PADW], 0.0)
                for tb in range(NB):
                    for c in range(8):
                        o.mm(ps[0][:], wu_[:, c, mt * 128:(mt + 1) * 128], hT[:, c, 1 + tb * 512:1 + (tb + 1) * 512], start=(c == 0), stop=(c == 7))
                    o.cp("act", u[:, PADW + tb * 512:PADW + (tb + 1) * 512], ps[0][:])
                wl, wh = ((2, 4), (8, 16))[mt]
                full = slice(PADW, PADW + S)

                def sh(t, d):
                    return t[:, PADW - d:PADW + S - d]
                o.tt("dve", sa[:, full], u[:, full], sh(u, 1), ALU.add)
                cur, oth, w = sa, sb_, 2
                res = {}
                res[2] = cur
                while w < wh:
                    o.tt("dve", oth[:, full], cur[:, full], sh(cur, w), ALU.add)
                    w *= 2
                    if w == wl:
                        pass
                    res[w] = oth
                    cur, oth = oth, cur
                lo_t, hi_t = sa, sb_
                for (pr, src) in ((slice(0, 64), lo_t), (slice(64, 128), hi_t)):
                    o.ts("dve", src[pr, full], src[pr, full], pool_c["rw"][pr, mt:mt + 1], ALU.mult)
                    o.tt("dve", src[pr, PADW:PADW + 16], src[pr, PADW:PADW + 16], pool_c["fix"][pr, mt, :], ALU.mult)
                    o.tt("dve", dm[pr, :], src[pr, full], u[pr, full], ALU.subtract)
                for tb in range(NB):
                    tsl = slice(tb * 512, (tb + 1) * 512)
                    o.mm(ps[1][:], pw[:, mt, :], dm[:, tsl])
                    o.act(mixT[:, mt, tsl], ps[1][:], AF.Identity, scale=pool_c["sc"][:, mt:mt + 1])

        if dbg.get("only"):
            o.load("pool", hT[:], dram["hT_in"].rearrange("(c p) t -> p c t", p=128))
            {"rwkv": rwkv, "dil": dilated, "nsa": nsa, "pool": pool_mixer}[dbg["only"]](dbg.get("layer", 0))
            o.store("pool", dbg_out["mix_only"].rearrange("(c p) t -> p c t", p=128), mixT[:])
        for l in range(0 if dbg.get("only") else n_layers):
            norm(l, gs1[l], 0)
            if "hT" in dbg_out and l == dbg.get("layer", 0):
                pass
            comp = dbg.get("compute", ("nsa", "pool", "rwkv", "dil"))
            for grp, nm, fn in ((3, "dil", dilated), (1, "pool", pool_mixer), (0, "nsa", nsa), (2, "rwkv", rwkv)):
                if nm in comp:
                    fn(l)
                    if f"mix{grp}_{l}" in dbg_out:
                        o.store("pool", dbg_out[f"mix{grp}_{l}"].rearrange("(c p) t -> p c t", p=128), mixT[:])
                elif dbg.get("mix_in"):
                    o.load("pool", mixT[:], dram[f"mix_in{l}"][grp * 256:(grp + 1) * 256, :].rearrange("(c p) t -> p c t", p=128))
                else:
                    continue
                out_proj(l, grp)
            if f"x1_{l}" in dbg_out:
                o.store("sp", dbg_out[f"x1_{l}"].rearrange("(c p) t -> p c t", p=128), xT[:])
            ffn_alloc()
            norm(l, gs2[l], 24, router=True)
            routing()
            if f"gates_{l}" in dbg_out:
                o.store("sp", dbg_out[f"gates_{l}"], F["gatesT"][:])
            moe(l)

        o.store("sp", outT.rearrange("(c p) t -> p c t", p=128), xT[:])
        k.final_wait("sp")
        k.emit()
        print("instr counts", k.count, "waits", k.n_waits, "sems", k.nsem, "sbuf left", nc.sbuf_bytes_remaining)
    return nc


_NP2DT = {np.dtype(np.float32): F32, np.dtype(np.int32): I32, np.dtype(ml_dtypes.bfloat16): BF16}


def run(inp, dbg=None, n_layers=2, extra_per=None):
    sh, per = _prep_inputs(inp)
    if extra_per:
        for b in range(8):
            per[b].update(extra_per[b])
    in_maps = []
    for b in range(8):
        d = dict(sh)
        d.update(per[b])
        in_maps.append(d)
    shapes = {n: (a.shape, _NP2DT[a.dtype]) for n, a in in_maps[0].items()}
    nc = build(shapes, dbg=dbg, n_layers=n_layers)
    ncore = dbg.get("ncores", 8) if dbg else 8
    res = run_bass_kernel_spmd(nc, in_maps[:ncore], core_ids=list(range(ncore)), **({"trace": True} if (dbg and dbg.get("trace")) else {}))
    return res


def kernel(**inputs):
    inp = {k_: np.asarray(v) for k_, v in inputs.items()}
    res = run(inp)
    out = np.stack([np.ascontiguousarray(res.results[b]["outT"].T) for b in range(8)], axis=0)
    return out.astype(np.float32)
```

```python
import numpy as np
import ml_dtypes
from contextlib import ExitStack
import concourse.bass as bass
import concourse.mybir as mybir
from concourse.bass_utils import run_bass_kernel_spmd

F32 = mybir.dt.float32
BF16 = mybir.dt.bfloat16
I32 = mybir.dt.int32
ALU = mybir.AluOpType
AF = mybir.ActivationFunctionType
AX = mybir.AxisListType
EPOCH = 30000

S = 2048
D = 1024
NT = 16
NB = 4
EPS = 1e-6


class V:
    __slots__ = ("t", "ap")

    def __init__(self, t, ap):
        self.t = t
        self.ap = ap

    def __getitem__(self, k):
        return V(self.t, self.ap[k])

    def re(self, s, **kw):
        return V(self.t, self.ap.rearrange(s, **kw))

    def bc(self, shape):
        return V(self.t, self.ap.to_broadcast(list(shape)))

    def cast(self, dt):
        return V(self.t, self.ap.bitcast(dt))


class T:
    def __init__(self, name, ap):
        self.name = name
        self.ap = ap
        self.last_w = None
        self.readers = {}
        self.dma_sem = None
        self.dma_cnt = 0

    def __getitem__(self, k):
        return V(self, self.ap[k])

    def view(self, name, ap):
        return T(name, ap)


class Sched:
    ENGS = ("pe", "act", "dve", "pool", "sp")

    def __init__(self, nc, stack):
        self.nc = nc
        self.stack = stack
        self.streams = {e: [] for e in self.ENGS}
        self.count = {e: 0 for e in self.ENGS}
        self.sems = {e: [] for e in self.ENGS}
        self.clock = {e: {} for e in self.ENGS}
        self.snap = {e: {} for e in self.ENGS}
        self.dma_known = {e: {} for e in self.ENGS}
        self.dma_tiles = []
        self.nsem = 0
        self.n_waits = 0

    def new_sem(self, name):
        self.nsem += 1
        return self.stack.enter_context(self.nc.semaphore(name))

    def eng_sem(self, e, n):
        idx = (n - 1) // EPOCH
        while len(self.sems[e]) <= idx:
            self.sems[e].append(self.new_sem(f"s_{e}_{len(self.sems[e])}"))
        return self.sems[e][idx], (n - 1) % EPOCH + 1

    def sbuf(self, name, shape, dtype):
        t = self.stack.enter_context(self.nc.sbuf_tensor("sb_" + name, list(shape), dtype))
        return T(name, t[:])

    def psum(self, name, shape, dtype):
        t = self.stack.enter_context(self.nc.psum_tensor("pp_" + name, list(shape), dtype))
        r = T(name, t[:])
        r.is_psum = True
        return r

    def _need(self, e, reads, writes):
        need = {}
        dneed = []

        def add(dep):
            if dep is None:
                return
            x, n = dep
            if x == e and e == "pe":
                return
            if need.get(x, 0) < n:
                need[x] = n
        for t in reads:
            add(t.last_w)
            if getattr(t, "is_psum", False):
                for x, n in t.readers.items():
                    if x != e:
                        add((x, n))
            if t.dma_cnt:
                dneed.append(t)
        for t in writes:
            add(t.last_w)
            for x, n in t.readers.items():
                if x == e:
                    continue
                add((x, n))
            if t.dma_cnt:
                dneed.append(t)
        return need, dneed

    def _emit_waits(self, e, need, dneed):
        waits = []
        ck = self.clock[e]
        for x, n in need.items():
            if ck.get(x, 0) >= n:
                continue
            waits.append(self.eng_sem(x, n))
            sn = self.snap[x].get(n)
            if sn:
                for y, m in sn.items():
                    if ck.get(y, 0) < m:
                        ck[y] = m
            if ck.get(x, 0) < n:
                ck[x] = n
        dk = self.dma_known[e]
        for t in dneed:
            if dk.get(id(t), 0) >= t.dma_cnt:
                continue
            waits.append((t.dma_sem, t.dma_cnt))
            dk[id(t)] = t.dma_cnt
        return waits

    limit = None
    limit_on = False
    phase = "init"
    annotate = False
    opcount = 0

    def op(self, e, fn, reads=(), writes=()):
        if self.limit is not None and self.limit_on:
            self.opcount += 1
            if self.opcount > self.limit:
                return 0
        need, dneed = self._need(e, reads, writes)
        waits = self._emit_waits(e, need, dneed)
        self.count[e] += 1
        n = self.count[e]
        sem, _ = self.eng_sem(e, n)
        self.n_waits += len(waits)

        ph = self.phase if self.annotate else None

        def run(h, fn=fn, waits=waits, sem=sem, ph=ph):
            for (s, v) in waits:
                h.wait_ge(s, v)
            ins = fn(h)
            if ph is not None:
                ins = ins.annotate(ph)
            ins.then_inc(sem, 1)
        self.streams[e].append(run)
        sn = dict(self.clock[e])
        sn[e] = n
        self.snap[e][n] = sn
        for t in reads:
            t.readers[e] = n
        for t in writes:
            t.last_w = (e, n)
            t.readers = {}
        return n

    def dma(self, e, out_ap, in_ap, tile, write, extra=()):
        reads, writes = ((), (tile,)) if write else ((tile,), ())
        need, dneed = self._need(e, reads, writes)
        waits = self._emit_waits(e, need, dneed)
        for xt in extra:
            if xt.dma_cnt:
                waits.append((xt.dma_sem, xt.dma_cnt))
        self.n_waits += len(waits)
        t = tile
        if t.dma_sem is None:
            t.dma_sem = self.new_sem(f"d{self.nsem}_" + t.name)
            self.dma_tiles.append(t)
        t.dma_cnt += 16
        dsem = t.dma_sem

        def run(h, waits=waits, dsem=dsem, out_ap=out_ap, in_ap=in_ap):
            for (s, v) in waits:
                h.wait_ge(s, v)
            h.dma_start(out=out_ap, in_=in_ap).then_inc(dsem, 16)
        self.streams[e].append(run)
        if write:
            t.last_w = None
            t.readers = {}

    def barrier(self, o, pstile, ones):
        if not hasattr(self, "_bs"):
            self._bs = {e: self.sbuf("bs_" + e, [128, 2], F32) for e in ("pe", "act", "dve", "pool")}
        bs = self._bs
        pap = pstile.ap[0:1, 0:1]
        oap = ones.ap[0:1, 0:1]
        self.op("pe", lambda h: h.matmul(pap, lhsT=oap, rhs=oap, start=True, stop=True),
                reads=[ones], writes=[pstile, bs["pe"]])
        self.op("act", lambda h: h.activation(out=bs["act"].ap[:, 0:1], in_=bs["act"].ap[:, 0:1], func=AF.Copy, scale=0.0), writes=[bs["act"]])
        self.op("dve", lambda h: h.memset(bs["dve"].ap[:, 0:1], 0.0), writes=[bs["dve"]])
        self.op("pool", lambda h: h.memset(bs["pool"].ap[:, 0:1], 0.0), writes=[bs["pool"]])
        allb = [bs[e] for e in ("pe", "act", "dve", "pool")]
        waits = [self.eng_sem(x, n) for x, n in (b.last_w for b in allb)]
        self.op("pe", lambda h: h.matmul(pap, lhsT=oap, rhs=oap, start=True, stop=True),
                reads=[ones] + allb, writes=[pstile])
        self.op("act", lambda h: h.activation(out=bs["act"].ap[:, 1:2], in_=bs["act"].ap[:, 0:1], func=AF.Copy), reads=allb, writes=[])
        self.op("dve", lambda h: h.memset(bs["dve"].ap[:, 1:2], 0.0), reads=allb, writes=[])
        self.op("pool", lambda h: h.memset(bs["pool"].ap[:, 1:2], 0.0), reads=allb, writes=[])

        def run(h, waits=waits):
            for (s_, v) in waits:
                h.wait_ge(s_, v)
        self.streams["sp"].append(run)

    def final_wait(self, e):
        waits = [(t.dma_sem, t.dma_cnt) for t in self.dma_tiles if t.dma_cnt]

        def run(h, waits=waits):
            for (s, v) in waits:
                h.wait_ge(s, v)
        self.streams[e].append(run)

    def emit(self):
        nc = self.nc
        with nc.Block() as block:
            @block.tensor
            def _(h):
                for r in self.streams["pe"]:
                    r(h)

            @block.scalar
            def _(h):
                for r in self.streams["act"]:
                    r(h)

            @block.vector
            def _(h):
                for r in self.streams["dve"]:
                    r(h)

            @block.gpsimd
            def _(h):
                for r in self.streams["pool"]:
                    r(h)

            @block.sync
            def _(h):
                for r in self.streams["sp"]:
                    r(h)


def _ap(x):
    return x.ap if isinstance(x, V) else x


def _ts(*xs):
    return [x.t for x in xs if isinstance(x, V)]


class Ops:
    def __init__(self, k):
        self.k = k

    def mm(self, out, lhsT, rhs, start=True, stop=True):
        self.k.op("pe", lambda h: h.matmul(out.ap, lhsT=lhsT.ap, rhs=rhs.ap, start=start, stop=stop),
                  reads=_ts(lhsT, rhs), writes=_ts(out))

    def tr(self, out, in_, ident):
        self.k.op("pe", lambda h: h.transpose(out.ap, in_.ap, ident.ap), reads=_ts(in_, ident), writes=_ts(out))

    def act(self, out, in_, func, bias=0.0, scale=1.0, accum=None):
        def f(h):
            kw = {}
            if accum is not None:
                kw["accum_out"] = accum.ap
            return h.activation(out=out.ap, in_=in_.ap, func=func, bias=_ap(bias), scale=_ap(scale), **kw)
        self.k.op("act", f, reads=_ts(in_, bias, scale), writes=_ts(out) + (_ts(accum) if accum is not None else []))

    def tt(self, e, out, a, b, op):
        self.k.op(e, lambda h: h.tensor_tensor(out=out.ap, in0=a.ap, in1=b.ap, op=op), reads=_ts(a, b), writes=_ts(out))

    def ts(self, e, out, a, s1, op0, s2=None, op1=None):
        def f(h):
            if op1 is None:
                return h.tensor_scalar(out=out.ap, in0=a.ap, scalar1=_ap(s1), scalar2=None, op0=op0)
            return h.tensor_scalar(out=out.ap, in0=a.ap, scalar1=_ap(s1), scalar2=_ap(s2), op0=op0, op1=op1)
        self.k.op(e, f, reads=_ts(a, s1, s2), writes=_ts(out))

    def stt(self, e, out, a, s, b, op0, op1):
        self.k.op(e, lambda h: h.scalar_tensor_tensor(out=out.ap, in0=a.ap, scalar=_ap(s), in1=b.ap, op0=op0, op1=op1),
                  reads=_ts(a, s, b), writes=_ts(out))

    def cp(self, e, out, in_):
        if e == "act":
            self.k.op("act", lambda h: h.activation(out=out.ap, in_=in_.ap, func=AF.Copy), reads=_ts(in_), writes=_ts(out))
        else:
            self.k.op(e, lambda h: h.tensor_copy(out=out.ap, in_=in_.ap), reads=_ts(in_), writes=_ts(out))

    def red(self, out, in_, op, axis=AX.X):
        self.k.op("dve", lambda h: h.tensor_reduce(out=out.ap, in_=in_.ap, axis=axis, op=op), reads=_ts(in_), writes=_ts(out))

    def recip(self, out, in_):
        self.k.op("dve", lambda h: h.reciprocal(out=out.ap, in_=in_.ap), reads=_ts(in_), writes=_ts(out))

    def memset(self, e, out, val):
        if e == "act_ms":
            self.k.op("act", lambda h: h.activation(out=out.ap, in_=out.ap, func=AF.Copy, scale=0.0), writes=_ts(out))
            return
        self.k.op(e, lambda h: h.memset(out.ap, val), writes=_ts(out))

    def load(self, e, out, src, extra=()):
        self.k.dma(e, out.ap, src, out.t, True, extra=extra)

    def store(self, e, dst, in_):
        self.k.dma(e, dst, in_.ap, in_.t, False)


def _cols(a, b):
    return list(range(a, b))


QK_COLS = (_cols(0, 256) + _cols(384, 448) * 2 + _cols(512, 576) * 2 + _cols(1964, 2220) + _cols(2220, 2476))
VN_COLS = _cols(448, 512) + _cols(576, 640)
FM_COLS = _cols(652, 908) + _cols(256, 384)
GL_COLS = _cols(640, 652)
VD_COLS = _cols(2476, 2732)
RW_COLS = _cols(908, 1964)


def _consts():
    c = {}
    c["ident_f"] = np.eye(128, dtype=np.float32)
    c["ident_b"] = np.eye(128, dtype=np.float32).astype(ml_dtypes.bfloat16)
    c["ones_f"] = np.ones((128, 128), np.float32)
    selE = np.zeros((16, 16, 128), np.float32)
    for e in range(16):
        selE[e, e, :] = 1.0
    c["selE"] = selE.reshape(16, 16 * 128)
    invf = (500000.0 ** (-2.0 * np.arange(8, dtype=np.float32) / 16.0)).astype(np.float32) / np.float32(2 * np.pi)
    c["invf"] = np.ascontiguousarray(np.broadcast_to(invf[None, :], (128, 8)), dtype=np.float32)
    p = np.arange(128)[:, None]
    q = np.arange(128)[None, :]
    c["trimask"] = np.concatenate([(p >= q), (p <= q)], axis=1).astype(np.float32).astype(ml_dtypes.bfloat16)
    bf = lambda a: np.ascontiguousarray(a, dtype=np.float32).astype(ml_dtypes.bfloat16)
    cc = np.arange(127)[:, None]
    tq = np.arange(2048)[None, :]
    c["cmpmask"] = bf(tq >= 16 * cc + 31)
    c_end = np.arange(127) * 16 + 31
    b_start = np.arange(32) * 64
    cover = np.clip(np.minimum(c_end[:, None] + 1, b_start[None, :] + 64) - np.maximum(c_end[:, None] + 1 - 32, b_start[None, :]), 0, None) / 32.0
    c["covones"] = bf(np.concatenate([cover, np.ones((127, 1))], axis=1))
    selG = np.zeros((12, 12, 128), np.float32)
    for e in range(12):
        selG[e, e, :] = 1.0
    c["selG"] = bf(selG.reshape(12, 12 * 128))
    E = np.zeros((32, 16, 128), np.float32)
    for j in range(16):
        for pp in range(128):
            E[2 * j + pp // 64, j, pp] = 1.0
    c["Esel"] = bf(E.reshape(32, 16 * 128))
    tt_ = np.arange(2048)
    cur = (tt_ // 64)[:, None]
    blk = np.arange(32)[None, :]
    forced = (blk == 0) | (blk == cur) | (blk == cur - 1)
    fut = blk > cur
    keep = (~forced & ~fut).astype(np.float32)
    add = np.where(fut, -1.0, np.where(forced, 1e4, 0.0)).astype(np.float32)
    c["selkeep"] = np.ascontiguousarray(keep.reshape(16, 128, 32).transpose(1, 0, 2))
    c["seladd"] = np.ascontiguousarray(add.reshape(16, 128, 32).transpose(1, 0, 2))
    c["trigt"] = bf(p > q)
    c["mU2"] = np.concatenate([(p < q), (p <= q)], axis=1).astype(np.float32)
    c["mL"] = (p > q).astype(np.float32)
    wwin = np.array([[2] * 64 + [4] * 64, [8] * 64 + [16] * 64], np.float32).T
    c["pool_rw"] = (1.0 / wwin).astype(np.float32)
    tcnt = np.arange(1, 17, dtype=np.float32)[None, None, :]
    c["pool_fix"] = (wwin[:, :, None] / np.minimum(tcnt, wwin[:, :, None])).astype(np.float32)
    return c


def _prep_inputs(inp):
    f = lambda a: np.ascontiguousarray(a, dtype=np.float32)
    sh = {}
    sh["ada_w"] = f(inp["ada_w"])
    sh["ada_b"] = f(inp["ada_b"].reshape(2, 48, 128).transpose(0, 2, 1))
    sh["g_mix"] = f(inp["norm_mix_g"].reshape(2, 8, 128).transpose(0, 2, 1))
    sh["g_ffn"] = f(inp["norm_ffn_g"].reshape(2, 8, 128).transpose(0, 2, 1))
    w_in = inp["w_in"]
    sh["w_qk"] = f(w_in[:, :, QK_COLS])
    sh["w_vn"] = f(w_in[:, :, VN_COLS])
    sh["w_fm"] = f(w_in[:, :, FM_COLS])
    sh["w_gl"] = f(w_in[:, :, GL_COLS])
    sh["w_vd"] = f(w_in[:, :, VD_COLS])
    sh["w_rw"] = f(w_in[:, :, RW_COLS])
    sh["w_dqk"] = f(w_in[:, :, _cols(1964, 2476)])
    sh["w_nqk"] = f(w_in[:, :, _cols(0, 256) + _cols(384, 448) * 2 + _cols(512, 576) * 2])
    gd = np.concatenate([np.tile(inp["dil_q_norm"], (1, 4)), np.tile(inp["dil_k_norm"], (1, 4))], axis=1)
    sh["g_dqk"] = f(np.broadcast_to(gd[:, None, :], (2, 128, 512)))
    gn = np.concatenate([np.tile(inp["nsa_q_norm"], (1, 4)), np.tile(inp["nsa_k_norm"][:, 1], (1, 2)),
                         np.tile(inp["nsa_k_norm"][:, 2], (1, 2))], axis=1)
    sh["g_nqk"] = f(np.broadcast_to(gn[:, None, :], (2, 128, 512)))
    sh["cmp_w1"] = f(inp["nsa_cmp_w1"])
    sh["cmp_w2"] = f(inp["nsa_cmp_w2"])
    sh["cmp_peT"] = f(inp["nsa_cmp_pe"].transpose(0, 1, 3, 2).reshape(2, 128, 32))
    gk = np.tile(inp["nsa_k_norm"][:, 0], (1, 2))
    sh["g_kcmp"] = f(np.broadcast_to(gk[:, None, :], (2, 128, 128)))
    rowb = lambda a: f(np.broadcast_to(a[:, None, :], (a.shape[0], 128, a.shape[1])))
    sh["rw_mu"] = rowb(inp["rwkv_mu"])
    sh["rw_w0"] = rowb(inp["rwkv_w0"])
    sh["rw_a0"] = rowb(inp["rwkv_a0"])
    sh["rw_kk"] = rowb(inp["rwkv_k_k"])
    sh["rw_ka"] = rowb(inp["rwkv_k_a"])
    sh["rw_rk"] = rowb(inp["rwkv_r_k"].reshape(2, 256))
    sh["rw_lnw"] = rowb(inp["rwkv_ln_w"])
    sh["rw_lnb"] = rowb(inp["rwkv_ln_b"])
    sh["rw_v0"] = rowb(np.concatenate([np.zeros_like(inp["rwkv_v0"]), inp["rwkv_v0"]], axis=0))
    sh["rw_w2"] = f(inp["rwkv_w2"])
    sh["rw_a2"] = f(inp["rwkv_a2"])
    sh["rw_g2"] = f(inp["rwkv_g2"])
    sh["rw_v1"] = f(inp["rwkv_v1"])
    sh["rw_v2"] = f(inp["rwkv_v2"])
    sh["pool_w"] = f(inp["pool_w"])
    sh["pool_scale"] = f(inp["pool_scale"].reshape(2, 2, 128).transpose(0, 2, 1))
    sh["w_out"] = f(inp["w_out"])
    sh["router_w"] = f(inp["router_w"])
    sh["router_b"] = f(np.broadcast_to(inp["router_b"][None, :], (128, 16)))
    sh["moe_wg"] = f(inp["moe_w_gate"])
    sh["moe_wu"] = f(inp["moe_w_up"])
    sh["moe_wd"] = f(inp["moe_w_down"])
    sh.update(_consts())
    per = []
    for b in range(8):
        d = {}
        d["xT"] = f(inp["x"][b].T)
        d["cvec"] = f(inp["c"][b].reshape(8, 128).T)
        d["pos_tm"] = np.ascontiguousarray(inp["positions"][b].reshape(16, 128).T.astype(np.int32))
        d["pos_ce"] = np.ascontiguousarray(inp["positions"][b][31::16][:127].reshape(127, 1).astype(np.int32))
        per.append(d)
    return sh, per


def build(shapes, dbg=None, n_layers=2):
    dbg = dbg or {}
    nc = bass.Bass("TRN2", target_bir_lowering=False)
    dram = {}
    for name, (shape, dt) in shapes.items():
        dram[name] = nc.dram_tensor(name, list(shape), dt, kind="ExternalInput").ap()
    outT = nc.dram_tensor("outT", [D, S], F32, kind="ExternalOutput").ap()
    dbg_out = {}
    for name, shape in dbg.get("outs", {}).items():
        dbg_out[name] = nc.dram_tensor(name, list(shape), F32, kind="ExternalOutput").ap()

    with ExitStack() as st:
        k = Sched(nc, st)
        k.annotate = bool(dbg.get("trace"))
        o = Ops(k)
        xT = k.sbuf("xT", [128, 8, S], F32)
        hT = k.sbuf("hT", [128, 8, S + 1], BF16)
        mixT = k.sbuf("mixT", [128, 2, S], BF16)
        wA = k.sbuf("wA", [128, 8704], BF16)
        wB = k.sbuf("wB", [128, 8704], BF16)
        wo = k.sbuf("wo", [128, 2, D], BF16)
        ident_f = k.sbuf("ident_f", [128, 128], F32)
        ident_b = k.sbuf("ident_b", [128, 128], BF16)
        ones_f = k.sbuf("ones_f", [128, 128], F32)
        cvec = k.sbuf("cvec", [128, 8], F32)
        scv = k.sbuf("scv", [128, 8], F32)
        mods = [k.sbuf(f"mod{l}", [128, 48], F32) for l in range(2)]
        adab = [k.sbuf(f"adab{l}", [128, 48], F32) for l in range(2)]
        gs1 = [k.sbuf(f"gs1_{l}", [128, 8], F32) for l in range(2)]
        gs2 = [k.sbuf(f"gs2_{l}", [128, 8], F32) for l in range(2)]
        gmx = [k.sbuf(f"gmx{l}", [128, 8], F32) for l in range(2)]
        gff = [k.sbuf(f"gff{l}", [128, 8], F32) for l in range(2)]
        rw_sb = k.sbuf("rw_sb", [128, 8, 16], F32)
        rb_sb = k.sbuf("rb_sb", [128, 16], F32)
        rbias = k.sbuf("rbias", [16, 1], F32)
        sq = [k.sbuf(f"sq{i}", [128, 512], F32) for i in range(2)]
        tmpf = [k.sbuf(f"tmpf{i}", [128, 512], F32) for i in range(2)]
        rstd = k.sbuf("rstd", [128, 512], F32)
        ps = [k.psum(f"ps{i}", [128, 512], F32) for i in range(7)]
        pT = k.psum("pT", [128, 1024], BF16)
        cosT = k.sbuf("cosT", [128, NT, 8], F32)
        sinT = k.sbuf("sinT", [128, NT, 8], F32)
        trimask = k.sbuf("trimask", [128, 256], BF16)
        ARENA = 48 * 1024
        arena = k.sbuf("arena", [128, ARENA // 2], BF16)
        ar_state = {"off": 0}

        def ar_reset():
            k.barrier(o, ps[6], ones_f)
            ar_state["off"] = 0
            ar_state["offA"] = 0
            ar_state["offB"] = 0

        def ar_rewind(off):
            k.barrier(o, ps[6], ones_f)
            ar_state["off"] = off

        def ar(name, shape, dt, pool="main"):
            esz = 4 if dt in (F32, I32) else 2
            n = int(np.prod(shape[1:]))
            nb = (n * esz + 3) // 4 * 4
            key = "off" if pool == "main" else "off" + pool
            base_ap, cap = {"main": (arena.ap, ARENA), "A": (wA.ap, 17408), "B": (wB.ap, 8192)}[pool]
            off = ar_state.get(key, 0)
            assert off + nb <= cap, (name, pool, off, nb)
            ar_state[key] = off + nb
            ap = base_ap[0:shape[0], off // 2:(off + nb) // 2]
            if dt != BF16:
                ap = ap.bitcast(dt)
            ap = ap[:, 0:n]
            if len(shape) == 3:
                ap = ap.rearrange("p (a b) -> p a b", a=shape[1])
            elif len(shape) == 4:
                ap = ap.rearrange("p (a b c) -> p a b c", a=shape[1], b=shape[2])
            return T(name, ap)

        o.load("sp", ident_f[:], dram["ident_f"])
        o.load("pool", ident_b[:], dram["ident_b"])
        o.load("sp", ones_f[:], dram["ones_f"])
        o.load("sp", cvec[:], dram["cvec"])
        o.load("sp", xT[:], dram["xT"].rearrange("(c p) t -> p c t", p=128))
        o.load("sp", rw_sb[:], dram["router_w"].rearrange("(c p) e -> p c e", p=128))
        o.load("sp", rb_sb[:], dram["router_b"])
        for l in range(2):
            o.load("sp", adab[l][:], dram["ada_b"][l])
            o.load("sp", gmx[l][:], dram["g_mix"][l])
            o.load("sp", gff[l][:], dram["g_ffn"][l])
        o.memset("pool", hT[:, :, 0:1], 0.0)

        o.act(scv[:], cvec[:], AF.Silu)
        stage = [wA[:, 0:4096].cast(F32).re("p (c e) -> p c e", c=8), wB[:, 0:4096].cast(F32).re("p (c e) -> p c e", c=8)]
        for l in range(0 if dbg.get("only") else n_layers):
            for blk in range(24):
                sg = stage[blk % 2]
                o.load("sp", sg, dram["ada_w"][l][:, blk * 256:(blk + 1) * 256].rearrange("(c p) e -> p c e", p=128))
                for jj in range(2):
                    j = blk * 2 + jj
                    for c in range(8):
                        o.mm(ps[0][:, j:j + 1], sg[:, c, jj * 128:(jj + 1) * 128], scv[:, c:c + 1], start=(c == 0), stop=(c == 7))
            o.tt("dve", mods[l][:], ps[0][:, 0:48], adab[l][:], ALU.add)
            o.stt("dve", gs1[l][:], mods[l][:, 8:16], 1.0, gmx[l][:], ALU.add, ALU.mult)
            o.stt("dve", gs2[l][:], mods[l][:, 32:40], 1.0, gff[l][:], ALU.add, ALU.mult)
        if "mods" in dbg_out:
            o.store("sp", dbg_out["mods"][0], mods[0][:])
            o.store("sp", dbg_out["mods"][1], mods[1][:])

        def norm(l, gs, sh_off, router=False):
            k.phase = "norm"
            sh = mods[l]
            if router:
                for c in range(8):
                    o.mm(ps[5][0:16, 0:1], rw_sb[:, c, :], sh[:, sh_off + c:sh_off + c + 1], start=(c == 0), stop=(c == 7))
                o.cp("dve", rbias[:], ps[5][0:16, 0:1])
            for tb in range(NB):
                tsl = slice(tb * 512, (tb + 1) * 512)
                for c in range(8):
                    s_ = sq[c % 2]
                    o.act(s_[:], xT[:, c, tsl], AF.Square)
                    o.mm(ps[0][:], ones_f[:], s_[:], start=(c == 0), stop=(c == 7))
                o.act(rstd[:], ps[0][:], AF.Sqrt, bias=EPS, scale=1.0 / D)
                o.recip(rstd[:], rstd[:])
                for c in range(8):
                    t_ = tmpf[c % 2]
                    o.stt("dve", t_[:], xT[:, c, tsl], gs[:, c:c + 1], rstd[:], ALU.mult, ALU.mult)
                    o.act(hT[:, c, 1 + tb * 512:1 + (tb + 1) * 512], t_[:], AF.Identity, bias=sh[:, sh_off + c:sh_off + c + 1])
                    if router:
                        o.mm(ps[1][0:16, :], rw_sb[:, c, :], t_[:], start=(c == 0), stop=(c == 7))
                if router:
                    o.act(F["affT"][:, tsl], ps[1][0:16, :], AF.Sigmoid, bias=rbias[:])

        def out_proj(l, grp):
            k.phase = "out_proj"
            o.load("pool", wo[:], dram["w_out"][l][grp * 256:(grp + 1) * 256, :].rearrange("(c p) e -> p c e", p=128))
            g1 = mods[l][:, 16:24]
            for tb in range(NB):
                tsl = slice(tb * 512, (tb + 1) * 512)
                for dc in range(8):
                    p_ = ps[2 + (dc % 2)]
                    for kc in range(2):
                        o.mm(p_[:], wo[:, kc, dc * 128:(dc + 1) * 128], mixT[:, kc, tsl], start=(kc == 0), stop=(kc == 1))
                    o.stt("dve", xT[:, dc, tsl], p_[:], g1[:, dc:dc + 1], xT[:, dc, tsl], ALU.mult, ALU.add)

        rt_tiles = {}
        moe_tiles = {}
        F = {}

        def ffn_alloc():
            ar_reset()
            F["affT"] = ar("affT", [16, S], F32)
            F["gatesT"] = ar("gatesT", [16, S], F32)
            F["selE"] = ar("selE", [16, 16, 128], F32)
            o.load("sp", F["selE"][:], dram["selE"].rearrange("k (e m) -> k e m", e=16))
            rt_tiles.update(dict(
                aff=ar("r_aff", [128, NT, 16], F32), sel=ar("r_sel", [128, NT, 16], F32),
                sel2=ar("r_sel2", [128, NT, 16], F32), m1=ar("r_m1", [128, NT * 4], F32),
                m2=ar("r_m2", [128, NT * 4], F32), gsc=ar("r_gsc", [128, NT, 4], F32),
                gm=ar("r_gm", [128, NT], F32), den=ar("r_den", [128, NT], F32)))
            moe_tiles["gb"] = [ar(f"m_gb{i}", [128, 512], F32) for i in range(2)]
            moe_tiles["sg"] = [ar(f"m_sg{i}", [128, 512], BF16) for i in range(2)]
            moe_tiles["u2"] = [ar(f"m_u2{i}", [128, 512], BF16) for i in range(2)]
            moe_tiles["aT"] = [ar(f"m_aT{i}", [128, 2, 512], BF16) for i in range(2)]

        def routing():
            k.phase = "routing"
            aff, sel, sel2, m1, m2, gsc, gm, den = (rt_tiles[n] for n in ("aff", "sel", "sel2", "m1", "m2", "gsc", "gm", "den"))
            for t in range(NT):
                o.tr(ps[4][:, t * 16:(t + 1) * 16], F["affT"][:, t * 128:(t + 1) * 128], ident_f[0:16, 0:16])
            o.cp("dve", aff[:].re("p t e -> p (t e)"), ps[4][:, 0:256])
            o.tt("dve", sel[:], aff[:], rb_sb[:].re("p (o e) -> p o e", o=1).bc([128, NT, 16]), ALU.add)
            s4 = sel[:].re("p t (g j) -> p (t g) j", g=4)
            o.red(m1[:], s4, ALU.max)
            o.tt("dve", sel2[:].re("p t (g j) -> p (t g) j", g=4), s4, m1[:].re("p (a o) -> p a o", o=1).bc([128, NT * 4, 4]), ALU.is_ge)
            o.stt("dve", sel2[:], sel2[:], -1e9, sel[:], ALU.mult, ALU.add)
            o.red(m2[:], sel2[:].re("p t (g j) -> p (t g) j", g=4), ALU.max)
            o.tt("dve", gsc[:].re("p t g -> p (t g)"), m1[:], m2[:], ALU.add)
            o.red(gm[:], gsc[:], ALU.max)
            o.tt("dve", gsc[:], gsc[:], gm[:].re("p (t o) -> p t o", o=1).bc([128, NT, 4]), ALU.is_ge)
            o.tt("dve", sel2[:].re("p t (g j) -> p (t g) j", g=4), s4, m2[:].re("p (a o) -> p a o", o=1).bc([128, NT * 4, 4]), ALU.is_ge)
            o.tt("dve", sel2[:].re("p t (g j) -> p (t g) j", g=4), sel2[:].re("p t (g j) -> p (t g) j", g=4),
                 gsc[:].re("p t (g o) -> p (t g) o", o=1).bc([128, NT * 4, 4]), ALU.mult)
            o.tt("dve", sel2[:], sel2[:], aff[:], ALU.mult)
            o.red(den[:], sel2[:], ALU.add)
            o.recip(den[:], den[:])
            o.tt("dve", sel2[:], sel2[:], den[:].re("p (t o) -> p t o", o=1).bc([128, NT, 16]), ALU.mult)
            for g4 in range(4):
                for t4 in range(4):
                    t = g4 * 4 + t4
                    o.tr(ps[5][0:16, t4 * 128:(t4 + 1) * 128], sel2[:, t, :], ident_f[:])
                o.cp("act", F["gatesT"][:, g4 * 512:(g4 + 1) * 512], ps[5][0:16, :])


        def moe(l):
            k.phase = "moe"
            g2 = mods[l][:, 40:48]

            def wviews(e):
                wb = (wA, wB)[e % 2]
                return (wb[:, 0:2048].re("p (c f) -> p c f", c=8), wb[:, 2048:4096].re("p (c f) -> p c f", c=8),
                        wb[:, 4096:6144].re("p (c d) -> p c d", c=2))

            def load_w(e):
                wg, wu, wd = wviews(e)
                o.load("pool", wg, dram["moe_wg"][l, e].rearrange("(c p) f -> p c f", p=128))
                o.load("pool", wu, dram["moe_wu"][l, e].rearrange("(c p) f -> p c f", p=128))
                o.load("pool", wd, dram["moe_wd"][l, e].rearrange("(c p) d -> p c d", p=128))

            def down(wd, aT, tsl):
                for dc in range(8):
                    pd = ps[4 + (dc % 2)]
                    for fc in range(2):
                        o.mm(pd[:], wd[:, fc, dc * 128:(dc + 1) * 128], aT[:, fc, :], start=(fc == 0), stop=(fc == 1))
                    o.stt("dve", xT[:, dc, tsl], pd[:], g2[:, dc:dc + 1], xT[:, dc, tsl], ALU.mult, ALU.add)

            load_w(0)
            pending = None
            it = 0
            for e in range(16):
                wg, wu, wd = wviews(e)
                for tb in range(NB):
                    tsl = slice(tb * 512, (tb + 1) * 512)
                    hsl = slice(1 + tb * 512, 1 + (tb + 1) * 512)
                    gb = moe_tiles["gb"][it % 2]
                    aT = moe_tiles["aT"][it % 2]
                    it += 1
                    o.mm(ps[6][:], F["selE"][:, e, :], F["gatesT"][:, tsl])
                    o.cp("act", gb[:], ps[6][:])
                    for fc in range(2):
                        pg, pu = ps[0 + fc], ps[2 + fc]
                        for c in range(8):
                            o.mm(pg[:], wg[:, c, fc * 128:(fc + 1) * 128], hT[:, c, hsl], start=(c == 0), stop=(c == 7))
                        for c in range(8):
                            o.mm(pu[:], wu[:, c, fc * 128:(fc + 1) * 128], hT[:, c, hsl], start=(c == 0), stop=(c == 7))
                        sg = moe_tiles["sg"][fc]
                        u2 = moe_tiles["u2"][fc]
                        o.act(sg[:], pg[:], AF.Silu)
                        o.tt("dve", u2[:], pu[:], gb[:], ALU.mult)
                        o.tt("pool", aT[:, fc, :], sg[:], u2[:], ALU.mult)
                    if pending is not None:
                        pending()
                    if tb == 0 and e + 1 < 16:
                        load_w(e + 1)
                    pending = (lambda wd=wd, aT=aT, tsl=tsl: down(wd, aT, tsl))
            pending()

        MAGIC = 12582912.0

        def rope_table(dst_cos, dst_sin, pos_i, nparts, ncol, tmp_y, tmp_r, posf, invf_t):
            o.cp("dve", posf, pos_i)
            o.tt("dve", tmp_y, posf.re("p (t o) -> p t o", o=1).bc([nparts, ncol, 8]),
                 invf_t.re("p (o f) -> p o f", o=1).bc([nparts, ncol, 8]), ALU.mult)
            for dst, shift in ((dst_sin, 0.0), (dst_cos, 0.25)):
                o.ts("dve", tmp_r, tmp_y, shift, ALU.add, MAGIC, ALU.add)
                o.ts("dve", tmp_r, tmp_r, MAGIC, ALU.subtract)
                o.stt("dve", tmp_r, tmp_y, shift, tmp_r, ALU.add, ALU.subtract)
                o.act(dst, tmp_r, AF.Sin, scale=6.28318)

        invf_t = k.sbuf("invf", [128, 8], F32)
        posi = k.sbuf("posi", [128, NT], I32)
        posf = k.sbuf("posf", [128, NT], F32)
        rt_y = k.sbuf("rt_y", [128, NT, 8], F32)
        rt_r = k.sbuf("rt_r", [128, NT, 8], F32)
        o.load("sp", invf_t[:], dram["invf"])
        o.load("sp", posi[:], dram["pos_tm"])
        o.load("pool", trimask[:], dram["trimask"])
        rope_table(cosT[:], sinT[:], posi[:], 128, NT, rt_y[:], rt_r[:], posf[:], invf_t[:])

        def proj_qk(l, wname, gname, qkT, wbuf, sc):
            k.phase = k.phase.split("/")[0] + "/projqk"
            o.load("pool", wbuf, dram[wname][l].rearrange("(c p) e -> p c e", p=128))
            o.load("sp", sc["gain"][:], dram[gname][l])
            for tt in range(NT):
                b = tt % 2
                t1, t2, yb, ss = sc["t1"][b], sc["t2"][b], sc["yb"][b], sc["ss"][b]
                ra, rb, rc, rd = sc["ra"][b], sc["rb"][b], sc["rc"][b], sc["rd"][b]
                pq = ps[b]
                pTh = pT[:, b * 512:(b + 1) * 512]
                hs = slice(1 + tt * 128, 1 + (tt + 1) * 128)
                for c in range(8):
                    o.mm(pq[:], hT[:, c, hs], wbuf[:, c, :], start=(c == 0), stop=(c == 7))
                o.act(t1[:], pq[:], AF.Square)
                o.red(ss[:], t1[:].re("p (h d) -> p h d", h=8), ALU.add)
                o.act(ss[:], ss[:], AF.Sqrt, bias=EPS, scale=1.0 / 64)
                o.recip(ss[:], ss[:])
                o.tt("dve", t2[:].re("p (h d) -> p h d", h=8), pq[:].re("p (h d) -> p h d", h=8),
                     ss[:].re("p (h o) -> p h o", o=1).bc([128, 8, 64]), ALU.mult)
                o.tt("pool", t2[:], t2[:], sc["gain"][:], ALU.mult)
                y3 = t2[:].re("p (h d) -> p h d", h=8)
                yb3 = yb[:].re("p (h d) -> p h d", h=8)
                cs = cosT[:, tt, :].re("p (o f) -> p o f", o=1).bc([128, 8, 8])
                sn = sinT[:, tt, :].re("p (o f) -> p o f", o=1).bc([128, 8, 8])
                o.tt("dve", ra[:], y3[:, :, 0:8], cs, ALU.mult)
                o.tt("pool", rb[:], y3[:, :, 8:16], sn, ALU.mult)
                o.tt("dve", yb3[:, :, 0:8], ra[:], rb[:], ALU.subtract)
                o.tt("pool", rc[:], y3[:, :, 8:16], cs, ALU.mult)
                o.tt("dve", rd[:], y3[:, :, 0:8], sn, ALU.mult)
                o.tt("pool", yb3[:, :, 8:16], rc[:], rd[:], ALU.add)
                o.cp("act", yb3[:, :, 16:64], y3[:, :, 16:64])
                for j_ in range(4):
                    o.tr(pTh[:, j_ * 128:(j_ + 1) * 128], yb[:, j_ * 128:(j_ + 1) * 128], ident_b[:])
                o.cp("act", qkT[:, :, tt * 128:(tt + 1) * 128], pTh.re("p (j t) -> p j t", j=4))

        def qk_scratch():
            two = lambda nm, shp, dt: [ar(f"{nm}{i}", shp, dt) for i in range(2)]
            return dict(gain=ar("gain", [128, 512], F32), t1=two("t1", [128, 512], F32), t2=two("t2", [128, 512], F32),
                        yb=two("yb", [128, 512], BF16), ss=two("ss", [128, 8], F32),
                        ra=two("ra", [128, 8, 8], F32), rb=two("rb", [128, 8, 8], F32),
                        rc=two("rc", [128, 8, 8], F32), rd=two("rd", [128, 8, 8], F32))

        def dilated(l):
            k.phase = "dilated"
            ar_reset()
            qkT = ar("qkT_d", [128, 4, S], BF16)
            mark = ar_state["off"]
            sc = qk_scratch()
            wq = wB[:, 0:4096].re("p (c e) -> p c e", c=8)
            wv = wB[:, 4096:6144].re("p (c e) -> p c e", c=8)
            proj_qk(l, "w_dqk", "g_dqk", qkT, wq, sc)
            ar_rewind(mark)
            k.phase = "dilated/attn"
            Vd = ar("Vd", [128, NT, 256], BF16)
            acc = [ar(f"acc{i}", [128, S], F32) for i in range(2)]
            pt = [ar(f"pt{i}", [128, 256], BF16) for i in range(3)]
            dtmp = sq[0]
            o.load("pool", wv, dram["w_vd"][l].rearrange("(c p) e -> p c e", p=128))
            o.memset("pool", Vd[:, :, 64:192], 1.0)
            it = 0
            for hp in range(2):
                for pi, dil in enumerate((1, 4, 16)):
                    nblk = (S // dil) // 128
                    for r in range(dil):
                        for m in range(nblk):
                            oi = r * nblk + m
                            st_ = 1 + r + dil * 128 * m
                            for c in range(8):
                                o.mm(ps[4][:, 0:128], hT[:, c, st_:st_ + dil * 127 + 1:dil], wv[:, c, hp * 128:(hp + 1) * 128], start=(c == 0), stop=(c == 7))
                            o.cp("act", Vd[:, oi, :].re("p (a b) -> p a b", b=64)[:, 0:4:3, :], ps[4][:, 0:128].re("p (a b) -> p a b", a=2))
                    items = []
                    for hh in range(2):
                        h = hp * 2 + hh
                        base = hh * 64
                        a_ = acc[hh]
                        for r in range(dil):
                            for n in range(nblk):
                                ms = [m for m in (n - 1, n) if m >= 0]
                                ncols = 128 * len(ms)
                                qcols = slice(r + dil * 128 * n, r + dil * 128 * n + dil * 127 + 1, dil)
                                pss = (ps[0], ps[1], ps[5])[it % 3]
                                pso = ps[2 + it % 2]
                                p_ = pt[it % 3]
                                it += 1

                                def A(ms=ms, ncols=ncols, qcols=qcols, pss=pss, p_=p_, base=base, r=r):
                                    for j, m in enumerate(ms):
                                        kcols = slice(r + dil * 128 * m, r + dil * 128 * m + dil * 127 + 1, dil)
                                        o.mm(pss[:, j * 128:(j + 1) * 128], qkT[base:base + 64, 2 + hp, kcols], qkT[base:base + 64, hp, qcols])
                                    o.act(p_[:, 0:ncols], pss[:, 0:ncols], AF.Exp, scale=0.125)
                                    o.tt("pool", p_[:, 0:ncols], p_[:, 0:ncols], trimask[:, 256 - ncols:256], ALU.mult)

                                def B(ms=ms, qcols=qcols, pso=pso, p_=p_, hh=hh, a_=a_, r=r):
                                    for j, m in enumerate(ms):
                                        oi = r * nblk + m
                                        lv = Vd[:, oi, hh * 128:(hh + 1) * 128]
                                        o.mm(pso[:, 0:128], lv, p_[:, j * 128:(j + 1) * 128], start=(j == 0), stop=(j == len(ms) - 1))
                                    if pi == 0:
                                        o.cp("dve", a_[:, qcols], pso[:, 0:128])
                                    else:
                                        o.tt("dve", a_[:, qcols], pso[:, 0:128], a_[:, qcols], ALU.add)
                                items.append((A, B))
                    LOOK = 2
                    for idx, (A, B) in enumerate(items):
                        A()
                        if idx >= LOOK:
                            items[idx - LOOK][1]()
                    for idx in range(max(0, len(items) - LOOK), len(items)):
                        items[idx][1]()
                for hh in range(2):
                    a_ = acc[hh]
                    ob, db = (0, 64) if hh == 0 else (64, 0)
                    for tb in range(NB):
                        tsl = slice(tb * 512, (tb + 1) * 512)
                        o.cp("act", dtmp[ob:ob + 64, :], a_[db:db + 64, tsl])
                        o.recip(dtmp[ob:ob + 64, :], dtmp[ob:ob + 64, :])
                        o.tt("dve", mixT[ob:ob + 64, hp, tsl], a_[ob:ob + 64, tsl], dtmp[ob:ob + 64, :], ALU.mult)


        def nsa(l):
            k.phase = "nsa"
            ar_reset()
            qkT = ar("qkT_n", [128, 4, S], BF16)
            mark = ar_state["off"]
            sc = qk_scratch()
            wq = wB[:, 0:4096].re("p (c e) -> p c e", c=8)
            wv = wB[:, 4096:5120].re("p (c e) -> p c e", c=8)
            wf = wB[:, 5120:6144].re("p (c e) -> p c e", c=8)
            wg = wB[:, 6144:6240].re("p (c e) -> p c e", c=8)
            proj_qk(l, "w_nqk", "g_nqk", qkT, wq, sc)
            ar_rewind(mark)
            Vn = ar("Vn", [128, NT, 4, 128], BF16)
            kcvc = ar("kcvc", [128, S], BF16, "A")
            sgT = ar("sgT", [12, S], BF16, "B")
            selT = ar("selT", [32, S], BF16, "B")
            cmpmask = ar("cmpmask", [127, 512], BF16)
            o.load("pool", wv, dram["w_vn"][l].rearrange("(c p) e -> p c e", p=128))
            o.load("pool", wf, dram["w_fm"][l][:, 256:384].rearrange("(c p) e -> p c e", p=128))
            o.load("pool", wg, dram["w_gl"][l].rearrange("(c p) e -> p c e", p=128))
            small = {}
            for nm, shp, dt, pl in (("covones", [127, 33], BF16, "main"), ("selG", [12, 12, 128], BF16, "main"), ("Esel", [32, 16, 128], BF16, "A"),
                                    ("selkeep", [128, 32], F32, "main"), ("seladd", [128, 32], F32, "main"), ("trigt", [128, 128], BF16, "main")):
                small[nm] = ar("c_" + nm, shp, dt, pl)
            o.load("pool", small["covones"][:], dram["covones"])
            o.load("pool", small["selG"][:], dram["selG"].rearrange("k (e m) -> k e m", e=12))
            o.load("pool", small["Esel"][:], dram["Esel"].rearrange("k (e m) -> k e m", e=16))
            o.load("pool", small["trigt"][:], dram["trigt"])
            k.phase = "nsa/vproj"
            o.memset("pool", Vn[:], 1.0)
            for tt in range(NT):
                hs = slice(1 + tt * 128, 1 + (tt + 1) * 128)
                for c in range(8):
                    o.mm(ps[4][:, 0:128], hT[:, c, hs], wv[:, c, :], start=(c == 0), stop=(c == 7))
                vflat = Vn[:, tt, :, :].re("p a b -> p (a b)")
                o.cp("act", vflat[:, 0:64], ps[4][:, 0:64])
                o.cp("act", vflat[:, 192:256], ps[4][:, 0:64])
                o.cp("dve", vflat[:, 256:320], ps[4][:, 64:128])
                o.cp("dve", vflat[:, 448:512], ps[4][:, 64:128])
            for tb in range(NB):
                hsl = slice(1 + tb * 512, 1 + (tb + 1) * 512)
                tsl = slice(tb * 512, (tb + 1) * 512)
                for c in range(8):
                    o.mm(ps[0][:], wf[:, c, :], hT[:, c, hsl], start=(c == 0), stop=(c == 7))
                o.cp("act", kcvc[:, tsl], ps[0][:])
                for c in range(8):
                    o.mm(ps[1][0:12, :], wg[:, c, :], hT[:, c, hsl], start=(c == 0), stop=(c == 7))
                o.act(sgT[:, tsl], ps[1][0:12, :], AF.Sigmoid)
            k.phase = "nsa/cmp"
            w1t = ar("w1t", [128, 32, 64], BF16, "A")
            peT = ar("peT", [128, 32], BF16, "A")
            w2t = ar("w2t", [64, 2, 64], BF16, "A")
            gel = ar("gel", [64, 2, 128], BF16, "A")
            for j in range(2):
                o.load("pool", w1t[j * 64:(j + 1) * 64, :, :], dram["cmp_w1"][l, j].rearrange("(i d) f -> d i f", d=64))
                o.load("pool", w2t[:, j, :], dram["cmp_w2"][l, j])
            o.load("pool", peT[:], dram["cmp_peT"][l])
            for j in range(2):
                b0 = j * 64
                for i in range(32):
                    o.mm(ps[2][0:64, 0:127], w1t[b0:b0 + 64, i, :], kcvc[b0:b0 + 64, i:i + 16 * 126 + 1:16], start=(i == 0), stop=False)
                for i in range(32):
                    o.mm(ps[2][0:64, 0:127], w1t[b0:b0 + 64, i, :], peT[b0:b0 + 64, i:i + 1].bc([64, 127]), start=False, stop=(i == 31))
                o.act(gel[:, j, 0:127], ps[2][0:64, 0:127], AF.Gelu_apprx_tanh)
            kc_f = ar("kc_f", [128, 128], F32, "A")
            kc_b = ar("kc_b", [128, 128], BF16, "A")
            kc_sq = ar("kc_sq", [128, 128], F32, "A")
            gkc = ar("gkc", [128, 128], F32, "A")
            kss = ar("kss", [128, 2], F32, "A")
            kcT = ar("kcT", [128, 128], BF16, "A")
            vc_aug = ar("vc_aug", [128, 2, 128], BF16)
            pce_i = ar("pce_i", [128, 1], I32, "A")
            pce_f = ar("pce_f", [128, 1], F32, "A")
            cy = ar("cy", [128, 1, 8], F32, "A")
            cr = ar("cr", [128, 1, 8], F32, "A")
            ccos = ar("ccos", [128, 1, 8], F32, "A")
            csin = ar("csin", [128, 1, 8], F32, "A")
            cra = ar("cra", [128, 2, 8], F32, "A")
            crb = ar("crb", [128, 2, 8], F32, "A")
            o.load("sp", gkc[:], dram["g_kcmp"][l])
            o.load("sp", pce_i[0:127, :], dram["pos_ce"])
            rope_table(ccos[0:127], csin[0:127], pce_i[0:127, :], 127, 1, cy[0:127], cr[0:127], pce_f[0:127, :], invf_t[0:127, :])
            for d2 in range(2):
                o.mm(ps[3][0:127, d2 * 64:(d2 + 1) * 64], gel[:, 0, 0:127], w2t[:, 0, :])
            o.mm(ps[3][0:127, 128:192], gel[:, 1, 0:127], w2t[:, 1, :])
            R = slice(0, 127)
            o.act(kc_sq[R, :], ps[3][R, 0:128], AF.Square)
            o.red(kss[R, :], kc_sq[R, :].re("p (h d) -> p h d", h=2), ALU.add)
            o.act(kss[R, :], kss[R, :], AF.Sqrt, bias=EPS, scale=1.0 / 64)
            o.recip(kss[R, :], kss[R, :])
            o.tt("dve", kc_f[R, :].re("p (h d) -> p h d", h=2), ps[3][R, 0:128].re("p (h d) -> p h d", h=2),
                 kss[R, :].re("p (h o) -> p h o", o=1).bc([127, 2, 64]), ALU.mult)
            o.tt("dve", kc_f[R, :], kc_f[R, :], gkc[R, :], ALU.mult)
            y3 = kc_f[R, :].re("p (h d) -> p h d", h=2)
            yb3 = kc_b[R, :].re("p (h d) -> p h d", h=2)
            cs = ccos[R, :, :].bc([127, 2, 8])
            sn = csin[R, :, :].bc([127, 2, 8])
            o.tt("dve", cra[R], y3[:, :, 0:8], cs, ALU.mult)
            o.tt("dve", crb[R], y3[:, :, 8:16], sn, ALU.mult)
            o.tt("dve", yb3[:, :, 0:8], cra[R], crb[R], ALU.subtract)
            o.tt("dve", cra[R], y3[:, :, 8:16], cs, ALU.mult)
            o.tt("dve", crb[R], y3[:, :, 0:8], sn, ALU.mult)
            o.tt("dve", yb3[:, :, 8:16], cra[R], crb[R], ALU.add)
            o.cp("act", yb3[:, :, 16:64], y3[:, :, 16:64])
            o.tr(pT[:, 0:127], kc_b[R, :], ident_b[0:127, 0:127])
            o.cp("act", kcT[:, 0:127], pT[:, 0:127])
            o.memset("pool", vc_aug[:], 1.0)
            o.cp("act", vc_aug[R, 0, 0:64], ps[3][R, 128:192])
            o.cp("act", vc_aug[R, 1, 64:128], ps[3][R, 128:192])

            wgt = ar("wgt", [128, 512], F32)
            ctr = ar("ctr", [128, 512], BF16)
            first = {}

            def epilogue(pso, ncol, h, br, tcol0, clamp=False):
                ob, db = (0, 64) if h % 2 == 0 else (64, 0)
                tsl_ = slice(tcol0, tcol0 + ncol)
                o.cp("act", wgt[ob:ob + 64, 0:ncol], pso[db:db + 64, 0:ncol])
                if clamp:
                    o.ts("dve", wgt[ob:ob + 64, 0:ncol], wgt[ob:ob + 64, 0:ncol], 1e-30, ALU.max)
                o.recip(wgt[ob:ob + 64, 0:ncol], wgt[ob:ob + 64, 0:ncol])
                o.mm(ps[6][:, 0:ncol], small["selG"][:, h * 3 + br, :], sgT[:, tsl_])
                o.tt("dve", wgt[ob:ob + 64, 0:ncol], wgt[ob:ob + 64, 0:ncol], ps[6][ob:ob + 64, 0:ncol], ALU.mult)
                dst = mixT[ob:ob + 64, h // 2, tsl_]
                if br == 0:
                    o.tt("dve", dst, pso[ob:ob + 64, 0:ncol], wgt[ob:ob + 64, 0:ncol], ALU.mult)
                else:
                    o.tt("dve", ctr[ob:ob + 64, 0:ncol], pso[ob:ob + 64, 0:ncol], wgt[ob:ob + 64, 0:ncol], ALU.mult)
                    o.tt("pool", dst, dst, ctr[ob:ob + 64, 0:ncol], ALU.add)

            eT = [ar(f"eT{i}", [128, 512], BF16) for i in range(2)]
            imp = ar("imp", [128, 32], F32)
            imp2 = ar("imp2", [128, 32], F32)
            mx8 = ar("mx8", [128, 8], F32)
            rdn = ar("rdn", [128, 4], F32)
            selm = ar("selm", [128, 32], F32)
            it = 0
            for tb in range(NB):
                tsl = slice(tb * 512, (tb + 1) * 512)
                o.load("pool", cmpmask[:], dram["cmpmask"][:, tsl])
                for h in range(4):
                    base = (h % 2) * 64
                    e_ = eT[it % 2]
                    it += 1
                    o.mm(ps[0][0:127, :], kcT[base:base + 64, 0:127], qkT[base:base + 64, h // 2, tsl])
                    o.act(e_[0:127, :], ps[0][0:127, :], AF.Exp, scale=0.125)
                    o.tt("pool", e_[0:127, :], e_[0:127, :], cmpmask[:, :], ALU.mult)
                    o.mm(ps[1][:], vc_aug[0:127, h % 2, :], e_[0:127, :])
                    for q4 in range(4):
                        o.mm((ps[2], ps[5])[q4 // 2][:, ((q4 % 2) * 4 + h) * 33:((q4 % 2) * 4 + h + 1) * 33], e_[0:127, q4 * 128:(q4 + 1) * 128], small["covones"][:, :])
                    epilogue(ps[1], 512, h, 0, tb * 512, clamp=True)
                for q4 in range(4):
                    tt = tb * 4 + q4
                    pv = (ps[2], ps[5])[q4 // 2][:, (q4 % 2) * 132:(q4 % 2 + 1) * 132].re("p (h c) -> p h c", h=4)
                    o.load("sp", small["selkeep"][:], dram["selkeep"][:, tt, :])
                    o.load("sp", small["seladd"][:], dram["seladd"][:, tt, :])
                    o.ts("dve", rdn[:], pv[:, :, 32], 1e-30, ALU.max)
                    o.recip(rdn[:], rdn[:])
                    o.ts("dve", imp[:], pv[:, 0, 0:32], rdn[:, 0:1], ALU.mult)
                    for h in range(1, 4):
                        o.stt("dve", imp[:], pv[:, h, 0:32], rdn[:, h:h + 1], imp[:], ALU.mult, ALU.add)
                    o.tt("dve", imp[:], imp[:], small["selkeep"][:, :], ALU.mult)
                    o.tt("dve", imp[:], imp[:], small["seladd"][:, :], ALU.add)
                    k.op("dve", lambda h_, a=mx8, b=imp: h_.max(out=a.ap, in_=b.ap), reads=[imp], writes=[mx8])
                    k.op("dve", lambda h_, a=imp2, b=mx8, c_=imp: h_.match_replace(out=a.ap, in_to_replace=b.ap, in_values=c_.ap, imm_value=-1e30),
                         reads=[imp, mx8], writes=[imp2])
                    k.op("dve", lambda h_, a=mx8, b=imp2: h_.max(out=a.ap, in_=b.ap), reads=[imp2], writes=[mx8])
                    o.ts("dve", selm[:], imp[:], mx8[:, 7:8], ALU.is_ge)
                    o.tr(ps[3][0:32, 0:128], selm[:], ident_f[:])
                    o.cp("act", selT[:, tt * 128:(tt + 1) * 128], ps[3][0:32, 0:128])

            k.phase = "nsa/selwin"
            mk = [ar(f"mk{i}", [128, 128], BF16) for i in range(3)]
            pp = [ar(f"pp{i}", [128, 256], BF16) for i in range(3)]
            it = 0
            items = []
            for br, kpair, vbase, span in ((1, 2, 0, 16), (2, 3, 2, 4)):
                for n in range(NT):
                    qsl = slice(n * 128, (n + 1) * 128)
                    js = [j for j in range(max(0, n - span), n + 1)]
                    for par in range(2):
                        base = par * 64
                        pso = ps[2 + par]
                        for ji, j in enumerate(js):
                            ksl = slice(j * 128, (j + 1) * 128)
                            p_ = pp[it % 3]
                            m_ = mk[it % 3]
                            pss = (ps[0], ps[1], ps[4])[it % 3]
                            it += 1

                            def A(br=br, kpair=kpair, span=span, n=n, j=j, qsl=qsl, ksl=ksl, base=base, p_=p_, m_=m_, pss=pss):
                                for hh in range(2):
                                    o.mm(pss[:, hh * 128:(hh + 1) * 128], qkT[base:base + 64, kpair, ksl], qkT[base:base + 64, hh, qsl])
                                o.act(p_[:], pss[:, 0:256], AF.Exp, scale=0.125)
                                msk = None
                                if br == 1:
                                    o.mm(ps[5][:, 0:128], small["Esel"][:, j, :], selT[:, qsl])
                                    if j == n:
                                        o.tt("dve", m_[:], ps[5][:, 0:128], trimask[:, 128:256], ALU.mult)
                                    else:
                                        o.cp("act", m_[:], ps[5][:, 0:128])
                                    msk = m_[:]
                                else:
                                    if j == n:
                                        msk = trimask[:, 128:256]
                                    elif j == n - span:
                                        msk = small["trigt"][:]
                                if msk is not None:
                                    o.tt("pool", p_[:].re("p (h q) -> p h q", h=2), p_[:].re("p (h q) -> p h q", h=2),
                                         msk.re("p (o q) -> p o q", o=1).bc([128, 2, 128]), ALU.mult)

                            def B(br=br, vbase=vbase, par=par, n=n, j=j, ji=ji, nj=len(js), p_=p_, pso=pso):
                                o.mm(pso[:, 0:256], Vn[:, j, vbase + par, :], p_[:], start=(ji == 0), stop=(ji == nj - 1))
                                if ji == nj - 1:
                                    for hh in range(2):
                                        h = hh * 2 + par
                                        epilogue(pso[:, hh * 128:(hh + 1) * 128], 128, h, br, n * 128)
                            items.append((A, B))
            LOOK = 2
            for idx, (A, B) in enumerate(items):
                A()
                if idx >= LOOK:
                    items[idx - LOOK][1]()
            for idx in range(max(0, len(items) - LOOK), len(items)):
                items[idx][1]()

        vfirst = None if dbg.get("no_vstore") else nc.dram_tensor("vfirst_scratch", [S, 256], F32).ap()
        vstore_tiles = []

        def rwkv(l):
            k.phase = "rwkv"
            ar_reset()
            WA = wA[:, 0:8448].re("p (c e) -> p c e", c=8)
            WB = wB[:, 0:8448].re("p (c e) -> p c e", c=8)
            mark0 = ar_state["off"]
            mu_t = ar("mu_t", [128, 1056], F32)
            omu = ar("omu", [128, 1056], F32)
            stg = [ar(f"stg{i}", [128, 1056], F32) for i in range(2)]
            o.load("sp", mu_t[:], dram["rw_mu"][l])
            o.ts("dve", omu[:], mu_t[:], -1.0, ALU.mult, 1.0, ALU.add)
            for c in range(8):
                sg = stg[c % 2]
                o.load("sp", sg[:], dram["w_rw"][l][c * 128:(c + 1) * 128, :])
                o.tt("dve", WA[:, c, :], sg[:], omu[:], ALU.mult)
                o.tt("pool", WB[:, c, :], sg[:], mu_t[:], ALU.mult)
            ar_rewind(mark0)
            if dbg.get("rwkv_stage", 9) <= -3:
                return
            P = {}
            for nm in ("w0", "a0", "kk", "ka", "rk", "lnw", "lnb") + (("v0",) if l > 0 else ()):
                P[nm] = ar("p_" + nm, [128, 256], F32)
                o.load("sp", P[nm][:], dram["rw_" + nm][l])
            wa2 = ar("wa2", [64, 2, 256], F32)
            g2a = ar("g2a", [128, 256], F32)
            g2b = ar("g2b", [32, 256], F32)
            o.load("sp", wa2[:, 0, :], dram["rw_w2"][l])
            o.load("sp", wa2[:, 1, :], dram["rw_a2"][l])
            o.load("sp", g2a[:], dram["rw_g2"][l][0:128, :])
            o.load("sp", g2b[:], dram["rw_g2"][l][128:160, :])
            if l > 0:
                v1 = ar("v1", [128, 2, 32], F32)
                v2 = ar("v2", [32, 256], F32)
                o.load("sp", v1[:], dram["rw_v1"][0].rearrange("(c p) r -> p c r", p=128))
                o.load("sp", v2[:], dram["rw_v2"][0])
            mU2 = ar("mU2", [128, 256], F32)
            mL = ar("mL", [128, 128], F32)
            o.load("sp", mU2[:], dram["mU2"])
            o.load("sp", mL[:], dram["mL"])
            Hs = ar("Hs", [64, 4, 64], F32)
            o.memset("dve", Hs[:], 0.0)
            if dbg.get("rwkv_stage", 9) <= -2:
                return
            nm256 = ("t0", "t1", "lw", "a_", "kk", "kp", "b_", "vv", "gi", "ginv", "ge", "gC", "gsb")
            W = {n_: ar("rw_" + n_, [128, 256], F32) for n_ in nm256}
            t0, t1, lw, a_, kk, kp, b_, vv, gi, ginv, ge, gC, gsb = (W[n_] for n_ in nm256)
            vf = gC
            vT = gi[:].re("p (a t) -> p a t", a=2)
            vv1T = ge[0:32, 0:128]
            XTkr = ar("XTkr", [64, 4, 2, 128], F32)
            XTbk = ar("XTbk", [64, 4, 2, 128], F32)
            P1 = ar("P1", [128, 256], F32)
            P2 = ar("P2", [128, 256], BF16)
            MrbTb = ar("MrbTb", [128, 128], BF16)
            vvb = ar("vvb", [128, 256], BF16)
            BCb = ar("BCb", [128, 256], BF16)
            KCb = ar("KCb", [128, 256], BF16)
            Hsb = ar("Hsb", [64, 4, 64], BF16)
            Lc0 = ar("Lc0", [128, 128], F32)
            Lb = [ar(f"Lb{i}", [128, 128], F32) for i in range(2)]
            Ub = [ar(f"Ub{i}", [128, 128], F32) for i in range(2)]
            Y = [ar(f"Y{i}", [128, 128], F32) for i in range(2)]
            ILs = [ar(f"IL{i}", [128, 128], F32) for i in range(2)]
            RH = ar("RH", [128, 128], F32)
            lxg2 = RH
            nWU = ar("nWU", [128, 128], BF16)
            TT = ar("TT", [64, 64], BF16)
            QT = ar("QT", [64, 128], BF16)
            s4 = ar("s4", [128, 4], F32)
            s4b = ar("s4b", [128, 4], F32)
            bc4 = ar("bc4", [128, 4], F32)
            gcol = ar("gcol", [64, 4], F32)
            lxw = ar("lxw", [64, 2, 128], F32)
            lxg = ar("lxg", [128, 128], F32)
            yb_ = KCb
            o.memset("pool", Hsb[:], 0.0)
            h3 = lambda v_: v_.re("p (h d) -> p h d", h=4)
            b3 = lambda v_: v_.re("p (h o) -> p h o", o=1).bc([128, 4, 64])

            for tt in range(dbg.get("rwkv_tiles", NT)):
                cur = slice(1 + tt * 128, 1 + (tt + 1) * 128)
                prv = slice(tt * 128, (tt + 1) * 128)
                k.phase = "rwkv/front"
                for (pb, c0, c1) in ((ps[0][:, 0:512], 0, 512), (ps[1][:, 0:256], 512, 768)):
                    for c in range(8):
                        o.mm(pb, hT[:, c, cur], WA[:, c, c0:c1], start=(c == 0), stop=False)
                    for c in range(8):
                        o.mm(pb, hT[:, c, prv], WB[:, c, c0:c1], start=False, stop=(c == 7))
                for (pb, c0, c1) in ((ps[2][:, 0:128], 768, 896), (ps[2][:, 128:256], 896, 1024), (ps[2][0:32, 256:384], 1024, 1056)):
                    for c in range(8):
                        o.mm(pb, WA[:, c, c0:c1], hT[:, c, cur], start=(c == 0), stop=False)
                    for c in range(8):
                        o.mm(pb, WB[:, c, c0:c1], hT[:, c, prv], start=False, stop=(c == 7))
                if dbg.get("rwkv_stage", 9) <= -1:
                    continue
                sub = dbg.get("rwkv_sub", 99)
                if sub > 0:
                    o.act(lxw[:, 0, :], ps[2][0:64, 0:128], AF.Tanh)
                if sub > 1:
                    o.cp("act", lxw[:, 1, :], ps[2][64:128, 0:128])
                if sub > 2:
                    o.act(lxg[:], ps[2][:, 128:256], AF.Sigmoid)
                if sub > 3:
                    o.act(lxg2[0:32, :], ps[2][0:32, 256:384], AF.Sigmoid)
                if sub > 4:
                    o.mm(ps[3][:, 0:256], lxw[:, 0, :], wa2[:, 0, :])
                if sub > 5:
                    o.mm(ps[3][:, 256:512], lxw[:, 1, :], wa2[:, 1, :])
                if sub > 6:
                    o.mm(ps[4][:, 0:256], lxg[:], g2a[:], start=True, stop=False)
                if sub > 7:
                    o.mm(ps[4][:, 0:256], lxg2[0:32, :], g2b[:], start=False, stop=True)
                if sub > 8:
                    o.cp("act", gsb[:], ps[4][:, 0:256])
                if dbg.get("rwkv_stage", 9) < 1:
                    continue
                o.tt("dve", t0[:], ps[3][:, 0:256], P["w0"][:], ALU.add)
                o.act(lw[:], t0[:], AF.Sigmoid)
                o.ts("pool", lw[:], lw[:], -0.6065306597126334, ALU.mult)
                o.tt("dve", t0[:], ps[3][:, 256:512], P["a0"][:], ALU.add)
                o.act(a_[:], t0[:], AF.Sigmoid)
                o.cp("act", vv[:], ps[1][:, 0:256])
                if l == 0:
                    if not dbg.get("no_vstore"):
                        o.store("sp", vfirst[tt * 128:(tt + 1) * 128, :], vv[:])
                        if vv not in vstore_tiles:
                            vstore_tiles.append(vv)
                else:
                    o.load("sp", vf[:], vfirst[tt * 128:(tt + 1) * 128, :], extra=vstore_tiles)
                    for c2 in range(2):
                        o.tr(ps[6][:, c2 * 128:(c2 + 1) * 128], vv[:, c2 * 128:(c2 + 1) * 128], ident_f[:])
                    o.cp("act", vT, ps[6][:, 0:256].re("p (a t) -> p a t", a=2))
                    for c2 in range(2):
                        o.mm(ps[6][0:32, 256:384], v1[:, c2, :], vT[:, c2, :], start=(c2 == 0), stop=(c2 == 1))
                    o.cp("act", vv1T, ps[6][0:32, 256:384])
                    o.mm(ps[3][:, 0:256], vv1T, v2[:])
                    o.tt("dve", t0[:], ps[3][:, 0:256], P["v0"][:], ALU.add)
                    o.act(t0[:], t0[:], AF.Sigmoid)
                    o.tt("dve", t1[:], vf[:], vv[:], ALU.subtract)
                    o.tt("dve", t1[:], t1[:], t0[:], ALU.mult)
                    o.tt("dve", vv[:], vv[:], t1[:], ALU.add)
                kps = ps[0][:, 256:512]
                rps = ps[0][:, 0:256]
                o.tt("dve", kk[:], kps, P["kk"][:], ALU.mult)
                o.tt("pool", t0[:], kk[:], kk[:], ALU.mult)
                o.red(s4[:], h3(t0[:]), ALU.add)
                o.act(s4[:], s4[:], AF.Sqrt)
                o.ts("dve", s4[:], s4[:], 1e-12, ALU.max)
                o.recip(s4[:], s4[:])
                o.tt("dve", h3(kk[:]), h3(kk[:]), b3(s4[:]), ALU.mult)
                o.stt("dve", t0[:], a_[:], -1.0, P["ka"][:], ALU.add, ALU.mult)
                o.stt("dve", kp[:], t0[:], 1.0, kps, ALU.add, ALU.mult)
                o.tt("pool", b_[:], kk[:], a_[:], ALU.mult)
                o.tt("dve", t1[:], rps, kp[:], ALU.mult)
                o.tt("pool", t1[:], t1[:], P["rk"][:], ALU.mult)
                o.red(bc4[:], h3(t1[:]), ALU.add)
                if dbg.get("rwkv_stage", 9) < 2:
                    continue
                o.mm(ps[5][:, 0:256], mU2[:, 128:256], lw[:])
                o.mm(ps[5][:, 256:512], ones_f[:], lw[:])
                for h in range(4):
                    o.mm(ps[4][0:64, 256 + h:257 + h], lw[:, h * 64:(h + 1) * 64], ones_f[:, 0:1])
                o.act(gcol[:], ps[4][0:64, 256:260], AF.Exp)
                cum = ps[5][:, 0:256]
                o.act(gi[:], cum, AF.Exp)
                o.act(ginv[:], cum, AF.Exp, scale=-1.0)
                o.tt("dve", t0[:], cum, lw[:], ALU.subtract)
                o.act(ge[:], t0[:], AF.Exp)
                o.cp("act", t1[:], ps[5][:, 256:512])
                o.tt("dve", t1[:], t1[:], cum, ALU.subtract)
                o.act(gC[:], t1[:], AF.Exp)
                o.tt("pool", ge[:], kk[:], ge[:], ALU.mult)
                o.tt("dve", gi[:], rps, gi[:], ALU.mult)
                o.tt("pool", a_[:], b_[:], ginv[:], ALU.mult)
                o.tt("pool", ginv[:], kp[:], ginv[:], ALU.mult)
                o.tt("pool", BCb[:], b_[:], gC[:], ALU.mult)
                o.tt("pool", KCb[:], kp[:], gC[:], ALU.mult)
                o.cp("act", vvb[:], vv[:])
                KKt, Rt, Bh, Kh, BC, KC = ge, gi, a_, ginv, BCb, KCb
                for qi, (src, dst, idx) in enumerate(((KKt, XTkr, 0), (Rt, XTkr, 1), (Bh, XTbk, 0), (Kh, XTbk, 1))):
                    for h in range(4):
                        o.tr(ps[6][0:64, h * 128:(h + 1) * 128], src[:, h * 64:(h + 1) * 64], ident_f[:])
                    o.cp("act" if qi % 2 == 0 else "dve", dst[:, :, idx, :], ps[6][0:64, 0:512].re("p (a t) -> p a t", a=4))
                if dbg.get("rwkv_stage", 9) < 3:
                    continue
                k.phase = "rwkv/heads"
                k.limit = dbg.get("oplimit")
                k.limit_on = True
                for h in range(4):
                    hc = slice(h * 64, (h + 1) * 64)
                    BhT = XTbk[:, h, 0, :]
                    KhT = XTbk[:, h, 1, :]
                    KRT = XTkr[:, h, :, :].re("p a t -> p (a t)")
                    KKtT = XTkr[:, h, 0, :]
                    RtT = XTkr[:, h, 1, :]
                    o.mm(ps[0][:, 0:256], BhT, KRT)
                    o.mm(ps[1][:, 0:256], KhT, KRT)
                    o.mm(ps[1][:, 256:384], KKtT, BhT)
                    o.tt("dve", P1[:], ps[0][:, 0:256], mU2[:], ALU.mult)
                    o.tt("dve", P2[:], ps[1][:, 0:256], mU2[:], ALU.mult)
                    o.tt("dve", MrbTb[:], ps[0][:, 128:256], mU2[:, 128:256], ALU.mult)
                    o.tt("dve", Lc0[:], ps[1][:, 256:384], mL[:], ALU.mult)
                    o.tt("pool", Y[0][:], ident_f[:], P1[:, 0:128], ALU.subtract)
                    o.mm(ps[3][:, 0:64], P2[:, 0:128], vvb[:, hc])
                    o.cp("act", RH[:, 64:128], ps[3][:, 0:64])
                    o.cp("pool", RH[:, 0:64], KKt[:, hc])
                    Uc, Lcur = P1[:, 0:128], Lc0[:]

                    def sq_step(ks, Uc, Lcur):
                        last = ks == 5
                        o.mm(ps[2][:, 0:128], Uc, Lcur)
                        if not last:
                            o.mm(ps[6][:, 0:128], Lcur, Uc)
                        o.cp("act", Lb[ks % 2][:], ps[2][:, 0:128])
                        if not last:
                            o.cp("dve", Ub[ks % 2][:], ps[6][:, 0:128])
                        o.tt("pool", ILs[ks % 2][:], Lb[ks % 2][:], ident_f[:], ALU.add)
                        return Ub[ks % 2][:], Lb[ks % 2][:]

                    def y_step(ks):
                        o.mm(ps[0][:, 256:384], ILs[ks % 2][:], Y[ks % 2][:])
                        o.cp("act", Y[(ks + 1) % 2][:], ps[0][:, 256:384])

                    Uc, Lcur = sq_step(0, Uc, Lcur)
                    for ks in range(1, 6):
                        Uc, Lcur = sq_step(ks, Uc, Lcur)
                        y_step(ks - 1)
                    y_step(5)
                    Yf = Y[0]
                    o.mm(ps[3][:, 64:192], Yf[:], RH[:])
                    o.ts("dve", nWU[:], ps[3][:, 64:192], -1.0, ALU.mult)
                    o.mm(ps[3][0:64, 192:256], nWU[:, 0:64], BC[:, hc])
                    o.cp("act", TT[:], ps[3][0:64, 192:256])
                    o.mm(ps[3][0:64, 256:384], nWU[:, 0:64], MrbTb[:], start=True, stop=False)
                    o.mm(ps[3][0:64, 256:384], ident_f[0:64, 0:64], RtT, start=False, stop=True)
                    o.cp("act", QT[:], ps[3][0:64, 256:384])
                    o.mm(ps[5][:, hc], P2[:, 128:256], vvb[:, hc], start=True, stop=False)
                    o.mm(ps[5][:, hc], MrbTb[:], nWU[:, 64:128], start=False, stop=False)
                    o.mm(ps[5][:, hc], QT[:], Hsb[:, h, :], start=False, stop=True)
                    o.mm(ps[4][0:64, hc], KC[:, hc], vvb[:, hc], start=True, stop=False)
                    o.mm(ps[4][0:64, hc], BC[:, hc], nWU[:, 64:128], start=False, stop=False)
                    o.mm(ps[4][0:64, hc], TT[:], Hsb[:, h, :], start=False, stop=True)
                    o.stt("dve", Hs[:, h, :], Hs[:, h, :], gcol[:, h:h + 1], ps[4][0:64, hc], ALU.mult, ALU.add)
                    o.cp("act", Hsb[:, h, :], Hs[:, h, :])
                if dbg.get("rwkv_stage", 9) < 4:
                    continue
                k.limit_on = False
                if "dumpP1" in dbg_out:
                    o.store("sp", dbg_out["dumpP1"], P1[:])
                    o.store("sp", dbg_out["dumpP2"], P2[:])
                    o.store("sp", dbg_out["dumpL"], Lc0[:])
                    o.store("sp", dbg_out["dumpY"], Y[0][:])
                    o.store("sp", dbg_out["dumpX"], XTkr[:].re("p a b t -> p (a b t)"))
                k.phase = "rwkv/post"
                O_ = ps[5][:, 0:256]
                xc = kk
                o.red(s4[:], h3(O_), ALU.add)
                o.ts("dve", s4[:], s4[:], 1.0 / 64, ALU.mult)
                o.tt("dve", h3(xc[:]), h3(O_), b3(s4[:]), ALU.subtract)
                o.tt("pool", t0[:], xc[:], xc[:], ALU.mult)
                o.red(s4b[:], h3(t0[:]), ALU.add)
                o.act(s4b[:], s4b[:], AF.Sqrt, bias=64e-5, scale=1.0 / 64)
                o.recip(s4b[:], s4b[:])
                o.tt("dve", h3(xc[:]), h3(xc[:]), b3(s4b[:]), ALU.mult)
                o.tt("pool", xc[:], xc[:], P["lnw"][:], ALU.mult)
                o.tt("pool", xc[:], xc[:], P["lnb"][:], ALU.add)
                o.tt("dve", h3(t0[:]), h3(vv[:]), b3(bc4[:]), ALU.mult)
                o.tt("dve", xc[:], xc[:], t0[:], ALU.add)
                o.tt("dve", yb_[:], xc[:], gsb[:], ALU.mult)
                for c2 in range(2):
                    o.tr(pT[:, c2 * 128:(c2 + 1) * 128], yb_[:, c2 * 128:(c2 + 1) * 128], ident_b[:])
                o.cp("act", mixT[:, :, tt * 128:(tt + 1) * 128], pT[:, 0:256].re("p (a t) -> p a t", a=2))

        pool_c = {}

        def pool_mixer(l):
            k.phase = "pool"
            ar_reset()
            if not pool_c:
                pool_c["rw"] = k.sbuf("pool_rw", [128, 2], F32)
                pool_c["fix"] = k.sbuf("pool_fix", [128, 2, 16], F32)
                pool_c["sc"] = k.sbuf("pool_sc", [128, 2], F32)
                o.load("sp", pool_c["rw"][:], dram["pool_rw"])
                o.load("sp", pool_c["fix"][:], dram["pool_fix"])
            o.load("sp", pool_c["sc"][:], dram["pool_scale"][l])
            wu_ = wB[:, 0:2048].re("p (c e) -> p c e", c=8)
            o.load("pool", wu_, dram["w_fm"][l][:, 0:256].rearrange("(c p) e -> p c e", p=128))
            pw = ar("pw", [128, 2, 128], BF16)
            pwf = ar("pwf", [128, 2, 128], F32)
            o.memset("pool", pwf[:], 0.0)
            for gi in range(4):
                o.load("sp", pwf[(gi % 2) * 64:(gi % 2) * 64 + 64, gi // 2, (gi % 2) * 64:(gi % 2) * 64 + 64], dram["pool_w"][l, gi])
            o.cp("dve", pw[:], pwf[:])
            PADW = 16
            u = [ar(f"pu{i}", [128, PADW + S], F32) for i in range(1)][0]
            sa = ar("psa", [128, PADW + S], F32)
            sb_ = ar("psb", [128, PADW + S], F32)
            dm = ar("pdm", [128, S], BF16)
            for mt in range(2):
                o.memset("pool", u[:, 0:PADW], 0.0)
                o.memset("pool", sa[:, 0:PADW], 0.0)
                o.memset("pool", sb_[:, 0:PADW], 0.0)
                for tb in range(NB):
                    for c in range(8):
                        o.mm(ps[0][:], wu_[:, c, mt * 128:(mt + 1) * 128], hT[:, c, 1 + tb * 512:1 + (tb + 1) * 512], start=(c == 0), stop=(c == 7))
                    o.cp("act", u[:, PADW + tb * 512:PADW + (tb + 1) * 512], ps[0][:])
                wl, wh = ((2, 4), (8, 16))[mt]
                full = slice(PADW, PADW + S)

                def sh(t, d):
                    return t[:, PADW - d:PADW + S - d]
                o.tt("dve", sa[:, full], u[:, full], sh(u, 1), ALU.add)
                cur, oth, w = sa, sb_, 2
                res = {}
                res[2] = cur
                while w < wh:
                    o.tt("dve", oth[:, full], cur[:, full], sh(cur, w), ALU.add)
                    w *= 2
                    if w == wl:
                        pass
                    res[w] = oth
                    cur, oth = oth, cur
                lo_t, hi_t = sa, sb_
                for (pr, src) in ((slice(0, 64), lo_t), (slice(64, 128), hi_t)):
                    o.ts("dve", src[pr, full], src[pr, full], pool_c["rw"][pr, mt:mt + 1], ALU.mult)
                    o.tt("dve", src[pr, PADW:PADW + 16], src[pr, PADW:PADW + 16], pool_c["fix"][pr, mt, :], ALU.mult)
                    o.tt("dve", dm[pr, :], src[pr, full], u[pr, full], ALU.subtract)
                for tb in range(NB):
                    tsl = slice(tb * 512, (tb + 1) * 512)
                    o.mm(ps[1][:], pw[:, mt, :], dm[:, tsl])
                    o.act(mixT[:, mt, tsl], ps[1][:], AF.Identity, scale=pool_c["sc"][:, mt:mt + 1])

        if dbg.get("only"):
            o.load("pool", hT[:], dram["hT_in"].rearrange("(c p) t -> p c t", p=128))
            {"rwkv": rwkv, "dil": dilated, "nsa": nsa, "pool": pool_mixer}[dbg["only"]](dbg.get("layer", 0))
            o.store("pool", dbg_out["mix_only"].rearrange("(c p) t -> p c t", p=128), mixT[:])
        for l in range(0 if dbg.get("only") else n_layers):
            norm(l, gs1[l], 0)
            if "hT" in dbg_out and l == dbg.get("layer", 0):
                pass
            comp = dbg.get("compute", ("nsa", "pool", "rwkv", "dil"))
            for grp, nm, fn in ((3, "dil", dilated), (1, "pool", pool_mixer), (0, "nsa", nsa), (2, "rwkv", rwkv)):
                if nm in comp:
                    fn(l)
                    if f"mix{grp}_{l}" in dbg_out:
                        o.store("pool", dbg_out[f"mix{grp}_{l}"].rearrange("(c p) t -> p c t", p=128), mixT[:])
                elif dbg.get("mix_in"):
                    o.load("pool", mixT[:], dram[f"mix_in{l}"][grp * 256:(grp + 1) * 256, :].rearrange("(c p) t -> p c t", p=128))
                else:
                    continue
                out_proj(l, grp)
            if f"x1_{l}" in dbg_out:
                o.store("sp", dbg_out[f"x1_{l}"].rearrange("(c p) t -> p c t", p=128), xT[:])
            ffn_alloc()
            norm(l, gs2[l], 24, router=True)
            routing()
            if f"gates_{l}" in dbg_out:
                o.store("sp", dbg_out[f"gates_{l}"], F["gatesT"][:])
            moe(l)

        o.store("sp", outT.rearrange("(c p) t -> p c t", p=128), xT[:])
        k.final_wait("sp")
        k.emit()
        print("instr counts", k.count, "waits", k.n_waits, "sems", k.nsem, "sbuf left", nc.sbuf_bytes_remaining)
    return nc


_NP2DT = {np.dtype(np.float32): F32, np.dtype(np.int32): I32, np.dtype(ml_dtypes.bfloat16): BF16}


def run(inp, dbg=None, n_layers=2, extra_per=None):
    sh, per = _prep_inputs(inp)
    if extra_per:
        for b in range(8):
            per[b].update(extra_per[b])
    in_maps = []
    for b in range(8):
        d = dict(sh)
        d.update(per[b])
        in_maps.append(d)
    shapes = {n: (a.shape, _NP2DT[a.dtype]) for n, a in in_maps[0].items()}
    nc = build(shapes, dbg=dbg, n_layers=n_layers)
    ncore = dbg.get("ncores", 8) if dbg else 8
    res = run_bass_kernel_spmd(nc, in_maps[:ncore], core_ids=list(range(ncore)), **({"trace": True} if (dbg and dbg.get("trace")) else {}))
    return res


def kernel(**inputs):
    inp = {k_: np.asarray(v) for k_, v in inputs.items()}
    res = run(inp)
    out = np.stack([np.ascontiguousarray(res.results[b]["outT"].T) for b in range(8)], axis=0)
    return out.astype(np.float32)
```

```python
import numpy as np
import ml_dtypes
from contextlib import ExitStack
import concourse.bass as bass
import concourse.mybir as mybir
from concourse.bass_utils import run_bass_kernel_spmd

F32 = mybir.dt.float32
BF16 = mybir.dt.bfloat16
I32 = mybir.dt.int32
ALU = mybir.AluOpType
AF = mybir.ActivationFunctionType
AX = mybir.AxisListType
EPOCH = 30000

S = 2048
D = 1024
NT = 16
NB = 4
EPS = 1e-6


class V:
    __slots__ = ("t", "ap")

    def __init__(self, t, ap):
        self.t = t
        self.ap = ap

    def __getitem__(self, k):
        return V(self.t, self.ap[k])

    def re(self, s, **kw):
        return V(self.t, self.ap.rearrange(s, **kw))

    def bc(self, shape):
        return V(self.t, self.ap.to_broadcast(list(shape)))

    def cast(self, dt):
        return V(self.t, self.ap.bitcast(dt))


class T:
    def __init__(self, name, ap):
        self.name = name
        self.ap = ap
        self.last_w = None
        self.readers = {}
        self.dma_sem = None
        self.dma_cnt = 0

    def __getitem__(self, k):
        return V(self, self.ap[k])

    def view(self, name, ap):
        return T(name, ap)


class Sched:
    ENGS = ("pe", "act", "dve", "pool", "sp")

    def __init__(self, nc, stack):
        self.nc = nc
        self.stack = stack
        self.streams = {e: [] for e in self.ENGS}
        self.count = {e: 0 for e in self.ENGS}
        self.sems = {e: [] for e in self.ENGS}
        self.clock = {e: {} for e in self.ENGS}
        self.snap = {e: {} for e in self.ENGS}
        self.dma_known = {e: {} for e in self.ENGS}
        self.dma_tiles = []
        self.nsem = 0
        self.n_waits = 0

    def new_sem(self, name):
        self.nsem += 1
        return self.stack.enter_context(self.nc.semaphore(name))

    def eng_sem(self, e, n):
        idx = (n - 1) // EPOCH
        while len(self.sems[e]) <= idx:
            self.sems[e].append(self.new_sem(f"s_{e}_{len(self.sems[e])}"))
        return self.sems[e][idx], (n - 1) % EPOCH + 1

    def sbuf(self, name, shape, dtype):
        t = self.stack.enter_context(self.nc.sbuf_tensor("sb_" + name, list(shape), dtype))
        return T(name, t[:])

    def psum(self, name, shape, dtype):
        t = self.stack.enter_context(self.nc.psum_tensor("pp_" + name, list(shape), dtype))
        r = T(name, t[:])
        r.is_psum = True
        return r

    def _need(self, e, reads, writes):
        need = {}
        dneed = []

        def add(dep):
            if dep is None:
                return
            x, n = dep
            if x == e and e == "pe":
                return
            if need.get(x, 0) < n:
                need[x] = n
        for t in reads:
            add(t.last_w)
            if getattr(t, "is_psum", False):
                for x, n in t.readers.items():
                    if x != e:
                        add((x, n))
            if t.dma_cnt:
                dneed.append(t)
        for t in writes:
            add(t.last_w)
            for x, n in t.readers.items():
                if x == e:
                    continue
                add((x, n))
            if t.dma_cnt:
                dneed.append(t)
        return need, dneed

    def _emit_waits(self, e, need, dneed):
        waits = []
        ck = self.clock[e]
        for x, n in need.items():
            if ck.get(x, 0) >= n:
                continue
            waits.append(self.eng_sem(x, n))
            sn = self.snap[x].get(n)
            if sn:
                for y, m in sn.items():
                    if ck.get(y, 0) < m:
                        ck[y] = m
            if ck.get(x, 0) < n:
                ck[x] = n
        dk = self.dma_known[e]
        for t in dneed:
            if dk.get(id(t), 0) >= t.dma_cnt:
                continue
            waits.append((t.dma_sem, t.dma_cnt))
            dk[id(t)] = t.dma_cnt
        return waits

    limit = None
    limit_on = False
    phase = "init"
    annotate = False
    opcount = 0

    def op(self, e, fn, reads=(), writes=()):
        if self.limit is not None and self.limit_on:
            self.opcount += 1
            if self.opcount > self.limit:
                return 0
        need, dneed = self._need(e, reads, writes)
        waits = self._emit_waits(e, need, dneed)
        self.count[e] += 1
        n = self.count[e]
        sem, _ = self.eng_sem(e, n)
        self.n_waits += len(waits)

        ph = self.phase if self.annotate else None

        def run(h, fn=fn, waits=waits, sem=sem, ph=ph):
            for (s, v) in waits:
                h.wait_ge(s, v)
            ins = fn(h)
            if ph is not None:
                ins = ins.annotate(ph)
            ins.then_inc(sem, 1)
        self.streams[e].append(run)
        sn = dict(self.clock[e])
        sn[e] = n
        self.snap[e][n] = sn
        for t in reads:
            t.readers[e] = n
        for t in writes:
            t.last_w = (e, n)
            t.readers = {}
        return n

    def dma(self, e, out_ap, in_ap, tile, write, extra=()):
        reads, writes = ((), (tile,)) if write else ((tile,), ())
        need, dneed = self._need(e, reads, writes)
        waits = self._emit_waits(e, need, dneed)
        for xt in extra:
            if xt.dma_cnt:
                waits.append((xt.dma_sem, xt.dma_cnt))
        self.n_waits += len(waits)
        t = tile
        if t.dma_sem is None:
            t.dma_sem = self.new_sem(f"d{self.nsem}_" + t.name)
            self.dma_tiles.append(t)
        t.dma_cnt += 16
        dsem = t.dma_sem

        def run(h, waits=waits, dsem=dsem, out_ap=out_ap, in_ap=in_ap):
            for (s, v) in waits:
                h.wait_ge(s, v)
            h.dma_start(out=out_ap, in_=in_ap).then_inc(dsem, 16)
        self.streams[e].append(run)
        if write:
            t.last_w = None
            t.readers = {}

    def barrier(self, o, pstile, ones):
        if not hasattr(self, "_bs"):
            self._bs = {e: self.sbuf("bs_" + e, [128, 2], F32) for e in ("pe", "act", "dve", "pool")}
        bs = self._bs
        pap = pstile.ap[0:1, 0:1]
        oap = ones.ap[0:1, 0:1]
        self.op("pe", lambda h: h.matmul(pap, lhsT=oap, rhs=oap, start=True, stop=True),
                reads=[ones], writes=[pstile, bs["pe"]])
        self.op("act", lambda h: h.activation(out=bs["act"].ap[:, 0:1], in_=bs["act"].ap[:, 0:1], func=AF.Copy, scale=0.0), writes=[bs["act"]])
        self.op("dve", lambda h: h.memset(bs["dve"].ap[:, 0:1], 0.0), writes=[bs["dve"]])
        self.op("pool", lambda h: h.memset(bs["pool"].ap[:, 0:1], 0.0), writes=[bs["pool"]])
        allb = [bs[e] for e in ("pe", "act", "dve", "pool")]
        waits = [self.eng_sem(x, n) for x, n in (b.last_w for b in allb)]
        self.op("pe", lambda h: h.matmul(pap, lhsT=oap, rhs=oap, start=True, stop=True),
                reads=[ones] + allb, writes=[pstile])
        self.op("act", lambda h: h.activation(out=bs["act"].ap[:, 1:2], in_=bs["act"].ap[:, 0:1], func=AF.Copy), reads=allb, writes=[])
        self.op("dve", lambda h: h.memset(bs["dve"].ap[:, 1:2], 0.0), reads=allb, writes=[])
        self.op("pool", lambda h: h.memset(bs["pool"].ap[:, 1:2], 0.0), reads=allb, writes=[])

        def run(h, waits=waits):
            for (s_, v) in waits:
                h.wait_ge(s_, v)
        self.streams["sp"].append(run)

    def final_wait(self, e):
        waits = [(t.dma_sem, t.dma_cnt) for t in self.dma_tiles if t.dma_cnt]

        def run(h, waits=waits):
            for (s, v) in waits:
                h.wait_ge(s, v)
        self.streams[e].append(run)

    def emit(self):
        nc = self.nc
        with nc.Block() as block:
            @block.tensor
            def _(h):
                for r in self.streams["pe"]:
                    r(h)

            @block.scalar
            def _(h):
                for r in self.streams["act"]:
                    r(h)

            @block.vector
            def _(h):
                for r in self.streams["dve"]:
                    r(h)

            @block.gpsimd
            def _(h):
                for r in self.streams["pool"]:
                    r(h)

            @block.sync
            def _(h):
                for r in self.streams["sp"]:
                    r(h)


def _ap(x):
    return x.ap if isinstance(x, V) else x


def _ts(*xs):
    return [x.t for x in xs if isinstance(x, V)]


class Ops:
    def __init__(self, k):
        self.k = k

    def mm(self, out, lhsT, rhs, start=True, stop=True):
        self.k.op("pe", lambda h: h.matmul(out.ap, lhsT=lhsT.ap, rhs=rhs.ap, start=start, stop=stop),
                  reads=_ts(lhsT, rhs), writes=_ts(out))

    def tr(self, out, in_, ident):
        self.k.op("pe", lambda h: h.transpose(out.ap, in_.ap, ident.ap), reads=_ts(in_, ident), writes=_ts(out))

    def act(self, out, in_, func, bias=0.0, scale=1.0, accum=None):
        def f(h):
            kw = {}
            if accum is not None:
                kw["accum_out"] = accum.ap
            return h.activation(out=out.ap, in_=in_.ap, func=func, bias=_ap(bias), scale=_ap(scale), **kw)
        self.k.op("act", f, reads=_ts(in_, bias, scale), writes=_ts(out) + (_ts(accum) if accum is not None else []))

    def tt(self, e, out, a, b, op):
        self.k.op(e, lambda h: h.tensor_tensor(out=out.ap, in0=a.ap, in1=b.ap, op=op), reads=_ts(a, b), writes=_ts(out))

    def ts(self, e, out, a, s1, op0, s2=None, op1=None):
        def f(h):
            if op1 is None:
                return h.tensor_scalar(out=out.ap, in0=a.ap, scalar1=_ap(s1), scalar2=None, op0=op0)
            return h.tensor_scalar(out=out.ap, in0=a.ap, scalar1=_ap(s1), scalar2=_ap(s2), op0=op0, op1=op1)
        self.k.op(e, f, reads=_ts(a, s1, s2), writes=_ts(out))

    def stt(self, e, out, a, s, b, op0, op1):
        self.k.op(e, lambda h: h.scalar_tensor_tensor(out=out.ap, in0=a.ap, scalar=_ap(s), in1=b.ap, op0=op0, op1=op1),
                  reads=_ts(a, s, b), writes=_ts(out))

    def cp(self, e, out, in_):
        if e == "act":
            self.k.op("act", lambda h: h.activation(out=out.ap, in_=in_.ap, func=AF.Copy), reads=_ts(in_), writes=_ts(out))
        else:
            self.k.op(e, lambda h: h.tensor_copy(out=out.ap, in_=in_.ap), reads=_ts(in_), writes=_ts(out))

    def red(self, out, in_, op, axis=AX.X):
        self.k.op("dve", lambda h: h.tensor_reduce(out=out.ap, in_=in_.ap, axis=axis, op=op), reads=_ts(in_), writes=_ts(out))

    def recip(self, out, in_):
        self.k.op("dve", lambda h: h.reciprocal(out=out.ap, in_=in_.ap), reads=_ts(in_), writes=_ts(out))

    def memset(self, e, out, val):
        if e == "act_ms":
            self.k.op("act", lambda h: h.activation(out=out.ap, in_=out.ap, func=AF.Copy, scale=0.0), writes=_ts(out))
            return
        self.k.op(e, lambda h: h.memset(out.ap, val), writes=_ts(out))

    def load(self, e, out, src, extra=()):
        self.k.dma(e, out.ap, src, out.t, True, extra=extra)

    def store(self, e, dst, in_):
        self.k.dma(e, dst, in_.ap, in_.t, False)


def _cols(a, b):
    return list(range(a, b))


QK_COLS = (_cols(0, 256) + _cols(384, 448) * 2 + _cols(512, 576) * 2 + _cols(1964, 2220) + _cols(2220, 2476))
VN_COLS = _cols(448, 512) + _cols(576, 640)
FM_COLS = _cols(652, 908) + _cols(256, 384)
GL_COLS = _cols(640, 652)
VD_COLS = _cols(2476, 2732)
RW_COLS = _cols(908, 1964)


def _consts():
    c = {}
    c["ident_f"] = np.eye(128, dtype=np.float32)
    c["ident_b"] = np.eye(128, dtype=np.float32).astype(ml_dtypes.bfloat16)
    c["ones_f"] = np.ones((128, 128), np.float32)
    selE = np.zeros((16, 16, 128), np.float32)
    for e in range(16):
        selE[e, e, :] = 1.0
    c["selE"] = selE.reshape(16, 16 * 128)
    invf = (500000.0 ** (-2.0 * np.arange(8, dtype=np.float32) / 16.0)).astype(np.float32) / np.float32(2 * np.pi)
    c["invf"] = np.ascontiguousarray(np.broadcast_to(invf[None, :], (128, 8)), dtype=np.float32)
    p = np.arange(128)[:, None]
    q = np.arange(128)[None, :]
    c["trimask"] = np.concatenate([(p >= q), (p <= q)], axis=1).astype(np.float32).astype(ml_dtypes.bfloat16)
    bf = lambda a: np.ascontiguousarray(a, dtype=np.float32).astype(ml_dtypes.bfloat16)
    cc = np.arange(127)[:, None]
    tq = np.arange(2048)[None, :]
    c["cmpmask"] = bf(tq >= 16 * cc + 31)
    c_end = np.arange(127) * 16 + 31
    b_start = np.arange(32) * 64
    cover = np.clip(np.minimum(c_end[:, None] + 1, b_start[None, :] + 64) - np.maximum(c_end[:, None] + 1 - 32, b_start[None, :]), 0, None) / 32.0
    c["covones"] = bf(np.concatenate([cover, np.ones((127, 1))], axis=1))
    selG = np.zeros((12, 12, 128), np.float32)
    for e in range(12):
        selG[e, e, :] = 1.0
    c["selG"] = bf(selG.reshape(12, 12 * 128))
    E = np.zeros((32, 16, 128), np.float32)
    for j in range(16):
        for pp in range(128):
            E[2 * j + pp // 64, j, pp] = 1.0
    c["Esel"] = bf(E.reshape(32, 16 * 128))
    tt_ = np.arange(2048)
    cur = (tt_ // 64)[:, None]
    blk = np.arange(32)[None, :]
    forced = (blk == 0) | (blk == cur) | (blk == cur - 1)
    fut = blk > cur
    keep = (~forced & ~fut).astype(np.float32)
    add = np.where(fut, -1.0, np.where(forced, 1e4, 0.0)).astype(np.float32)
    c["selkeep"] = np.ascontiguousarray(keep.reshape(16, 128, 32).transpose(1, 0, 2))
    c["seladd"] = np.ascontiguousarray(add.reshape(16, 128, 32).transpose(1, 0, 2))
    c["trigt"] = bf(p > q)
    c["mU2"] = np.concatenate([(p < q), (p <= q)], axis=1).astype(np.float32)
    c["mL"] = (p > q).astype(np.float32)
    wwin = np.array([[2] * 64 + [4] * 64, [8] * 64 + [16] * 64], np.float32).T
    c["pool_rw"] = (1.0 / wwin).astype(np.float32)
    tcnt = np.arange(1, 17, dtype=np.float32)[None, None, :]
    c["pool_fix"] = (wwin[:, :, None] / np.minimum(tcnt, wwin[:, :, None])).astype(np.float32)
    return c


def _prep_inputs(inp):
    f = lambda a: np.ascontiguousarray(a, dtype=np.float32)
    sh = {}
    sh["ada_w"] = f(inp["ada_w"])
    sh["ada_b"] = f(inp["ada_b"].reshape(2, 48, 128).transpose(0, 2, 1))
    sh["g_mix"] = f(inp["norm_mix_g"].reshape(2, 8, 128).transpose(0, 2, 1))
    sh["g_ffn"] = f(inp["norm_ffn_g"].reshape(2, 8, 128).transpose(0, 2, 1))
    w_in = inp["w_in"]
    sh["w_qk"] = f(w_in[:, :, QK_COLS])
    sh["w_vn"] = f(w_in[:, :, VN_COLS])
    sh["w_fm"] = f(w_in[:, :, FM_COLS])
    sh["w_gl"] = f(w_in[:, :, GL_COLS])
    sh["w_vd"] = f(w_in[:, :, VD_COLS])
    sh["w_rw"] = f(w_in[:, :, RW_COLS])
    sh["w_dqk"] = f(w_in[:, :, _cols(1964, 2476)])
    sh["w_nqk"] = f(w_in[:, :, _cols(0, 256) + _cols(384, 448) * 2 + _cols(512, 576) * 2])
    gd = np.concatenate([np.tile(inp["dil_q_norm"], (1, 4)), np.tile(inp["dil_k_norm"], (1, 4))], axis=1)
    sh["g_dqk"] = f(np.broadcast_to(gd[:, None, :], (2, 128, 512)))
    gn = np.concatenate([np.tile(inp["nsa_q_norm"], (1, 4)), np.tile(inp["nsa_k_norm"][:, 1], (1, 2)),
                         np.tile(inp["nsa_k_norm"][:, 2], (1, 2))], axis=1)
    sh["g_nqk"] = f(np.broadcast_to(gn[:, None, :], (2, 128, 512)))
    sh["cmp_w1"] = f(inp["nsa_cmp_w1"])
    sh["cmp_w2"] = f(inp["nsa_cmp_w2"])
    sh["cmp_peT"] = f(inp["nsa_cmp_pe"].transpose(0, 1, 3, 2).reshape(2, 128, 32))
    gk = np.tile(inp["nsa_k_norm"][:, 0], (1, 2))
    sh["g_kcmp"] = f(np.broadcast_to(gk[:, None, :], (2, 128, 128)))
    rowb = lambda a: f(np.broadcast_to(a[:, None, :], (a.shape[0], 128, a.shape[1])))
    sh["rw_mu"] = rowb(inp["rwkv_mu"])
    sh["rw_w0"] = rowb(inp["rwkv_w0"])
    sh["rw_a0"] = rowb(inp["rwkv_a0"])
    sh["rw_kk"] = rowb(inp["rwkv_k_k"])
    sh["rw_ka"] = rowb(inp["rwkv_k_a"])
    sh["rw_rk"] = rowb(inp["rwkv_r_k"].reshape(2, 256))
    sh["rw_lnw"] = rowb(inp["rwkv_ln_w"])
    sh["rw_lnb"] = rowb(inp["rwkv_ln_b"])
    sh["rw_v0"] = rowb(np.concatenate([np.zeros_like(inp["rwkv_v0"]), inp["rwkv_v0"]], axis=0))
    sh["rw_w2"] = f(inp["rwkv_w2"])
    sh["rw_a2"] = f(inp["rwkv_a2"])
    sh["rw_g2"] = f(inp["rwkv_g2"])
    sh["rw_v1"] = f(inp["rwkv_v1"])
    sh["rw_v2"] = f(inp["rwkv_v2"])
    sh["pool_w"] = f(inp["pool_w"])
    sh["pool_scale"] = f(inp["pool_scale"].reshape(2, 2, 128).transpose(0, 2, 1))
    sh["w_out"] = f(inp["w_out"])
    sh["router_w"] = f(inp["router_w"])
    sh["router_b"] = f(np.broadcast_to(inp["router_b"][None, :], (128, 16)))
    sh["moe_wg"] = f(inp["moe_w_gate"])
    sh["moe_wu"] = f(inp["moe_w_up"])
    sh["moe_wd"] = f(inp["moe_w_down"])
    sh.update(_consts())
    per = []
    for b in range(8):
        d = {}
        d["xT"] = f(inp["x"][b].T)
        d["cvec"] = f(inp["c"][b].reshape(8, 128).T)
        d["pos_tm"] = np.ascontiguousarray(inp["positions"][b].reshape(16, 128).T.astype(np.int32))
        d["pos_ce"] = np.ascontiguousarray(inp["positions"][b][31::16][:127].reshape(127, 1).astype(np.int32))
        per.append(d)
    return sh, per


def build(shapes, dbg=None, n_layers=2):
    dbg = dbg or {}
    nc = bass.Bass("TRN2", target_bir_lowering=False)
    dram = {}
    for name, (shape, dt) in shapes.items():
        dram[name] = nc.dram_tensor(name, list(shape), dt, kind="ExternalInput").ap()
    outT = nc.dram_tensor("outT", [D, S], F32, kind="ExternalOutput").ap()
    dbg_out = {}
    for name, shape in dbg.get("outs", {}).items():
        dbg_out[name] = nc.dram_tensor(name, list(shape), F32, kind="ExternalOutput").ap()

    with ExitStack() as st:
        k = Sched(nc, st)
        k.annotate = bool(dbg.get("trace"))
        o = Ops(k)
        xT = k.sbuf("xT", [128, 8, S], F32)
        hT = k.sbuf("hT", [128, 8, S + 1], BF16)
        mixT = k.sbuf("mixT", [128, 2, S], BF16)
        wA = k.sbuf("wA", [128, 8704], BF16)
        wB = k.sbuf("wB", [128, 8704], BF16)
        wo = k.sbuf("wo", [128, 2, D], BF16)
        ident_f = k.sbuf("ident_f", [128, 128], F32)
        ident_b = k.sbuf("ident_b", [128, 128], BF16)
        ones_f = k.sbuf("ones_f", [128, 128], F32)
        cvec = k.sbuf("cvec", [128, 8], F32)
        scv = k.sbuf("scv", [128, 8], F32)
        mods = [k.sbuf(f"mod{l}", [128, 48], F32) for l in range(2)]
        adab = [k.sbuf(f"adab{l}", [128, 48], F32) for l in range(2)]
        gs1 = [k.sbuf(f"gs1_{l}", [128, 8], F32) for l in range(2)]
        gs2 = [k.sbuf(f"gs2_{l}", [128, 8], F32) for l in range(2)]
        gmx = [k.sbuf(f"gmx{l}", [128, 8], F32) for l in range(2)]
        gff = [k.sbuf(f"gff{l}", [128, 8], F32) for l in range(2)]
        rw_sb = k.sbuf("rw_sb", [128, 8, 16], F32)
        rb_sb = k.sbuf("rb_sb", [128, 16], F32)
        rbias = k.sbuf("rbias", [16, 1], F32)
        sq = [k.sbuf(f"sq{i}", [128, 512], F32) for i in range(2)]
        tmpf = [k.sbuf(f"tmpf{i}", [128, 512], F32) for i in range(2)]
        rstd = k.sbuf("rstd", [128, 512], F32)
        ps = [k.psum(f"ps{i}", [128, 512], F32) for i in range(7)]
        pT = k.psum("pT", [128, 1024], BF16)
        cosT = k.sbuf("cosT", [128, NT, 8], F32)
        sinT = k.sbuf("sinT", [128, NT, 8], F32)
        trimask = k.sbuf("trimask", [128, 256], BF16)
        ARENA = 48 * 1024
        arena = k.sbuf("arena", [128, ARENA // 2], BF16)
        ar_state = {"off": 0}

        def ar_reset():
            k.barrier(o, ps[6], ones_f)
            ar_state["off"] = 0
            ar_state["offA"] = 0
            ar_state["offB"] = 0

        def ar_rewind(off):
            k.barrier(o, ps[6], ones_f)
            ar_state["off"] = off

        def ar(name, shape, dt, pool="main"):
            esz = 4 if dt in (F32, I32) else 2
            n = int(np.prod(shape[1:]))
            nb = (n * esz + 3) // 4 * 4
            key = "off" if pool == "main" else "off" + pool
            base_ap, cap = {"main": (arena.ap, ARENA), "A": (wA.ap, 17408), "B": (wB.ap, 8192)}[pool]
            off = ar_state.get(key, 0)
            assert off + nb <= cap, (name, pool, off, nb)
            ar_state[key] = off + nb
            ap = base_ap[0:shape[0], off // 2:(off + nb) // 2]
            if dt != BF16:
                ap = ap.bitcast(dt)
            ap = ap[:, 0:n]
            if len(shape) == 3:
                ap = ap.rearrange("p (a b) -> p a b", a=shape[1])
            elif len(shape) == 4:
                ap = ap.rearrange("p (a b c) -> p a b c", a=shape[1], b=shape[2])
            return T(name, ap)

        o.load("sp", ident_f[:], dram["ident_f"])
        o.load("pool", ident_b[:], dram["ident_b"])
        o.load("sp", ones_f[:], dram["ones_f"])
        o.load("sp", cvec[:], dram["cvec"])
        o.load("sp", xT[:], dram["xT"].rearrange("(c p) t -> p c t", p=128))
        o.load("sp", rw_sb[:], dram["router_w"].rearrange("(c p) e -> p c e", p=128))
        o.load("sp", rb_sb[:], dram["router_b"])
        for l in range(2):
            o.load("sp", adab[l][:], dram["ada_b"][l])
            o.load("sp", gmx[l][:], dram["g_mix"][l])
            o.load("sp", gff[l][:], dram["g_ffn"][l])
        o.memset("pool", hT[:, :, 0:1], 0.0)

        o.act(scv[:], cvec[:], AF.Silu)
        stage = [wA[:, 0:4096].cast(F32).re("p (c e) -> p c e", c=8), wB[:, 0:4096].cast(F32).re("p (c e) -> p c e", c=8)]
        for l in range(0 if dbg.get("only") else n_layers):
            for blk in range(24):
                sg = stage[blk % 2]
                o.load("sp", sg, dram["ada_w"][l][:, blk * 256:(blk + 1) * 256].rearrange("(c p) e -> p c e", p=128))
                for jj in range(2):
                    j = blk * 2 + jj
                    for c in range(8):
                        o.mm(ps[0][:, j:j + 1], sg[:, c, jj * 128:(jj + 1) * 128], scv[:, c:c + 1], start=(c == 0), stop=(c == 7))
            o.tt("dve", mods[l][:], ps[0][:, 0:48], adab[l][:], ALU.add)
            o.stt("dve", gs1[l][:], mods[l][:, 8:16], 1.0, gmx[l][:], ALU.add, ALU.mult)
            o.stt("dve", gs2[l][:], mods[l][:, 32:40], 1.0, gff[l][:], ALU.add, ALU.mult)
        if "mods" in dbg_out:
            o.store("sp", dbg_out["mods"][0], mods[0][:])
            o.store("sp", dbg_out["mods"][1], mods[1][:])

        def norm(l, gs, sh_off, router=False):
            k.phase = "norm"
            sh = mods[l]
            if router:
                for c in range(8):
                    o.mm(ps[5][0:16, 0:1], rw_sb[:, c, :], sh[:, sh_off + c:sh_off + c + 1], start=(c == 0), stop=(c == 7))
                o.cp("dve", rbias[:], ps[5][0:16, 0:1])
            for tb in range(NB):
                tsl = slice(tb * 512, (tb + 1) * 512)
                for c in range(8):
                    s_ = sq[c % 2]
                    o.act(s_[:], xT[:, c, tsl], AF.Square)
                    o.mm(ps[0][:], ones_f[:], s_[:], start=(c == 0), stop=(c == 7))
                o.act(rstd[:], ps[0][:], AF.Sqrt, bias=EPS, scale=1.0 / D)
                o.recip(rstd[:], rstd[:])
                for c in range(8):
                    t_ = tmpf[c % 2]
                    o.stt("dve", t_[:], xT[:, c, tsl], gs[:, c:c + 1], rstd[:], ALU.mult, ALU.mult)
                    o.act(hT[:, c, 1 + tb * 512:1 + (tb + 1) * 512], t_[:], AF.Identity, bias=sh[:, sh_off + c:sh_off + c + 1])
                    if router:
                        o.mm(ps[1][0:16, :], rw_sb[:, c, :], t_[:], start=(c == 0), stop=(c == 7))
                if router:
                    o.act(F["affT"][:, tsl], ps[1][0:16, :], AF.Sigmoid, bias=rbias[:])

        wo_state = {}

        def load_wo(l, grp):
            o.load("pool", wo[:], dram["w_out"][l][grp * 256:(grp + 1) * 256, :].rearrange("(c p) e -> p c e", p=128))
            wo_state["cur"] = (l, grp)

        def out_proj(l, grp):
            k.phase = "out_proj"
            if wo_state.get("cur") != (l, grp):
                load_wo(l, grp)
            g1 = mods[l][:, 16:24]
            for tb in range(NB):
                tsl = slice(tb * 512, (tb + 1) * 512)
                for dc in range(8):
                    p_ = ps[2 + (dc % 2)]
                    for kc in range(2):
                        o.mm(p_[:], wo[:, kc, dc * 128:(dc + 1) * 128], mixT[:, kc, tsl], start=(kc == 0), stop=(kc == 1))
                    o.stt("dve", xT[:, dc, tsl], p_[:], g1[:, dc:dc + 1], xT[:, dc, tsl], ALU.mult, ALU.add)

        rt_tiles = {}
        moe_tiles = {}
        F = {}

        def ffn_alloc():
            ar_reset()
            F["affT"] = ar("affT", [16, S], F32)
            F["gatesT"] = ar("gatesT", [16, S], F32)
            F["selE"] = ar("selE", [16, 16, 128], F32)
            o.load("sp", F["selE"][:], dram["selE"].rearrange("k (e m) -> k e m", e=16))
            rt_tiles.update(dict(
                aff=ar("r_aff", [128, NT, 16], F32), sel=ar("r_sel", [128, NT, 16], F32),
                sel2=ar("r_sel2", [128, NT, 16], F32), m1=ar("r_m1", [128, NT * 4], F32),
                m2=ar("r_m2", [128, NT * 4], F32), gsc=ar("r_gsc", [128, NT, 4], F32),
                gm=ar("r_gm", [128, NT], F32), den=ar("r_den", [128, NT], F32)))
            moe_tiles["gb"] = [ar(f"m_gb{i}", [128, 512], F32) for i in range(2)]
            moe_tiles["sg"] = [ar(f"m_sg{i}", [128, 512], BF16) for i in range(2)]
            moe_tiles["u2"] = [ar(f"m_u2{i}", [128, 512], BF16) for i in range(2)]
            moe_tiles["aT"] = [ar(f"m_aT{i}", [128, 2, 512], BF16) for i in range(2)]

        def routing():
            k.phase = "routing"
            aff, sel, sel2, m1, m2, gsc, gm, den = (rt_tiles[n] for n in ("aff", "sel", "sel2", "m1", "m2", "gsc", "gm", "den"))
            for t in range(NT):
                o.tr(ps[4][:, t * 16:(t + 1) * 16], F["affT"][:, t * 128:(t + 1) * 128], ident_f[0:16, 0:16])
            o.cp("dve", aff[:].re("p t e -> p (t e)"), ps[4][:, 0:256])
            o.tt("dve", sel[:], aff[:], rb_sb[:].re("p (o e) -> p o e", o=1).bc([128, NT, 16]), ALU.add)
            s4 = sel[:].re("p t (g j) -> p (t g) j", g=4)
            o.red(m1[:], s4, ALU.max)
            o.tt("dve", sel2[:].re("p t (g j) -> p (t g) j", g=4), s4, m1[:].re("p (a o) -> p a o", o=1).bc([128, NT * 4, 4]), ALU.is_ge)
            o.stt("dve", sel2[:], sel2[:], -1e9, sel[:], ALU.mult, ALU.add)
            o.red(m2[:], sel2[:].re("p t (g j) -> p (t g) j", g=4), ALU.max)
            o.tt("dve", gsc[:].re("p t g -> p (t g)"), m1[:], m2[:], ALU.add)
            o.red(gm[:], gsc[:], ALU.max)
            o.tt("dve", gsc[:], gsc[:], gm[:].re("p (t o) -> p t o", o=1).bc([128, NT, 4]), ALU.is_ge)
            o.tt("dve", sel2[:].re("p t (g j) -> p (t g) j", g=4), s4, m2[:].re("p (a o) -> p a o", o=1).bc([128, NT * 4, 4]), ALU.is_ge)
            o.tt("dve", sel2[:].re("p t (g j) -> p (t g) j", g=4), sel2[:].re("p t (g j) -> p (t g) j", g=4),
                 gsc[:].re("p t (g o) -> p (t g) o", o=1).bc([128, NT * 4, 4]), ALU.mult)
            o.tt("dve", sel2[:], sel2[:], aff[:], ALU.mult)
            o.red(den[:], sel2[:], ALU.add)
            o.recip(den[:], den[:])
            o.tt("dve", sel2[:], sel2[:], den[:].re("p (t o) -> p t o", o=1).bc([128, NT, 16]), ALU.mult)
            for g4 in range(4):
                for t4 in range(4):
                    t = g4 * 4 + t4
                    o.tr(ps[5][0:16, t4 * 128:(t4 + 1) * 128], sel2[:, t, :], ident_f[:])
                o.cp("act", F["gatesT"][:, g4 * 512:(g4 + 1) * 512], ps[5][0:16, :])


        moe_state = {}

        def moe_prefetch(l):
            wb = wA
            o.load("pool", wb[:, 0:2048].re("p (c f) -> p c f", c=8), dram["moe_wg"][l, 0].rearrange("(c p) f -> p c f", p=128))
            o.load("pool", wb[:, 2048:4096].re("p (c f) -> p c f", c=8), dram["moe_wu"][l, 0].rearrange("(c p) f -> p c f", p=128))
            o.load("pool", wb[:, 4096:6144].re("p (c d) -> p c d", c=2), dram["moe_wd"][l, 0].rearrange("(c p) d -> p c d", p=128))
            moe_state["pref"] = l

        def moe(l):
            k.phase = "moe"
            g2 = mods[l][:, 40:48]

            def wviews(e):
                wb = (wA, wB)[e % 2]
                return (wb[:, 0:2048].re("p (c f) -> p c f", c=8), wb[:, 2048:4096].re("p (c f) -> p c f", c=8),
                        wb[:, 4096:6144].re("p (c d) -> p c d", c=2))

            def load_w(e):
                wg, wu, wd = wviews(e)
                o.load("pool", wg, dram["moe_wg"][l, e].rearrange("(c p) f -> p c f", p=128))
                o.load("pool", wu, dram["moe_wu"][l, e].rearrange("(c p) f -> p c f", p=128))
                o.load("pool", wd, dram["moe_wd"][l, e].rearrange("(c p) d -> p c d", p=128))

            def down(wd, aT, tsl):
                for dc in range(8):
                    pd = ps[4 + (dc % 2)]
                    for fc in range(2):
                        o.mm(pd[:], wd[:, fc, dc * 128:(dc + 1) * 128], aT[:, fc, :], start=(fc == 0), stop=(fc == 1))
                    o.stt("dve", xT[:, dc, tsl], pd[:], g2[:, dc:dc + 1], xT[:, dc, tsl], ALU.mult, ALU.add)

            if moe_state.get("pref") != l:
                load_w(0)
            pending = None
            it = 0
            for e in range(16):
                wg, wu, wd = wviews(e)
                for tb in range(NB):
                    tsl = slice(tb * 512, (tb + 1) * 512)
                    hsl = slice(1 + tb * 512, 1 + (tb + 1) * 512)
                    gb = moe_tiles["gb"][it % 2]
                    aT = moe_tiles["aT"][it % 2]
                    it += 1
                    o.mm(ps[6][:], F["selE"][:, e, :], F["gatesT"][:, tsl])
                    o.cp("act", gb[:], ps[6][:])
                    for fc in range(2):
                        pg, pu = ps[0 + fc], ps[2 + fc]
                        for c in range(8):
                            o.mm(pg[:], wg[:, c, fc * 128:(fc + 1) * 128], hT[:, c, hsl], start=(c == 0), stop=(c == 7))
                        for c in range(8):
                            o.mm(pu[:], wu[:, c, fc * 128:(fc + 1) * 128], hT[:, c, hsl], start=(c == 0), stop=(c == 7))
                        sg = moe_tiles["sg"][fc]
                        u2 = moe_tiles["u2"][fc]
                        o.act(sg[:], pg[:], AF.Silu)
                        o.tt("dve", u2[:], pu[:], gb[:], ALU.mult)
                        o.tt("pool", aT[:, fc, :], sg[:], u2[:], ALU.mult)
                    if pending is not None:
                        pending()
                    if tb == 0 and e + 1 < 16:
                        load_w(e + 1)
                    pending = (lambda wd=wd, aT=aT, tsl=tsl: down(wd, aT, tsl))
            pending()

        MAGIC = 12582912.0

        def rope_table(dst_cos, dst_sin, pos_i, nparts, ncol, tmp_y, tmp_r, posf, invf_t):
            o.cp("dve", posf, pos_i)
            o.tt("dve", tmp_y, posf.re("p (t o) -> p t o", o=1).bc([nparts, ncol, 8]),
                 invf_t.re("p (o f) -> p o f", o=1).bc([nparts, ncol, 8]), ALU.mult)
            for dst, shift in ((dst_sin, 0.0), (dst_cos, 0.25)):
                o.ts("dve", tmp_r, tmp_y, shift, ALU.add, MAGIC, ALU.add)
                o.ts("dve", tmp_r, tmp_r, MAGIC, ALU.subtract)
                o.stt("dve", tmp_r, tmp_y, shift, tmp_r, ALU.add, ALU.subtract)
                o.act(dst, tmp_r, AF.Sin, scale=6.28318)

        invf_t = k.sbuf("invf", [128, 8], F32)
        posi = k.sbuf("posi", [128, NT], I32)
        posf = k.sbuf("posf", [128, NT], F32)
        rt_y = k.sbuf("rt_y", [128, NT, 8], F32)
        rt_r = k.sbuf("rt_r", [128, NT, 8], F32)
        o.load("sp", invf_t[:], dram["invf"])
        o.load("sp", posi[:], dram["pos_tm"])
        o.load("pool", trimask[:], dram["trimask"])
        rope_table(cosT[:], sinT[:], posi[:], 128, NT, rt_y[:], rt_r[:], posf[:], invf_t[:])

        def proj_qk(l, wname, gname, qkT, wbuf, sc, preloaded=False):
            k.phase = k.phase.split("/")[0] + "/projqk"
            if not preloaded:
                o.load("pool", wbuf, dram[wname][l].rearrange("(c p) e -> p c e", p=128))
            o.load("sp", sc["gain"][:], dram[gname][l])
            for tt in range(NT):
                b = tt % 2
                t1, t2, yb, ss = sc["t1"][b], sc["t2"][b], sc["yb"][b], sc["ss"][b]
                ra, rb, rc, rd = sc["ra"][b], sc["rb"][b], sc["rc"][b], sc["rd"][b]
                pq = ps[b]
                pTh = pT[:, b * 512:(b + 1) * 512]
                hs = slice(1 + tt * 128, 1 + (tt + 1) * 128)
                for c in range(8):
                    o.mm(pq[:], hT[:, c, hs], wbuf[:, c, :], start=(c == 0), stop=(c == 7))
                o.act(t1[:], pq[:], AF.Square)
                o.red(ss[:], t1[:].re("p (h d) -> p h d", h=8), ALU.add)
                o.act(ss[:], ss[:], AF.Sqrt, bias=EPS, scale=1.0 / 64)
                o.recip(ss[:], ss[:])
                o.tt("dve", t2[:].re("p (h d) -> p h d", h=8), pq[:].re("p (h d) -> p h d", h=8),
                     ss[:].re("p (h o) -> p h o", o=1).bc([128, 8, 64]), ALU.mult)
                o.tt("pool", t2[:], t2[:], sc["gain"][:], ALU.mult)
                y3 = t2[:].re("p (h d) -> p h d", h=8)
                yb3 = yb[:].re("p (h d) -> p h d", h=8)
                cs = cosT[:, tt, :].re("p (o f) -> p o f", o=1).bc([128, 8, 8])
                sn = sinT[:, tt, :].re("p (o f) -> p o f", o=1).bc([128, 8, 8])
                o.tt("dve", ra[:], y3[:, :, 0:8], cs, ALU.mult)
                o.tt("pool", rb[:], y3[:, :, 8:16], sn, ALU.mult)
                o.tt("dve", yb3[:, :, 0:8], ra[:], rb[:], ALU.subtract)
                o.tt("pool", rc[:], y3[:, :, 8:16], cs, ALU.mult)
                o.tt("dve", rd[:], y3[:, :, 0:8], sn, ALU.mult)
                o.tt("pool", yb3[:, :, 8:16], rc[:], rd[:], ALU.add)
                o.cp("act", yb3[:, :, 16:64], y3[:, :, 16:64])
                for j_ in range(4):
                    o.tr(pTh[:, j_ * 128:(j_ + 1) * 128], yb[:, j_ * 128:(j_ + 1) * 128], ident_b[:])
                o.cp("act", qkT[:, :, tt * 128:(tt + 1) * 128], pTh.re("p (j t) -> p j t", j=4))

        def qk_scratch():
            two = lambda nm, shp, dt: [ar(f"{nm}{i}", shp, dt) for i in range(2)]
            return dict(gain=ar("gain", [128, 512], F32), t1=two("t1", [128, 512], F32), t2=two("t2", [128, 512], F32),
                        yb=two("yb", [128, 512], BF16), ss=two("ss", [128, 8], F32),
                        ra=two("ra", [128, 8, 8], F32), rb=two("rb", [128, 8, 8], F32),
                        rc=two("rc", [128, 8, 8], F32), rd=two("rd", [128, 8, 8], F32))

        dil_state = {}

        def dil_prefetch(l):
            o.load("pool", wB[:, 0:4096].re("p (c e) -> p c e", c=8), dram["w_dqk"][l].rearrange("(c p) e -> p c e", p=128))
            o.load("pool", wB[:, 4096:6144].re("p (c e) -> p c e", c=8), dram["w_vd"][l].rearrange("(c p) e -> p c e", p=128))
            dil_state["pref"] = l

        def dilated(l):
            k.phase = "dilated"
            ar_reset()
            qkT = ar("qkT_d", [128, 4, S], BF16)
            mark = ar_state["off"]
            sc = qk_scratch()
            wq = wB[:, 0:4096].re("p (c e) -> p c e", c=8)
            wv = wB[:, 4096:6144].re("p (c e) -> p c e", c=8)
            pre = dil_state.get("pref") == l
            proj_qk(l, "w_dqk", "g_dqk", qkT, wq, sc, preloaded=pre)
            ar_rewind(mark)
            k.phase = "dilated/attn"
            Vd = ar("Vd", [128, NT, 256], BF16)
            acc = [ar(f"acc{i}", [128, S], F32) for i in range(2)]
            pt = [ar(f"pt{i}", [128, 256], BF16) for i in range(3)]
            dtmp = sq[0]
            if not pre:
                o.load("pool", wv, dram["w_vd"][l].rearrange("(c p) e -> p c e", p=128))
            o.memset("pool", Vd[:, :, 64:192], 1.0)
            it = 0
            for hp in range(2):
                for pi, dil in enumerate((1, 4, 16)):
                    nblk = (S // dil) // 128
                    for r in range(dil):
                        for m in range(nblk):
                            oi = r * nblk + m
                            st_ = 1 + r + dil * 128 * m
                            for c in range(8):
                                o.mm(ps[4][:, 0:128], hT[:, c, st_:st_ + dil * 127 + 1:dil], wv[:, c, hp * 128:(hp + 1) * 128], start=(c == 0), stop=(c == 7))
                            o.cp("act", Vd[:, oi, :].re("p (a b) -> p a b", b=64)[:, 0:4:3, :], ps[4][:, 0:128].re("p (a b) -> p a b", a=2))
                    items = []
                    for hh in range(2):
                        h = hp * 2 + hh
                        base = hh * 64
                        a_ = acc[hh]
                        for r in range(dil):
                            for n in range(nblk):
                                ms = [m for m in (n - 1, n) if m >= 0]
                                ncols = 128 * len(ms)
                                qcols = slice(r + dil * 128 * n, r + dil * 128 * n + dil * 127 + 1, dil)
                                pss = (ps[0], ps[1], ps[5])[it % 3]
                                pso = ps[2 + it % 2]
                                p_ = pt[it % 3]
                                it += 1

                                def A(ms=ms, ncols=ncols, qcols=qcols, pss=pss, p_=p_, base=base, r=r):
                                    for j, m in enumerate(ms):
                                        kcols = slice(r + dil * 128 * m, r + dil * 128 * m + dil * 127 + 1, dil)
                                        o.mm(pss[:, j * 128:(j + 1) * 128], qkT[base:base + 64, 2 + hp, kcols], qkT[base:base + 64, hp, qcols])
                                    o.act(p_[:, 0:ncols], pss[:, 0:ncols], AF.Exp, scale=0.125)
                                    o.tt("pool", p_[:, 0:ncols], p_[:, 0:ncols], trimask[:, 256 - ncols:256], ALU.mult)

                                def B(ms=ms, qcols=qcols, pso=pso, p_=p_, hh=hh, a_=a_, r=r):
                                    for j, m in enumerate(ms):
                                        oi = r * nblk + m
                                        lv = Vd[:, oi, hh * 128:(hh + 1) * 128]
                                        o.mm(pso[:, 0:128], lv, p_[:, j * 128:(j + 1) * 128], start=(j == 0), stop=(j == len(ms) - 1))
                                    if pi == 0:
                                        o.cp("dve", a_[:, qcols], pso[:, 0:128])
                                    else:
                                        o.tt("dve", a_[:, qcols], pso[:, 0:128], a_[:, qcols], ALU.add)
                                items.append((A, B))
                    LOOK = 2
                    for idx, (A, B) in enumerate(items):
                        A()
                        if idx >= LOOK:
                            items[idx - LOOK][1]()
                    for idx in range(max(0, len(items) - LOOK), len(items)):
                        items[idx][1]()
                for hh in range(2):
                    a_ = acc[hh]
                    ob, db = (0, 64) if hh == 0 else (64, 0)
                    for tb in range(NB):
                        tsl = slice(tb * 512, (tb + 1) * 512)
                        o.cp("act", dtmp[ob:ob + 64, :], a_[db:db + 64, tsl])
                        o.recip(dtmp[ob:ob + 64, :], dtmp[ob:ob + 64, :])
                        o.tt("dve", mixT[ob:ob + 64, hp, tsl], a_[ob:ob + 64, tsl], dtmp[ob:ob + 64, :], ALU.mult)


        def nsa(l):
            k.phase = "nsa"
            ar_reset()
            qkT = ar("qkT_n", [128, 4, S], BF16)
            mark = ar_state["off"]
            sc = qk_scratch()
            wq = wB[:, 0:4096].re("p (c e) -> p c e", c=8)
            wv = wB[:, 4096:5120].re("p (c e) -> p c e", c=8)
            wf = wB[:, 5120:6144].re("p (c e) -> p c e", c=8)
            wg = wB[:, 6144:6240].re("p (c e) -> p c e", c=8)
            proj_qk(l, "w_nqk", "g_nqk", qkT, wq, sc)
            ar_rewind(mark)
            Vn = ar("Vn", [128, NT, 4, 128], BF16)
            kcvc = ar("kcvc", [128, S], BF16, "A")
            sgT = ar("sgT", [12, S], BF16, "B")
            selT = ar("selT", [32, S], BF16, "B")
            cmpmask = ar("cmpmask", [127, 512], BF16)
            o.load("pool", wv, dram["w_vn"][l].rearrange("(c p) e -> p c e", p=128))
            o.load("pool", wf, dram["w_fm"][l][:, 256:384].rearrange("(c p) e -> p c e", p=128))
            o.load("pool", wg, dram["w_gl"][l].rearrange("(c p) e -> p c e", p=128))
            small = {}
            for nm, shp, dt, pl in (("covones", [127, 33], BF16, "main"), ("selG", [12, 12, 128], BF16, "main"), ("Esel", [32, 16, 128], BF16, "A"),
                                    ("selkeep", [128, 32], F32, "main"), ("seladd", [128, 32], F32, "main"), ("trigt", [128, 128], BF16, "main")):
                small[nm] = ar("c_" + nm, shp, dt, pl)
            o.load("pool", small["covones"][:], dram["covones"])
            o.load("pool", small["selG"][:], dram["selG"].rearrange("k (e m) -> k e m", e=12))
            o.load("pool", small["Esel"][:], dram["Esel"].rearrange("k (e m) -> k e m", e=16))
            o.load("pool", small["trigt"][:], dram["trigt"])
            k.phase = "nsa/vproj"
            o.memset("pool", Vn[:], 1.0)
            for tt in range(NT):
                hs = slice(1 + tt * 128, 1 + (tt + 1) * 128)
                for c in range(8):
                    o.mm(ps[4][:, 0:128], hT[:, c, hs], wv[:, c, :], start=(c == 0), stop=(c == 7))
                vflat = Vn[:, tt, :, :].re("p a b -> p (a b)")
                o.cp("act", vflat[:, 0:64], ps[4][:, 0:64])
                o.cp("act", vflat[:, 192:256], ps[4][:, 0:64])
                o.cp("dve", vflat[:, 256:320], ps[4][:, 64:128])
                o.cp("dve", vflat[:, 448:512], ps[4][:, 64:128])
            for tb in range(NB):
                hsl = slice(1 + tb * 512, 1 + (tb + 1) * 512)
                tsl = slice(tb * 512, (tb + 1) * 512)
                for c in range(8):
                    o.mm(ps[0][:], wf[:, c, :], hT[:, c, hsl], start=(c == 0), stop=(c == 7))
                o.cp("act", kcvc[:, tsl], ps[0][:])
                for c in range(8):
                    o.mm(ps[1][0:12, :], wg[:, c, :], hT[:, c, hsl], start=(c == 0), stop=(c == 7))
                o.act(sgT[:, tsl], ps[1][0:12, :], AF.Sigmoid)
            k.phase = "nsa/cmp"
            w1t = ar("w1t", [128, 32, 64], BF16, "A")
            peT = ar("peT", [128, 32], BF16, "A")
            w2t = ar("w2t", [64, 2, 64], BF16, "A")
            gel = ar("gel", [64, 2, 128], BF16, "A")
            for j in range(2):
                o.load("pool", w1t[j * 64:(j + 1) * 64, :, :], dram["cmp_w1"][l, j].rearrange("(i d) f -> d i f", d=64))
                o.load("pool", w2t[:, j, :], dram["cmp_w2"][l, j])
            o.load("pool", peT[:], dram["cmp_peT"][l])
            for j in range(2):
                b0 = j * 64
                for i in range(32):
                    o.mm(ps[2][0:64, 0:127], w1t[b0:b0 + 64, i, :], kcvc[b0:b0 + 64, i:i + 16 * 126 + 1:16], start=(i == 0), stop=False)
                for i in range(32):
                    o.mm(ps[2][0:64, 0:127], w1t[b0:b0 + 64, i, :], peT[b0:b0 + 64, i:i + 1].bc([64, 127]), start=False, stop=(i == 31))
                o.act(gel[:, j, 0:127], ps[2][0:64, 0:127], AF.Gelu_apprx_tanh)
            kc_f = ar("kc_f", [128, 128], F32, "A")
            kc_b = ar("kc_b", [128, 128], BF16, "A")
            kc_sq = ar("kc_sq", [128, 128], F32, "A")
            gkc = ar("gkc", [128, 128], F32, "A")
            kss = ar("kss", [128, 2], F32, "A")
            kcT = ar("kcT", [128, 128], BF16, "A")
            vc_aug = ar("vc_aug", [128, 2, 128], BF16)
            pce_i = ar("pce_i", [128, 1], I32, "A")
            pce_f = ar("pce_f", [128, 1], F32, "A")
            cy = ar("cy", [128, 1, 8], F32, "A")
            cr = ar("cr", [128, 1, 8], F32, "A")
            ccos = ar("ccos", [128, 1, 8], F32, "A")
            csin = ar("csin", [128, 1, 8], F32, "A")
            cra = ar("cra", [128, 2, 8], F32, "A")
            crb = ar("crb", [128, 2, 8], F32, "A")
            o.load("sp", gkc[:], dram["g_kcmp"][l])
            o.load("sp", pce_i[0:127, :], dram["pos_ce"])
            rope_table(ccos[0:127], csin[0:127], pce_i[0:127, :], 127, 1, cy[0:127], cr[0:127], pce_f[0:127, :], invf_t[0:127, :])
            for d2 in range(2):
                o.mm(ps[3][0:127, d2 * 64:(d2 + 1) * 64], gel[:, 0, 0:127], w2t[:, 0, :])
            o.mm(ps[3][0:127, 128:192], gel[:, 1, 0:127], w2t[:, 1, :])
            R = slice(0, 127)
            o.act(kc_sq[R, :], ps[3][R, 0:128], AF.Square)
            o.red(kss[R, :], kc_sq[R, :].re("p (h d) -> p h d", h=2), ALU.add)
            o.act(kss[R, :], kss[R, :], AF.Sqrt, bias=EPS, scale=1.0 / 64)
            o.recip(kss[R, :], kss[R, :])
            o.tt("dve", kc_f[R, :].re("p (h d) -> p h d", h=2), ps[3][R, 0:128].re("p (h d) -> p h d", h=2),
                 kss[R, :].re("p (h o) -> p h o", o=1).bc([127, 2, 64]), ALU.mult)
            o.tt("dve", kc_f[R, :], kc_f[R, :], gkc[R, :], ALU.mult)
            y3 = kc_f[R, :].re("p (h d) -> p h d", h=2)
            yb3 = kc_b[R, :].re("p (h d) -> p h d", h=2)
            cs = ccos[R, :, :].bc([127, 2, 8])
            sn = csin[R, :, :].bc([127, 2, 8])
            o.tt("dve", cra[R], y3[:, :, 0:8], cs, ALU.mult)
            o.tt("dve", crb[R], y3[:, :, 8:16], sn, ALU.mult)
            o.tt("dve", yb3[:, :, 0:8], cra[R], crb[R], ALU.subtract)
            o.tt("dve", cra[R], y3[:, :, 8:16], cs, ALU.mult)
            o.tt("dve", crb[R], y3[:, :, 0:8], sn, ALU.mult)
            o.tt("dve", yb3[:, :, 8:16], cra[R], crb[R], ALU.add)
            o.cp("act", yb3[:, :, 16:64], y3[:, :, 16:64])
            o.tr(pT[:, 0:127], kc_b[R, :], ident_b[0:127, 0:127])
            o.cp("act", kcT[:, 0:127], pT[:, 0:127])
            o.memset("pool", vc_aug[:], 1.0)
            o.cp("act", vc_aug[R, 0, 0:64], ps[3][R, 128:192])
            o.cp("act", vc_aug[R, 1, 64:128], ps[3][R, 128:192])

            wgt = ar("wgt", [128, 512], F32)
            ctr = ar("ctr", [128, 512], BF16)
            first = {}

            def epilogue(pso, ncol, h, br, tcol0, clamp=False):
                ob, db = (0, 64) if h % 2 == 0 else (64, 0)
                tsl_ = slice(tcol0, tcol0 + ncol)
                o.cp("act", wgt[ob:ob + 64, 0:ncol], pso[db:db + 64, 0:ncol])
                if clamp:
                    o.ts("dve", wgt[ob:ob + 64, 0:ncol], wgt[ob:ob + 64, 0:ncol], 1e-30, ALU.max)
                o.recip(wgt[ob:ob + 64, 0:ncol], wgt[ob:ob + 64, 0:ncol])
                o.mm(ps[6][:, 0:ncol], small["selG"][:, h * 3 + br, :], sgT[:, tsl_])
                o.tt("dve", wgt[ob:ob + 64, 0:ncol], wgt[ob:ob + 64, 0:ncol], ps[6][ob:ob + 64, 0:ncol], ALU.mult)
                dst = mixT[ob:ob + 64, h // 2, tsl_]
                if br == 0:
                    o.tt("dve", dst, pso[ob:ob + 64, 0:ncol], wgt[ob:ob + 64, 0:ncol], ALU.mult)
                else:
                    o.tt("dve", ctr[ob:ob + 64, 0:ncol], pso[ob:ob + 64, 0:ncol], wgt[ob:ob + 64, 0:ncol], ALU.mult)
                    o.tt("pool", dst, dst, ctr[ob:ob + 64, 0:ncol], ALU.add)

            eT = [ar(f"eT{i}", [128, 512], BF16) for i in range(2)]
            imp = ar("imp", [128, 32], F32)
            imp2 = ar("imp2", [128, 32], F32)
            mx8 = ar("mx8", [128, 8], F32)
            rdn = ar("rdn", [128, 4], F32)
            selm = ar("selm", [128, 32], F32)
            it = 0
            for tb in range(NB):
                tsl = slice(tb * 512, (tb + 1) * 512)
                o.load("pool", cmpmask[:], dram["cmpmask"][:, tsl])
                for h in range(4):
                    base = (h % 2) * 64
                    e_ = eT[it % 2]
                    it += 1
                    o.mm(ps[0][0:127, :], kcT[base:base + 64, 0:127], qkT[base:base + 64, h // 2, tsl])
                    o.act(e_[0:127, :], ps[0][0:127, :], AF.Exp, scale=0.125)
                    o.tt("pool", e_[0:127, :], e_[0:127, :], cmpmask[:, :], ALU.mult)
                    o.mm(ps[1][:], vc_aug[0:127, h % 2, :], e_[0:127, :])
                    for q4 in range(4):
                        o.mm((ps[2], ps[5])[q4 // 2][:, ((q4 % 2) * 4 + h) * 33:((q4 % 2) * 4 + h + 1) * 33], e_[0:127, q4 * 128:(q4 + 1) * 128], small["covones"][:, :])
                    epilogue(ps[1], 512, h, 0, tb * 512, clamp=True)
                for q4 in range(4):
                    tt = tb * 4 + q4
                    pv = (ps[2], ps[5])[q4 // 2][:, (q4 % 2) * 132:(q4 % 2 + 1) * 132].re("p (h c) -> p h c", h=4)
                    o.load("sp", small["selkeep"][:], dram["selkeep"][:, tt, :])
                    o.load("sp", small["seladd"][:], dram["seladd"][:, tt, :])
                    o.ts("dve", rdn[:], pv[:, :, 32], 1e-30, ALU.max)
                    o.recip(rdn[:], rdn[:])
                    o.ts("dve", imp[:], pv[:, 0, 0:32], rdn[:, 0:1], ALU.mult)
                    for h in range(1, 4):
                        o.stt("dve", imp[:], pv[:, h, 0:32], rdn[:, h:h + 1], imp[:], ALU.mult, ALU.add)
                    o.tt("dve", imp[:], imp[:], small["selkeep"][:, :], ALU.mult)
                    o.tt("dve", imp[:], imp[:], small["seladd"][:, :], ALU.add)
                    k.op("dve", lambda h_, a=mx8, b=imp: h_.max(out=a.ap, in_=b.ap), reads=[imp], writes=[mx8])
                    k.op("dve", lambda h_, a=imp2, b=mx8, c_=imp: h_.match_replace(out=a.ap, in_to_replace=b.ap, in_values=c_.ap, imm_value=-1e30),
                         reads=[imp, mx8], writes=[imp2])
                    k.op("dve", lambda h_, a=mx8, b=imp2: h_.max(out=a.ap, in_=b.ap), reads=[imp2], writes=[mx8])
                    o.ts("dve", selm[:], imp[:], mx8[:, 7:8], ALU.is_ge)
                    o.tr(ps[3][0:32, 0:128], selm[:], ident_f[:])
                    o.cp("act", selT[:, tt * 128:(tt + 1) * 128], ps[3][0:32, 0:128])

            k.phase = "nsa/selwin"
            mk = [ar(f"mk{i}", [128, 128], BF16) for i in range(3)]
            pp = [ar(f"pp{i}", [128, 256], BF16) for i in range(3)]
            it = 0
            items = []
            for br, kpair, vbase, span in ((1, 2, 0, 16), (2, 3, 2, 4)):
                for n in range(NT):
                    qsl = slice(n * 128, (n + 1) * 128)
                    js = [j for j in range(max(0, n - span), n + 1)]
                    for par in range(2):
                        base = par * 64
                        pso = ps[2 + par]
                        for ji, j in enumerate(js):
                            ksl = slice(j * 128, (j + 1) * 128)
                            p_ = pp[it % 3]
                            m_ = mk[it % 3]
                            pss = (ps[0], ps[1], ps[4])[it % 3]
                            it += 1

                            def A(br=br, kpair=kpair, span=span, n=n, j=j, qsl=qsl, ksl=ksl, base=base, p_=p_, m_=m_, pss=pss):
                                for hh in range(2):
                                    o.mm(pss[:, hh * 128:(hh + 1) * 128], qkT[base:base + 64, kpair, ksl], qkT[base:base + 64, hh, qsl])
                                o.act(p_[:], pss[:, 0:256], AF.Exp, scale=0.125)
                                msk = None
                                if br == 1:
                                    o.mm(ps[5][:, 0:128], small["Esel"][:, j, :], selT[:, qsl])
                                    if j == n:
                                        o.tt("dve", m_[:], ps[5][:, 0:128], trimask[:, 128:256], ALU.mult)
                                    else:
                                        o.cp("act", m_[:], ps[5][:, 0:128])
                                    msk = m_[:]
                                else:
                                    if j == n:
                                        msk = trimask[:, 128:256]
                                    elif j == n - span:
                                        msk = small["trigt"][:]
                                if msk is not None:
                                    o.tt("pool", p_[:].re("p (h q) -> p h q", h=2), p_[:].re("p (h q) -> p h q", h=2),
                                         msk.re("p (o q) -> p o q", o=1).bc([128, 2, 128]), ALU.mult)

                            def B(br=br, vbase=vbase, par=par, n=n, j=j, ji=ji, nj=len(js), p_=p_, pso=pso):
                                o.mm(pso[:, 0:256], Vn[:, j, vbase + par, :], p_[:], start=(ji == 0), stop=(ji == nj - 1))
                                if ji == nj - 1:
                                    for hh in range(2):
                                        h = hh * 2 + par
                                        epilogue(pso[:, hh * 128:(hh + 1) * 128], 128, h, br, n * 128)
                            items.append((A, B))
            LOOK = 2
            for idx, (A, B) in enumerate(items):
                A()
                if idx >= LOOK:
                    items[idx - LOOK][1]()
            for idx in range(max(0, len(items) - LOOK), len(items)):
                items[idx][1]()

        vfirst = None if dbg.get("no_vstore") else nc.dram_tensor("vfirst_scratch", [S, 256], F32).ap()
        vstore_tiles = []

        def rwkv(l):
            k.phase = "rwkv"
            ar_reset()
            WA = wA[:, 0:8448].re("p (c e) -> p c e", c=8)
            WB = wB[:, 0:8448].re("p (c e) -> p c e", c=8)
            mark0 = ar_state["off"]
            mu_t = ar("mu_t", [128, 1056], F32)
            omu = ar("omu", [128, 1056], F32)
            stg = [ar(f"stg{i}", [128, 1056], F32) for i in range(2)]
            o.load("sp", mu_t[:], dram["rw_mu"][l])
            o.ts("dve", omu[:], mu_t[:], -1.0, ALU.mult, 1.0, ALU.add)
            for c in range(8):
                sg = stg[c % 2]
                o.load("sp", sg[:], dram["w_rw"][l][c * 128:(c + 1) * 128, :])
                o.tt("dve", WA[:, c, :], sg[:], omu[:], ALU.mult)
                o.tt("pool", WB[:, c, :], sg[:], mu_t[:], ALU.mult)
            ar_rewind(mark0)
            if dbg.get("rwkv_stage", 9) <= -3:
                return
            P = {}
            for nm in ("w0", "a0", "kk", "ka", "rk", "lnw", "lnb") + (("v0",) if l > 0 else ()):
                P[nm] = ar("p_" + nm, [128, 256], F32)
                o.load("sp", P[nm][:], dram["rw_" + nm][l])
            wa2 = ar("wa2", [64, 2, 256], F32)
            g2a = ar("g2a", [128, 256], F32)
            g2b = ar("g2b", [32, 256], F32)
            o.load("sp", wa2[:, 0, :], dram["rw_w2"][l])
            o.load("sp", wa2[:, 1, :], dram["rw_a2"][l])
            o.load("sp", g2a[:], dram["rw_g2"][l][0:128, :])
            o.load("sp", g2b[:], dram["rw_g2"][l][128:160, :])
            if l > 0:
                v1 = ar("v1", [128, 2, 32], F32)
                v2 = ar("v2", [32, 256], F32)
                o.load("sp", v1[:], dram["rw_v1"][0].rearrange("(c p) r -> p c r", p=128))
                o.load("sp", v2[:], dram["rw_v2"][0])
            mU2 = ar("mU2", [128, 256], F32)
            mL = ar("mL", [128, 128], F32)
            o.load("sp", mU2[:], dram["mU2"])
            o.load("sp", mL[:], dram["mL"])
            Hs = ar("Hs", [64, 4, 64], F32)
            o.memset("dve", Hs[:], 0.0)
            if dbg.get("rwkv_stage", 9) <= -2:
                return
            nm256 = ("t0", "t1", "lw", "a_", "kk", "kp", "b_", "vv", "gi", "ginv", "ge", "gC", "gsb")
            W = {n_: ar("rw_" + n_, [128, 256], F32) for n_ in nm256}
            t0, t1, lw, a_, kk, kp, b_, vv, gi, ginv, ge, gC, gsb = (W[n_] for n_ in nm256)
            vf = gC
            vT = gi[:].re("p (a t) -> p a t", a=2)
            vv1T = ge[0:32, 0:128]
            XTkr = ar("XTkr", [64, 4, 2, 128], F32)
            XTbk = ar("XTbk", [64, 4, 2, 128], F32)
            P1 = ar("P1", [128, 256], F32)
            P2 = ar("P2", [128, 256], BF16)
            MrbTb = ar("MrbTb", [128, 128], BF16)
            vvb = ar("vvb", [128, 256], BF16)
            BCb = ar("BCb", [128, 256], BF16)
            KCb = ar("KCb", [128, 256], BF16)
            Hsb = ar("Hsb", [64, 4, 64], BF16)
            Lc0 = ar("Lc0", [128, 128], F32)
            Lb = [ar(f"Lb{i}", [128, 128], F32) for i in range(2)]
            Ub = [ar(f"Ub{i}", [128, 128], F32) for i in range(2)]
            Y = [ar(f"Y{i}", [128, 128], F32) for i in range(2)]
            ILs = [ar(f"IL{i}", [128, 128], F32) for i in range(2)]
            RH = ar("RH", [128, 128], F32)
            lxg2 = RH
            nWU = ar("nWU", [128, 128], BF16)
            TT = ar("TT", [64, 64], BF16)
            QT = ar("QT", [64, 128], BF16)
            s4 = ar("s4", [128, 4], F32)
            s4b = ar("s4b", [128, 4], F32)
            bc4 = ar("bc4", [128, 4], F32)
            gcol = ar("gcol", [64, 4], F32)
            lxw = ar("lxw", [64, 2, 128], F32)
            lxg = ar("lxg", [128, 128], F32)
            yb_ = KCb
            o.memset("pool", Hsb[:], 0.0)
            h3 = lambda v_: v_.re("p (h d) -> p h d", h=4)
            b3 = lambda v_: v_.re("p (h o) -> p h o", o=1).bc([128, 4, 64])

            for tt in range(dbg.get("rwkv_tiles", NT)):
                cur = slice(1 + tt * 128, 1 + (tt + 1) * 128)
                prv = slice(tt * 128, (tt + 1) * 128)
                k.phase = "rwkv/front"
                for (pb, c0, c1) in ((ps[0][:, 0:512], 0, 512), (ps[1][:, 0:256], 512, 768)):
                    for c in range(8):
                        o.mm(pb, hT[:, c, cur], WA[:, c, c0:c1], start=(c == 0), stop=False)
                    for c in range(8):
                        o.mm(pb, hT[:, c, prv], WB[:, c, c0:c1], start=False, stop=(c == 7))
                for (pb, c0, c1) in ((ps[2][:, 0:128], 768, 896), (ps[2][:, 128:256], 896, 1024), (ps[2][0:32, 256:384], 1024, 1056)):
                    for c in range(8):
                        o.mm(pb, WA[:, c, c0:c1], hT[:, c, cur], start=(c == 0), stop=False)
                    for c in range(8):
                        o.mm(pb, WB[:, c, c0:c1], hT[:, c, prv], start=False, stop=(c == 7))
                if dbg.get("rwkv_stage", 9) <= -1:
                    continue
                sub = dbg.get("rwkv_sub", 99)
                if sub > 0:
                    o.act(lxw[:, 0, :], ps[2][0:64, 0:128], AF.Tanh)
                if sub > 1:
                    o.cp("act", lxw[:, 1, :], ps[2][64:128, 0:128])
                if sub > 2:
                    o.act(lxg[:], ps[2][:, 128:256], AF.Sigmoid)
                if sub > 3:
                    o.act(lxg2[0:32, :], ps[2][0:32, 256:384], AF.Sigmoid)
                if sub > 4:
                    o.mm(ps[3][:, 0:256], lxw[:, 0, :], wa2[:, 0, :])
                if sub > 5:
                    o.mm(ps[3][:, 256:512], lxw[:, 1, :], wa2[:, 1, :])
                if sub > 6:
                    o.mm(ps[4][:, 0:256], lxg[:], g2a[:], start=True, stop=False)
                if sub > 7:
                    o.mm(ps[4][:, 0:256], lxg2[0:32, :], g2b[:], start=False, stop=True)
                if sub > 8:
                    o.cp("act", gsb[:], ps[4][:, 0:256])
                if dbg.get("rwkv_stage", 9) < 1:
                    continue
                o.tt("dve", t0[:], ps[3][:, 0:256], P["w0"][:], ALU.add)
                o.act(lw[:], t0[:], AF.Sigmoid)
                o.ts("pool", lw[:], lw[:], -0.6065306597126334, ALU.mult)
                o.tt("dve", t0[:], ps[3][:, 256:512], P["a0"][:], ALU.add)
                o.act(a_[:], t0[:], AF.Sigmoid)
                o.cp("act", vv[:], ps[1][:, 0:256])
                if l == 0:
                    if not dbg.get("no_vstore"):
                        o.store("sp", vfirst[tt * 128:(tt + 1) * 128, :], vv[:])
                        if vv not in vstore_tiles:
                            vstore_tiles.append(vv)
                else:
                    o.load("sp", vf[:], vfirst[tt * 128:(tt + 1) * 128, :], extra=vstore_tiles)
                    for c2 in range(2):
                        o.tr(ps[6][:, c2 * 128:(c2 + 1) * 128], vv[:, c2 * 128:(c2 + 1) * 128], ident_f[:])
                    o.cp("act", vT, ps[6][:, 0:256].re("p (a t) -> p a t", a=2))
                    for c2 in range(2):
                        o.mm(ps[6][0:32, 256:384], v1[:, c2, :], vT[:, c2, :], start=(c2 == 0), stop=(c2 == 1))
                    o.cp("act", vv1T, ps[6][0:32, 256:384])
                    o.mm(ps[3][:, 0:256], vv1T, v2[:])
                    o.tt("dve", t0[:], ps[3][:, 0:256], P["v0"][:], ALU.add)
                    o.act(t0[:], t0[:], AF.Sigmoid)
                    o.tt("dve", t1[:], vf[:], vv[:], ALU.subtract)
                    o.tt("dve", t1[:], t1[:], t0[:], ALU.mult)
                    o.tt("dve", vv[:], vv[:], t1[:], ALU.add)
                kps = ps[0][:, 256:512]
                rps = ps[0][:, 0:256]
                o.tt("dve", kk[:], kps, P["kk"][:], ALU.mult)
                o.tt("pool", t0[:], kk[:], kk[:], ALU.mult)
                o.red(s4[:], h3(t0[:]), ALU.add)
                o.act(s4[:], s4[:], AF.Sqrt)
                o.ts("dve", s4[:], s4[:], 1e-12, ALU.max)
                o.recip(s4[:], s4[:])
                o.tt("dve", h3(kk[:]), h3(kk[:]), b3(s4[:]), ALU.mult)
                o.stt("dve", t0[:], a_[:], -1.0, P["ka"][:], ALU.add, ALU.mult)
                o.stt("dve", kp[:], t0[:], 1.0, kps, ALU.add, ALU.mult)
                o.tt("pool", b_[:], kk[:], a_[:], ALU.mult)
                o.tt("dve", t1[:], rps, kp[:], ALU.mult)
                o.tt("pool", t1[:], t1[:], P["rk"][:], ALU.mult)
                o.red(bc4[:], h3(t1[:]), ALU.add)
                if dbg.get("rwkv_stage", 9) < 2:
                    continue
                o.mm(ps[5][:, 0:256], mU2[:, 128:256], lw[:])
                o.mm(ps[5][:, 256:512], ones_f[:], lw[:])
                for h in range(4):
                    o.mm(ps[4][0:64, 256 + h:257 + h], lw[:, h * 64:(h + 1) * 64], ones_f[:, 0:1])
                o.act(gcol[:], ps[4][0:64, 256:260], AF.Exp)
                cum = ps[5][:, 0:256]
                o.act(gi[:], cum, AF.Exp)
                o.act(ginv[:], cum, AF.Exp, scale=-1.0)
                o.tt("dve", t0[:], cum, lw[:], ALU.subtract)
                o.act(ge[:], t0[:], AF.Exp)
                o.cp("act", t1[:], ps[5][:, 256:512])
                o.tt("dve", t1[:], t1[:], cum, ALU.subtract)
                o.act(gC[:], t1[:], AF.Exp)
                o.tt("pool", ge[:], kk[:], ge[:], ALU.mult)
                o.tt("dve", gi[:], rps, gi[:], ALU.mult)
                o.tt("pool", a_[:], b_[:], ginv[:], ALU.mult)
                o.tt("pool", ginv[:], kp[:], ginv[:], ALU.mult)
                o.tt("pool", BCb[:], b_[:], gC[:], ALU.mult)
                o.tt("pool", KCb[:], kp[:], gC[:], ALU.mult)
                o.cp("act", vvb[:], vv[:])
                KKt, Rt, Bh, Kh, BC, KC = ge, gi, a_, ginv, BCb, KCb
                for qi, (src, dst, idx) in enumerate(((KKt, XTkr, 0), (Rt, XTkr, 1), (Bh, XTbk, 0), (Kh, XTbk, 1))):
                    for h in range(4):
                        o.tr(ps[6][0:64, h * 128:(h + 1) * 128], src[:, h * 64:(h + 1) * 64], ident_f[:])
                    o.cp("act" if qi % 2 == 0 else "dve", dst[:, :, idx, :], ps[6][0:64, 0:512].re("p (a t) -> p a t", a=4))
                if dbg.get("rwkv_stage", 9) < 3:
                    continue
                k.phase = "rwkv/heads"
                k.limit = dbg.get("oplimit")
                k.limit_on = True
                for h in range(4):
                    hc = slice(h * 64, (h + 1) * 64)
                    BhT = XTbk[:, h, 0, :]
                    KhT = XTbk[:, h, 1, :]
                    KRT = XTkr[:, h, :, :].re("p a t -> p (a t)")
                    KKtT = XTkr[:, h, 0, :]
                    RtT = XTkr[:, h, 1, :]
                    o.mm(ps[0][:, 0:256], BhT, KRT)
                    o.mm(ps[1][:, 0:256], KhT, KRT)
                    o.mm(ps[1][:, 256:384], KKtT, BhT)
                    o.tt("dve", P1[:], ps[0][:, 0:256], mU2[:], ALU.mult)
                    o.tt("dve", P2[:], ps[1][:, 0:256], mU2[:], ALU.mult)
                    o.tt("dve", MrbTb[:], ps[0][:, 128:256], mU2[:, 128:256], ALU.mult)
                    o.tt("dve", Lc0[:], ps[1][:, 256:384], mL[:], ALU.mult)
                    o.tt("pool", Y[0][:], ident_f[:], P1[:, 0:128], ALU.subtract)
                    o.mm(ps[3][:, 0:64], P2[:, 0:128], vvb[:, hc])
                    o.cp("act", RH[:, 64:128], ps[3][:, 0:64])
                    o.cp("pool", RH[:, 0:64], KKt[:, hc])
                    Uc, Lcur = P1[:, 0:128], Lc0[:]

                    def sq_step(ks, Uc, Lcur):
                        last = ks == 5
                        o.mm(ps[2][:, 0:128], Uc, Lcur)
                        if not last:
                            o.mm(ps[6][:, 0:128], Lcur, Uc)
                        o.cp("act", Lb[ks % 2][:], ps[2][:, 0:128])
                        if not last:
                            o.cp("dve", Ub[ks % 2][:], ps[6][:, 0:128])
                        o.tt("pool", ILs[ks % 2][:], Lb[ks % 2][:], ident_f[:], ALU.add)
                        return Ub[ks % 2][:], Lb[ks % 2][:]

                    def y_step(ks):
                        o.mm(ps[0][:, 256:384], ILs[ks % 2][:], Y[ks % 2][:])
                        o.cp("act", Y[(ks + 1) % 2][:], ps[0][:, 256:384])

                    Uc, Lcur = sq_step(0, Uc, Lcur)
                    for ks in range(1, 6):
                        Uc, Lcur = sq_step(ks, Uc, Lcur)
                        y_step(ks - 1)
                    y_step(5)
                    Yf = Y[0]
                    o.mm(ps[3][:, 64:192], Yf[:], RH[:])
                    o.ts("dve", nWU[:], ps[3][:, 64:192], -1.0, ALU.mult)
                    o.mm(ps[3][0:64, 192:256], nWU[:, 0:64], BC[:, hc])
                    o.cp("act", TT[:], ps[3][0:64, 192:256])
                    o.mm(ps[3][0:64, 256:384], nWU[:, 0:64], MrbTb[:], start=True, stop=False)
                    o.mm(ps[3][0:64, 256:384], ident_f[0:64, 0:64], RtT, start=False, stop=True)
                    o.cp("act", QT[:], ps[3][0:64, 256:384])
                    o.mm(ps[5][:, hc], P2[:, 128:256], vvb[:, hc], start=True, stop=False)
                    o.mm(ps[5][:, hc], MrbTb[:], nWU[:, 64:128], start=False, stop=False)
                    o.mm(ps[5][:, hc], QT[:], Hsb[:, h, :], start=False, stop=True)
                    o.mm(ps[4][0:64, hc], KC[:, hc], vvb[:, hc], start=True, stop=False)
                    o.mm(ps[4][0:64, hc], BC[:, hc], nWU[:, 64:128], start=False, stop=False)
                    o.mm(ps[4][0:64, hc], TT[:], Hsb[:, h, :], start=False, stop=True)
                    o.stt("dve", Hs[:, h, :], Hs[:, h, :], gcol[:, h:h + 1], ps[4][0:64, hc], ALU.mult, ALU.add)
                    o.cp("act", Hsb[:, h, :], Hs[:, h, :])
                if dbg.get("rwkv_stage", 9) < 4:
                    continue
                k.limit_on = False
                if "dumpP1" in dbg_out:
                    o.store("sp", dbg_out["dumpP1"], P1[:])
                    o.store("sp", dbg_out["dumpP2"], P2[:])
                    o.store("sp", dbg_out["dumpL"], Lc0[:])
                    o.store("sp", dbg_out["dumpY"], Y[0][:])
                    o.store("sp", dbg_out["dumpX"], XTkr[:].re("p a b t -> p (a b t)"))
                k.phase = "rwkv/post"
                O_ = ps[5][:, 0:256]
                xc = kk
                o.red(s4[:], h3(O_), ALU.add)
                o.ts("dve", s4[:], s4[:], 1.0 / 64, ALU.mult)
                o.tt("dve", h3(xc[:]), h3(O_), b3(s4[:]), ALU.subtract)
                o.tt("pool", t0[:], xc[:], xc[:], ALU.mult)
                o.red(s4b[:], h3(t0[:]), ALU.add)
                o.act(s4b[:], s4b[:], AF.Sqrt, bias=64e-5, scale=1.0 / 64)
                o.recip(s4b[:], s4b[:])
                o.tt("dve", h3(xc[:]), h3(xc[:]), b3(s4b[:]), ALU.mult)
                o.tt("pool", xc[:], xc[:], P["lnw"][:], ALU.mult)
                o.tt("pool", xc[:], xc[:], P["lnb"][:], ALU.add)
                o.tt("dve", h3(t0[:]), h3(vv[:]), b3(bc4[:]), ALU.mult)
                o.tt("dve", xc[:], xc[:], t0[:], ALU.add)
                o.tt("dve", yb_[:], xc[:], gsb[:], ALU.mult)
                for c2 in range(2):
                    o.tr(pT[:, c2 * 128:(c2 + 1) * 128], yb_[:, c2 * 128:(c2 + 1) * 128], ident_b[:])
                o.cp("act", mixT[:, :, tt * 128:(tt + 1) * 128], pT[:, 0:256].re("p (a t) -> p a t", a=2))

        pool_c = {}

        def pool_mixer(l):
            k.phase = "pool"
            ar_reset()
            if not pool_c:
                pool_c["rw"] = k.sbuf("pool_rw", [128, 2], F32)
                pool_c["fix"] = k.sbuf("pool_fix", [128, 2, 16], F32)
                pool_c["sc"] = k.sbuf("pool_sc", [128, 2], F32)
                o.load("sp", pool_c["rw"][:], dram["pool_rw"])
                o.load("sp", pool_c["fix"][:], dram["pool_fix"])
            o.load("sp", pool_c["sc"][:], dram["pool_scale"][l])
            wu_ = wB[:, 0:2048].re("p (c e) -> p c e", c=8)
            o.load("pool", wu_, dram["w_fm"][l][:, 0:256].rearrange("(c p) e -> p c e", p=128))
            pw = ar("pw", [128, 2, 128], BF16)
            pwf = ar("pwf", [128, 2, 128], F32)
            o.memset("pool", pwf[:], 0.0)
            for gi in range(4):
                o.load("sp", pwf[(gi % 2) * 64:(gi % 2) * 64 + 64, gi // 2, (gi % 2) * 64:(gi % 2) * 64 + 64], dram["pool_w"][l, gi])
            o.cp("dve", pw[:], pwf[:])
            PADW = 16
            u = [ar(f"pu{i}", [128, PADW + S], F32) for i in range(1)][0]
            sa = ar("psa", [128, PADW + S], F32)
            sb_ = ar("psb", [128, PADW + S], F32)
            dm = ar("pdm", [128, S], BF16)
            for mt in range(2):
                o.memset("pool", u[:, 0:PADW], 0.0)
                o.memset("pool", sa[:, 0:PADW], 0.0)
                o.memset("pool", sb_[:, 0:PADW], 0.0)
                for tb in range(NB):
                    for c in range(8):
                        o.mm(ps[0][:], wu_[:, c, mt * 128:(mt + 1) * 128], hT[:, c, 1 + tb * 512:1 + (tb + 1) * 512], start=(c == 0), stop=(c == 7))
                    o.cp("act", u[:, PADW + tb * 512:PADW + (tb + 1) * 512], ps[0][:])
                wl, wh = ((2, 4), (8, 16))[mt]
                full = slice(PADW, PADW + S)

                def sh(t, d):
                    return t[:, PADW - d:PADW + S - d]
                o.tt("dve", sa[:, full], u[:, full], sh(u, 1), ALU.add)
                cur, oth, w = sa, sb_, 2
                res = {}
                res[2] = cur
                while w < wh:
                    o.tt("dve", oth[:, full], cur[:, full], sh(cur, w), ALU.add)
                    w *= 2
                    if w == wl:
                        pass
                    res[w] = oth
                    cur, oth = oth, cur
                lo_t, hi_t = sa, sb_
                for (pr, src) in ((slice(0, 64), lo_t), (slice(64, 128), hi_t)):
                    o.ts("dve", src[pr, full], src[pr, full], pool_c["rw"][pr, mt:mt + 1], ALU.mult)
                    o.tt("dve", src[pr, PADW:PADW + 16], src[pr, PADW:PADW + 16], pool_c["fix"][pr, mt, :], ALU.mult)
                    o.tt("dve", dm[pr, :], src[pr, full], u[pr, full], ALU.subtract)
                for tb in range(NB):
                    tsl = slice(tb * 512, (tb + 1) * 512)
                    o.mm(ps[1][:], pw[:, mt, :], dm[:, tsl])
                    o.act(mixT[:, mt, tsl], ps[1][:], AF.Identity, scale=pool_c["sc"][:, mt:mt + 1])

        if dbg.get("only"):
            o.load("pool", hT[:], dram["hT_in"].rearrange("(c p) t -> p c t", p=128))
            {"rwkv": rwkv, "dil": dilated, "nsa": nsa, "pool": pool_mixer}[dbg["only"]](dbg.get("layer", 0))
            o.store("pool", dbg_out["mix_only"].rearrange("(c p) t -> p c t", p=128), mixT[:])
        GORDER = (3, 1, 0, 2)
        for l in range(0 if dbg.get("only") else n_layers):
            full = not dbg.get("mix_in") and "compute" not in dbg
            if full:
                dil_prefetch(l)
                if l == 0:
                    load_wo(l, GORDER[0])
            norm(l, gs1[l], 0)
            if "hT" in dbg_out and l == dbg.get("layer", 0):
                pass
            comp = dbg.get("compute", ("nsa", "pool", "rwkv", "dil"))
            for grp, nm, fn in ((3, "dil", dilated), (1, "pool", pool_mixer), (0, "nsa", nsa), (2, "rwkv", rwkv)):
                if nm in comp:
                    fn(l)
                    if f"mix{grp}_{l}" in dbg_out:
                        o.store("pool", dbg_out[f"mix{grp}_{l}"].rearrange("(c p) t -> p c t", p=128), mixT[:])
                elif dbg.get("mix_in"):
                    o.load("pool", mixT[:], dram[f"mix_in{l}"][grp * 256:(grp + 1) * 256, :].rearrange("(c p) t -> p c t", p=128))
                else:
                    continue
                out_proj(l, grp)
                if full:
                    gi_ = GORDER.index(grp)
                    if gi_ + 1 < 4:
                        load_wo(l, GORDER[gi_ + 1])
                    elif l + 1 < n_layers:
                        load_wo(l + 1, GORDER[0])
            if f"x1_{l}" in dbg_out:
                o.store("sp", dbg_out[f"x1_{l}"].rearrange("(c p) t -> p c t", p=128), xT[:])
            ffn_alloc()
            if full:
                moe_prefetch(l)
            norm(l, gs2[l], 24, router=True)
            routing()
            if f"gates_{l}" in dbg_out:
                o.store("sp", dbg_out[f"gates_{l}"], F["gatesT"][:])
            moe(l)

        o.store("sp", outT.rearrange("(c p) t -> p c t", p=128), xT[:])
        k.final_wait("sp")
        k.emit()
        print("instr counts", k.count, "waits", k.n_waits, "sems", k.nsem, "sbuf left", nc.sbuf_bytes_remaining)
    return nc


_NP2DT = {np.dtype(np.float32): F32, np.dtype(np.int32): I32, np.dtype(ml_dtypes.bfloat16): BF16}


def run(inp, dbg=None, n_layers=2, extra_per=None):
    sh, per = _prep_inputs(inp)
    if extra_per:
        for b in range(8):
            per[b].update(extra_per[b])
    in_maps = []
    for b in range(8):
        d = dict(sh)
        d.update(per[b])
        in_maps.append(d)
    shapes = {n: (a.shape, _NP2DT[a.dtype]) for n, a in in_maps[0].items()}
    nc = build(shapes, dbg=dbg, n_layers=n_layers)
    ncore = dbg.get("ncores", 8) if dbg else 8
    res = run_bass_kernel_spmd(nc, in_maps[:ncore], core_ids=list(range(ncore)), **({"trace": True} if (dbg and dbg.get("trace")) else {}))
    return res


def kernel(**inputs):
    inp = {k_: np.asarray(v) for k_, v in inputs.items()}
    res = run(inp)
    out = np.stack([np.ascontiguousarray(res.results[b]["outT"].T) for b in range(8)], axis=0)
    return out.astype(np.float32)
```

```python
import numpy as np
import ml_dtypes
from contextlib import ExitStack
import concourse.bass as bass
import concourse.mybir as mybir
from concourse.bass_utils import run_bass_kernel_spmd

F32 = mybir.dt.float32
BF16 = mybir.dt.bfloat16
I32 = mybir.dt.int32
ALU = mybir.AluOpType
AF = mybir.ActivationFunctionType
AX = mybir.AxisListType
EPOCH = 30000

S = 2048
D = 1024
NT = 16
NB = 4
EPS = 1e-6


class V:
    __slots__ = ("t", "ap")

    def __init__(self, t, ap):
        self.t = t
        self.ap = ap

    def __getitem__(self, k):
        return V(self.t, self.ap[k])

    def re(self, s, **kw):
        return V(self.t, self.ap.rearrange(s, **kw))

    def bc(self, shape):
        return V(self.t, self.ap.to_broadcast(list(shape)))

    def cast(self, dt):
        return V(self.t, self.ap.bitcast(dt))


class T:
    def __init__(self, name, ap):
        self.name = name
        self.ap = ap
        self.last_w = None
        self.readers = {}
        self.dma_sem = None
        self.dma_cnt = 0

    def __getitem__(self, k):
        return V(self, self.ap[k])

    def view(self, name, ap):
        return T(name, ap)


class Sched:
    ENGS = ("pe", "act", "dve", "pool", "sp")

    def __init__(self, nc, stack):
        self.nc = nc
        self.stack = stack
        self.streams = {e: [] for e in self.ENGS}
        self.count = {e: 0 for e in self.ENGS}
        self.sems = {e: [] for e in self.ENGS}
        self.clock = {e: {} for e in self.ENGS}
        self.snap = {e: {} for e in self.ENGS}
        self.dma_known = {e: {} for e in self.ENGS}
        self.dma_tiles = []
        self.nsem = 0
        self.n_waits = 0

    def new_sem(self, name):
        self.nsem += 1
        return self.stack.enter_context(self.nc.semaphore(name))

    def eng_sem(self, e, n):
        idx = (n - 1) // EPOCH
        while len(self.sems[e]) <= idx:
            self.sems[e].append(self.new_sem(f"s_{e}_{len(self.sems[e])}"))
        return self.sems[e][idx], (n - 1) % EPOCH + 1

    def sbuf(self, name, shape, dtype):
        t = self.stack.enter_context(self.nc.sbuf_tensor("sb_" + name, list(shape), dtype))
        return T(name, t[:])

    def psum(self, name, shape, dtype):
        t = self.stack.enter_context(self.nc.psum_tensor("pp_" + name, list(shape), dtype))
        r = T(name, t[:])
        r.is_psum = True
        return r

    def _need(self, e, reads, writes):
        need = {}
        dneed = []

        def add(dep):
            if dep is None:
                return
            x, n = dep
            if x == e and e == "pe":
                return
            if need.get(x, 0) < n:
                need[x] = n
        for t in reads:
            add(t.last_w)
            if getattr(t, "is_psum", False):
                for x, n in t.readers.items():
                    if x != e:
                        add((x, n))
            if t.dma_cnt:
                dneed.append(t)
        for t in writes:
            add(t.last_w)
            for x, n in t.readers.items():
                if x == e:
                    continue
                add((x, n))
            if t.dma_cnt:
                dneed.append(t)
        return need, dneed

    def _emit_waits(self, e, need, dneed):
        waits = []
        ck = self.clock[e]
        for x, n in need.items():
            if ck.get(x, 0) >= n:
                continue
            waits.append(self.eng_sem(x, n))
            sn = self.snap[x].get(n)
            if sn:
                for y, m in sn.items():
                    if ck.get(y, 0) < m:
                        ck[y] = m
            if ck.get(x, 0) < n:
                ck[x] = n
        dk = self.dma_known[e]
        for t in dneed:
            if dk.get(id(t), 0) >= t.dma_cnt:
                continue
            waits.append((t.dma_sem, t.dma_cnt))
            dk[id(t)] = t.dma_cnt
        return waits

    limit = None
    limit_on = False
    phase = "init"
    annotate = False
    opcount = 0

    def op(self, e, fn, reads=(), writes=()):
        if self.limit is not None and self.limit_on:
            self.opcount += 1
            if self.opcount > self.limit:
                return 0
        need, dneed = self._need(e, reads, writes)
        waits = self._emit_waits(e, need, dneed)
        self.count[e] += 1
        n = self.count[e]
        sem, _ = self.eng_sem(e, n)
        self.n_waits += len(waits)

        ph = self.phase if self.annotate else None

        def run(h, fn=fn, waits=waits, sem=sem, ph=ph):
            for (s, v) in waits:
                h.wait_ge(s, v)
            ins = fn(h)
            if ph is not None:
                ins = ins.annotate(ph)
            ins.then_inc(sem, 1)
        self.streams[e].append(run)
        sn = dict(self.clock[e])
        sn[e] = n
        self.snap[e][n] = sn
        for t in reads:
            t.readers[e] = n
        for t in writes:
            t.last_w = (e, n)
            t.readers = {}
        return n

    def dma(self, e, out_ap, in_ap, tile, write, extra=()):
        reads, writes = ((), (tile,)) if write else ((tile,), ())
        need, dneed = self._need(e, reads, writes)
        waits = self._emit_waits(e, need, dneed)
        for xt in extra:
            if xt.dma_cnt:
                waits.append((xt.dma_sem, xt.dma_cnt))
        self.n_waits += len(waits)
        t = tile
        if t.dma_sem is None:
            t.dma_sem = self.new_sem(f"d{self.nsem}_" + t.name)
            self.dma_tiles.append(t)
        t.dma_cnt += 16
        dsem = t.dma_sem

        def run(h, waits=waits, dsem=dsem, out_ap=out_ap, in_ap=in_ap):
            for (s, v) in waits:
                h.wait_ge(s, v)
            h.dma_start(out=out_ap, in_=in_ap).then_inc(dsem, 16)
        self.streams[e].append(run)
        if write:
            t.last_w = None
            t.readers = {}

    def barrier(self, o, pstile, ones):
        if not hasattr(self, "_bs"):
            self._bs = {e: self.sbuf("bs_" + e, [128, 2], F32) for e in ("pe", "act", "dve", "pool")}
        bs = self._bs
        pap = pstile.ap[0:1, 0:1]
        oap = ones.ap[0:1, 0:1]
        self.op("pe", lambda h: h.matmul(pap, lhsT=oap, rhs=oap, start=True, stop=True),
                reads=[ones], writes=[pstile, bs["pe"]])
        self.op("act", lambda h: h.activation(out=bs["act"].ap[:, 0:1], in_=bs["act"].ap[:, 0:1], func=AF.Copy, scale=0.0), writes=[bs["act"]])
        self.op("dve", lambda h: h.memset(bs["dve"].ap[:, 0:1], 0.0), writes=[bs["dve"]])
        self.op("pool", lambda h: h.memset(bs["pool"].ap[:, 0:1], 0.0), writes=[bs["pool"]])
        allb = [bs[e] for e in ("pe", "act", "dve", "pool")]
        waits = [self.eng_sem(x, n) for x, n in (b.last_w for b in allb)]
        self.op("pe", lambda h: h.matmul(pap, lhsT=oap, rhs=oap, start=True, stop=True),
                reads=[ones] + allb, writes=[pstile])
        self.op("act", lambda h: h.activation(out=bs["act"].ap[:, 1:2], in_=bs["act"].ap[:, 0:1], func=AF.Copy), reads=allb, writes=[])
        self.op("dve", lambda h: h.memset(bs["dve"].ap[:, 1:2], 0.0), reads=allb, writes=[])
        self.op("pool", lambda h: h.memset(bs["pool"].ap[:, 1:2], 0.0), reads=allb, writes=[])

        def run(h, waits=waits):
            for (s_, v) in waits:
                h.wait_ge(s_, v)
        self.streams["sp"].append(run)

    def final_wait(self, e):
        waits = [(t.dma_sem, t.dma_cnt) for t in self.dma_tiles if t.dma_cnt]

        def run(h, waits=waits):
            for (s, v) in waits:
                h.wait_ge(s, v)
        self.streams[e].append(run)

    def emit(self):
        nc = self.nc
        with nc.Block() as block:
            @block.tensor
            def _(h):
                for r in self.streams["pe"]:
                    r(h)

            @block.scalar
            def _(h):
                for r in self.streams["act"]:
                    r(h)

            @block.vector
            def _(h):
                for r in self.streams["dve"]:
                    r(h)

            @block.gpsimd
            def _(h):
                for r in self.streams["pool"]:
                    r(h)

            @block.sync
            def _(h):
                for r in self.streams["sp"]:
                    r(h)


def _ap(x):
    return x.ap if isinstance(x, V) else x


def _ts(*xs):
    return [x.t for x in xs if isinstance(x, V)]


class Ops:
    def __init__(self, k):
        self.k = k

    def mm(self, out, lhsT, rhs, start=True, stop=True):
        self.k.op("pe", lambda h: h.matmul(out.ap, lhsT=lhsT.ap, rhs=rhs.ap, start=start, stop=stop),
                  reads=_ts(lhsT, rhs), writes=_ts(out))

    def tr(self, out, in_, ident):
        self.k.op("pe", lambda h: h.transpose(out.ap, in_.ap, ident.ap), reads=_ts(in_, ident), writes=_ts(out))

    def act(self, out, in_, func, bias=0.0, scale=1.0, accum=None):
        def f(h):
            kw = {}
            if accum is not None:
                kw["accum_out"] = accum.ap
            return h.activation(out=out.ap, in_=in_.ap, func=func, bias=_ap(bias), scale=_ap(scale), **kw)
        self.k.op("act", f, reads=_ts(in_, bias, scale), writes=_ts(out) + (_ts(accum) if accum is not None else []))

    def tt(self, e, out, a, b, op):
        self.k.op(e, lambda h: h.tensor_tensor(out=out.ap, in0=a.ap, in1=b.ap, op=op), reads=_ts(a, b), writes=_ts(out))

    def ts(self, e, out, a, s1, op0, s2=None, op1=None):
        def f(h):
            if op1 is None:
                return h.tensor_scalar(out=out.ap, in0=a.ap, scalar1=_ap(s1), scalar2=None, op0=op0)
            return h.tensor_scalar(out=out.ap, in0=a.ap, scalar1=_ap(s1), scalar2=_ap(s2), op0=op0, op1=op1)
        self.k.op(e, f, reads=_ts(a, s1, s2), writes=_ts(out))

    def stt(self, e, out, a, s, b, op0, op1):
        self.k.op(e, lambda h: h.scalar_tensor_tensor(out=out.ap, in0=a.ap, scalar=_ap(s), in1=b.ap, op0=op0, op1=op1),
                  reads=_ts(a, s, b), writes=_ts(out))

    def cp(self, e, out, in_):
        if e == "act":
            self.k.op("act", lambda h: h.activation(out=out.ap, in_=in_.ap, func=AF.Copy), reads=_ts(in_), writes=_ts(out))
        else:
            self.k.op(e, lambda h: h.tensor_copy(out=out.ap, in_=in_.ap), reads=_ts(in_), writes=_ts(out))

    def red(self, out, in_, op, axis=AX.X):
        self.k.op("dve", lambda h: h.tensor_reduce(out=out.ap, in_=in_.ap, axis=axis, op=op), reads=_ts(in_), writes=_ts(out))

    def recip(self, out, in_):
        self.k.op("dve", lambda h: h.reciprocal(out=out.ap, in_=in_.ap), reads=_ts(in_), writes=_ts(out))

    def memset(self, e, out, val):
        if e == "act_ms":
            self.k.op("act", lambda h: h.activation(out=out.ap, in_=out.ap, func=AF.Copy, scale=0.0), writes=_ts(out))
            return
        self.k.op(e, lambda h: h.memset(out.ap, val), writes=_ts(out))

    def load(self, e, out, src, extra=()):
        self.k.dma(e, out.ap, src, out.t, True, extra=extra)

    def store(self, e, dst, in_):
        self.k.dma(e, dst, in_.ap, in_.t, False)


def _cols(a, b):
    return list(range(a, b))


QK_COLS = (_cols(0, 256) + _cols(384, 448) * 2 + _cols(512, 576) * 2 + _cols(1964, 2220) + _cols(2220, 2476))
VN_COLS = _cols(448, 512) + _cols(576, 640)
FM_COLS = _cols(652, 908) + _cols(256, 384)
GL_COLS = _cols(640, 652)
VD_COLS = _cols(2476, 2732)
RW_COLS = _cols(908, 1964)


def _consts():
    c = {}
    c["ident_f"] = np.eye(128, dtype=np.float32)
    c["ident_b"] = np.eye(128, dtype=np.float32).astype(ml_dtypes.bfloat16)
    c["ones_f"] = np.ones((128, 128), np.float32)
    selE = np.zeros((16, 16, 128), np.float32)
    for e in range(16):
        selE[e, e, :] = 1.0
    c["selE"] = selE.reshape(16, 16 * 128)
    invf = (500000.0 ** (-2.0 * np.arange(8, dtype=np.float32) / 16.0)).astype(np.float32) / np.float32(2 * np.pi)
    c["invf"] = np.ascontiguousarray(np.broadcast_to(invf[None, :], (128, 8)), dtype=np.float32)
    p = np.arange(128)[:, None]
    q = np.arange(128)[None, :]
    c["trimask"] = np.concatenate([(p >= q), (p <= q)], axis=1).astype(np.float32).astype(ml_dtypes.bfloat16)
    bf = lambda a: np.ascontiguousarray(a, dtype=np.float32).astype(ml_dtypes.bfloat16)
    cc = np.arange(127)[:, None]
    tq = np.arange(2048)[None, :]
    c["cmpmask"] = bf(tq >= 16 * cc + 31)
    c_end = np.arange(127) * 16 + 31
    b_start = np.arange(32) * 64
    cover = np.clip(np.minimum(c_end[:, None] + 1, b_start[None, :] + 64) - np.maximum(c_end[:, None] + 1 - 32, b_start[None, :]), 0, None) / 32.0
    c["covones"] = bf(np.concatenate([cover, np.ones((127, 1))], axis=1))
    selG = np.zeros((12, 12, 128), np.float32)
    for e in range(12):
        selG[e, e, :] = 1.0
    c["selG"] = bf(selG.reshape(12, 12 * 128))
    E = np.zeros((32, 16, 128), np.float32)
    for j in range(16):
        for pp in range(128):
            E[2 * j + pp // 64, j, pp] = 1.0
    c["Esel"] = bf(E.reshape(32, 16 * 128))
    tt_ = np.arange(2048)
    cur = (tt_ // 64)[:, None]
    blk = np.arange(32)[None, :]
    forced = (blk == 0) | (blk == cur) | (blk == cur - 1)
    fut = blk > cur
    keep = (~forced & ~fut).astype(np.float32)
    add = np.where(fut, -1.0, np.where(forced, 1e4, 0.0)).astype(np.float32)
    c["selkeep"] = np.ascontiguousarray(keep.reshape(16, 128, 32).transpose(1, 0, 2))
    c["seladd"] = np.ascontiguousarray(add.reshape(16, 128, 32).transpose(1, 0, 2))
    c["trigt"] = bf(p > q)
    c["mU2"] = np.concatenate([(p < q), (p <= q)], axis=1).astype(np.float32)
    c["mL"] = (p > q).astype(np.float32)
    wwin = np.array([[2] * 64 + [4] * 64, [8] * 64 + [16] * 64], np.float32).T
    c["pool_rw"] = (1.0 / wwin).astype(np.float32)
    tcnt = np.arange(1, 17, dtype=np.float32)[None, None, :]
    c["pool_fix"] = (wwin[:, :, None] / np.minimum(tcnt, wwin[:, :, None])).astype(np.float32)
    return c


def _prep_inputs(inp):
    f = lambda a: np.ascontiguousarray(a, dtype=np.float32)
    sh = {}
    sh["ada_w"] = f(inp["ada_w"])
    sh["ada_b"] = f(inp["ada_b"].reshape(2, 48, 128).transpose(0, 2, 1))
    sh["g_mix"] = f(inp["norm_mix_g"].reshape(2, 8, 128).transpose(0, 2, 1))
    sh["g_ffn"] = f(inp["norm_ffn_g"].reshape(2, 8, 128).transpose(0, 2, 1))
    w_in = inp["w_in"]
    sh["w_qk"] = f(w_in[:, :, QK_COLS])
    sh["w_vn"] = f(w_in[:, :, VN_COLS])
    sh["w_fm"] = f(w_in[:, :, FM_COLS])
    sh["w_gl"] = f(w_in[:, :, GL_COLS])
    sh["w_vd"] = f(w_in[:, :, VD_COLS])
    sh["w_rw"] = f(w_in[:, :, RW_COLS])
    sh["w_dqk"] = f(w_in[:, :, _cols(1964, 2476)])
    sh["w_nqk"] = f(w_in[:, :, _cols(0, 256) + _cols(384, 448) * 2 + _cols(512, 576) * 2])
    gd = np.concatenate([np.tile(inp["dil_q_norm"], (1, 4)), np.tile(inp["dil_k_norm"], (1, 4))], axis=1)
    sh["g_dqk"] = f(np.broadcast_to(gd[:, None, :], (2, 128, 512)))
    gn = np.concatenate([np.tile(inp["nsa_q_norm"], (1, 4)), np.tile(inp["nsa_k_norm"][:, 1], (1, 2)),
                         np.tile(inp["nsa_k_norm"][:, 2], (1, 2))], axis=1)
    sh["g_nqk"] = f(np.broadcast_to(gn[:, None, :], (2, 128, 512)))
    sh["cmp_w1"] = f(inp["nsa_cmp_w1"])
    sh["cmp_w2"] = f(inp["nsa_cmp_w2"])
    sh["cmp_peT"] = f(inp["nsa_cmp_pe"].transpose(0, 1, 3, 2).reshape(2, 128, 32))
    gk = np.tile(inp["nsa_k_norm"][:, 0], (1, 2))
    sh["g_kcmp"] = f(np.broadcast_to(gk[:, None, :], (2, 128, 128)))
    rowb = lambda a: f(np.broadcast_to(a[:, None, :], (a.shape[0], 128, a.shape[1])))
    sh["rw_mu"] = rowb(inp["rwkv_mu"])
    sh["rw_w0"] = rowb(inp["rwkv_w0"])
    sh["rw_a0"] = rowb(inp["rwkv_a0"])
    sh["rw_kk"] = rowb(inp["rwkv_k_k"])
    sh["rw_ka"] = rowb(inp["rwkv_k_a"])
    sh["rw_rk"] = rowb(inp["rwkv_r_k"].reshape(2, 256))
    sh["rw_lnw"] = rowb(inp["rwkv_ln_w"])
    sh["rw_lnb"] = rowb(inp["rwkv_ln_b"])
    sh["rw_v0"] = rowb(np.concatenate([np.zeros_like(inp["rwkv_v0"]), inp["rwkv_v0"]], axis=0))
    sh["rw_w2"] = f(inp["rwkv_w2"])
    sh["rw_a2"] = f(inp["rwkv_a2"])
    sh["rw_g2"] = f(inp["rwkv_g2"])
    sh["rw_v1"] = f(inp["rwkv_v1"])
    sh["rw_v2"] = f(inp["rwkv_v2"])
    sh["pool_w"] = f(inp["pool_w"])
    sh["pool_scale"] = f(inp["pool_scale"].reshape(2, 2, 128).transpose(0, 2, 1))
    sh["w_out"] = f(inp["w_out"])
    sh["router_w"] = f(inp["router_w"])
    sh["router_b"] = f(np.broadcast_to(inp["router_b"][None, :], (128, 16)))
    sh["moe_wg"] = f(inp["moe_w_gate"])
    sh["moe_wu"] = f(inp["moe_w_up"])
    sh["moe_wd"] = f(inp["moe_w_down"])
    sh.update(_consts())
    per = []
    for b in range(8):
        d = {}
        d["xT"] = f(inp["x"][b].T)
        d["cvec"] = f(inp["c"][b].reshape(8, 128).T)
        d["pos_tm"] = np.ascontiguousarray(inp["positions"][b].reshape(16, 128).T.astype(np.int32))
        d["pos_ce"] = np.ascontiguousarray(inp["positions"][b][31::16][:127].reshape(127, 1).astype(np.int32))
        per.append(d)
    return sh, per


def build(shapes, dbg=None, n_layers=2):
    dbg = dbg or {}
    nc = bass.Bass("TRN2", target_bir_lowering=False)
    dram = {}
    for name, (shape, dt) in shapes.items():
        dram[name] = nc.dram_tensor(name, list(shape), dt, kind="ExternalInput").ap()
    outT = nc.dram_tensor("outT", [D, S], F32, kind="ExternalOutput").ap()
    dbg_out = {}
    for name, shape in dbg.get("outs", {}).items():
        dbg_out[name] = nc.dram_tensor(name, list(shape), F32, kind="ExternalOutput").ap()

    with ExitStack() as st:
        k = Sched(nc, st)
        k.annotate = bool(dbg.get("trace"))
        o = Ops(k)
        xT = k.sbuf("xT", [128, 8, S], F32)
        hT = k.sbuf("hT", [128, 8, S + 1], BF16)
        mixT = k.sbuf("mixT", [128, 2, S], BF16)
        wA = k.sbuf("wA", [128, 8704], BF16)
        wB = k.sbuf("wB", [128, 8704], BF16)
        wo = k.sbuf("wo", [128, 2, D], BF16)
        ident_f = k.sbuf("ident_f", [128, 128], F32)
        ident_b = k.sbuf("ident_b", [128, 128], BF16)
        ones_f = k.sbuf("ones_f", [128, 128], F32)
        cvec = k.sbuf("cvec", [128, 8], F32)
        scv = k.sbuf("scv", [128, 8], F32)
        mods = [k.sbuf(f"mod{l}", [128, 48], F32) for l in range(2)]
        adab = [k.sbuf(f"adab{l}", [128, 48], F32) for l in range(2)]
        gs1 = [k.sbuf(f"gs1_{l}", [128, 8], F32) for l in range(2)]
        gs2 = [k.sbuf(f"gs2_{l}", [128, 8], F32) for l in range(2)]
        gmx = [k.sbuf(f"gmx{l}", [128, 8], F32) for l in range(2)]
        gff = [k.sbuf(f"gff{l}", [128, 8], F32) for l in range(2)]
        rw_sb = k.sbuf("rw_sb", [128, 8, 16], F32)
        rb_sb = k.sbuf("rb_sb", [128, 16], F32)
        rbias = k.sbuf("rbias", [16, 1], F32)
        sq = [k.sbuf(f"sq{i}", [128, 512], F32) for i in range(2)]
        tmpf = [k.sbuf(f"tmpf{i}", [128, 512], F32) for i in range(2)]
        rstd = k.sbuf("rstd", [128, 512], F32)
        ps = [k.psum(f"ps{i}", [128, 512], F32) for i in range(7)]
        pT = k.psum("pT", [128, 1024], BF16)
        cosT = k.sbuf("cosT", [128, NT, 8], F32)
        sinT = k.sbuf("sinT", [128, NT, 8], F32)
        trimask = k.sbuf("trimask", [128, 256], BF16)
        ARENA = 48 * 1024
        arena = k.sbuf("arena", [128, ARENA // 2], BF16)
        ar_state = {"off": 0}

        def ar_reset():
            k.barrier(o, ps[6], ones_f)
            ar_state["off"] = 0
            ar_state["offA"] = 0
            ar_state["offB"] = 0

        def ar_rewind(off):
            k.barrier(o, ps[6], ones_f)
            ar_state["off"] = off

        def ar(name, shape, dt, pool="main"):
            esz = 4 if dt in (F32, I32) else 2
            n = int(np.prod(shape[1:]))
            nb = (n * esz + 3) // 4 * 4
            key = "off" if pool == "main" else "off" + pool
            base_ap, cap = {"main": (arena.ap, ARENA), "A": (wA.ap, 17408), "B": (wB.ap, 8192)}[pool]
            off = ar_state.get(key, 0)
            assert off + nb <= cap, (name, pool, off, nb)
            ar_state[key] = off + nb
            ap = base_ap[0:shape[0], off // 2:(off + nb) // 2]
            if dt != BF16:
                ap = ap.bitcast(dt)
            ap = ap[:, 0:n]
            if len(shape) == 3:
                ap = ap.rearrange("p (a b) -> p a b", a=shape[1])
            elif len(shape) == 4:
                ap = ap.rearrange("p (a b c) -> p a b c", a=shape[1], b=shape[2])
            return T(name, ap)

        o.load("sp", ident_f[:], dram["ident_f"])
        o.load("pool", ident_b[:], dram["ident_b"])
        o.load("sp", ones_f[:], dram["ones_f"])
        o.load("sp", cvec[:], dram["cvec"])
        o.load("sp", xT[:], dram["xT"].rearrange("(c p) t -> p c t", p=128))
        o.load("sp", rw_sb[:], dram["router_w"].rearrange("(c p) e -> p c e", p=128))
        o.load("sp", rb_sb[:], dram["router_b"])
        for l in range(2):
            o.load("sp", adab[l][:], dram["ada_b"][l])
            o.load("sp", gmx[l][:], dram["g_mix"][l])
            o.load("sp", gff[l][:], dram["g_ffn"][l])
        o.memset("pool", hT[:, :, 0:1], 0.0)

        o.act(scv[:], cvec[:], AF.Silu)
        stage = [wA[:, 0:4096].cast(F32).re("p (c e) -> p c e", c=8), wB[:, 0:4096].cast(F32).re("p (c e) -> p c e", c=8)]
        for l in range(0 if dbg.get("only") else n_layers):
            for blk in range(24):
                sg = stage[blk % 2]
                o.load("sp", sg, dram["ada_w"][l][:, blk * 256:(blk + 1) * 256].rearrange("(c p) e -> p c e", p=128))
                for jj in range(2):
                    j = blk * 2 + jj
                    for c in range(8):
                        o.mm(ps[0][:, j:j + 1], sg[:, c, jj * 128:(jj + 1) * 128], scv[:, c:c + 1], start=(c == 0), stop=(c == 7))
            o.tt("dve", mods[l][:], ps[0][:, 0:48], adab[l][:], ALU.add)
            o.stt("dve", gs1[l][:], mods[l][:, 8:16], 1.0, gmx[l][:], ALU.add, ALU.mult)
            o.stt("dve", gs2[l][:], mods[l][:, 32:40], 1.0, gff[l][:], ALU.add, ALU.mult)
        if "mods" in dbg_out:
            o.store("sp", dbg_out["mods"][0], mods[0][:])
            o.store("sp", dbg_out["mods"][1], mods[1][:])

        def norm(l, gs, sh_off, router=False):
            k.phase = "norm"
            sh = mods[l]
            if router:
                for c in range(8):
                    o.mm(ps[5][0:16, 0:1], rw_sb[:, c, :], sh[:, sh_off + c:sh_off + c + 1], start=(c == 0), stop=(c == 7))
                o.cp("dve", rbias[:], ps[5][0:16, 0:1])
            for tb in range(NB):
                tsl = slice(tb * 512, (tb + 1) * 512)
                for c in range(8):
                    s_ = sq[c % 2]
                    o.act(s_[:], xT[:, c, tsl], AF.Square)
                    o.mm(ps[0][:], ones_f[:], s_[:], start=(c == 0), stop=(c == 7))
                o.act(rstd[:], ps[0][:], AF.Sqrt, bias=EPS, scale=1.0 / D)
                o.recip(rstd[:], rstd[:])
                for c in range(8):
                    t_ = tmpf[c % 2]
                    o.stt("dve", t_[:], xT[:, c, tsl], gs[:, c:c + 1], rstd[:], ALU.mult, ALU.mult)
                    o.act(hT[:, c, 1 + tb * 512:1 + (tb + 1) * 512], t_[:], AF.Identity, bias=sh[:, sh_off + c:sh_off + c + 1])
                    if router:
                        o.mm(ps[1][0:16, :], rw_sb[:, c, :], t_[:], start=(c == 0), stop=(c == 7))
                if router:
                    o.act(F["affT"][:, tsl], ps[1][0:16, :], AF.Sigmoid, bias=rbias[:])

        wo_state = {}

        def load_wo(l, grp):
            o.load("pool", wo[:], dram["w_out"][l][grp * 256:(grp + 1) * 256, :].rearrange("(c p) e -> p c e", p=128))
            wo_state["cur"] = (l, grp)

        def out_proj(l, grp):
            k.phase = "out_proj"
            if wo_state.get("cur") != (l, grp):
                load_wo(l, grp)
            g1 = mods[l][:, 16:24]
            for tb in range(NB):
                tsl = slice(tb * 512, (tb + 1) * 512)
                for dc in range(8):
                    p_ = ps[2 + (dc % 2)]
                    for kc in range(2):
                        o.mm(p_[:], wo[:, kc, dc * 128:(dc + 1) * 128], mixT[:, kc, tsl], start=(kc == 0), stop=(kc == 1))
                    o.stt("dve", xT[:, dc, tsl], p_[:], g1[:, dc:dc + 1], xT[:, dc, tsl], ALU.mult, ALU.add)

        rt_tiles = {}
        moe_tiles = {}
        F = {}

        def ffn_alloc():
            ar_reset()
            F["affT"] = ar("affT", [16, S], F32)
            F["gatesT"] = ar("gatesT", [16, S], F32)
            F["selE"] = ar("selE", [16, 16, 128], F32)
            o.load("sp", F["selE"][:], dram["selE"].rearrange("k (e m) -> k e m", e=16))
            rt_tiles.update(dict(
                aff=ar("r_aff", [128, NT, 16], F32), sel=ar("r_sel", [128, NT, 16], F32),
                sel2=ar("r_sel2", [128, NT, 16], F32), m1=ar("r_m1", [128, NT * 4], F32),
                m2=ar("r_m2", [128, NT * 4], F32), gsc=ar("r_gsc", [128, NT, 4], F32),
                gm=ar("r_gm", [128, NT], F32), den=ar("r_den", [128, NT], F32)))
            moe_tiles["gb"] = [ar(f"m_gb{i}", [128, 512], F32) for i in range(2)]
            moe_tiles["sg"] = [ar(f"m_sg{i}", [128, 512], BF16) for i in range(2)]
            moe_tiles["u2"] = [ar(f"m_u2{i}", [128, 512], BF16) for i in range(2)]
            moe_tiles["aT"] = [ar(f"m_aT{i}", [128, 2, 512], BF16) for i in range(2)]

        def routing():
            k.phase = "routing"
            aff, sel, sel2, m1, m2, gsc, gm, den = (rt_tiles[n] for n in ("aff", "sel", "sel2", "m1", "m2", "gsc", "gm", "den"))
            for t in range(NT):
                o.tr(ps[4][:, t * 16:(t + 1) * 16], F["affT"][:, t * 128:(t + 1) * 128], ident_f[0:16, 0:16])
            o.cp("dve", aff[:].re("p t e -> p (t e)"), ps[4][:, 0:256])
            o.tt("dve", sel[:], aff[:], rb_sb[:].re("p (o e) -> p o e", o=1).bc([128, NT, 16]), ALU.add)
            s4 = sel[:].re("p t (g j) -> p (t g) j", g=4)
            o.red(m1[:], s4, ALU.max)
            o.tt("dve", sel2[:].re("p t (g j) -> p (t g) j", g=4), s4, m1[:].re("p (a o) -> p a o", o=1).bc([128, NT * 4, 4]), ALU.is_ge)
            o.stt("dve", sel2[:], sel2[:], -1e9, sel[:], ALU.mult, ALU.add)
            o.red(m2[:], sel2[:].re("p t (g j) -> p (t g) j", g=4), ALU.max)
            o.tt("dve", gsc[:].re("p t g -> p (t g)"), m1[:], m2[:], ALU.add)
            o.red(gm[:], gsc[:], ALU.max)
            o.tt("dve", gsc[:], gsc[:], gm[:].re("p (t o) -> p t o", o=1).bc([128, NT, 4]), ALU.is_ge)
            o.tt("dve", sel2[:].re("p t (g j) -> p (t g) j", g=4), s4, m2[:].re("p (a o) -> p a o", o=1).bc([128, NT * 4, 4]), ALU.is_ge)
            o.tt("dve", sel2[:].re("p t (g j) -> p (t g) j", g=4), sel2[:].re("p t (g j) -> p (t g) j", g=4),
                 gsc[:].re("p t (g o) -> p (t g) o", o=1).bc([128, NT * 4, 4]), ALU.mult)
            o.tt("dve", sel2[:], sel2[:], aff[:], ALU.mult)
            o.red(den[:], sel2[:], ALU.add)
            o.recip(den[:], den[:])
            o.tt("dve", sel2[:], sel2[:], den[:].re("p (t o) -> p t o", o=1).bc([128, NT, 16]), ALU.mult)
            for g4 in range(4):
                for t4 in range(4):
                    t = g4 * 4 + t4
                    o.tr(ps[5][0:16, t4 * 128:(t4 + 1) * 128], sel2[:, t, :], ident_f[:])
                o.cp("act", F["gatesT"][:, g4 * 512:(g4 + 1) * 512], ps[5][0:16, :])


        moe_state = {}

        def moe_prefetch(l):
            wb = wA
            o.load("pool", wb[:, 0:2048].re("p (c f) -> p c f", c=8), dram["moe_wg"][l, 0].rearrange("(c p) f -> p c f", p=128))
            o.load("pool", wb[:, 2048:4096].re("p (c f) -> p c f", c=8), dram["moe_wu"][l, 0].rearrange("(c p) f -> p c f", p=128))
            o.load("pool", wb[:, 4096:6144].re("p (c d) -> p c d", c=2), dram["moe_wd"][l, 0].rearrange("(c p) d -> p c d", p=128))
            moe_state["pref"] = l

        def moe(l):
            k.phase = "moe"
            g2 = mods[l][:, 40:48]

            def wviews(e):
                wb = (wA, wB)[e % 2]
                return (wb[:, 0:2048].re("p (c f) -> p c f", c=8), wb[:, 2048:4096].re("p (c f) -> p c f", c=8),
                        wb[:, 4096:6144].re("p (c d) -> p c d", c=2))

            def load_w(e):
                wg, wu, wd = wviews(e)
                o.load("pool", wg, dram["moe_wg"][l, e].rearrange("(c p) f -> p c f", p=128))
                o.load("pool", wu, dram["moe_wu"][l, e].rearrange("(c p) f -> p c f", p=128))
                o.load("pool", wd, dram["moe_wd"][l, e].rearrange("(c p) d -> p c d", p=128))

            def down(wd, aT, tsl):
                for dc in range(8):
                    pd = ps[4 + (dc % 2)]
                    for fc in range(2):
                        o.mm(pd[:], wd[:, fc, dc * 128:(dc + 1) * 128], aT[:, fc, :], start=(fc == 0), stop=(fc == 1))
                    o.stt("dve", xT[:, dc, tsl], pd[:], g2[:, dc:dc + 1], xT[:, dc, tsl], ALU.mult, ALU.add)

            if moe_state.get("pref") != l:
                load_w(0)
            pending = None
            it = 0
            for e in range(16):
                wg, wu, wd = wviews(e)
                for tb in range(NB):
                    tsl = slice(tb * 512, (tb + 1) * 512)
                    hsl = slice(1 + tb * 512, 1 + (tb + 1) * 512)
                    gb = moe_tiles["gb"][it % 2]
                    aT = moe_tiles["aT"][it % 2]
                    it += 1
                    o.mm(ps[6][:], F["selE"][:, e, :], F["gatesT"][:, tsl])
                    o.cp("act", gb[:], ps[6][:])
                    for fc in range(2):
                        pg, pu = ps[0 + fc], ps[2 + fc]
                        for c in range(8):
                            o.mm(pg[:], wg[:, c, fc * 128:(fc + 1) * 128], hT[:, c, hsl], start=(c == 0), stop=(c == 7))
                        for c in range(8):
                            o.mm(pu[:], wu[:, c, fc * 128:(fc + 1) * 128], hT[:, c, hsl], start=(c == 0), stop=(c == 7))
                        sg = moe_tiles["sg"][fc]
                        u2 = moe_tiles["u2"][fc]
                        o.act(sg[:], pg[:], AF.Silu)
                        o.tt("dve", u2[:], pu[:], gb[:], ALU.mult)
                        o.tt("pool", aT[:, fc, :], sg[:], u2[:], ALU.mult)
                    if pending is not None:
                        pending()
                    if tb == 0 and e + 1 < 16:
                        load_w(e + 1)
                    pending = (lambda wd=wd, aT=aT, tsl=tsl: down(wd, aT, tsl))
            pending()

        MAGIC = 12582912.0

        def rope_table(dst_cos, dst_sin, pos_i, nparts, ncol, tmp_y, tmp_r, posf, invf_t):
            o.cp("dve", posf, pos_i)
            o.tt("dve", tmp_y, posf.re("p (t o) -> p t o", o=1).bc([nparts, ncol, 8]),
                 invf_t.re("p (o f) -> p o f", o=1).bc([nparts, ncol, 8]), ALU.mult)
            for dst, shift in ((dst_sin, 0.0), (dst_cos, 0.25)):
                o.ts("dve", tmp_r, tmp_y, shift, ALU.add, MAGIC, ALU.add)
                o.ts("dve", tmp_r, tmp_r, MAGIC, ALU.subtract)
                o.stt("dve", tmp_r, tmp_y, shift, tmp_r, ALU.add, ALU.subtract)
                o.act(dst, tmp_r, AF.Sin, scale=6.28318)

        invf_t = k.sbuf("invf", [128, 8], F32)
        posi = k.sbuf("posi", [128, NT], I32)
        posf = k.sbuf("posf", [128, NT], F32)
        rt_y = k.sbuf("rt_y", [128, NT, 8], F32)
        rt_r = k.sbuf("rt_r", [128, NT, 8], F32)
        o.load("sp", invf_t[:], dram["invf"])
        o.load("sp", posi[:], dram["pos_tm"])
        o.load("pool", trimask[:], dram["trimask"])
        rope_table(cosT[:], sinT[:], posi[:], 128, NT, rt_y[:], rt_r[:], posf[:], invf_t[:])

        def proj_qk(l, wname, gname, qkT, wbuf, sc, preloaded=False):
            k.phase = k.phase.split("/")[0] + "/projqk"
            if not preloaded:
                o.load("pool", wbuf, dram[wname][l].rearrange("(c p) e -> p c e", p=128))
            o.load("sp", sc["gain"][:], dram[gname][l])
            for tt in range(NT):
                b = tt % 2
                t1, t2, yb, ss = sc["t1"][b], sc["t2"][b], sc["yb"][b], sc["ss"][b]
                ra, rb, rc, rd = sc["ra"][b], sc["rb"][b], sc["rc"][b], sc["rd"][b]
                pq = ps[b]
                pTh = pT[:, b * 512:(b + 1) * 512]
                hs = slice(1 + tt * 128, 1 + (tt + 1) * 128)
                for c in range(8):
                    o.mm(pq[:], hT[:, c, hs], wbuf[:, c, :], start=(c == 0), stop=(c == 7))
                o.act(t1[:], pq[:], AF.Square)
                o.red(ss[:], t1[:].re("p (h d) -> p h d", h=8), ALU.add)
                o.act(ss[:], ss[:], AF.Sqrt, bias=EPS, scale=1.0 / 64)
                o.recip(ss[:], ss[:])
                o.tt("dve", t2[:].re("p (h d) -> p h d", h=8), pq[:].re("p (h d) -> p h d", h=8),
                     ss[:].re("p (h o) -> p h o", o=1).bc([128, 8, 64]), ALU.mult)
                o.tt("pool", t2[:], t2[:], sc["gain"][:], ALU.mult)
                y3 = t2[:].re("p (h d) -> p h d", h=8)
                yb3 = yb[:].re("p (h d) -> p h d", h=8)
                cs = cosT[:, tt, :].re("p (o f) -> p o f", o=1).bc([128, 8, 8])
                sn = sinT[:, tt, :].re("p (o f) -> p o f", o=1).bc([128, 8, 8])
                o.tt("dve", ra[:], y3[:, :, 0:8], cs, ALU.mult)
                o.tt("pool", rb[:], y3[:, :, 8:16], sn, ALU.mult)
                o.tt("dve", yb3[:, :, 0:8], ra[:], rb[:], ALU.subtract)
                o.tt("pool", rc[:], y3[:, :, 8:16], cs, ALU.mult)
                o.tt("dve", rd[:], y3[:, :, 0:8], sn, ALU.mult)
                o.tt("pool", yb3[:, :, 8:16], rc[:], rd[:], ALU.add)
                o.cp("act", yb3[:, :, 16:64], y3[:, :, 16:64])
                for j_ in range(4):
                    o.tr(pTh[:, j_ * 128:(j_ + 1) * 128], yb[:, j_ * 128:(j_ + 1) * 128], ident_b[:])
                o.cp("act", qkT[:, :, tt * 128:(tt + 1) * 128], pTh.re("p (j t) -> p j t", j=4))

        def qk_scratch():
            two = lambda nm, shp, dt: [ar(f"{nm}{i}", shp, dt) for i in range(2)]
            return dict(gain=ar("gain", [128, 512], F32), t1=two("t1", [128, 512], F32), t2=two("t2", [128, 512], F32),
                        yb=two("yb", [128, 512], BF16), ss=two("ss", [128, 8], F32),
                        ra=two("ra", [128, 8, 8], F32), rb=two("rb", [128, 8, 8], F32),
                        rc=two("rc", [128, 8, 8], F32), rd=two("rd", [128, 8, 8], F32))

        dil_state = {}

        def dil_prefetch(l):
            o.load("pool", wB[:, 0:4096].re("p (c e) -> p c e", c=8), dram["w_dqk"][l].rearrange("(c p) e -> p c e", p=128))
            o.load("pool", wB[:, 4096:6144].re("p (c e) -> p c e", c=8), dram["w_vd"][l].rearrange("(c p) e -> p c e", p=128))
            dil_state["pref"] = l

        def dilated(l):
            k.phase = "dilated"
            ar_reset()
            qkT = ar("qkT_d", [128, 4, S], BF16)
            mark = ar_state["off"]
            sc = qk_scratch()
            wq = wB[:, 0:4096].re("p (c e) -> p c e", c=8)
            wv = wB[:, 4096:6144].re("p (c e) -> p c e", c=8)
            pre = dil_state.get("pref") == l
            proj_qk(l, "w_dqk", "g_dqk", qkT, wq, sc, preloaded=pre)
            ar_rewind(mark)
            k.phase = "dilated/attn"
            Vd = ar("Vd", [128, NT, 256], BF16)
            acc = [ar(f"acc{i}", [128, S], F32) for i in range(2)]
            pt = [ar(f"pt{i}", [128, 256], BF16) for i in range(3)]
            dtmp = sq[0]
            if not pre:
                o.load("pool", wv, dram["w_vd"][l].rearrange("(c p) e -> p c e", p=128))
            o.memset("pool", Vd[:, :, 64:192], 1.0)
            it = 0
            for hp in range(2):
                for pi, dil in enumerate((1, 4, 16)):
                    nblk = (S // dil) // 128
                    for r in range(dil):
                        for m in range(nblk):
                            oi = r * nblk + m
                            st_ = 1 + r + dil * 128 * m
                            for c in range(8):
                                o.mm(ps[4][:, 0:128], hT[:, c, st_:st_ + dil * 127 + 1:dil], wv[:, c, hp * 128:(hp + 1) * 128], start=(c == 0), stop=(c == 7))
                            o.cp("act", Vd[:, oi, :].re("p (a b) -> p a b", b=64)[:, 0:4:3, :], ps[4][:, 0:128].re("p (a b) -> p a b", a=2))
                    items = []
                    for hh in range(2):
                        h = hp * 2 + hh
                        base = hh * 64
                        a_ = acc[hh]
                        for r in range(dil):
                            for n in range(nblk):
                                ms = [m for m in (n - 1, n) if m >= 0]
                                ncols = 128 * len(ms)
                                qcols = slice(r + dil * 128 * n, r + dil * 128 * n + dil * 127 + 1, dil)
                                pss = (ps[0], ps[1], ps[5])[it % 3]
                                pso = ps[2 + it % 2]
                                p_ = pt[it % 3]
                                it += 1

                                def A(ms=ms, ncols=ncols, qcols=qcols, pss=pss, p_=p_, base=base, r=r):
                                    for j, m in enumerate(ms):
                                        kcols = slice(r + dil * 128 * m, r + dil * 128 * m + dil * 127 + 1, dil)
                                        o.mm(pss[:, j * 128:(j + 1) * 128], qkT[base:base + 64, 2 + hp, kcols], qkT[base:base + 64, hp, qcols])
                                    o.act(p_[:, 0:ncols], pss[:, 0:ncols], AF.Exp, scale=0.125)
                                    o.tt("pool", p_[:, 0:ncols], p_[:, 0:ncols], trimask[:, 256 - ncols:256], ALU.mult)

                                def B(ms=ms, qcols=qcols, pso=pso, p_=p_, hh=hh, a_=a_, r=r):
                                    for j, m in enumerate(ms):
                                        oi = r * nblk + m
                                        lv = Vd[:, oi, hh * 128:(hh + 1) * 128]
                                        o.mm(pso[:, 0:128], lv, p_[:, j * 128:(j + 1) * 128], start=(j == 0), stop=(j == len(ms) - 1))
                                    if pi == 0:
                                        o.cp("dve", a_[:, qcols], pso[:, 0:128])
                                    else:
                                        o.tt("dve", a_[:, qcols], pso[:, 0:128], a_[:, qcols], ALU.add)
                                items.append((A, B))
                    LOOK = 2
                    for idx, (A, B) in enumerate(items):
                        A()
                        if idx >= LOOK:
                            items[idx - LOOK][1]()
                    for idx in range(max(0, len(items) - LOOK), len(items)):
                        items[idx][1]()
                for hh in range(2):
                    a_ = acc[hh]
                    ob, db = (0, 64) if hh == 0 else (64, 0)
                    for tb in range(NB):
                        tsl = slice(tb * 512, (tb + 1) * 512)
                        o.cp("act", dtmp[ob:ob + 64, :], a_[db:db + 64, tsl])
                        o.recip(dtmp[ob:ob + 64, :], dtmp[ob:ob + 64, :])
                        o.tt("dve", mixT[ob:ob + 64, hp, tsl], a_[ob:ob + 64, tsl], dtmp[ob:ob + 64, :], ALU.mult)


        def nsa(l):
            k.phase = "nsa"
            ar_reset()
            qkT = ar("qkT_n", [128, 4, S], BF16)
            mark = ar_state["off"]
            sc = qk_scratch()
            wq = wB[:, 0:4096].re("p (c e) -> p c e", c=8)
            wv = wB[:, 4096:5120].re("p (c e) -> p c e", c=8)
            wf = wB[:, 5120:6144].re("p (c e) -> p c e", c=8)
            wg = wB[:, 6144:6240].re("p (c e) -> p c e", c=8)
            proj_qk(l, "w_nqk", "g_nqk", qkT, wq, sc)
            ar_rewind(mark)
            Vn = ar("Vn", [128, NT, 4, 128], BF16)
            kcvc = ar("kcvc", [128, S], BF16, "A")
            sgT = ar("sgT", [12, S], BF16, "B")
            selT = ar("selT", [32, S], BF16, "B")
            cmpmask = ar("cmpmask", [127, 512], BF16)
            o.load("pool", wv, dram["w_vn"][l].rearrange("(c p) e -> p c e", p=128))
            o.load("pool", wf, dram["w_fm"][l][:, 256:384].rearrange("(c p) e -> p c e", p=128))
            o.load("pool", wg, dram["w_gl"][l].rearrange("(c p) e -> p c e", p=128))
            small = {}
            for nm, shp, dt, pl in (("covones", [127, 33], BF16, "main"), ("selG", [12, 12, 128], BF16, "main"), ("Esel", [32, 16, 128], BF16, "A"),
                                    ("selkeep", [128, 32], F32, "main"), ("seladd", [128, 32], F32, "main"), ("trigt", [128, 128], BF16, "main")):
                small[nm] = ar("c_" + nm, shp, dt, pl)
            o.load("pool", small["covones"][:], dram["covones"])
            o.load("pool", small["selG"][:], dram["selG"].rearrange("k (e m) -> k e m", e=12))
            o.load("pool", small["Esel"][:], dram["Esel"].rearrange("k (e m) -> k e m", e=16))
            o.load("pool", small["trigt"][:], dram["trigt"])
            k.phase = "nsa/vproj"
            o.memset("pool", Vn[:], 1.0)
            for tt in range(NT):
                hs = slice(1 + tt * 128, 1 + (tt + 1) * 128)
                for c in range(8):
                    o.mm(ps[4][:, 0:128], hT[:, c, hs], wv[:, c, :], start=(c == 0), stop=(c == 7))
                vflat = Vn[:, tt, :, :].re("p a b -> p (a b)")
                o.cp("act", vflat[:, 0:64], ps[4][:, 0:64])
                o.cp("act", vflat[:, 192:256], ps[4][:, 0:64])
                o.cp("dve", vflat[:, 256:320], ps[4][:, 64:128])
                o.cp("dve", vflat[:, 448:512], ps[4][:, 64:128])
            for tb in range(NB):
                hsl = slice(1 + tb * 512, 1 + (tb + 1) * 512)
                tsl = slice(tb * 512, (tb + 1) * 512)
                for c in range(8):
                    o.mm(ps[0][:], wf[:, c, :], hT[:, c, hsl], start=(c == 0), stop=(c == 7))
                o.cp("act", kcvc[:, tsl], ps[0][:])
                for c in range(8):
                    o.mm(ps[1][0:12, :], wg[:, c, :], hT[:, c, hsl], start=(c == 0), stop=(c == 7))
                o.act(sgT[:, tsl], ps[1][0:12, :], AF.Sigmoid)
            k.phase = "nsa/cmp"
            w1t = ar("w1t", [128, 32, 64], BF16, "A")
            peT = ar("peT", [128, 32], BF16, "A")
            w2t = ar("w2t", [64, 2, 64], BF16, "A")
            gel = ar("gel", [64, 2, 128], BF16, "A")
            for j in range(2):
                o.load("pool", w1t[j * 64:(j + 1) * 64, :, :], dram["cmp_w1"][l, j].rearrange("(i d) f -> d i f", d=64))
                o.load("pool", w2t[:, j, :], dram["cmp_w2"][l, j])
            o.load("pool", peT[:], dram["cmp_peT"][l])
            for j in range(2):
                b0 = j * 64
                for i in range(32):
                    o.mm(ps[2][0:64, 0:127], w1t[b0:b0 + 64, i, :], kcvc[b0:b0 + 64, i:i + 16 * 126 + 1:16], start=(i == 0), stop=False)
                for i in range(32):
                    o.mm(ps[2][0:64, 0:127], w1t[b0:b0 + 64, i, :], peT[b0:b0 + 64, i:i + 1].bc([64, 127]), start=False, stop=(i == 31))
                o.act(gel[:, j, 0:127], ps[2][0:64, 0:127], AF.Gelu_apprx_tanh)
            kc_f = ar("kc_f", [128, 128], F32, "A")
            kc_b = ar("kc_b", [128, 128], BF16, "A")
            kc_sq = ar("kc_sq", [128, 128], F32, "A")
            gkc = ar("gkc", [128, 128], F32, "A")
            kss = ar("kss", [128, 2], F32, "A")
            kcT = ar("kcT", [128, 128], BF16, "A")
            vc_aug = ar("vc_aug", [128, 2, 128], BF16)
            pce_i = ar("pce_i", [128, 1], I32, "A")
            pce_f = ar("pce_f", [128, 1], F32, "A")
            cy = ar("cy", [128, 1, 8], F32, "A")
            cr = ar("cr", [128, 1, 8], F32, "A")
            ccos = ar("ccos", [128, 1, 8], F32, "A")
            csin = ar("csin", [128, 1, 8], F32, "A")
            cra = ar("cra", [128, 2, 8], F32, "A")
            crb = ar("crb", [128, 2, 8], F32, "A")
            o.load("sp", gkc[:], dram["g_kcmp"][l])
            o.load("sp", pce_i[0:127, :], dram["pos_ce"])
            rope_table(ccos[0:127], csin[0:127], pce_i[0:127, :], 127, 1, cy[0:127], cr[0:127], pce_f[0:127, :], invf_t[0:127, :])
            for d2 in range(2):
                o.mm(ps[3][0:127, d2 * 64:(d2 + 1) * 64], gel[:, 0, 0:127], w2t[:, 0, :])
            o.mm(ps[3][0:127, 128:192], gel[:, 1, 0:127], w2t[:, 1, :])
            R = slice(0, 127)
            o.act(kc_sq[R, :], ps[3][R, 0:128], AF.Square)
            o.red(kss[R, :], kc_sq[R, :].re("p (h d) -> p h d", h=2), ALU.add)
            o.act(kss[R, :], kss[R, :], AF.Sqrt, bias=EPS, scale=1.0 / 64)
            o.recip(kss[R, :], kss[R, :])
            o.tt("dve", kc_f[R, :].re("p (h d) -> p h d", h=2), ps[3][R, 0:128].re("p (h d) -> p h d", h=2),
                 kss[R, :].re("p (h o) -> p h o", o=1).bc([127, 2, 64]), ALU.mult)
            o.tt("dve", kc_f[R, :], kc_f[R, :], gkc[R, :], ALU.mult)
            y3 = kc_f[R, :].re("p (h d) -> p h d", h=2)
            yb3 = kc_b[R, :].re("p (h d) -> p h d", h=2)
            cs = ccos[R, :, :].bc([127, 2, 8])
            sn = csin[R, :, :].bc([127, 2, 8])
            o.tt("dve", cra[R], y3[:, :, 0:8], cs, ALU.mult)
            o.tt("dve", crb[R], y3[:, :, 8:16], sn, ALU.mult)
            o.tt("dve", yb3[:, :, 0:8], cra[R], crb[R], ALU.subtract)
            o.tt("dve", cra[R], y3[:, :, 8:16], cs, ALU.mult)
            o.tt("dve", crb[R], y3[:, :, 0:8], sn, ALU.mult)
            o.tt("dve", yb3[:, :, 8:16], cra[R], crb[R], ALU.add)
            o.cp("act", yb3[:, :, 16:64], y3[:, :, 16:64])
            o.tr(pT[:, 0:127], kc_b[R, :], ident_b[0:127, 0:127])
            o.cp("act", kcT[:, 0:127], pT[:, 0:127])
            o.memset("pool", vc_aug[:], 1.0)
            o.cp("act", vc_aug[R, 0, 0:64], ps[3][R, 128:192])
            o.cp("act", vc_aug[R, 1, 64:128], ps[3][R, 128:192])

            wgt = ar("wgt", [128, 512], F32)
            ctr = ar("ctr", [128, 512], BF16)
            first = {}

            def epilogue(pso, ncol, h, br, tcol0, clamp=False):
                ob, db = (0, 64) if h % 2 == 0 else (64, 0)
                tsl_ = slice(tcol0, tcol0 + ncol)
                o.cp("act", wgt[ob:ob + 64, 0:ncol], pso[db:db + 64, 0:ncol])
                if clamp:
                    o.ts("dve", wgt[ob:ob + 64, 0:ncol], wgt[ob:ob + 64, 0:ncol], 1e-30, ALU.max)
                o.recip(wgt[ob:ob + 64, 0:ncol], wgt[ob:ob + 64, 0:ncol])
                o.mm(ps[6][:, 0:ncol], small["selG"][:, h * 3 + br, :], sgT[:, tsl_])
                o.tt("dve", wgt[ob:ob + 64, 0:ncol], wgt[ob:ob + 64, 0:ncol], ps[6][ob:ob + 64, 0:ncol], ALU.mult)
                dst = mixT[ob:ob + 64, h // 2, tsl_]
                if br == 0:
                    o.tt("dve", dst, pso[ob:ob + 64, 0:ncol], wgt[ob:ob + 64, 0:ncol], ALU.mult)
                else:
                    o.tt("dve", ctr[ob:ob + 64, 0:ncol], pso[ob:ob + 64, 0:ncol], wgt[ob:ob + 64, 0:ncol], ALU.mult)
                    o.tt("pool", dst, dst, ctr[ob:ob + 64, 0:ncol], ALU.add)

            eT = [ar(f"eT{i}", [128, 512], BF16) for i in range(2)]
            imp = ar("imp", [128, 32], F32)
            imp2 = ar("imp2", [128, 32], F32)
            mx8 = ar("mx8", [128, 8], F32)
            rdn = ar("rdn", [128, 4], F32)
            selm = ar("selm", [128, 32], F32)
            it = 0
            for tb in range(NB):
                tsl = slice(tb * 512, (tb + 1) * 512)
                o.load("pool", cmpmask[:], dram["cmpmask"][:, tsl])
                for h in range(4):
                    base = (h % 2) * 64
                    e_ = eT[it % 2]
                    it += 1
                    o.mm(ps[0][0:127, :], kcT[base:base + 64, 0:127], qkT[base:base + 64, h // 2, tsl])
                    o.act(e_[0:127, :], ps[0][0:127, :], AF.Exp, scale=0.125)
                    o.tt("pool", e_[0:127, :], e_[0:127, :], cmpmask[:, :], ALU.mult)
                    o.mm(ps[1][:], vc_aug[0:127, h % 2, :], e_[0:127, :])
                    for q4 in range(4):
                        o.mm((ps[2], ps[5])[q4 // 2][:, ((q4 % 2) * 4 + h) * 33:((q4 % 2) * 4 + h + 1) * 33], e_[0:127, q4 * 128:(q4 + 1) * 128], small["covones"][:, :])
                    epilogue(ps[1], 512, h, 0, tb * 512, clamp=True)
                for q4 in range(4):
                    tt = tb * 4 + q4
                    pv = (ps[2], ps[5])[q4 // 2][:, (q4 % 2) * 132:(q4 % 2 + 1) * 132].re("p (h c) -> p h c", h=4)
                    o.load("sp", small["selkeep"][:], dram["selkeep"][:, tt, :])
                    o.load("sp", small["seladd"][:], dram["seladd"][:, tt, :])
                    o.ts("dve", rdn[:], pv[:, :, 32], 1e-30, ALU.max)
                    o.recip(rdn[:], rdn[:])
                    o.ts("dve", imp[:], pv[:, 0, 0:32], rdn[:, 0:1], ALU.mult)
                    for h in range(1, 4):
                        o.stt("dve", imp[:], pv[:, h, 0:32], rdn[:, h:h + 1], imp[:], ALU.mult, ALU.add)
                    o.tt("dve", imp[:], imp[:], small["selkeep"][:, :], ALU.mult)
                    o.tt("dve", imp[:], imp[:], small["seladd"][:, :], ALU.add)
                    k.op("dve", lambda h_, a=mx8, b=imp: h_.max(out=a.ap, in_=b.ap), reads=[imp], writes=[mx8])
                    k.op("dve", lambda h_, a=imp2, b=mx8, c_=imp: h_.match_replace(out=a.ap, in_to_replace=b.ap, in_values=c_.ap, imm_value=-1e30),
                         reads=[imp, mx8], writes=[imp2])
                    k.op("dve", lambda h_, a=mx8, b=imp2: h_.max(out=a.ap, in_=b.ap), reads=[imp2], writes=[mx8])
                    o.ts("dve", selm[:], imp[:], mx8[:, 7:8], ALU.is_ge)
                    o.tr(ps[3][0:32, 0:128], selm[:], ident_f[:])
                    o.cp("act", selT[:, tt * 128:(tt + 1) * 128], ps[3][0:32, 0:128])

            k.phase = "nsa/selwin"
            mk = [ar(f"mk{i}", [128, 128], BF16) for i in range(3)]
            pp = [ar(f"pp{i}", [128, 256], BF16) for i in range(3)]
            it = 0
            items = []
            for br, kpair, vbase, span in ((1, 2, 0, 16), (2, 3, 2, 4)):
                for n in range(NT):
                    qsl = slice(n * 128, (n + 1) * 128)
                    js = [j for j in range(max(0, n - span), n + 1)]
                    for par in range(2):
                        base = par * 64
                        pso = ps[2 + par]
                        for ji, j in enumerate(js):
                            ksl = slice(j * 128, (j + 1) * 128)
                            p_ = pp[it % 3]
                            m_ = mk[it % 3]
                            pss = (ps[0], ps[1], ps[4])[it % 3]
                            it += 1

                            def A(br=br, kpair=kpair, span=span, n=n, j=j, qsl=qsl, ksl=ksl, base=base, p_=p_, m_=m_, pss=pss):
                                for hh in range(2):
                                    o.mm(pss[:, hh * 128:(hh + 1) * 128], qkT[base:base + 64, kpair, ksl], qkT[base:base + 64, hh, qsl])
                                o.act(p_[:], pss[:, 0:256], AF.Exp, scale=0.125)
                                msk = None
                                if br == 1 and n < 8:
                                    msk = trimask[:, 128:256] if j == n else None
                                elif br == 1:
                                    o.mm(ps[5][:, 0:128], small["Esel"][:, j, :], selT[:, qsl])
                                    if j == n:
                                        o.tt("dve", m_[:], ps[5][:, 0:128], trimask[:, 128:256], ALU.mult)
                                    else:
                                        o.cp("act", m_[:], ps[5][:, 0:128])
                                    msk = m_[:]
                                else:
                                    if j == n:
                                        msk = trimask[:, 128:256]
                                    elif j == n - span:
                                        msk = small["trigt"][:]
                                if msk is not None:
                                    o.tt("pool", p_[:].re("p (h q) -> p h q", h=2), p_[:].re("p (h q) -> p h q", h=2),
                                         msk.re("p (o q) -> p o q", o=1).bc([128, 2, 128]), ALU.mult)

                            def B(br=br, vbase=vbase, par=par, n=n, j=j, ji=ji, nj=len(js), p_=p_, pso=pso):
                                o.mm(pso[:, 0:256], Vn[:, j, vbase + par, :], p_[:], start=(ji == 0), stop=(ji == nj - 1))
                                if ji == nj - 1:
                                    for hh in range(2):
                                        h = hh * 2 + par
                                        epilogue(pso[:, hh * 128:(hh + 1) * 128], 128, h, br, n * 128)
                            items.append((A, B))
            LOOK = 2
            for idx, (A, B) in enumerate(items):
                A()
                if idx >= LOOK:
                    items[idx - LOOK][1]()
            for idx in range(max(0, len(items) - LOOK), len(items)):
                items[idx][1]()

        vfirst = None if dbg.get("no_vstore") else nc.dram_tensor("vfirst_scratch", [S, 256], F32).ap()
        vstore_tiles = []

        def rwkv(l):
            k.phase = "rwkv"
            ar_reset()
            WA = wA[:, 0:8448].re("p (c e) -> p c e", c=8)
            WB = wB[:, 0:8448].re("p (c e) -> p c e", c=8)
            mark0 = ar_state["off"]
            mu_t = ar("mu_t", [128, 1056], F32)
            omu = ar("omu", [128, 1056], F32)
            stg = [ar(f"stg{i}", [128, 1056], F32) for i in range(2)]
            o.load("sp", mu_t[:], dram["rw_mu"][l])
            o.ts("dve", omu[:], mu_t[:], -1.0, ALU.mult, 1.0, ALU.add)
            for c in range(8):
                sg = stg[c % 2]
                o.load("sp", sg[:], dram["w_rw"][l][c * 128:(c + 1) * 128, :])
                o.tt("dve", WA[:, c, :], sg[:], omu[:], ALU.mult)
                o.tt("pool", WB[:, c, :], sg[:], mu_t[:], ALU.mult)
            ar_rewind(mark0)
            if dbg.get("rwkv_stage", 9) <= -3:
                return
            P = {}
            for nm in ("w0", "a0", "kk", "ka", "rk", "lnw", "lnb") + (("v0",) if l > 0 else ()):
                P[nm] = ar("p_" + nm, [128, 256], F32)
                o.load("sp", P[nm][:], dram["rw_" + nm][l])
            wa2 = ar("wa2", [64, 2, 256], F32)
            g2a = ar("g2a", [128, 256], F32)
            g2b = ar("g2b", [32, 256], F32)
            o.load("sp", wa2[:, 0, :], dram["rw_w2"][l])
            o.load("sp", wa2[:, 1, :], dram["rw_a2"][l])
            o.load("sp", g2a[:], dram["rw_g2"][l][0:128, :])
            o.load("sp", g2b[:], dram["rw_g2"][l][128:160, :])
            if l > 0:
                v1 = ar("v1", [128, 2, 32], F32)
                v2 = ar("v2", [32, 256], F32)
                o.load("sp", v1[:], dram["rw_v1"][0].rearrange("(c p) r -> p c r", p=128))
                o.load("sp", v2[:], dram["rw_v2"][0])
            mU2 = ar("mU2", [128, 256], F32)
            mL = ar("mL", [128, 128], F32)
            o.load("sp", mU2[:], dram["mU2"])
            o.load("sp", mL[:], dram["mL"])
            Hs = ar("Hs", [64, 4, 64], F32)
            o.memset("dve", Hs[:], 0.0)
            if dbg.get("rwkv_stage", 9) <= -2:
                return
            nm256 = ("t0", "t1", "lw", "a_", "kk", "kp", "b_", "vv", "gi", "ginv", "ge", "gC", "gsb")
            W = {n_: ar("rw_" + n_, [128, 256], F32) for n_ in nm256}
            t0, t1, lw, a_, kk, kp, b_, vv, gi, ginv, ge, gC, gsb = (W[n_] for n_ in nm256)
            vf = gC
            vT = gi[:].re("p (a t) -> p a t", a=2)
            vv1T = ge[0:32, 0:128]
            XTkr = ar("XTkr", [64, 4, 2, 128], F32)
            XTbk = ar("XTbk", [64, 4, 2, 128], F32)
            P1 = ar("P1", [128, 256], F32)
            P2 = ar("P2", [128, 256], BF16)
            MrbTb = ar("MrbTb", [128, 128], BF16)
            vvb = ar("vvb", [128, 256], BF16)
            BCb = ar("BCb", [128, 256], BF16)
            KCb = ar("KCb", [128, 256], BF16)
            Hsb = ar("Hsb", [64, 4, 64], BF16)
            Lc0 = ar("Lc0", [128, 128], F32)
            Lb = [ar(f"Lb{i}", [128, 128], F32) for i in range(2)]
            Ub = [ar(f"Ub{i}", [128, 128], F32) for i in range(2)]
            Y = [ar(f"Y{i}", [128, 128], F32) for i in range(2)]
            ILs = [ar(f"IL{i}", [128, 128], F32) for i in range(2)]
            RH = ar("RH", [128, 128], F32)
            lxg2 = RH
            nWU = ar("nWU", [128, 128], BF16)
            TT = ar("TT", [64, 64], BF16)
            QT = ar("QT", [64, 128], BF16)
            s4 = ar("s4", [128, 4], F32)
            s4b = ar("s4b", [128, 4], F32)
            bc4 = ar("bc4", [128, 4], F32)
            gcol = ar("gcol", [64, 4], F32)
            lxw = ar("lxw", [64, 2, 128], F32)
            lxg = ar("lxg", [128, 128], F32)
            yb_ = KCb
            o.memset("pool", Hsb[:], 0.0)
            h3 = lambda v_: v_.re("p (h d) -> p h d", h=4)
            b3 = lambda v_: v_.re("p (h o) -> p h o", o=1).bc([128, 4, 64])

            for tt in range(dbg.get("rwkv_tiles", NT)):
                cur = slice(1 + tt * 128, 1 + (tt + 1) * 128)
                prv = slice(tt * 128, (tt + 1) * 128)
                k.phase = "rwkv/front"
                for (pb, c0, c1) in ((ps[0][:, 0:512], 0, 512), (ps[1][:, 0:256], 512, 768)):
                    for c in range(8):
                        o.mm(pb, hT[:, c, cur], WA[:, c, c0:c1], start=(c == 0), stop=False)
                    for c in range(8):
                        o.mm(pb, hT[:, c, prv], WB[:, c, c0:c1], start=False, stop=(c == 7))
                for (pb, c0, c1) in ((ps[2][:, 0:128], 768, 896), (ps[2][:, 128:256], 896, 1024), (ps[2][0:32, 256:384], 1024, 1056)):
                    for c in range(8):
                        o.mm(pb, WA[:, c, c0:c1], hT[:, c, cur], start=(c == 0), stop=False)
                    for c in range(8):
                        o.mm(pb, WB[:, c, c0:c1], hT[:, c, prv], start=False, stop=(c == 7))
                if dbg.get("rwkv_stage", 9) <= -1:
                    continue
                sub = dbg.get("rwkv_sub", 99)
                if sub > 0:
                    o.act(lxw[:, 0, :], ps[2][0:64, 0:128], AF.Tanh)
                if sub > 1:
                    o.cp("act", lxw[:, 1, :], ps[2][64:128, 0:128])
                if sub > 2:
                    o.act(lxg[:], ps[2][:, 128:256], AF.Sigmoid)
                if sub > 3:
                    o.act(lxg2[0:32, :], ps[2][0:32, 256:384], AF.Sigmoid)
                if sub > 4:
                    o.mm(ps[3][:, 0:256], lxw[:, 0, :], wa2[:, 0, :])
                if sub > 5:
                    o.mm(ps[3][:, 256:512], lxw[:, 1, :], wa2[:, 1, :])
                if sub > 6:
                    o.mm(ps[4][:, 0:256], lxg[:], g2a[:], start=True, stop=False)
                if sub > 7:
                    o.mm(ps[4][:, 0:256], lxg2[0:32, :], g2b[:], start=False, stop=True)
                if sub > 8:
                    o.cp("act", gsb[:], ps[4][:, 0:256])
                if dbg.get("rwkv_stage", 9) < 1:
                    continue
                o.tt("dve", t0[:], ps[3][:, 0:256], P["w0"][:], ALU.add)
                o.act(lw[:], t0[:], AF.Sigmoid)
                o.ts("pool", lw[:], lw[:], -0.6065306597126334, ALU.mult)
                o.tt("dve", t0[:], ps[3][:, 256:512], P["a0"][:], ALU.add)
                o.act(a_[:], t0[:], AF.Sigmoid)
                o.cp("act", vv[:], ps[1][:, 0:256])
                if l == 0:
                    if not dbg.get("no_vstore"):
                        o.store("sp", vfirst[tt * 128:(tt + 1) * 128, :], vv[:])
                        if vv not in vstore_tiles:
                            vstore_tiles.append(vv)
                else:
                    o.load("sp", vf[:], vfirst[tt * 128:(tt + 1) * 128, :], extra=vstore_tiles)
                    for c2 in range(2):
                        o.tr(ps[6][:, c2 * 128:(c2 + 1) * 128], vv[:, c2 * 128:(c2 + 1) * 128], ident_f[:])
                    o.cp("act", vT, ps[6][:, 0:256].re("p (a t) -> p a t", a=2))
                    for c2 in range(2):
                        o.mm(ps[6][0:32, 256:384], v1[:, c2, :], vT[:, c2, :], start=(c2 == 0), stop=(c2 == 1))
                    o.cp("act", vv1T, ps[6][0:32, 256:384])
                    o.mm(ps[3][:, 0:256], vv1T, v2[:])
                    o.tt("dve", t0[:], ps[3][:, 0:256], P["v0"][:], ALU.add)
                    o.act(t0[:], t0[:], AF.Sigmoid)
                    o.tt("dve", t1[:], vf[:], vv[:], ALU.subtract)
                    o.tt("dve", t1[:], t1[:], t0[:], ALU.mult)
                    o.tt("dve", vv[:], vv[:], t1[:], ALU.add)
                kps = ps[0][:, 256:512]
                rps = ps[0][:, 0:256]
                o.tt("dve", kk[:], kps, P["kk"][:], ALU.mult)
                o.tt("pool", t0[:], kk[:], kk[:], ALU.mult)
                o.red(s4[:], h3(t0[:]), ALU.add)
                o.act(s4[:], s4[:], AF.Sqrt)
                o.ts("dve", s4[:], s4[:], 1e-12, ALU.max)
                o.recip(s4[:], s4[:])
                o.tt("dve", h3(kk[:]), h3(kk[:]), b3(s4[:]), ALU.mult)
                o.stt("dve", t0[:], a_[:], -1.0, P["ka"][:], ALU.add, ALU.mult)
                o.stt("dve", kp[:], t0[:], 1.0, kps, ALU.add, ALU.mult)
                o.tt("pool", b_[:], kk[:], a_[:], ALU.mult)
                o.tt("dve", t1[:], rps, kp[:], ALU.mult)
                o.tt("pool", t1[:], t1[:], P["rk"][:], ALU.mult)
                o.red(bc4[:], h3(t1[:]), ALU.add)
                if dbg.get("rwkv_stage", 9) < 2:
                    continue
                o.mm(ps[5][:, 0:256], mU2[:, 128:256], lw[:])
                o.mm(ps[5][:, 256:512], ones_f[:], lw[:])
                for h in range(4):
                    o.mm(ps[4][0:64, 256 + h:257 + h], lw[:, h * 64:(h + 1) * 64], ones_f[:, 0:1])
                o.act(gcol[:], ps[4][0:64, 256:260], AF.Exp)
                cum = ps[5][:, 0:256]
                o.act(gi[:], cum, AF.Exp)
                o.act(ginv[:], cum, AF.Exp, scale=-1.0)
                o.tt("dve", t0[:], cum, lw[:], ALU.subtract)
                o.act(ge[:], t0[:], AF.Exp)
                o.cp("act", t1[:], ps[5][:, 256:512])
                o.tt("dve", t1[:], t1[:], cum, ALU.subtract)
                o.act(gC[:], t1[:], AF.Exp)
                o.tt("pool", ge[:], kk[:], ge[:], ALU.mult)
                o.tt("dve", gi[:], rps, gi[:], ALU.mult)
                o.tt("pool", a_[:], b_[:], ginv[:], ALU.mult)
                o.tt("pool", ginv[:], kp[:], ginv[:], ALU.mult)
                o.tt("pool", BCb[:], b_[:], gC[:], ALU.mult)
                o.tt("pool", KCb[:], kp[:], gC[:], ALU.mult)
                o.cp("act", vvb[:], vv[:])
                KKt, Rt, Bh, Kh, BC, KC = ge, gi, a_, ginv, BCb, KCb
                for qi, (src, dst, idx) in enumerate(((KKt, XTkr, 0), (Rt, XTkr, 1), (Bh, XTbk, 0), (Kh, XTbk, 1))):
                    for h in range(4):
                        o.tr(ps[6][0:64, h * 128:(h + 1) * 128], src[:, h * 64:(h + 1) * 64], ident_f[:])
                    o.cp("act" if qi % 2 == 0 else "dve", dst[:, :, idx, :], ps[6][0:64, 0:512].re("p (a t) -> p a t", a=4))
                if dbg.get("rwkv_stage", 9) < 3:
                    continue
                k.phase = "rwkv/heads"
                k.limit = dbg.get("oplimit")
                k.limit_on = True
                for h in range(4):
                    hc = slice(h * 64, (h + 1) * 64)
                    BhT = XTbk[:, h, 0, :]
                    KhT = XTbk[:, h, 1, :]
                    KRT = XTkr[:, h, :, :].re("p a t -> p (a t)")
                    KKtT = XTkr[:, h, 0, :]
                    RtT = XTkr[:, h, 1, :]
                    o.mm(ps[0][:, 0:256], BhT, KRT)
                    o.mm(ps[1][:, 0:256], KhT, KRT)
                    o.mm(ps[1][:, 256:384], KKtT, BhT)
                    o.tt("dve", P1[:], ps[0][:, 0:256], mU2[:], ALU.mult)
                    o.tt("dve", P2[:], ps[1][:, 0:256], mU2[:], ALU.mult)
                    o.tt("dve", MrbTb[:], ps[0][:, 128:256], mU2[:, 128:256], ALU.mult)
                    o.tt("dve", Lc0[:], ps[1][:, 256:384], mL[:], ALU.mult)
                    o.tt("pool", Y[0][:], ident_f[:], P1[:, 0:128], ALU.subtract)
                    o.mm(ps[3][:, 0:64], P2[:, 0:128], vvb[:, hc])
                    o.cp("act", RH[:, 64:128], ps[3][:, 0:64])
                    o.cp("pool", RH[:, 0:64], KKt[:, hc])
                    Uc, Lcur = P1[:, 0:128], Lc0[:]

                    def sq_step(ks, Uc, Lcur):
                        last = ks == 5
                        o.mm(ps[2][:, 0:128], Uc, Lcur)
                        if not last:
                            o.mm(ps[6][:, 0:128], Lcur, Uc)
                        o.cp("act", Lb[ks % 2][:], ps[2][:, 0:128])
                        if not last:
                            o.cp("dve", Ub[ks % 2][:], ps[6][:, 0:128])
                        o.tt("pool", ILs[ks % 2][:], Lb[ks % 2][:], ident_f[:], ALU.add)
                        return Ub[ks % 2][:], Lb[ks % 2][:]

                    def y_step(ks):
                        o.mm(ps[0][:, 256:384], ILs[ks % 2][:], Y[ks % 2][:])
                        o.cp("act", Y[(ks + 1) % 2][:], ps[0][:, 256:384])

                    Uc, Lcur = sq_step(0, Uc, Lcur)
                    for ks in range(1, 6):
                        Uc, Lcur = sq_step(ks, Uc, Lcur)
                        y_step(ks - 1)
                    y_step(5)
                    Yf = Y[0]
                    o.mm(ps[3][:, 64:192], Yf[:], RH[:])
                    o.ts("dve", nWU[:], ps[3][:, 64:192], -1.0, ALU.mult)
                    o.mm(ps[3][0:64, 192:256], nWU[:, 0:64], BC[:, hc])
                    o.cp("act", TT[:], ps[3][0:64, 192:256])
                    o.mm(ps[3][0:64, 256:384], nWU[:, 0:64], MrbTb[:], start=True, stop=False)
                    o.mm(ps[3][0:64, 256:384], ident_f[0:64, 0:64], RtT, start=False, stop=True)
                    o.cp("act", QT[:], ps[3][0:64, 256:384])
                    o.mm(ps[5][:, hc], P2[:, 128:256], vvb[:, hc], start=True, stop=False)
                    o.mm(ps[5][:, hc], MrbTb[:], nWU[:, 64:128], start=False, stop=False)
                    o.mm(ps[5][:, hc], QT[:], Hsb[:, h, :], start=False, stop=True)
                    o.mm(ps[4][0:64, hc], KC[:, hc], vvb[:, hc], start=True, stop=False)
                    o.mm(ps[4][0:64, hc], BC[:, hc], nWU[:, 64:128], start=False, stop=False)
                    o.mm(ps[4][0:64, hc], TT[:], Hsb[:, h, :], start=False, stop=True)
                    o.stt("dve", Hs[:, h, :], Hs[:, h, :], gcol[:, h:h + 1], ps[4][0:64, hc], ALU.mult, ALU.add)
                    o.cp("act", Hsb[:, h, :], Hs[:, h, :])
                if dbg.get("rwkv_stage", 9) < 4:
                    continue
                k.limit_on = False
                if "dumpP1" in dbg_out:
                    o.store("sp", dbg_out["dumpP1"], P1[:])
                    o.store("sp", dbg_out["dumpP2"], P2[:])
                    o.store("sp", dbg_out["dumpL"], Lc0[:])
                    o.store("sp", dbg_out["dumpY"], Y[0][:])
                    o.store("sp", dbg_out["dumpX"], XTkr[:].re("p a b t -> p (a b t)"))
                k.phase = "rwkv/post"
                O_ = ps[5][:, 0:256]
                xc = kk
                o.red(s4[:], h3(O_), ALU.add)
                o.ts("dve", s4[:], s4[:], 1.0 / 64, ALU.mult)
                o.tt("dve", h3(xc[:]), h3(O_), b3(s4[:]), ALU.subtract)
                o.tt("pool", t0[:], xc[:], xc[:], ALU.mult)
                o.red(s4b[:], h3(t0[:]), ALU.add)
                o.act(s4b[:], s4b[:], AF.Sqrt, bias=64e-5, scale=1.0 / 64)
                o.recip(s4b[:], s4b[:])
                o.tt("dve", h3(xc[:]), h3(xc[:]), b3(s4b[:]), ALU.mult)
                o.tt("pool", xc[:], xc[:], P["lnw"][:], ALU.mult)
                o.tt("pool", xc[:], xc[:], P["lnb"][:], ALU.add)
                o.tt("dve", h3(t0[:]), h3(vv[:]), b3(bc4[:]), ALU.mult)
                o.tt("dve", xc[:], xc[:], t0[:], ALU.add)
                o.tt("dve", yb_[:], xc[:], gsb[:], ALU.mult)
                for c2 in range(2):
                    o.tr(pT[:, c2 * 128:(c2 + 1) * 128], yb_[:, c2 * 128:(c2 + 1) * 128], ident_b[:])
                o.cp("act", mixT[:, :, tt * 128:(tt + 1) * 128], pT[:, 0:256].re("p (a t) -> p a t", a=2))

        pool_c = {}

        def pool_mixer(l):
            k.phase = "pool"
            ar_reset()
            if not pool_c:
                pool_c["rw"] = k.sbuf("pool_rw", [128, 2], F32)
                pool_c["fix"] = k.sbuf("pool_fix", [128, 2, 16], F32)
                pool_c["sc"] = k.sbuf("pool_sc", [128, 2], F32)
                o.load("sp", pool_c["rw"][:], dram["pool_rw"])
                o.load("sp", pool_c["fix"][:], dram["pool_fix"])
            o.load("sp", pool_c["sc"][:], dram["pool_scale"][l])
            wu_ = wB[:, 0:2048].re("p (c e) -> p c e", c=8)
            o.load("pool", wu_, dram["w_fm"][l][:, 0:256].rearrange("(c p) e -> p c e", p=128))
            pw = ar("pw", [128, 2, 128], BF16)
            pwf = ar("pwf", [128, 2, 128], F32)
            o.memset("pool", pwf[:], 0.0)
            for gi in range(4):
                o.load("sp", pwf[(gi % 2) * 64:(gi % 2) * 64 + 64, gi // 2, (gi % 2) * 64:(gi % 2) * 64 + 64], dram["pool_w"][l, gi])
            o.cp("dve", pw[:], pwf[:])
            PADW = 16
            u = [ar(f"pu{i}", [128, PADW + S], F32) for i in range(1)][0]
            sa = ar("psa", [128, PADW + S], F32)
            sb_ = ar("psb", [128, PADW + S], F32)
            dm = ar("pdm", [128, S], BF16)
            for mt in range(2):
                o.memset("pool", u[:, 0:PADW], 0.0)
                o.memset("pool", sa[:, 0:PADW], 0.0)
                o.memset("pool", sb_[:, 0:PADW], 0.0)
                for tb in range(NB):
                    for c in range(8):
                        o.mm(ps[0][:], wu_[:, c, mt * 128:(mt + 1) * 128], hT[:, c, 1 + tb * 512:1 + (tb + 1) * 512], start=(c == 0), stop=(c == 7))
                    o.cp("act", u[:, PADW + tb * 512:PADW + (tb + 1) * 512], ps[0][:])
                wl, wh = ((2, 4), (8, 16))[mt]
                full = slice(PADW, PADW + S)

                def sh(t, d):
                    return t[:, PADW - d:PADW + S - d]
                o.tt("dve", sa[:, full], u[:, full], sh(u, 1), ALU.add)
                cur, oth, w = sa, sb_, 2
                res = {}
                res[2] = cur
                while w < wh:
                    o.tt("dve", oth[:, full], cur[:, full], sh(cur, w), ALU.add)
                    w *= 2
                    if w == wl:
                        pass
                    res[w] = oth
                    cur, oth = oth, cur
                lo_t, hi_t = sa, sb_
                for (pr, src) in ((slice(0, 64), lo_t), (slice(64, 128), hi_t)):
                    o.ts("dve", src[pr, full], src[pr, full], pool_c["rw"][pr, mt:mt + 1], ALU.mult)
                    o.tt("dve", src[pr, PADW:PADW + 16], src[pr, PADW:PADW + 16], pool_c["fix"][pr, mt, :], ALU.mult)
                    o.tt("dve", dm[pr, :], src[pr, full], u[pr, full], ALU.subtract)
                for tb in range(NB):
                    tsl = slice(tb * 512, (tb + 1) * 512)
                    o.mm(ps[1][:], pw[:, mt, :], dm[:, tsl])
                    o.act(mixT[:, mt, tsl], ps[1][:], AF.Identity, scale=pool_c["sc"][:, mt:mt + 1])

        if dbg.get("only"):
            o.load("pool", hT[:], dram["hT_in"].rearrange("(c p) t -> p c t", p=128))
            {"rwkv": rwkv, "dil": dilated, "nsa": nsa, "pool": pool_mixer}[dbg["only"]](dbg.get("layer", 0))
            o.store("pool", dbg_out["mix_only"].rearrange("(c p) t -> p c t", p=128), mixT[:])
        GORDER = (3, 1, 0, 2)
        for l in range(0 if dbg.get("only") else n_layers):
            full = not dbg.get("mix_in") and "compute" not in dbg
            if full:
                dil_prefetch(l)
                if l == 0:
                    load_wo(l, GORDER[0])
            norm(l, gs1[l], 0)
            if "hT" in dbg_out and l == dbg.get("layer", 0):
                pass
            comp = dbg.get("compute", ("nsa", "pool", "rwkv", "dil"))
            for grp, nm, fn in ((3, "dil", dilated), (1, "pool", pool_mixer), (0, "nsa", nsa), (2, "rwkv", rwkv)):
                if nm in comp:
                    fn(l)
                    if f"mix{grp}_{l}" in dbg_out:
                        o.store("pool", dbg_out[f"mix{grp}_{l}"].rearrange("(c p) t -> p c t", p=128), mixT[:])
                elif dbg.get("mix_in"):
                    o.load("pool", mixT[:], dram[f"mix_in{l}"][grp * 256:(grp + 1) * 256, :].rearrange("(c p) t -> p c t", p=128))
                else:
                    continue
                out_proj(l, grp)
                if full:
                    gi_ = GORDER.index(grp)
                    if gi_ + 1 < 4:
                        load_wo(l, GORDER[gi_ + 1])
                    elif l + 1 < n_layers:
                        load_wo(l + 1, GORDER[0])
            if f"x1_{l}" in dbg_out:
                o.store("sp", dbg_out[f"x1_{l}"].rearrange("(c p) t -> p c t", p=128), xT[:])
            ffn_alloc()
            if full:
                moe_prefetch(l)
            norm(l, gs2[l], 24, router=True)
            routing()
            if f"gates_{l}" in dbg_out:
                o.store("sp", dbg_out[f"gates_{l}"], F["gatesT"][:])
            moe(l)

        o.store("sp", outT.rearrange("(c p) t -> p c t", p=128), xT[:])
        k.final_wait("sp")
        k.emit()
        print("instr counts", k.count, "waits", k.n_waits, "sems", k.nsem, "sbuf left", nc.sbuf_bytes_remaining)
    return nc


_NP2DT = {np.dtype(np.float32): F32, np.dtype(np.int32): I32, np.dtype(ml_dtypes.bfloat16): BF16}


def run(inp, dbg=None, n_layers=2, extra_per=None):
    sh, per = _prep_inputs(inp)
    if extra_per:
        for b in range(8):
            per[b].update(extra_per[b])
    in_maps = []
    for b in range(8):
        d = dict(sh)
        d.update(per[b])
        in_maps.append(d)
    shapes = {n: (a.shape, _NP2DT[a.dtype]) for n, a in in_maps[0].items()}
    nc = build(shapes, dbg=dbg, n_layers=n_layers)
    ncore = dbg.get("ncores", 8) if dbg else 8
    res = run_bass_kernel_spmd(nc, in_maps[:ncore], core_ids=list(range(ncore)), **({"trace": True} if (dbg and dbg.get("trace")) else {}))
    return res


def kernel(**inputs):
    inp = {k_: np.asarray(v) for k_, v in inputs.items()}
    res = run(inp)
    out = np.stack([np.ascontiguousarray(res.results[b]["outT"].T) for b in range(8)], axis=0)
    return out.astype(np.float32)
```
